# Optimizing a Trainium2 kernel written in Bass

```python
import jax, jax.numpy as jnp
from jax import lax
import numpy as np

D_MODEL = 1024
BATCH = 8
SEQ = 4096
DEPTH = 2

N_BRANCH = 4
BRANCH_W = D_MODEL // 2
GLA_HEADS = 4
GLA_DV = BRANCH_W // GLA_HEADS
GLA_DK = GLA_DV // 2
GLA_KW = GLA_HEADS * GLA_DK
GLA_GATE_RANK = 16
GLA_LOGIT_NORM = 16.0
GLA_CHUNK = 64
RWKV_HEAD = 64
RWKV_HEADS = BRANCH_W // RWKV_HEAD
RWKV_W_LORA = 64
RWKV_A_LORA = 64
RWKV_G_LORA = 128
RWKV_GN_EPS = 64e-5
FOX_HEADS = 8
FOX_DH = BRANCH_W // FOX_HEADS
FOX_QBLOCK = 128
MASK_VALUE = -1e30
HGRN_HEADS = 4
HGRN_EXPAND = 128
HGRN_DV = BRANCH_W // HGRN_HEADS
HGRN_KW = HGRN_HEADS * HGRN_EXPAND
HGRN_CHUNK = 64
N_EXPERTS = 16
N_GROUPS = 4
EXPERTS_PER_GROUP = N_EXPERTS // N_GROUPS
TOP_K = 2
GROUP_SCORE_TOPK = 2
D_FF_EXPERT = D_MODEL // 2
DN_ALPHA = (2.0 * DEPTH) ** 0.25
DN_BETA = (8.0 * DEPTH) ** -0.25
N_ADA = 6

GLA_COLS = 2 * GLA_KW + 2 * BRANCH_W + GLA_GATE_RANK
RWKV_COLS = 3 * BRANCH_W + RWKV_W_LORA + RWKV_A_LORA + RWKV_G_LORA
FOX_COLS = 3 * BRANCH_W + FOX_HEADS
HGRN_COLS = 2 * HGRN_KW + 2 * BRANCH_W
N_IN_COLS = GLA_COLS + RWKV_COLS + FOX_COLS + HGRN_COLS

kernel_name = "hybrid_gla_rwkv7_fox_hgrn2_moe_block"


def _split(t, sizes):
    return jnp.split(t, [int(s) for s in np.cumsum(sizes)[:-1]], axis=-1)


def _heads(t, n):
    b, s, w = t.shape
    return t.reshape(b, s, n, w // n).transpose(0, 2, 1, 3)


def _merge(t):
    b, n, s, d = t.shape
    return t.transpose(0, 2, 1, 3).reshape(b, s, n * d)


def _layer_norm(t, g=None, b=None, eps=1e-5):
    tf = t.astype(jnp.float32)
    mu = tf.mean(-1, keepdims=True)
    var = jnp.square(tf - mu).mean(-1, keepdims=True)
    y = (tf - mu) * lax.rsqrt(var + eps)
    if g is not None:
        y = y * g + b
    return y.astype(t.dtype)


def _rms_norm(t, g, eps=1e-6):
    tf = t.astype(jnp.float32)
    y = tf * lax.rsqrt(jnp.mean(jnp.square(tf), -1, keepdims=True) + eps) * g
    return y.astype(t.dtype)


def chunk_gla(q, k, v, log_g, chunk):
    out_dtype = v.dtype
    bsz, nh, t_len, dk = q.shape
    dv = v.shape[-1]
    n_chunks = t_len // chunk

    def to_chunks(a):
        return jnp.moveaxis(a.astype(jnp.float32).reshape(bsz, nh, n_chunks, chunk, a.shape[-1]), 2, 0)

    causal = jnp.tril(jnp.ones((chunk, chunk), bool))[:, :, None]

    def step(state, inp):
        qi, ki, vi, gi = inp
        b = jnp.cumsum(gi, axis=-2)
        o_inter = jnp.einsum('bhck,bhkv->bhcv', qi * jnp.exp(b), state)
        diff = b[..., :, None, :] - b[..., None, :, :]
        decay = jnp.where(causal, jnp.exp(jnp.where(causal, diff, 0.0)), 0.0)
        att = jnp.einsum('bhik,bhjk,bhijk->bhij', qi, ki, decay)
        o = o_inter + jnp.einsum('bhij,bhjv->bhiv', att, vi)
        b_last = b[..., -1:, :]
        state = jnp.exp(b_last)[..., 0, :, None] * state + jnp.einsum(
            'bhck,bhcv->bhkv', ki * jnp.exp(b_last - b), vi)
        return state, o

    s0 = jnp.zeros((bsz, nh, dk, dv), jnp.float32)
    _, o = lax.scan(step, s0, tuple(map(to_chunks, (q, k, v, log_g))))
    return jnp.moveaxis(o, 0, 2).reshape(bsz, nh, t_len, dv).astype(out_dtype)


def rwkv7_scan(r, log_w, k, v, kk, a):
    out_dtype = v.dtype
    bsz, nh, _, n = r.shape

    def to_time(t):
        return jnp.moveaxis(t.astype(jnp.float32), 2, 0)

    def step(state, inp):
        r_t, lw_t, k_t, v_t, kk_t, a_t = inp
        sa = jnp.einsum('bhvk,bhk->bhv', state, -kk_t)
        state = (state * jnp.exp(lw_t)[:, :, None, :]
                 + sa[..., :, None] * (kk_t * a_t)[..., None, :]
                 + v_t[..., :, None] * k_t[..., None, :])
        return state, jnp.einsum('bhvk,bhk->bhv', state, r_t)

    s0 = jnp.zeros((bsz, nh, n, n), jnp.float32)
    _, y = lax.scan(step, s0, tuple(map(to_time, (r, log_w, k, v, kk, a))))
    return jnp.moveaxis(y, 0, 2).astype(out_dtype)


def fox_attention(q, k, v, log_f):
    bsz, nh, t_len, dh = q.shape
    n_blocks = t_len // FOX_QBLOCK
    cum_f = jnp.cumsum(log_f.astype(jnp.float32), axis=-1)
    q_blocks = jnp.moveaxis(q.reshape(bsz, nh, n_blocks, FOX_QBLOCK, dh), 2, 0)
    f_blocks = jnp.moveaxis(cum_f.reshape(bsz, nh, n_blocks, FOX_QBLOCK), 2, 0)
    key_pos = jnp.arange(t_len)
    scale = dh ** -0.5

    def one_block(args):
        blk, qi, fi = args
        s = jnp.einsum('bhqd,bhkd->bhqk', qi, k).astype(jnp.float32) * scale
        s = s + fi[..., :, None] - cum_f[..., None, :]
        q_pos = blk * FOX_QBLOCK + jnp.arange(FOX_QBLOCK)
        s = jnp.where(key_pos[None, :] <= q_pos[:, None], s, MASK_VALUE)
        p = jax.nn.softmax(s, axis=-1)
        return jnp.einsum('bhqk,bhkd->bhqd', p.astype(v.dtype), v)

    o = lax.map(one_block, (jnp.arange(n_blocks), q_blocks, f_blocks))
    return jnp.moveaxis(o, 0, 2).reshape(bsz, nh, t_len, dh)


def gla_branch(cols, alpha_up, alpha_b, norm_g):
    q, k, v, g, al = _split(cols, [GLA_KW, GLA_KW, BRANCH_W, BRANCH_W, GLA_GATE_RANK])
    log_a = jax.nn.log_sigmoid((al @ alpha_up + alpha_b).astype(jnp.float32)) / GLA_LOGIT_NORM
    o = chunk_gla(_heads(q, GLA_HEADS) * GLA_DK ** -0.5, _heads(k, GLA_HEADS),
                  _heads(v, GLA_HEADS), _heads(log_a, GLA_HEADS), GLA_CHUNK)
    return _merge(_rms_norm(o, norm_g)) * jax.nn.silu(g)


def rwkv7_branch(cols, mu, w0, w2, a0, a2, g2, k_k, k_a, r_k, ln_g, ln_b):
    prev = jnp.pad(cols, ((0, 0), (1, 0), (0, 0)))[:, :-1]
    xs = cols + (prev - cols) * mu
    r, k, v, wl, al, gl = _split(xs, [BRANCH_W, BRANCH_W, BRANCH_W, RWKV_W_LORA, RWKV_A_LORA, RWKV_G_LORA])
    w_raw = -jax.nn.softplus(-(w0 + jnp.tanh(wl) @ w2)) - 0.5
    log_w = -jnp.exp(w_raw.astype(jnp.float32))
    a = jax.nn.sigmoid(a0 + al @ a2)
    g = jax.nn.sigmoid(gl) @ g2
    kk = _heads(k * k_k, RWKV_HEADS)
    kk = kk / jnp.maximum(jnp.sqrt(jnp.sum(jnp.square(kk), -1, keepdims=True)), 1e-12)
    k = k * (1 + (a - 1) * k_a)
    rh, kh, vh = _heads(r, RWKV_HEADS), _heads(k, RWKV_HEADS), _heads(v, RWKV_HEADS)
    y = rwkv7_scan(rh, _heads(log_w, RWKV_HEADS), kh, vh, kk, _heads(a, RWKV_HEADS))
    y = _merge(_layer_norm(y, eps=RWKV_GN_EPS)) * ln_g + ln_b
    bonus = jnp.sum(rh * kh * r_k[:, None, :], -1, keepdims=True) * vh
    return (y + _merge(bonus)) * g


def fox_branch(cols, f_bias):
    q, k, v, fl = _split(cols, [BRANCH_W, BRANCH_W, BRANCH_W, FOX_HEADS])
    log_f = jax.nn.log_sigmoid((fl + f_bias).astype(jnp.float32)).transpose(0, 2, 1)
    o = fox_attention(_heads(q, FOX_HEADS), _heads(k, FOX_HEADS), _heads(v, FOX_HEADS), log_f)
    return _merge(o)


def hgrn2_branch(cols, lb, norm_g):
    q, fg, i_in, g = _split(cols, [HGRN_KW, HGRN_KW, BRANCH_W, BRANCH_W])
    fg = fg.astype(jnp.float32)
    lbf = lb.astype(jnp.float32)
    f = lbf + (1 - lbf) * jax.nn.sigmoid(fg)
    log_f = jnp.log(f)
    one_minus_f = (1 - lbf) * jax.nn.sigmoid(-fg)
    o = chunk_gla(_heads(q, HGRN_HEADS), _heads(one_minus_f, HGRN_HEADS),
                  _heads(i_in, HGRN_HEADS), _heads(log_f, HGRN_HEADS), HGRN_CHUNK)
    return _merge(_rms_norm(o, norm_g)) * jax.nn.silu(g)


def grouped_moe(h, router_w, router_b, w_g, w_u, w_d):
    bsz, t_len, _ = h.shape
    probs = jax.nn.softmax((h @ router_w).astype(jnp.float32), axis=-1)
    sel = (probs + router_b).reshape(bsz, t_len, N_GROUPS, EXPERTS_PER_GROUP)
    group_score = lax.top_k(sel, GROUP_SCORE_TOPK)[0].sum(-1)
    g_sel = jnp.argmax(group_score, axis=-1)
    in_group = jnp.take_along_axis(sel, g_sel[..., None, None], axis=2)[..., 0, :]
    _, local = lax.top_k(in_group, TOP_K)
    idx = g_sel[..., None] * EXPERTS_PER_GROUP + local
    w_sel = jnp.take_along_axis(probs, idx, axis=-1)
    w_sel = w_sel / jnp.sum(w_sel, -1, keepdims=True)
    combine = jnp.sum(jax.nn.one_hot(idx, N_EXPERTS, dtype=jnp.float32) * w_sel[..., None], axis=-2)
    combine = combine.astype(h.dtype)
    y = jnp.zeros_like(h)
    for e in range(N_EXPERTS):
        he = jax.nn.silu(h @ w_g[e]) * (h @ w_u[e])
        y = y + combine[..., e:e + 1] * (he @ w_d[e])
    return y


def setup_inputs(seed: int = 0) -> dict:
    key = jax.random.key(seed)
    ks = jax.random.split(key, 64)
    counter = [0]

    def nk():
        counter[0] += 1
        return ks[counter[0]]

    def nrm(shape, scale):
        return scale * jax.random.normal(nk(), shape, jnp.float32)

    gate_offset = jnp.zeros((N_ADA, 1), jnp.float32).at[jnp.array([2, 5])].set(1.0)
    return {
        "x": nrm((BATCH, SEQ, D_MODEL), 1.0),
        "c": nrm((BATCH, D_MODEL), 1.0),
        "ada_w": nrm((DEPTH, D_MODEL, N_ADA * D_MODEL), 0.1 * D_MODEL ** -0.5),
        "ada_b": nrm((DEPTH, N_ADA, D_MODEL), 0.02) + gate_offset[None],
        "w_in": nrm((DEPTH, D_MODEL, N_IN_COLS), D_MODEL ** -0.5),
        "gla_alpha_up": nrm((DEPTH, GLA_GATE_RANK, GLA_KW), GLA_GATE_RANK ** -0.5),
        "gla_alpha_b": nrm((DEPTH, GLA_KW), 0.5),
        "gla_norm_g": 1.0 + nrm((DEPTH, GLA_DV), 0.05),
        "rwkv_mu": jax.random.uniform(nk(), (DEPTH, RWKV_COLS), jnp.float32),
        "rwkv_w0": jax.random.uniform(nk(), (DEPTH, BRANCH_W), jnp.float32, -6.0, -1.0),
        "rwkv_w2": nrm((DEPTH, RWKV_W_LORA, BRANCH_W), 0.1 * RWKV_W_LORA ** -0.5),
        "rwkv_a0": nrm((DEPTH, BRANCH_W), 0.3),
        "rwkv_a2": nrm((DEPTH, RWKV_A_LORA, BRANCH_W), RWKV_A_LORA ** -0.5),
        "rwkv_g2": nrm((DEPTH, RWKV_G_LORA, BRANCH_W), RWKV_G_LORA ** -0.5),
        "rwkv_k_k": 0.85 + nrm((DEPTH, BRANCH_W), 0.05),
        "rwkv_k_a": 1.0 + nrm((DEPTH, BRANCH_W), 0.05),
        "rwkv_r_k": nrm((DEPTH, RWKV_HEADS, RWKV_HEAD), 0.1),
        "rwkv_ln_g": 1.0 + nrm((DEPTH, BRANCH_W), 0.05),
        "rwkv_ln_b": nrm((DEPTH, BRANCH_W), 0.02),
        "fox_f_bias": 2.0 + nrm((DEPTH, FOX_HEADS), 0.5),
        "hgrn_lb_logits": nrm((DEPTH, HGRN_KW), 1.0),
        "hgrn_norm_g": 1.0 + nrm((DEPTH, HGRN_DV), 0.05),
        "w_br": nrm((DEPTH, N_BRANCH, BRANCH_W, D_MODEL), DN_BETA * BRANCH_W ** -0.5),
        "w_gate": nrm((DEPTH, N_BRANCH, D_MODEL, D_MODEL), D_MODEL ** -0.5),
        "b_gate": nrm((DEPTH, N_BRANCH, D_MODEL), 0.1),
        "w_o": nrm((DEPTH, D_MODEL, D_MODEL), DN_BETA * D_MODEL ** -0.5),
        "ln1_g": 1.0 + nrm((DEPTH, D_MODEL), 0.05),
        "ln1_b": nrm((DEPTH, D_MODEL), 0.02),
        "router_w": nrm((D_MODEL, N_EXPERTS), D_MODEL ** -0.5),
        "router_b": nrm((N_EXPERTS,), 0.01),
        "exp_w_gate": nrm((DEPTH, N_EXPERTS, D_MODEL, D_FF_EXPERT), D_MODEL ** -0.5),
        "exp_w_up": nrm((DEPTH, N_EXPERTS, D_MODEL, D_FF_EXPERT), DN_BETA * D_MODEL ** -0.5),
        "exp_w_down": nrm((DEPTH, N_EXPERTS, D_FF_EXPERT, D_MODEL), DN_BETA * D_FF_EXPERT ** -0.5),
        "ln2_g": 1.0 + nrm((DEPTH, D_MODEL), 0.05),
        "ln2_b": nrm((DEPTH, D_MODEL), 0.02),
    }


def reference(x, c, ada_w, ada_b, w_in, gla_alpha_up, gla_alpha_b, gla_norm_g,
              rwkv_mu, rwkv_w0, rwkv_w2, rwkv_a0, rwkv_a2, rwkv_g2, rwkv_k_k, rwkv_k_a,
              rwkv_r_k, rwkv_ln_g, rwkv_ln_b, fox_f_bias, hgrn_lb_logits, hgrn_norm_g,
              w_br, w_gate, b_gate, w_o, ln1_g, ln1_b, router_w, router_b,
              exp_w_gate, exp_w_up, exp_w_down, ln2_g, ln2_b):
    bsz = x.shape[0]
    p_lb = jax.nn.softmax(hgrn_lb_logits.astype(jnp.float32), axis=0)
    lb_all = jnp.cumsum(p_lb, axis=0) - p_lb[0]
    cond = jax.nn.silu(c)
    for i in range(DEPTH):
        mod = (cond @ ada_w[i]).reshape(bsz, N_ADA, D_MODEL) + ada_b[i]
        sh1, sc1, gt1, sh2, sc2, gt2 = (mod[:, j, None, :] for j in range(N_ADA))

        h = _layer_norm(x) * (1 + sc1) + sh1
        proj = h @ w_in[i]
        a_cols, b_cols, c_cols, d_cols = _split(proj, [GLA_COLS, RWKV_COLS, FOX_COLS, HGRN_COLS])
        branches = (
            gla_branch(a_cols, gla_alpha_up[i], gla_alpha_b[i], gla_norm_g[i]),
            rwkv7_branch(b_cols, rwkv_mu[i], rwkv_w0[i], rwkv_w2[i], rwkv_a0[i], rwkv_a2[i],
                         rwkv_g2[i], rwkv_k_k[i], rwkv_k_a[i], rwkv_r_k[i], rwkv_ln_g[i], rwkv_ln_b[i]),
            fox_branch(c_cols, fox_f_bias[i]),
            hgrn2_branch(d_cols, lb_all[i], hgrn_norm_g[i]),
        )
        merged = sum(jax.nn.sigmoid(h @ w_gate[i, n] + b_gate[i, n]) * (br @ w_br[i, n])
                     for n, br in enumerate(branches))
        y = merged @ w_o[i]
        x = _layer_norm(DN_ALPHA * x + gt1 * y, ln1_g[i], ln1_b[i])

        h = _layer_norm(x) * (1 + sc2) + sh2
        y = grouped_moe(h, router_w, router_b, exp_w_gate[i], exp_w_up[i], exp_w_down[i])
        x = _layer_norm(DN_ALPHA * x + gt2 * y, ln2_g[i], ln2_b[i])
    return x
```

```python
import math
from contextlib import ExitStack

import numpy as np
import concourse.bass as bass
import concourse.mybir as mybir
from concourse.bass_utils import run_bass_kernel_spmd

F32 = mybir.dt.float32
BF16 = mybir.dt.bfloat16
AF = mybir.ActivationFunctionType
ALU = mybir.AluOpType
AX = mybir.AxisListType

ENGS = ("pe", "act", "dve", "pool", "sp")


class _Op:
    __slots__ = ("eng", "fn", "dma", "waits", "key", "val", "know", "inc")


def _region(ap):
    shape = list(ap.tensor.shape)
    off = int(ap.offset)
    if str(ap.space) == "DRAM":
        ext = 0
        for step, cnt in ap.ap:
            ext += (int(cnt) - 1) * abs(int(step))
        return (ap.name, 0, 1, off, off + ext + 1)
    if str(ap.space) == "PSUM":
        return (ap.name, 0, 128, 0, 1 << 30)
    ps = 1
    for s in shape[1:]:
        ps *= int(s)
    p0 = off // ps
    f0 = off % ps
    npart = 1
    ext = 0
    for step, cnt in ap.ap:
        step = int(step)
        cnt = int(cnt)
        if step == ps:
            npart = max(npart, cnt)
        elif step > ps:
            npart = max(npart, (cnt - 1) * (step // ps) + 1)
        else:
            ext += (cnt - 1) * step
    return (ap.name, p0, p0 + npart, f0, f0 + ext + 1)


class Sched:
    def __init__(self, nc, n_dma_sems=12):
        self.nc = nc
        self.streams = {e: [] for e in ENGS}
        self.clock = {e: {} for e in ENGS}
        self.cpos = {e: 0 for e in ENGS}
        self.last_op = {e: None for e in ENGS}
        self.n_dma_sems = n_dma_sems
        self.dma_uses = {}
        self.dma_next = {e: 0 for e in ENGS}
        self.dma_last = {}
        self.acc = {}
        self.readonly = set()
        self.pe_bank = {}
        self.epoch = 0
        self.n_ops = 0

    def _add(self, eng, fn, reads, writes, dma, force=()):
        op = _Op()
        op.eng, op.fn, op.dma = eng, fn, dma
        clk = self.clock[eng]
        need = {}

        def dep(d, forced=False):
            if (not forced) and (not dma) and (not d.dma) and d.eng == "pe" and eng == "pe":
                return
            if clk.get(d.key, 0) >= d.val:
                return
            if need.get(d.key, 0) < d.val:
                need[d.key] = d.val
            for k, v in d.know.items():
                if clk.get(k, 0) < v:
                    clk[k] = v

        for d_ in force:
            dep(d_, True)
        regs = []
        for ap in reads:
            if ap.name in self.readonly:
                continue
            regs.append((_region(ap), False))
        for ap in writes:
            regs.append((_region(ap), True))
        for (name, p0, p1, f0, f1), isw in regs:
            lst = self.acc.get(name)
            if lst is None:
                continue
            for r in lst:
                if r[4] is op:
                    continue
                if r[0] < p1 and p0 < r[1] and r[2] < f1 and f0 < r[3]:
                    if isw or r[5] or (f1 == (1 << 30) and r[4].eng != eng):
                        dep(r[4])
        if dma:
            slot = self.dma_next[eng]
            self.dma_next[eng] = (slot + 1) % self.n_dma_sems
            k = ("s", eng, slot)
            prev = self.dma_last.get(k)
            if prev is not None:
                dep(prev)
            cnt = self.dma_uses.get(k, 0) + 1
            self.dma_uses[k] = cnt
            op.key, op.val, op.inc = k, 16 * cnt, 16
            self.dma_last[k] = op
        else:
            self.cpos[eng] += 1
            op.key, op.val, op.inc = ("e", eng, self.epoch), self.cpos[eng], 1
        for k, v in need.items():
            if clk.get(k, 0) < v:
                clk[k] = v
        op.waits = list(need.items())
        op.know = dict(clk)
        self.streams[eng].append(op)
        self.last_op[eng] = op
        for (name, p0, p1, f0, f1), isw in regs:
            lst = self.acc.setdefault(name, [])
            if isw:
                lst[:] = [r for r in lst if not (p0 <= r[0] and r[1] <= p1 and f0 <= r[2] and r[3] <= f1)]
            else:
                if not dma:
                    lst[:] = [r for r in lst if not ((not r[5]) and (not r[4].dma) and r[4].eng == eng
                                                     and r[0] == p0 and r[1] == p1 and r[2] == f0 and r[3] == f1)]
            lst.append([p0, p1, f0, f1, op, isw])
        self.n_ops += 1
        return op

    def barrier(self):
        targets = []
        for e in ENGS:
            if self.cpos[e] > 0:
                targets.append((("e", e, self.epoch), self.cpos[e], self.last_op[e]))
        for k, o in self.dma_last.items():
            targets.append((k, o.val, o))
        for e in ENGS:
            clk = self.clock[e]
            op = _Op()
            op.eng, op.fn, op.dma = e, None, False
            need = {}
            for k, v, o in targets:
                if clk.get(k, 0) < v:
                    need[k] = v
                    clk[k] = v
            op.waits = list(need.items())
            op.key = None
            op.know = dict(clk)
            self.streams[e].append(op)
        self.acc = {}
        self.pe_bank = {}
        if max(self.cpos.values()) > 16000:
            self.epoch += 1
            self.cpos = {e: 0 for e in ENGS}

    def _pe_bank(self, out, lhsT):
        kpos = (int(lhsT.base_partition()) if hasattr(lhsT, "base_partition") else 0, int(lhsT.partition_size()))
        prev = self.pe_bank.get(out.name)
        force = ()
        if prev is not None and prev[0] != kpos:
            force = (prev[1],)
        return kpos, force

    def mm(self, out, lhsT, rhs, start=True, stop=True):
        rd = [lhsT, rhs] + ([] if start else [out])
        kpos, force = self._pe_bank(out, lhsT)
        op = self._add("pe", lambda e: e.matmul(out, lhsT, rhs, start=start, stop=stop), rd, [out], False, force=force)
        self.pe_bank[out.name] = (kpos, op)
        return op

    def tr(self, out, in_, ident):
        kpos, force = self._pe_bank(out, in_)
        op = self._add("pe", lambda e: e.transpose(out, in_, ident), [in_, ident], [out], False, force=force)
        self.pe_bank[out.name] = (kpos, op)
        return op

    def act(self, out, in_, func, bias=None, scale=None, accum_out=None):
        rd = [in_]
        kw = {}
        if bias is not None:
            kw["bias"] = bias
            if not isinstance(bias, (int, float)):
                rd.append(bias)
        if scale is not None:
            kw["scale"] = scale
            if not isinstance(scale, (int, float)):
                rd.append(scale)
        wr = [out]
        if accum_out is not None:
            kw["accum_out"] = accum_out
            wr.append(accum_out)
        return self._add("act", lambda e: e.activation(out, in_, func, **kw), rd, wr, False)

    def tt(self, out, in0, in1, op, eng="dve"):
        return self._add(eng, lambda e: e.tensor_tensor(out, in0, in1, op), [in0, in1], [out], False)

    def ts(self, out, in0, s1, s2, op0, op1=None, eng="dve", accum_out=None):
        rd = [in0]
        if not isinstance(s1, (int, float)):
            rd.append(s1)
        if s2 is not None and not isinstance(s2, (int, float)):
            rd.append(s2)
        wr = [out]
        kw = {}
        if accum_out is not None:
            kw["accum_out"] = accum_out
            wr.append(accum_out)
        if op1 is None:
            return self._add(eng, lambda e: e.tensor_scalar(out, in0, s1, None, op0, **kw), rd, wr, False)
        return self._add(eng, lambda e: e.tensor_scalar(out, in0, s1, s2, op0, op1, **kw), rd, wr, False)

    def stt(self, out, in0, scalar, in1, op0, op1):
        rd = [in0, in1]
        if not isinstance(scalar, (int, float)):
            rd.append(scalar)
        return self._add("dve", lambda e: e.scalar_tensor_tensor(out, in0, scalar, in1, op0, op1), rd, [out], False)

    def copy(self, out, in_, eng="dve"):
        if eng == "act":
            return self._add("act", lambda e: e.copy(out, in_), [in_], [out], False)
        return self._add(eng, lambda e: e.tensor_copy(out, in_), [in_], [out], False)

    def memset(self, out, val, eng="dve"):
        return self._add(eng, lambda e: e.memset(out, val), [], [out], False)

    def reduce(self, out, in_, op, axis=AX.X, eng="dve"):
        return self._add(eng, lambda e: e.tensor_reduce(out, in_, axis, op), [in_], [out], False)

    def bn_stats(self, out, in_):
        return self._add("dve", lambda e: e.bn_stats(out, in_), [in_], [out], False)

    def bn_aggr(self, out, in_):
        return self._add("dve", lambda e: e.bn_aggr(out, in_), [in_], [out], False)

    def recip(self, out, in_):
        return self._add("dve", lambda e: e.reciprocal(out, in_), [in_], [out], False)

    def dma(self, out, in_, eng="sp", **kw):
        return self._add(eng, lambda e: e.dma_start(out=out, in_=in_, **kw), [in_], [out], True)

    def emit(self):
        nc = self.nc
        with ExitStack() as es:
            sems = {}
            for e in ENGS:
                for op in self.streams[e]:
                    if op.fn is not None and (not op.dma) and op.key not in sems:
                        sems[op.key] = es.enter_context(nc.semaphore("c_%s_%d" % (op.key[1], op.key[2])))
            for k in self.dma_uses:
                sems[k] = es.enter_context(nc.semaphore("d_%s_%d" % (k[1], k[2])))
            final = [(k, o.val) for k, o in self.dma_last.items()]
            for e in ENGS:
                if self.cpos[e] > 0:
                    final.append((("e", e, self.epoch), self.cpos[e]))
            block = es.enter_context(nc.Block())
            streams = self.streams

            def run(engname, e):
                for op in streams[engname]:
                    for k, v in op.waits:
                        e.wait_ge(sems[k], v)
                    if op.fn is not None:
                        op.fn(e).then_inc(sems[op.key], op.inc)
                if engname == "sp":
                    for k, v in final:
                        e.wait_ge(sems[k], v)

            @block.tensor
            def _(e):
                run("pe", e)

            @block.scalar
            def _(e):
                run("act", e)

            @block.vector
            def _(e):
                run("dve", e)

            @block.gpsimd
            def _(e):
                run("pool", e)

            @block.sync
            def _(e):
                run("sp", e)


DBG = {}
T = 4096
D = 1024
NT = 32
DEPTH = 2
NCOLS = 6936
GLA_OFF, RWKV_OFF, FOX_OFF, HGRN_OFF = 0, 1552, 3344, 4888
DN_ALPHA = (2.0 * DEPTH) ** 0.25
NE = 16


def make_consts():
    c = {}
    i = np.arange(128)
    same = (i[:, None] // 64) == (i[None, :] // 64)
    c["ident"] = np.eye(128, dtype=np.float32)
    c["ones"] = np.ones((128, 128), np.float32)
    c["triu"] = (i[:, None] <= i[None, :]).astype(np.float32)
    c["triu64"] = ((i[:, None] <= i[None, :]) & same).astype(np.float32)
    c["sup64"] = ((i[:, None] < i[None, :]) & same).astype(np.float32)
    c["slo64"] = ((i[:, None] > i[None, :]) & same).astype(np.float32)
    c["blk64"] = same.astype(np.float32)
    return c


CONST_NAMES = ["ident", "ones", "triu", "triu64", "sup64", "slo64", "blk64"]

PARAMS = [
    ("c", [1, D]), ("ada_w", [2, D, 6 * D]), ("ada_b", [2, 6, D]), ("w_in", [2, D, NCOLS]),
    ("gla_alpha_up", [2, 16, 256]), ("gla_alpha_b", [2, 256]), ("gla_norm_g", [2, 128]),
    ("rwkv_mu", [2, 1792]), ("rwkv_w0", [2, 512]), ("rwkv_w2", [2, 64, 512]), ("rwkv_a0", [2, 512]),
    ("rwkv_a2", [2, 64, 512]), ("rwkv_g2", [2, 128, 512]), ("rwkv_k_k", [2, 512]), ("rwkv_k_a", [2, 512]),
    ("rwkv_r_k", [2, 8, 64]), ("rwkv_ln_g", [2, 512]), ("rwkv_ln_b", [2, 512]), ("fox_f_bias", [2, 8]),
    ("hgrn_lb_logits", [2, 512]), ("hgrn_norm_g", [2, 128]), ("w_br", [2, 4, 512, D]),
    ("w_gate", [2, 4, D, D]), ("b_gate", [2, 4, D]), ("w_o", [2, D, D]), ("ln1_g", [2, D]), ("ln1_b", [2, D]),
    ("router_w", [D, NE]), ("router_b", [NE]), ("exp_w_gate", [2, NE, D, 512]), ("exp_w_up", [2, NE, D, 512]),
    ("exp_w_down", [2, NE, 512, D]), ("ln2_g", [2, D]), ("ln2_b", [2, D]),
]


class KB:
    def __init__(self, io=None):
        self.nc = bass.Bass("TRN2", target_bir_lowering=False)
        self.S = Sched(self.nc)
        self.io = io or {}
        self.d = {}
        nc = self.nc
        self.x = self.ext_in("x", [T, D], F32)
        for n, shp in PARAMS:
            self.d[n] = self.ext_in(n, shp, F32)
        self.cst_d = {n: self.ext_in("k_" + n, [128, 128], F32) for n in CONST_NAMES}
        self.psb = [nc.alloc_psum_tensor("psb%d" % i, [128, 512], F32) for i in range(8)]
        self.cst = {n: nc.alloc_sbuf_tensor("c_" + n, [128, 128], F32) for n in CONST_NAMES}
        for n in CONST_NAMES:
            self.S.dma(self.cst[n][:], self.cst_d[n])
        self.hT = None

    def ext_in(self, name, shape, dt):
        self.S.readonly.add(name)
        return self.nc.dram_tensor(name, list(shape), dt, kind="ExternalInput").ap()

    def dram(self, name, shape, dt):
        role = self.io.get(name)
        if role == "in":
            return self.nc.dram_tensor(name, list(shape), dt, kind="ExternalInput").ap()
        if role == "out" or name == "out":
            return self.nc.dram_tensor(name, list(shape), dt, kind="ExternalOutput").ap()
        return self.nc.dram_tensor(name, list(shape), dt).ap()


class Pool_:
    def __init__(self, kb):
        self.kb = kb
        self.es = ExitStack()

    _uid = [0]

    def sb(self, name, shape, dt=F32):
        Pool_._uid[0] += 1
        return self.es.enter_context(self.kb.nc.sbuf_tensor("%s_u%d" % (name, Pool_._uid[0]), list(shape), dt))

    def close(self):
        self.kb.S.barrier()
        self.es.close()


def phase_mod(kb, mod_d):
    S = kb.S
    P = Pool_(kb)
    condT = P.sb("condT", [128, 8])
    load_T(kb, P, condT[:], kb.d["c"].rearrange("o (c p) -> (o c) p", p=128), 8)
    S.act(condT[:], condT[:], AF.Silu)
    wst = [P.sb("adaw%d" % k, [128, 3072]) for k in range(2)]
    mrow = P.sb("mrow", [1, 6144])
    brow = P.sb("brow", [1, 6144])
    n = 0
    for i in range(2):
        S.dma(brow[:], kb.d["ada_b"][i:i + 1].rearrange("o j d -> o (j d)"))
        for half in range(2):
            for kc in range(8):
                w = wst[n % 2]
                n += 1
                S.dma(w[:], kb.d["ada_w"][i, kc * 128:(kc + 1) * 128, half * 3072:(half + 1) * 3072],
                      eng="sp" if n % 2 else "pool")
                for b in range(6):
                    S.mm(kb.psb[b][0:1, :], condT[:, kc:kc + 1], w[:, b * 512:(b + 1) * 512],
                         start=(kc == 0), stop=(kc == 7))
            for b in range(6):
                o = half * 3072 + b * 512
                S.tt(mrow[0:1, o:o + 512], kb.psb[b][0:1, :], brow[0:1, o:o + 512], ALU.add)
        for j in (1, 4):
            S.ts(mrow[0:1, j * 1024:(j + 1) * 1024], mrow[0:1, j * 1024:(j + 1) * 1024], 1.0, None, ALU.add)
        S.dma(mod_d[i:i + 1, :], mrow[:])
    P.close()


def rsqrt_eps(S, out, in_, eps, scale=1.0):
    S.act(out, in_, AF.Ln, bias=float(eps), scale=float(scale))
    S.act(out, out, AF.Exp, scale=-0.5)


def ln_stats(S, xin, st, mv, rstd, eps=1e-5):
    S.bn_stats(st[:, 0:6], xin[:, 0:512])
    S.bn_stats(st[:, 6:12], xin[:, 512:1024])
    S.bn_aggr(mv[:], st[:])
    rsqrt_eps(S, rstd[:], mv[:, 1:2], eps)


def load_T(kb, P, dst, src_rows, n, psum=None):
    S = kb.S
    tmp = P.sb("ldT_tmp", [n, 128])
    S.dma(tmp[:], src_rows)
    ps = kb.psb[7] if psum is None else psum
    S.mm(ps[:, 0:n], tmp[:], kb.cst["ident"][0:n, 0:n], start=True, stop=True)
    S.copy(dst, ps[:, 0:n])


def load_modT(kb, P, mod_d, layer, name):
    modT = P.sb(name, [128, 6, 8])
    load_T(kb, P, modT[:].rearrange("p j c -> p (j c)"), mod_d[layer].rearrange("(r p) -> r p", p=128), 48)
    return modT


def phase_ln_mixer(kb, x_src, mod_d, layer):
    S = kb.S
    P = Pool_(kb)
    modT = load_modT(kb, P, mod_d, layer, "modT_a")
    xb = [P.sb("lnx%d" % k, [128, 1024]) for k in range(2)]
    st = [P.sb("lnst%d" % k, [128, 12]) for k in range(2)]
    mv = [P.sb("lnmv%d" % k, [128, 2]) for k in range(2)]
    rs = [P.sb("lnrs%d" % k, [128, 1]) for k in range(2)]
    ident = kb.cst["ident"]
    for t in range(DBG.get("ln_nt", NT)):
        k = t % 2
        xin = xb[k]
        S.dma(xin[:], x_src[t * 128:(t + 1) * 128, :])
        if DBG.get("ln_lvl", 9) < 1:
            continue
        ln_stats(S, xin, st[k], mv[k], rs[k])
        S.ts(xin[:], xin[:], mv[k][:, 0:1], rs[k][:, 0:1], ALU.subtract, ALU.mult)
        if DBG.get("ln_lvl", 9) < 2:
            continue
        for c in range(8):
            pb = kb.psb[(t % 2) * 2 + c // 4]
            S.tr(pb[:, (c % 4) * 128:(c % 4 + 1) * 128], xin[:, c * 128:(c + 1) * 128], ident[:])
        if DBG.get("ln_lvl", 9) < 3:
            continue
        for c in range(DBG.get("ln_nc", 8)):
            pb = kb.psb[(t % 2) * 2 + c // 4]
            src = pb[:, (c % 4) * 128:(c % 4 + 1) * 128]
            off = DBG.get("ln_off", 1)
            dst = kb.hT[:, c, off + t * 128:off + (t + 1) * 128]
            ev = DBG.get("ln_evac", "both")
            if (c % 2 == 0 and ev == "both") or ev == "dve":
                S.ts(dst, src, modT[:, 1, c:c + 1], modT[:, 0, c:c + 1], ALU.mult, ALU.add)
            else:
                S.act(dst, src, AF.Identity, bias=modT[:, 0, c:c + 1], scale=modT[:, 1, c:c + 1])
    P.close()


def prep_cast(kb, P, jobs, stg, stb):
    S = kb.S
    n = 0
    for dst, src in jobs:
        R, N = src.shape
        for r in range(0, R, 128):
            a, b = stg[n % 2], stb[n % 2]
            n += 1
            S.dma(a[:, 0:N], src[r:r + 128, :], eng="sp")
            S.copy(b[:, 0:N], a[:, 0:N], eng="pool" if n % 2 else "act")
            S.dma(dst[r:r + 128, :], b[:, 0:N], eng="pool")


class Epi:
    def __init__(self, kb, P, mod_d, layer, gt_idx, g_name, b_name, tag, with_z=True):
        S = kb.S
        self.kb = kb
        self.gt = P.sb("epi_gt" + tag, [128, 1024])
        self.g = P.sb("epi_g" + tag, [128, 1024])
        self.b = P.sb("epi_b" + tag, [128, 1024])
        S.dma(self.gt[:], mod_d[layer:layer + 1, gt_idx * 1024:(gt_idx + 1) * 1024].partition_broadcast(128))
        S.dma(self.g[:], kb.d[g_name][layer:layer + 1, :].partition_broadcast(128))
        S.dma(self.b[:], kb.d[b_name][layer:layer + 1, :].partition_broadcast(128))
        self.xb = [P.sb("epi_x%s%d" % (tag, k), [128, 1024]) for k in range(2)]
        self.zb = [P.sb("epi_z%s%d" % (tag, k), [128, 1024]) for k in range(2)] if with_z else None
        self.st = [P.sb("epi_st%s%d" % (tag, k), [128, 12]) for k in range(2)]
        self.mv = [P.sb("epi_mv%s%d" % (tag, k), [128, 2]) for k in range(2)]
        self.rs = [P.sb("epi_rs%s%d" % (tag, k), [128, 1]) for k in range(2)]
        self.n = 0

    def prefetch_x(self, x_src, t):
        k = self.n % 2
        self.kb.S.dma(self.xb[k][:], x_src[t * 128:(t + 1) * 128, :])

    def run(self, y_halves, x_dst, t, x_src=None, z=None):
        S = self.kb.S
        k = self.n % 2
        self.n += 1
        if x_src is not None:
            S.dma(self.xb[k][:], x_src[t * 128:(t + 1) * 128, :])
        x = self.xb[k]
        if z is None:
            z = self.zb[k]
        for h in range(2):
            S.tt(z[:, h * 512:(h + 1) * 512], y_halves[h], self.gt[:, h * 512:(h + 1) * 512], ALU.mult)
        S.stt(z[:], x[:], DN_ALPHA, z[:], ALU.mult, ALU.add)
        ln_stats(S, z, self.st[k], self.mv[k], self.rs[k])
        S.ts(z[:], z[:], self.mv[k][:, 0:1], self.rs[k][:, 0:1], ALU.subtract, ALU.mult)
        S.tt(z[:], z[:], self.g[:], ALU.mult, eng="pool")
        S.tt(z[:], z[:], self.b[:], ALU.add, eng="pool")
        S.dma(x_dst[t * 128:(t + 1) * 128, :], z[:], eng="pool")


def phase_prep_merge(kb, layer, wg_d, wb_d, wo_d):
    P = Pool_(kb)
    stg = [P.sb("pst%d" % k, [128, 1024]) for k in range(2)]
    stb = [P.sb("psb%d" % k, [128, 1024], BF16) for k in range(2)]
    jobs = []
    for n in range(4):
        jobs.append((wg_d[n], kb.d["w_gate"][layer, n]))
        jobs.append((wb_d[n], kb.d["w_br"][layer, n]))
    jobs.append((wo_d, kb.d["w_o"][layer]))
    prep_cast(kb, P, jobs, stg, stb)
    P.close()


def phase_merge(kb, layer, mod_d, brT_d, wg_d, wb_d, wo_d, x_src, x_dst):
    S = kb.S
    P = Pool_(kb)
    epi = Epi(kb, P, mod_d, layer, 2, "ln1_g", "ln1_b", "m")
    bgT = P.sb("bgT", [128, 4, 8])
    load_T(kb, P, bgT[:].rearrange("p n c -> p (n c)"), kb.d["b_gate"][layer].rearrange("n (c p) -> (n c) p", p=128), 32)
    wo = P.sb("wo", [128, 8, 1024], BF16)
    S.dma(wo[:], wo_d.rearrange("(c p) n -> p c n", p=128))
    wg = [P.sb("wg%d" % k, [128, 8, 1024], BF16) for k in range(2)]
    wb = [P.sb("wb%d" % k, [128, 4, 1024], BF16) for k in range(2)]
    brt = [P.sb("brt%d" % k, [128, 4, 512], BF16) for k in range(2)]
    mT = P.sb("mT", [128, 8, 512])
    mTb = P.sb("mTb", [128, 8, 512], BF16)
    sig = [P.sb("sig%d" % k, [128, 512]) for k in range(2)]
    tmp = [P.sb("mtmp%d" % k, [128, 512]) for k in range(2)]
    cnt = 0
    q = 0
    for g in range(8):
        tok = slice(g * 512, (g + 1) * 512)
        for n in range(4):
            k = cnt % 2
            cnt += 1
            S.dma(wg[k][:], wg_d[n].rearrange("(c p) n -> p c n", p=128), eng="sp")
            S.dma(wb[k][:], wb_d[n].rearrange("(c p) n -> p c n", p=128), eng="sp")
            S.dma(brt[k][:], brT_d[n, :, :, tok].rearrange("c p t -> p c t"), eng="sp")
            for cc in range(8):
                pa = kb.psb[(q % 2) * 2]
                pb = kb.psb[(q % 2) * 2 + 1]
                for kc in range(8):
                    S.mm(pa[:], wg[k][:, kc, cc * 128:(cc + 1) * 128], kb.hT[:, kc, 1 + g * 512:1 + (g + 1) * 512],
                         start=(kc == 0), stop=(kc == 7))
                for kc in range(4):
                    S.mm(pb[:], wb[k][:, kc, cc * 128:(cc + 1) * 128], brt[k][:, kc, :],
                         start=(kc == 0), stop=(kc == 3))
                sg = sig[q % 2]
                S.act(sg[:], pa[:], AF.Sigmoid, bias=bgT[:, n, cc:cc + 1])
                if n == 0:
                    S.tt(mT[:, cc, :], sg[:], pb[:], ALU.mult)
                else:
                    tp = tmp[q % 2]
                    S.tt(tp[:], sg[:], pb[:], ALU.mult)
                    S.tt(mT[:, cc, :], mT[:, cc, :], tp[:], ALU.add, eng="pool")
                q += 1
        for cc in range(8):
            S.copy(mTb[:, cc, :], mT[:, cc, :], eng="act" if cc % 2 else "pool")
        for tt in range(4):
            t = g * 4 + tt
            epi.prefetch_x(x_src, t)
            ys = []
            for h in range(2):
                py = kb.psb[4 + (t % 2) * 2 + h]
                for kc in range(8):
                    S.mm(py[:], mTb[:, kc, tt * 128:(tt + 1) * 128], wo[:, kc, h * 512:(h + 1) * 512],
                         start=(kc == 0), stop=(kc == 7))
                ys.append(py[:])
            epi.run(ys, x_dst, t)
    P.close()


def phase_moe(kb, layer, mod_d, x_src, x_dst):
    S = kb.S
    P = Pool_(kb)
    NSG = 2
    TSG = T // NSG
    NTS = TSG // 128
    epi = Epi(kb, P, mod_d, layer, 5, "ln2_g", "ln2_b", "e", with_z=False)
    modT = load_modT(kb, P, mod_d, layer, "modT_e")
    rw = P.sb("rw", [128, 8, NE])
    S.dma(rw[:], kb.d["router_w"].rearrange("(c p) e -> p c e", p=128))
    rb = P.sb("rb", [128, NE])
    S.dma(rb[:], kb.d["router_b"].rearrange("(o e) -> o e", o=1).partition_broadcast(128))
    hT = P.sb("hTm", [128, 8, TSG + 1], BF16)
    yacc = P.sb("yacc", [128, NTS, 1024])
    comb = P.sb("comb", [128, NTS, NE])
    h32 = [P.sb("h32_%d" % k, [128, 8, 128]) for k in range(2)]
    st = [P.sb("mst%d" % k, [128, 12]) for k in range(2)]
    mv = [P.sb("mmv%d" % k, [128, 2]) for k in range(2)]
    rs = [P.sb("mrs%d" % k, [128, 1]) for k in range(2)]
    lg = P.sb("r_lg", [128, NE])
    pr = P.sb("r_pr", [128, NE])
    sel = P.sb("r_sel", [128, NE])
    sel2 = P.sb("r_sel2", [128, NE])
    eq = P.sb("r_eq", [128, NE])
    m1 = P.sb("r_m1", [128, 4])
    m2 = P.sb("r_m2", [128, 4])
    gs = P.sb("r_gs", [128, 4])
    gm = P.sb("r_gm", [128, 1])
    og = P.sb("r_og", [128, 4])
    thr = P.sb("r_thr", [128, 4])
    msk = P.sb("r_msk", [128, NE])
    sm = P.sb("r_sm", [128, 1])
    mx = P.sb("r_mx", [128, 1])
    wg = [P.sb("ewg%d" % k, [128, 8, 512], BF16) for k in range(2)]
    wu = [P.sb("ewu%d" % k, [128, 8, 512], BF16) for k in range(2)]
    wd = [P.sb("ewd%d" % k, [128, 4, 1024], BF16) for k in range(2)]
    stg = [P.sb("estg%d" % k, [128, 2, 512]) for k in range(3)]
    heT = [P.sb("heT%d" % k, [128, 4, 512], BF16) for k in range(2)]
    sl = [P.sb("esl%d" % k, [128, 512]) for k in range(2)]
    ident = kb.cst["ident"]
    nld = [0]

    def load_expert(e, k):
        for (dst, src, kcn) in ((wg[k], kb.d["exp_w_gate"][layer, e], 8), (wu[k], kb.d["exp_w_up"][layer, e], 8)):
            sv = src.rearrange("(c p) n -> p c n", p=128)
            for c2 in range(0, kcn, 2):
                sg_ = stg[nld[0] % 3]
                nld[0] += 1
                S.dma(sg_[:], sv[:, c2:c2 + 2, :], eng="sp")
                S.copy(dst[:, c2:c2 + 2, :], sg_[:], eng="pool")
        sv = kb.d["exp_w_down"][layer, e].rearrange("(c p) n -> p c n", p=128)
        for c in range(4):
            sg_ = stg[nld[0] % 3]
            nld[0] += 1
            S.dma(sg_[:].rearrange("p a b -> p (a b)"), sv[:, c, :], eng="sp")
            S.copy(wd[k][:, c, :], sg_[:].rearrange("p a b -> p (a b)"), eng="pool")

    for sgi in range(NSG):
        t0 = sgi * NTS
        for tl in range(NTS):
            t = t0 + tl
            k = tl % 2
            xin = epi.xb[k]
            S.dma(xin[:], x_src[t * 128:(t + 1) * 128, :])
            ln_stats(S, xin, st[k], mv[k], rs[k])
            S.ts(xin[:], xin[:], mv[k][:, 0:1], rs[k][:, 0:1], ALU.subtract, ALU.mult)
            for c in range(8):
                pb = kb.psb[k * 2 + c // 4]
                S.tr(pb[:, (c % 4) * 128:(c % 4 + 1) * 128], xin[:, c * 128:(c + 1) * 128], ident[:])
            for c in range(8):
                pb = kb.psb[k * 2 + c // 4]
                src = pb[:, (c % 4) * 128:(c % 4 + 1) * 128]
                if c % 2 == 0:
                    S.ts(h32[k][:, c, :], src, modT[:, 4, c:c + 1], modT[:, 3, c:c + 1], ALU.mult, ALU.add)
                else:
                    S.act(h32[k][:, c, :], src, AF.Identity, bias=modT[:, 3, c:c + 1], scale=modT[:, 4, c:c + 1])
                S.copy(hT[:, c, 1 + tl * 128:1 + (tl + 1) * 128], h32[k][:, c, :], eng="pool")
            pl = kb.psb[4 + k]
            for c in range(8):
                S.mm(pl[:, 0:NE], h32[k][:, c, :], rw[:, c, :], start=(c == 0), stop=(c == 7))
            S.copy(lg[:], pl[:, 0:NE])
            S.reduce(mx[:], lg[:], ALU.max)
            S.ts(mx[:], mx[:], -1.0, None, ALU.mult)
            S.act(pr[:], lg[:], AF.Exp, bias=mx[:, 0:1], scale=1.0, accum_out=sm[:])
            S.recip(sm[:], sm[:])
            S.ts(pr[:], pr[:], sm[:, 0:1], None, ALU.mult)
            S.tt(sel[:], pr[:], rb[:], ALU.add)
            sel3 = sel[:].rearrange("p (g e) -> p g e", g=4)
            S.reduce(m1[:], sel3, ALU.max)
            S.tt(eq[:].rearrange("p (g e) -> p g e", g=4), sel3, m1[:].unsqueeze(2).to_broadcast([128, 4, 4]), ALU.is_ge)
            S.stt(sel2[:], eq[:], -1e9, sel[:], ALU.mult, ALU.add)
            S.reduce(m2[:], sel2[:].rearrange("p (g e) -> p g e", g=4), ALU.max)
            S.tt(gs[:], m1[:], m2[:], ALU.add)
            S.reduce(gm[:], gs[:], ALU.max)
            S.ts(og[:], gs[:], gm[:, 0:1], None, ALU.is_ge)
            S.ts(thr[:], og[:], -1e9, 1e9, ALU.mult, ALU.add)
            S.tt(thr[:], thr[:], m2[:], ALU.add)
            S.tt(msk[:].rearrange("p (g e) -> p g e", g=4), sel3, thr[:].unsqueeze(2).to_broadcast([128, 4, 4]), ALU.is_ge)
            S.tt(msk[:], msk[:], pr[:], ALU.mult)
            S.reduce(sm[:], msk[:], ALU.add)
            S.recip(sm[:], sm[:])
            S.ts(comb[:, tl, :], msk[:], sm[:, 0:1], None, ALU.mult)
        if sgi == 0:
            load_expert(0, 0)
        q = 0
        for e in range(NE):
            k = (sgi * NE + e) % 2
            nxt = sgi * NE + e + 1
            if nxt < NSG * NE:
                load_expert(nxt % NE, nxt % 2)
            for gq in range(NTS // 4):
                he = heT[gq % 2]
                for fc in range(4):
                    pg = kb.psb[(q % 2) * 2]
                    pu = kb.psb[(q % 2) * 2 + 1]
                    for kc in range(8):
                        S.mm(pg[:], wg[k][:, kc, fc * 128:(fc + 1) * 128], hT[:, kc, 1 + gq * 512:1 + (gq + 1) * 512],
                             start=(kc == 0), stop=(kc == 7))
                    for kc in range(8):
                        S.mm(pu[:], wu[k][:, kc, fc * 128:(fc + 1) * 128], hT[:, kc, 1 + gq * 512:1 + (gq + 1) * 512],
                             start=(kc == 0), stop=(kc == 7))
                    s_ = sl[q % 2]
                    S.act(s_[:], pg[:], AF.Silu)
                    S.tt(he[:, fc, :], s_[:], pu[:], ALU.mult)
                    q += 1
                for tt in range(4):
                    tl = gq * 4 + tt
                    for h in range(2):
                        py = kb.psb[4 + (tl * 2 + h) % 4]
                        for fc in range(4):
                            S.mm(py[:], he[:, fc, tt * 128:(tt + 1) * 128], wd[k][:, fc, h * 512:(h + 1) * 512],
                                 start=(fc == 0), stop=(fc == 3))
                        ya = yacc[:, tl, h * 512:(h + 1) * 512]
                        if e == 0:
                            S.ts(ya, py[:], comb[:, tl, e:e + 1], None, ALU.mult)
                        else:
                            S.stt(ya, py[:], comb[:, tl, e:e + 1], ya, ALU.mult, ALU.add)
        for tl in range(NTS):
            t = t0 + tl
            epi.run([yacc[:, tl, 0:512], yacc[:, tl, 512:1024]], x_dst, t, x_src=x_src, z=yacc[:, tl, :])
    P.close()


def build(io=None, layers=(0, 1), stages=("mod", "ln", "prep", "br", "merge", "moe"), last_out=None):
    kb = KB(io)
    S = kb.S
    mod_d = kb.dram("mod_d", [2, 6144], F32)
    xa = kb.dram("xa", [T, D], F32)
    xbd = kb.dram("xbd", [T, D], F32)
    out = kb.dram("out", [T, D], F32)
    brT_d = kb.dram("brT", [4, 4, 128, T], BF16)
    wg_d = kb.dram("wg_bf", [4, D, D], BF16)
    wb_d = kb.dram("wb_bf", [4, 512, D], BF16)
    wo_d = kb.dram("wo_bf", [D, D], BF16)
    kb.lb_d = kb.dram("lb_d", [2, 512], F32)
    if "mod" in stages:
        phase_mod(kb, mod_d)
    for layer in layers:
        x_src = kb.x if layer == 0 else xbd
        x_fin = out if layer == layers[-1] else xbd
        MP = Pool_(kb)
        kb.hT = MP.sb("hT", [128, 8, T + 1], BF16)
        for c in range(8):
            S.memset(kb.hT[:, c, 0:1], 0.0, eng="pool")
        if "ln" in stages:
            phase_ln_mixer(kb, x_src, mod_d, layer)
        if "prep" in stages:
            phase_prep_merge(kb, layer, wg_d, wb_d, wo_d)
        if "br" in stages:
            phase_branches(kb, layer, brT_d)
        if "merge" in stages:
            phase_merge(kb, layer, mod_d, brT_d, wg_d, wb_d, wo_d, x_src, xa if "moe" in stages else x_fin)
        MP.close()
        if "moe" in stages:
            phase_moe(kb, layer, mod_d, xa, x_fin)
    S.emit()
    return kb


def load_w(kb, dst, src, stg, cast_eng="pool", dma_eng="sp"):
    S = kb.S
    kc = src.shape[0] // 128
    n = src.shape[1]
    sv = stg[:, 0:kc, 0:n]
    S.dma(sv, src.rearrange("(c p) n -> p c n", p=128), eng=dma_eng)
    S.copy(dst, sv, eng=cast_eng)


def tok(t0, n=128):
    return slice(1 + t0, 1 + t0 + n)


def fox_branch(kb, layer, brT_d):
    S = kb.S
    P = Pool_(kb)
    hT = kb.hT
    W = kb.d["w_in"][layer]
    o = FOX_OFF
    psb = kb.psb
    stg = P.sb("fstg", [128, 8, 520])
    wq = P.sb("fwq", [128, 8, 512], BF16)
    wk = P.sb("fwk", [128, 8, 512], BF16)
    wvf = P.sb("fwvf", [128, 8, 520], BF16)
    load_w(kb, wq[:], W[:, o:o + 512], stg)
    load_w(kb, wk[:], W[:, o + 512:o + 1024], stg)
    load_w(kb, wvf[:], W[:, o + 1024:o + 1544], stg)
    fb = P.sb("ffb", [128, 8])
    S.dma(fb[:], kb.d["fox_f_bias"][layer:layer + 1, :].partition_broadcast(128))
    maskb = P.sb("fmask", [128, 128], BF16)
    S.copy(maskb[:], kb.cst["triu"][:])
    lf = P.sb("flf", [128, 32, 8])
    tA = P.sb("ftA", [128, 32, 8])
    tB = P.sb("ftB", [128, 32, 8])
    Fs = P.sb("fFs", [128, 32, 8])
    Cs = P.sb("fCs", [128, 32, 8])
    vp = P.sb("fvp", [128, 32, 8, 65], BF16)
    S.memset(vp[:].rearrange("p a b c -> p (a b c)"), 1.0, eng="pool")
    for g in range(8):
        pb = psb[6 + g % 2]
        for tt in range(4):
            t = g * 4 + tt
            for kc in range(8):
                S.mm(pb[:, tt * 8:(tt + 1) * 8], hT[:, kc, tok(t * 128)], wvf[:, kc, 512:520], start=(kc == 0), stop=(kc == 7))
        S.tt(lf[:, g * 4:(g + 1) * 4, :], pb[:, 0:32].rearrange("p (a b) -> p a b", a=4),
             fb[:].unsqueeze(1).to_broadcast([128, 4, 8]), ALU.add)
    lf2 = lf[:].rearrange("p a b -> p (a b)")
    S.act(lf2, lf2, AF.Exp, scale=-1.0)
    S.act(lf2, lf2, AF.Ln, bias=1.0)
    S.ts(lf2, lf2, -1.0, None, ALU.mult)
    a, b = lf, tA
    d = 1
    while d < 32:
        nb = tA if b is tA else tB
        if a is lf:
            nb = tA
        S.tt(nb[:, d:32, :], a[:, d:32, :], a[:, 0:32 - d, :], ALU.add)
        S.copy(nb[:, 0:d, :], a[:, 0:d, :])
        a = nb
        b = tB if nb is tA else tA
        d *= 2
    incl = a
    excl = tB if incl is tA else tA
    S.tt(excl[:], incl[:], lf[:], ALU.subtract)
    pF, pC = psb[6], psb[7]
    S.mm(pF[:, 0:256], kb.cst["triu"][:], lf2, start=True, stop=False)
    S.mm(pF[:, 0:256], kb.cst["ones"][:], excl[:].rearrange("p a b -> p (a b)"), start=False, stop=True)
    S.mm(pC[:, 0:256], kb.cst["ones"][:], incl[:].rearrange("p a b -> p (a b)"), start=True, stop=True)
    S.copy(Fs[:].rearrange("p a b -> p (a b)"), pF[:, 0:256])
    S.copy(Cs[:].rearrange("p a b -> p (a b)"), pC[:, 0:256], eng="act")
    for t in range(32):
        pb = psb[6 + t % 2]
        for kc in range(8):
            S.mm(pb[:], hT[:, kc, tok(t * 128)], wvf[:, kc, 0:512], start=(kc == 0), stop=(kc == 7))
        src = pb[:].rearrange("p (h d) -> p h d", h=8)
        if t % 2 == 0:
            S.copy(vp[:, t, :, 0:64], src, eng="dve")
        else:
            S.copy(vp[:, t, :, 0:64], src, eng="act")
    QT = P.sb("fQT", [128, T], BF16)
    KA = P.sb("fKA", [128, T], BF16)
    KB_ = P.sb("fKB", [128, T], BF16)
    S.memset(KA[64:128, :], 0.0, eng="pool")
    S.memset(KB_[0:64, :], 0.0, eng="pool")
    otok = P.sb("fotok", [128, 32, 128])
    brs = P.sb("fbrs", [128, T], BF16)
    pts = [P.sb("fpt%d" % k, [128, 128], BF16) for k in range(4)]
    biases = [P.sb("fbias%d" % k, [128, 32]) for k in range(2)]
    rcs = [P.sb("frc%d" % k, [128, 1]) for k in range(2)]
    q = 0
    nb_ = 0
    for p in range(4):
        for g in range(8):
            pq = psb[6]
            pk = psb[7]
            for kc in range(8):
                S.mm(pq[:], wq[:, kc, p * 128:(p + 1) * 128], hT[:, kc, tok(g * 512, 512)], start=(kc == 0), stop=(kc == 7))
            for kc in range(8):
                S.mm(pk[:], wk[:, kc, p * 128:(p + 1) * 128], hT[:, kc, tok(g * 512, 512)], start=(kc == 0), stop=(kc == 7))
            S.act(QT[:, g * 512:(g + 1) * 512], pq[:], AF.Copy, scale=0.125)
            S.copy(KA[0:64, g * 512:(g + 1) * 512], pk[0:64, :])
            S.copy(KB_[64:128, g * 512:(g + 1) * 512], pk[64:128, :])
        for a_ in range(2):
            h = 2 * p + a_
            Kh = KA if a_ == 0 else KB_
            for i in range(32):
                bias = biases[nb_ % 2]
                rc = rcs[nb_ % 2]
                po = psb[4 + nb_ % 2]
                nb_ += 1
                S.ts(bias[:, 0:i + 1], Fs[:, 0:i + 1, h], Cs[:, i, h:h + 1], -1.0, ALU.subtract, ALU.mult)
                for j in range(i + 1):
                    ps_s = psb[q % 4]
                    pt = pts[q % 4]
                    q += 1
                    S.mm(ps_s[:, 0:128], Kh[:, j * 128:(j + 1) * 128], QT[:, i * 128:(i + 1) * 128])
                    S.act(pt[:], ps_s[:, 0:128], AF.Exp, bias=bias[:, j:j + 1])
                    if j == i:
                        S.tt(pt[:], pt[:], maskb[:], ALU.mult, eng="pool")
                    S.mm(po[:, 0:65], pt[:], vp[:, j, h, :], start=(j == 0), stop=(j == i))
                S.recip(rc[:], po[:, 64:65])
                S.ts(otok[:, i, a_ * 64:(a_ + 1) * 64], po[:, 0:64], rc[:, 0:1], None, ALU.mult)
                if a_ == 1:
                    pt_ = psb[6 + i % 2]
                    S.tr(pt_[:, 0:128], otok[:, i, :], kb.cst["ident"][:])
                    S.copy(brs[:, i * 128:(i + 1) * 128], pt_[:, 0:128], eng="act" if i % 2 else "dve")
        S.dma(brT_d[2, p], brs[:], eng="pool")
    P.close()


def phase_branches(kb, layer, brT_d):
    which = DBG.get("branches", (0, 1, 2, 3))
    if 2 in which:
        fox_branch(kb, layer, brT_d)
    if 0 in which:
        gla_branch(kb, layer, brT_d)
    if 3 in which:
        hgrn_branch(kb, layer, brT_d)
    if 1 in which:
        rwkv_branch(kb, layer, brT_d)


def cgla_branch(kb, layer, brT_d, kind, lb_d=None):
    S = kb.S
    P = Pool_(kb)
    hT = kb.hT
    W = kb.d["w_in"][layer]
    psb = kb.psb
    cst = kb.cst
    gla = (kind == "gla")
    NU = 2 if gla else 4
    HPU = 2 if gla else 1
    DK = 64 if gla else 128
    KW = NU * 128
    o = GLA_OFF if gla else HGRN_OFF
    bidx = 0 if gla else 3
    qscale = 0.125 if gla else 1.0
    stg = P.sb("cstg", [128, 8, 528])
    if gla:
        wq = P.sb("cwq", [128, 8, 256], BF16)
        wk = P.sb("cwk", [128, 8, 256], BF16)
        wv = P.sb("cwv", [128, 8, 512], BF16)
        wg = P.sb("cwg", [128, 8, 528], BF16)
        load_w(kb, wq[:], W[:, o:o + 256], stg)
        load_w(kb, wk[:], W[:, o + 256:o + 512], stg)
        load_w(kb, wv[:], W[:, o + 512:o + 1024], stg)
        load_w(kb, wg[:], W[:, o + 1024:o + 1552], stg)
        aup = P.sb("caup", [16, 256])
        S.dma(aup[:], kb.d["gla_alpha_up"][layer])
        abr = P.sb("cabr", [1, 256])
        S.dma(abr[:], kb.d["gla_alpha_b"][layer:layer + 1, :])
        alT = P.sb("calT", [16, 512])
        Uc = P.sb("cUc", [128, 128])
        SUl = P.sb("cSUl", [128, 128])
        S.ts(Uc[:], cst["triu64"][:], -1.0 / 16.0, None, ALU.mult)
        S.ts(SUl[:], cst["slo64"][:], -1.0 / 16.0, None, ALU.mult)
        ng_src = kb.d["gla_norm_g"]
    else:
        wq = P.sb("cwq", [128, 8, 512], BF16)
        wk = P.sb("cwk", [128, 8, 512], BF16)
        wv = P.sb("cwv", [128, 8, 512], BF16)
        wg = P.sb("cwg", [128, 8, 512], BF16)
        load_w(kb, wq[:], W[:, o:o + 512], stg)
        load_w(kb, wk[:], W[:, o + 512:o + 1024], stg)
        load_w(kb, wv[:], W[:, o + 1024:o + 1536], stg)
        load_w(kb, wg[:], W[:, o + 1536:o + 2048], stg)
        Uc, SUl = cst["triu64"], cst["slo64"]
        lbB = P.sb("clbB", [128, 512])
        omlB = P.sb("comlB", [128, 512])
        S.dma(lbB[:], lb_d[0:1, :].partition_broadcast(128))
        S.ts(omlB[:], lbB[:], -1.0, 1.0, ALU.mult, ALU.add)
        lbT = P.sb("clbT", [128, 4])
        omlT = P.sb("comlT", [128, 4])
        load_T(kb, P, lbT[:], lb_d[0].rearrange("(c p) -> c p", p=128), 4)
        S.ts(omlT[:], lbT[:], -1.0, 1.0, ALU.mult, ALU.add)
        ng_src = kb.d["hgrn_norm_g"]
    ngb = P.sb("cngb", [128, 128])
    S.dma(ngb[:], ng_src[layer:layer + 1, :].partition_broadcast(128))
    qTs = [P.sb("cqTs%d" % u, [128, 512]) for u in range(NU)]
    kTs = [P.sb("ckTs%d" % u, [128, 512]) for u in range(NU)]
    brs = P.sb("cbrs", [128, 4, T], BF16)
    l_tok = P.sb("cltok", [128, KW])
    k_tok = P.sb("cktok", [128, KW])
    f_tok = P.sb("cftok", [128, KW])
    v_tok = P.sb("cvtok", [128, 512], BF16)
    sg_tok = P.sb("csgtok", [128, 512])
    br_tok = P.sb("cbrtok", [128, 512])
    bTs = P.sb("cbTs", [128, 128])
    kdec = P.sb("ckdec", [128, 128])
    khat = P.sb("ckhat", [128, 128], BF16)
    bm = P.sb("cbm", [128, 2])
    nbm = P.sb("cnbm", [128, 2])
    E1 = P.sb("cE1", [128, 128])
    E2 = P.sb("cE2", [128, 128])
    E3 = P.sb("cE3", [128, 128])
    qt = P.sb("cqt", [128, 128], BF16)
    kt = P.sb("ckt", [128, 128], BF16)
    qA = P.sb("cqA", [128, 128], BF16)
    qB = P.sb("cqB", [128, 128], BF16)
    attb = [P.sb("cattb%d" % a, [128, 128], BF16) for a in range(HPU)]
    Sf = [[P.sb("cSf%d_%d" % (u, k), [128, 128]) for k in range(2)] for u in range(NU)]
    Sb = [[P.sb("cSb%d_%d" % (u, k), [128, 128], BF16) for k in range(2)] for u in range(NU)]
    for u in range(NU):
        S.memset(Sf[u][0][:], 0.0)
        S.memset(Sb[u][0][:], 0.0)
    ss = P.sb("css", [128, 2])
    junk = P.sb("cjunk", [128, 128])
    for g in range(DBG.get('cg_ng', 8)):
        gt = tok(g * 512, 512)
        for u in range(NU):
            pq, pk = psb[0], psb[1]
            for kc in range(8):
                S.mm(pq[:], wq[:, kc, u * 128:(u + 1) * 128], hT[:, kc, gt], start=(kc == 0), stop=(kc == 7))
            for kc in range(8):
                S.mm(pk[:], wk[:, kc, u * 128:(u + 1) * 128], hT[:, kc, gt], start=(kc == 0), stop=(kc == 7))
            S.copy(qTs[u][:], pq[:], eng="act")
            if gla:
                S.copy(kTs[u][:], pk[:], eng="dve")
            else:
                S.act(kTs[u][:], pk[:], AF.Sigmoid)
                S.ts(kTs[u][:], kTs[u][:], omlT[:, u:u + 1], lbT[:, u:u + 1], ALU.mult, ALU.add)
                S.ts(kTs[u][:], kTs[u][:], -1.0, 1.0, ALU.mult, ALU.add)
        if gla:
            pa = psb[2]
            for kc in range(8):
                S.mm(pa[0:16, :], wg[:, kc, 512:528], hT[:, kc, gt], start=(kc == 0), stop=(kc == 7))
            S.copy(alT[:], pa[0:16, :])
        for tt in range(4):
            t = g * 4 + tt
            tk = tok(t * 128)
            tcol = slice(tt * 128, (tt + 1) * 128)
            pv, pg = psb[2], psb[3]
            for kc in range(8):
                S.mm(pv[:], hT[:, kc, tk], wv[:, kc, 0:512], start=(kc == 0), stop=(kc == 7))
            S.copy(v_tok[:], pv[:], eng="act")
            for kc in range(8):
                S.mm(pg[:], hT[:, kc, tk], wg[:, kc, 0:512], start=(kc == 0), stop=(kc == 7))
            S.act(sg_tok[:], pg[:], AF.Silu)
            pk2 = psb[2]
            if gla:
                for kc in range(8):
                    S.mm(pk2[:, 0:256], hT[:, kc, tk], wk[:, kc, 0:256], start=(kc == 0), stop=(kc == 7))
                S.mm(pk2[:, 256:512], alT[:, tcol], aup[:], start=True, stop=False)
                S.mm(pk2[:, 256:512], cst["ones"][0:1, :], abr[:], start=False, stop=True)
                S.copy(k_tok[:], pk2[:, 0:256])
                S.act(l_tok[:], pk2[:, 256:512], AF.Exp, scale=-1.0)
                S.act(l_tok[:], l_tok[:], AF.Ln, bias=1.0)
            else:
                for kc in range(8):
                    S.mm(pk2[:], hT[:, kc, tk], wk[:, kc, 0:512], start=(kc == 0), stop=(kc == 7))
                S.act(f_tok[:], pk2[:], AF.Sigmoid)
                S.tt(f_tok[:], f_tok[:], omlB[:], ALU.mult)
                S.tt(f_tok[:], f_tok[:], lbB[:], ALU.add)
                S.act(l_tok[:], f_tok[:], AF.Ln)
                S.ts(k_tok[:], f_tok[:], -1.0, 1.0, ALU.mult, ALU.add)
            for u in range(NU):
                cu = slice(u * 128, (u + 1) * 128)
                S0f, S1f = Sf[u][0], Sf[u][1]
                S0b, S1b = Sb[u][0], Sb[u][1]
                if DBG.get('cg_lvl', 99) < 1:
                    continue
                pX = psb[4]
                S.mm(pX[:, 0:128], l_tok[:, cu], Uc[:])
                S.mm(pX[:, 128:256], SUl[:], l_tok[:, cu])
                S.copy(bTs[:], pX[:, 0:128])
                S.act(kdec[:], pX[:, 128:256], AF.Exp)
                S.tt(khat[:], k_tok[:, cu], kdec[:], ALU.mult, eng="pool")
                if DBG.get('cg_lvl', 99) < 2:
                    continue
                mid = bTs[:].rearrange("p (c s) -> p c s", c=2)[:, :, 32]
                S.copy(bm[:], mid)
                S.ts(nbm[:], mid, -1.0, None, ALU.mult)
                for c in range(2):
                    cs = slice(c * 64, (c + 1) * 64)
                    S.act(E1[:, cs], bTs[:, cs], AF.Exp, bias=nbm[:, c:c + 1])
                    S.act(E2[:, cs], bTs[:, cs], AF.Exp, bias=bm[:, c:c + 1], scale=-1.0)
                S.act(E3[:], bTs[:], AF.Exp)
                if DBG.get('cg_sub', 9) < 1:
                    continue
                S.stt(qt[:], qTs[u][:, tcol], qscale, E1[:], ALU.mult, ALU.mult)
                if DBG.get('cg_sub', 9) < 2:
                    continue
                S.tt(kt[:], kTs[u][:, tcol], E2[:], ALU.mult, eng="pool")
                if DBG.get('cg_sub', 9) < 3:
                    continue
                S.stt(qA[:], qTs[u][:, tcol], qscale, E3[:], ALU.mult, ALU.mult)
                if DBG.get('cg_lvl', 99) < 3:
                    continue
                pAtt = psb[5]
                for a in range(HPU):
                    ra = slice(a * DK, (a + 1) * DK)
                    S.mm(pAtt[:, a * 128:(a + 1) * 128], kt[ra, :], qt[ra, :])
                for a in range(HPU):
                    am = DBG.get("att_mode", "swap")
                    if am == "tt":
                        S.tt(attb[a][:], pAtt[:, a * 128:(a + 1) * 128], cst["triu64"][:], ALU.mult)
                    elif am == "swap":
                        S.tt(attb[a][:], cst["triu64"][:], pAtt[:, a * 128:(a + 1) * 128], ALU.mult)
                    elif am == "act":
                        S.copy(junk[:], pAtt[:, a * 128:(a + 1) * 128], eng="act")
                        S.tt(attb[a][:], junk[:], cst["triu64"][:], ALU.mult, eng="pool")
                if DBG.get('cg_lvl', 99) < 4:
                    continue
                pS = psb[6]
                for c in range(2):
                    rows = slice(c * 64, (c + 1) * 64)
                    for a in range(HPU):
                        h = u * HPU + a
                        ra = slice(a * DK, (a + 1) * DK)
                        S.mm(pS[ra, c * 128:(c + 1) * 128], khat[rows, a * DK:(a + 1) * DK], v_tok[rows, h * 128:(h + 1) * 128])
                S.stt(S1f[:], S0f[:], E3[:, 63:64], pS[:, 0:128], ALU.mult, ALU.add)
                S.copy(S1b[:], S1f[:], eng="act")
                if DBG.get('cg_lvl', 99) < 5:
                    continue
                pO = psb[7]
                for a in range(HPU):
                    h = u * HPU + a
                    ra = slice(a * DK, (a + 1) * DK)
                    oc = slice(a * 128, (a + 1) * 128)
                    S.mm(pO[0:64, oc], qA[ra, 0:64], S0b[ra, :], start=True, stop=False)
                    S.mm(pO[64:128, oc], qA[ra, 64:128], S1b[ra, :], start=True, stop=False)
                    S.mm(pO[:, oc], attb[a][:], v_tok[:, h * 128:(h + 1) * 128], start=False, stop=True)
                S.stt(S0f[:], S1f[:], E3[:, 127:128], pS[:, 128:256], ALU.mult, ALU.add)
                S.copy(S0b[:], S0f[:], eng="act")
                if DBG.get('cg_lvl', 99) < 6:
                    continue
                for a in range(HPU):
                    h = u * HPU + a
                    oc = slice(a * 128, (a + 1) * 128)
                    hc = slice(h * 128, (h + 1) * 128)
                    S.act(junk[:], pO[:, oc], AF.Square, accum_out=ss[:, a:a + 1])
                    rsqrt_eps(S, ss[:, a:a + 1], ss[:, a:a + 1], 1e-6, scale=1.0 / 128.0)
                    S.stt(br_tok[:, hc], pO[:, oc], ss[:, a:a + 1], ngb[:], ALU.mult, ALU.mult)
                    S.tt(br_tok[:, hc], br_tok[:, hc], sg_tok[:, hc], ALU.mult, eng="pool")
            pT = psb[0] if tt % 2 else psb[1]
            for kc in range(4):
                S.tr(pT[:, kc * 128:(kc + 1) * 128], br_tok[:, kc * 128:(kc + 1) * 128], cst["ident"][:])
            S.copy(brs[:, :, t * 128:(t + 1) * 128], pT[:].rearrange("p (c t) -> p c t", c=4), eng="act" if t % 2 else "dve")
    S.dma(brT_d[bidx].rearrange("c p t -> p c t"), brs[:], eng="pool")
    P.close()


def gla_branch(kb, layer, brT_d):
    cgla_branch(kb, layer, brT_d, "gla")


def hgrn_branch(kb, layer, brT_d):
    S = kb.S
    P = Pool_(kb)
    lb_d = kb.lb_d
    row = P.sb("hlbrow", [1, 512])
    if layer == 0:
        S.memset(row[:], 0.0)
    else:
        r0 = P.sb("hlb0", [1, 512])
        S.dma(r0[:], kb.d["hgrn_lb_logits"][0:1, :])
        S.dma(row[:], kb.d["hgrn_lb_logits"][1:2, :])
        S.tt(row[:], row[:], r0[:], ALU.subtract)
        S.act(row[:], row[:], AF.Sigmoid)
    S.dma(lb_d[layer:layer + 1, :], row[:])
    P.close()
    cgla_branch(kb, layer, brT_d, "hgrn", lb_d=lb_d[layer:layer + 1, :])


def rwkv_branch(kb, layer, brT_d):
    S = kb.S
    hT = kb.hT
    W = kb.d["w_in"][layer]
    psb = kb.psb
    cst = kb.cst
    o = RWKV_OFF
    P = Pool_(kb)
    CW = math.exp(-0.5)
    Wr = [P.sb("rWr%d" % k, [128, 8, 512], BF16) for k in range(2)]
    Wk = [P.sb("rWk%d" % k, [128, 8, 512], BF16) for k in range(2)]
    Wv = [P.sb("rWv%d" % k, [128, 8, 512], BF16) for k in range(2)]
    Wl = [P.sb("rWl%d" % k, [128, 8, 256], BF16) for k in range(2)]
    PP = Pool_(kb)
    stg = PP.sb("rstg", [128, 8, 512])
    muB = PP.sb("rmuB", [128, 1792])
    omuB = PP.sb("romuB", [128, 1792])
    S.dma(muB[:], kb.d["rwkv_mu"][layer:layer + 1, :].partition_broadcast(128))
    S.ts(omuB[:], muB[:], -1.0, 1.0, ALU.mult, ALU.add)
    for (dst, c0, n) in ((Wr, 0, 512), (Wk, 512, 512), (Wv, 1024, 512), (Wl, 1536, 256)):
        sv = stg[:, :, 0:n]
        S.dma(sv, W[:, o + c0:o + c0 + n].rearrange("(c p) n -> p c n", p=128))
        S.tt(dst[0][:], sv, omuB[:, c0:c0 + n].unsqueeze(1).to_broadcast([128, 8, n]), ALU.mult)
        S.tt(dst[1][:], sv, muB[:, c0:c0 + n].unsqueeze(1).to_broadcast([128, 8, n]), ALU.mult, eng="pool")
    PP.close()
    lw = P.sb("rlw", [128, 512])
    S.dma(lw[0:64, :], kb.d["rwkv_w2"][layer])
    S.dma(lw[64:128, :], kb.d["rwkv_a2"][layer])
    g2f = P.sb("rg2f", [128, 512])
    g2b = P.sb("rg2b", [128, 512], BF16)
    S.dma(g2f[:], kb.d["rwkv_g2"][layer])
    S.copy(g2b[:], g2f[:])
    w0r = P.sb("rw0r", [1, 512])
    S.dma(w0r[:], kb.d["rwkv_w0"][layer:layer + 1, :])
    a0T = P.sb("ra0T", [128, 4])
    kkT_ = P.sb("rkkT", [128, 4])
    kaT = P.sb("rkaT", [128, 4])
    okaT = P.sb("rokaT", [128, 4])
    rkT_ = P.sb("rrkT", [128, 4])
    load_T(kb, P, a0T[:], kb.d["rwkv_a0"][layer].rearrange("(c p) -> c p", p=128), 4)
    load_T(kb, P, kkT_[:], kb.d["rwkv_k_k"][layer].rearrange("(c p) -> c p", p=128), 4)
    load_T(kb, P, kaT[:], kb.d["rwkv_k_a"][layer].rearrange("(c p) -> c p", p=128), 4)
    load_T(kb, P, rkT_[:], kb.d["rwkv_r_k"][layer].rearrange("(c two) d -> c (two d)", two=2), 4)
    S.ts(okaT[:], kaT[:], -1.0, 1.0, ALU.mult, ALU.add)
    lngB = P.sb("rlngB", [128, 512])
    lnbB = P.sb("rlnbB", [128, 512])
    S.dma(lngB[:], kb.d["rwkv_ln_g"][layer:layer + 1, :].partition_broadcast(128))
    S.dma(lnbB[:], kb.d["rwkv_ln_b"][layer:layer + 1, :].partition_broadcast(128))
    Uc = P.sb("rUc", [128, 128])
    Ux = P.sb("rUx", [128, 128])
    SUl = P.sb("rSUl", [128, 128])
    nsup = P.sb("rnsup", [128, 128])
    nslo = P.sb("rnslo", [128, 128])
    ntriu = P.sb("rntriu", [128, 128])
    S.ts(Uc[:], cst["triu64"][:], -CW, None, ALU.mult)
    S.ts(Ux[:], cst["sup64"][:], -CW, None, ALU.mult)
    S.ts(SUl[:], cst["slo64"][:], -CW, None, ALU.mult)
    S.ts(nsup[:], cst["sup64"][:], -1.0, None, ALU.mult)
    S.ts(nslo[:], cst["slo64"][:], -1.0, None, ALU.mult)
    S.ts(ntriu[:], cst["triu64"][:], -1.0, None, ALU.mult)
    hsel = P.sb("rhsel", [128, 2])
    S.copy(hsel[:], cst["blk64"][:].rearrange("p (a s) -> p a s", a=2)[:, :, 0])
    ident = cst["ident"]

    def f512(name):
        return P.sb(name, [128, 512])

    def f128(name, dt=F32):
        return P.sb(name, [128, 128], dt)

    lo = f512("rlo")
    sgl = P.sb("rsgl", [128, 512], BF16)
    rT, kT, aT, kaT_, bT_, kpT, tmpF, prodT = (f512("r_" + n) for n in ("rT", "kT", "aT", "kapT", "bbT", "kpT", "tmpF", "prodT"))
    v_tok, l_tok, g_tok = f128("rvtok"), f128("rltok"), f128("rgtok")
    bTs, bxTs = f128("rbTs"), f128("rbxTs")
    bm, nbm, bl = P.sb("rbm", [128, 2]), P.sb("rnbm", [128, 2]), P.sb("rbl", [128, 2])
    E = {n: f128("rE_" + n) for n in ("r", "kx", "inv", "abs", "absx", "last")}
    rt, kxt, kt, bt, rbar, kbar, KhT, BhT = (f128("r_" + n) for n in ("rt", "kxt", "kt", "bt", "rbar", "kbar", "KhT", "BhT"))
    Khat, Bhat = f128("rKhat"), f128("rBhat")
    Mm = [[f128("rM%d_%d" % (a, k)) for k in range(2)] for a in range(2)]
    MT = [[f128("rMT%d_%d" % (a, k)) for k in range(2)] for a in range(2)]
    Pm = [[f128("rP%d_%d" % (a, k)) for k in range(2)] for a in range(2)]
    Akk = [f128("rAkk%d" % a) for a in range(2)]
    Ark = [f128("rArk%d" % a) for a in range(2)]
    Arb = [f128("rArb%d" % a) for a in range(2)]
    Ws, Us, ytok = f128("rWs"), f128("rUs"), f128("rytok")
    Hs = [[P.sb("rHs%d_%d" % (u, k), [128, 64]) for k in range(2)] for u in range(4)]
    for u in range(4):
        S.memset(Hs[u][0][:], 0.0)
    st6 = P.sb("rst6", [128, 2, 6])
    mv2 = P.sb("rmv2", [128, 2, 2])
    rs2 = P.sb("rrs2", [128, 2])
    sb2 = P.sb("rsb2", [128, 2])
    yn = f128("ryn")
    brt_ = f128("rbrt")
    brb = [P.sb("rbrb%d" % k, [128, 128], BF16) for k in range(2)]
    nbr = 0

    def proj_fm(ps, W2, c0, n, gcol):
        for kc in range(8):
            S.mm(ps[0:n, :], W2[0][:, kc, c0:c0 + n], hT[:, kc, slice(1 + gcol, 1 + gcol + 512)], start=(kc == 0), stop=False)
        for kc in range(8):
            S.mm(ps[0:n, :], W2[1][:, kc, c0:c0 + n], hT[:, kc, slice(gcol, gcol + 512)], start=False, stop=(kc == 7))

    for g in range(DBG.get("rw_ng", 8)):
        gcol = g * 512
        proj_fm(psb[0], Wl, 0, 128, gcol)
        S.act(lo[0:64, :], psb[0][0:64, :], AF.Tanh)
        S.copy(lo[64:128, :], psb[0][64:128, :])
        proj_fm(psb[1], Wl, 128, 128, gcol)
        S.act(sgl[:], psb[1][:], AF.Sigmoid)
        for u in range(4):
            uc = slice(u * 128, (u + 1) * 128)
            proj_fm(psb[0], Wr, u * 128, 128, gcol)
            S.copy(rT[:], psb[0][:], eng="act")
            proj_fm(psb[1], Wk, u * 128, 128, gcol)
            S.copy(kT[:], psb[1][:])
            S.mm(psb[0][:], lw[64:128, uc], lo[64:128, :])
            S.act(aT[:], psb[0][:], AF.Sigmoid, bias=a0T[:, u:u + 1])
            S.ts(kaT_[:], kT[:], kkT_[:, u:u + 1], None, ALU.mult)
            S.tt(tmpF[:], kaT_[:], kaT_[:], ALU.mult, eng="pool")
            S.mm(psb[1][:], cst["blk64"][:], tmpF[:])
            S.act(tmpF[:], psb[1][:], AF.Ln, bias=1e-24)
            S.act(tmpF[:], tmpF[:], AF.Exp, scale=-0.5)
            S.tt(kaT_[:], kaT_[:], tmpF[:], ALU.mult)
            S.tt(bT_[:], kaT_[:], aT[:], ALU.mult, eng="pool")
            S.ts(tmpF[:], aT[:], kaT[:, u:u + 1], okaT[:, u:u + 1], ALU.mult, ALU.add)
            S.tt(kpT[:], kT[:], tmpF[:], ALU.mult)
            S.stt(prodT[:], rT[:], rkT_[:, u:u + 1], kpT[:], ALU.mult, ALU.mult)
            for tt_ in range(4):
                t = g * 4 + tt_
                t0 = t * 128
                tc_ = slice(tt_ * 128, (tt_ + 1) * 128)
                H0, H1 = Hs[u][0], Hs[u][1]
                pt_ = psb[2]
                for kc in range(8):
                    S.mm(pt_[:, 0:128], hT[:, kc, slice(1 + t0, 1 + t0 + 128)], Wv[0][:, kc, uc], start=(kc == 0), stop=False)
                for kc in range(8):
                    S.mm(pt_[:, 0:128], hT[:, kc, slice(t0, t0 + 128)], Wv[1][:, kc, uc], start=False, stop=(kc == 7))
                S.mm(pt_[:, 128:256], lo[0:64, tc_], lw[0:64, uc], start=True, stop=False)
                S.mm(pt_[:, 128:256], cst["ones"][0:1, :], w0r[0:1, uc], start=False, stop=True)
                S.mm(pt_[:, 256:384], sgl[:, tc_], g2b[:, uc])
                S.mm(pt_[:, 384:386], prodT[:, tc_], hsel[:])
                S.copy(v_tok[:], pt_[:, 0:128])
                S.act(l_tok[:], pt_[:, 128:256], AF.Sigmoid)
                S.copy(g_tok[:], pt_[:, 256:384], eng="act")
                S.copy(sb2[:], pt_[:, 384:386])
                pX = psb[3]
                S.mm(pX[:, 0:128], l_tok[:], Uc[:])
                S.mm(pX[:, 128:256], l_tok[:], Ux[:])
                S.copy(bTs[:], pX[:, 0:128])
                S.copy(bxTs[:], pX[:, 128:256], eng="act")
                b3 = bTs[:].rearrange("p (c s) -> p c s", c=2)
                S.copy(bm[:], b3[:, :, 32])
                S.ts(nbm[:], b3[:, :, 32], -1.0, None, ALU.mult)
                S.copy(bl[:], b3[:, :, 63])
                for c in range(2):
                    cs = slice(c * 64, (c + 1) * 64)
                    S.act(E["r"][:, cs], bTs[:, cs], AF.Exp, bias=nbm[:, c:c + 1])
                    S.act(E["kx"][:, cs], bxTs[:, cs], AF.Exp, bias=nbm[:, c:c + 1])
                    S.act(E["inv"][:, cs], bTs[:, cs], AF.Exp, bias=bm[:, c:c + 1], scale=-1.0)
                    S.act(E["last"][:, cs], bTs[:, cs], AF.Exp, bias=bl[:, c:c + 1], scale=-1.0)
                S.act(E["abs"][:], bTs[:], AF.Exp)
                S.act(E["absx"][:], bxTs[:], AF.Exp)
                S.tt(rt[:], rT[:, tc_], E["r"][:], ALU.mult)
                S.tt(kxt[:], kaT_[:, tc_], E["kx"][:], ALU.mult, eng="pool")
                S.tt(kt[:], kpT[:, tc_], E["inv"][:], ALU.mult)
                S.tt(bt[:], bT_[:, tc_], E["inv"][:], ALU.mult, eng="pool")
                S.tt(rbar[:], rT[:, tc_], E["abs"][:], ALU.mult)
                S.tt(kbar[:], kaT_[:, tc_], E["absx"][:], ALU.mult, eng="pool")
                S.tt(KhT[:], kpT[:, tc_], E["last"][:], ALU.mult)
                S.stt(BhT[:], bT_[:, tc_], -1.0, E["last"][:], ALU.mult, ALU.mult)
                S.tr(pX[:, 256:384], KhT[:], ident[:])
                S.tr(pX[:, 384:512], BhT[:], ident[:])
                S.copy(Khat[:], pX[:, 256:384])
                S.copy(Bhat[:], pX[:, 384:512], eng="act")
                for a in range(2):
                    ra = slice(a * 64, (a + 1) * 64)
                    pA = psb[4 + a]
                    S.mm(pA[:, 0:128], bt[ra, :], kxt[ra, :])
                    S.mm(pA[:, 128:256], kxt[ra, :], bt[ra, :])
                    S.mm(pA[:, 256:384], kt[ra, :], kxt[ra, :])
                    S.mm(pA[:, 384:512], kt[ra, :], rt[ra, :])
                    S.mm(psb[6][:, a * 128:(a + 1) * 128], bt[ra, :], rt[ra, :])
                for a in range(2):
                    pA = psb[4 + a]
                    S.tt(Mm[a][0][:], nsup[:], pA[:, 0:128], ALU.mult)
                    S.tt(MT[a][0][:], nslo[:], pA[:, 128:256], ALU.mult)
                    S.tt(Akk[a][:], cst["sup64"][:], pA[:, 256:384], ALU.mult)
                    S.tt(Ark[a][:], cst["triu64"][:], pA[:, 384:512], ALU.mult)
                    S.tt(Arb[a][:], ntriu[:], psb[6][:, a * 128:(a + 1) * 128], ALU.mult)
                    S.tt(Pm[a][0][:], Mm[a][0][:], ident[:], ALU.add, eng="pool")
                cur = 0
                for lvl in range(1, 6):
                    nxt = 1 - cur
                    for a in range(2):
                        pA = psb[4 + a]
                        if lvl < 5:
                            S.mm(pA[:, 0:128], MT[a][cur][:], Mm[a][cur][:])
                        S.mm(pA[:, 128:256], Mm[a][cur][:], MT[a][cur][:])
                    for a in range(2):
                        pA = psb[4 + a]
                        if lvl < 5:
                            S.copy(Mm[a][nxt][:], pA[:, 0:128])
                        S.copy(MT[a][nxt][:], pA[:, 128:256], eng="act")
                    for a in range(2):
                        pA = psb[4 + a]
                        S.mm(pA[:, 256:384], MT[a][nxt][:], Pm[a][cur][:])
                    for a in range(2):
                        pA = psb[4 + a]
                        S.tt(Pm[a][nxt][:], Pm[a][cur][:], pA[:, 256:384], ALU.add)
                    cur = nxt
                Pf = [Pm[a][cur] for a in range(2)]
                pC = psb[7]
                Hc = [H0, H1, H0]
                for c in range(2):
                    rc = slice(c * 64, (c + 1) * 64)
                    Hin, Hout = Hc[c], Hc[c + 1]
                    for a in range(2):
                        ra = slice(a * 64, (a + 1) * 64)
                        S.mm(pC[rc, a * 64:(a + 1) * 64], kbar[ra, rc], Hin[ra, :], start=True, stop=False)
                        S.mm(pC[rc, a * 64:(a + 1) * 64], Akk[a][rc, rc], v_tok[rc, ra], start=False, stop=True)
                    S.copy(Ws[rc, :], pC[rc, 0:128])
                    for a in range(2):
                        ra = slice(a * 64, (a + 1) * 64)
                        S.mm(pC[rc, 128 + a * 64:128 + (a + 1) * 64], Pf[a][rc, rc], Ws[rc, ra])
                    S.copy(Us[rc, :], pC[rc, 128:256])
                    for a in range(2):
                        ra = slice(a * 64, (a + 1) * 64)
                        oy = slice(256 + a * 64, 256 + (a + 1) * 64)
                        S.mm(pC[rc, oy], rbar[ra, rc], Hin[ra, :], start=True, stop=False)
                        S.mm(pC[rc, oy], Ark[a][rc, rc], v_tok[rc, ra], start=False, stop=False)
                        S.mm(pC[rc, oy], Arb[a][rc, rc], Us[rc, ra], start=False, stop=True)
                    for a in range(2):
                        ra = slice(a * 64, (a + 1) * 64)
                        S.mm(pC[ra, 384:448], Khat[rc, ra], v_tok[rc, ra], start=True, stop=False)
                        S.mm(pC[ra, 384:448], Bhat[rc, ra], Us[rc, ra], start=False, stop=True)
                    S.stt(Hout[:], Hin[:], E["abs"][:, c * 64 + 63:c * 64 + 64], pC[:, 384:448], ALU.mult, ALU.add)
                    S.copy(ytok[rc, :], pC[rc, 256:384], eng="act")
                for a in range(2):
                    S.bn_stats(st6[:, a, :], ytok[:, a * 64:(a + 1) * 64])
                    S.bn_aggr(mv2[:, a, :], st6[:, a, :])
                rsqrt_eps(S, rs2[:], mv2[:, :, 1], 64e-5)
                for a in range(2):
                    ra = slice(a * 64, (a + 1) * 64)
                    gc = slice(u * 128 + a * 64, u * 128 + (a + 1) * 64)
                    S.ts(yn[:, ra], ytok[:, ra], mv2[:, a, 0:1], rs2[:, a:a + 1], ALU.subtract, ALU.mult)
                    S.tt(yn[:, ra], yn[:, ra], lngB[:, gc], ALU.mult, eng="pool")
                    S.tt(yn[:, ra], yn[:, ra], lnbB[:, gc], ALU.add, eng="pool")
                    S.stt(yn[:, ra], v_tok[:, ra], sb2[:, a:a + 1], yn[:, ra], ALU.mult, ALU.add)
                S.tt(brt_[:], yn[:], g_tok[:], ALU.mult, eng="pool")
                S.tr(pX[:, 0:128], brt_[:], ident[:])
                bb_ = brb[nbr % 2]
                nbr += 1
                S.copy(bb_[:], pX[:, 0:128])
                S.dma(brT_d[1, u, :, t0:t0 + 128], bb_[:], eng="pool")
    P.close()


_CACHE = {}


def kernel(**inputs):
    if "kb" not in _CACHE:
        _CACHE["kb"] = build()
    kb = _CACHE["kb"]
    consts = make_consts()
    shared = {}
    for n, shp in PARAMS:
        if n == "c":
            continue
        shared[n] = np.ascontiguousarray(np.asarray(inputs[n], dtype=np.float32))
    for n, v in consts.items():
        shared["k_" + n] = v
    x = np.asarray(inputs["x"], dtype=np.float32)
    c = np.asarray(inputs["c"], dtype=np.float32)
    in_maps = []
    for b in range(8):
        m = dict(shared)
        m["x"] = np.ascontiguousarray(x[b])
        m["c"] = np.ascontiguousarray(c[b:b + 1])
        in_maps.append(m)
    res = run_bass_kernel_spmd(kb.nc, in_maps, core_ids=list(range(8)))
    return np.stack([np.asarray(r["out"], dtype=np.float32) for r in res.results], axis=0)
```

```python
import math
from contextlib import ExitStack

import numpy as np
import concourse.bass as bass
import concourse.mybir as mybir
from concourse.bass_utils import run_bass_kernel_spmd

F32 = mybir.dt.float32
BF16 = mybir.dt.bfloat16
AF = mybir.ActivationFunctionType
ALU = mybir.AluOpType
AX = mybir.AxisListType

ENGS = ("pe", "act", "dve", "pool", "sp")


class _Op:
    __slots__ = ("eng", "fn", "dma", "waits", "key", "val", "know", "inc")


def _region(ap):
    shape = list(ap.tensor.shape)
    off = int(ap.offset)
    if str(ap.space) == "DRAM":
        ext = 0
        for step, cnt in ap.ap:
            ext += (int(cnt) - 1) * abs(int(step))
        return (ap.name, 0, 1, off, off + ext + 1)
    if str(ap.space) == "PSUM":
        return (ap.name, 0, 128, 0, 1 << 30)
    ps = 1
    for s in shape[1:]:
        ps *= int(s)
    p0 = off // ps
    f0 = off % ps
    npart = 1
    ext = 0
    for step, cnt in ap.ap:
        step = int(step)
        cnt = int(cnt)
        if step == ps:
            npart = max(npart, cnt)
        elif step > ps:
            npart = max(npart, (cnt - 1) * (step // ps) + 1)
        else:
            ext += (cnt - 1) * step
    return (ap.name, p0, p0 + npart, f0, f0 + ext + 1)


class Sched:
    def __init__(self, nc, n_dma_sems=12):
        self.nc = nc
        self.streams = {e: [] for e in ENGS}
        self.clock = {e: {} for e in ENGS}
        self.cpos = {e: 0 for e in ENGS}
        self.last_op = {e: None for e in ENGS}
        self.n_dma_sems = n_dma_sems
        self.dma_uses = {}
        self.dma_next = {e: 0 for e in ENGS}
        self.dma_last = {}
        self.acc = {}
        self.readonly = set()
        self.pe_bank = {}
        self.epoch = 0
        self.n_ops = 0

    def _add(self, eng, fn, reads, writes, dma, force=()):
        op = _Op()
        op.eng, op.fn, op.dma = eng, fn, dma
        clk = self.clock[eng]
        need = {}

        def dep(d, forced=False):
            if (not forced) and (not dma) and (not d.dma) and d.eng == "pe" and eng == "pe":
                return
            if clk.get(d.key, 0) >= d.val:
                return
            if need.get(d.key, 0) < d.val:
                need[d.key] = d.val
            for k, v in d.know.items():
                if clk.get(k, 0) < v:
                    clk[k] = v

        for d_ in force:
            dep(d_, True)
        regs = []
        for ap in reads:
            if ap.name in self.readonly:
                continue
            regs.append((_region(ap), False))
        for ap in writes:
            regs.append((_region(ap), True))
        for (name, p0, p1, f0, f1), isw in regs:
            lst = self.acc.get(name)
            if lst is None:
                continue
            for r in lst:
                if r[4] is op:
                    continue
                if r[0] < p1 and p0 < r[1] and r[2] < f1 and f0 < r[3]:
                    if isw or r[5] or (f1 == (1 << 30) and r[4].eng != eng):
                        dep(r[4])
        if dma:
            slot = self.dma_next[eng]
            self.dma_next[eng] = (slot + 1) % self.n_dma_sems
            k = ("s", eng, slot)
            prev = self.dma_last.get(k)
            if prev is not None:
                dep(prev)
            cnt = self.dma_uses.get(k, 0) + 1
            self.dma_uses[k] = cnt
            op.key, op.val, op.inc = k, 16 * cnt, 16
            self.dma_last[k] = op
        else:
            self.cpos[eng] += 1
            op.key, op.val, op.inc = ("e", eng, self.epoch), self.cpos[eng], 1
        for k, v in need.items():
            if clk.get(k, 0) < v:
                clk[k] = v
        op.waits = list(need.items())
        op.know = dict(clk)
        self.streams[eng].append(op)
        self.last_op[eng] = op
        for (name, p0, p1, f0, f1), isw in regs:
            lst = self.acc.setdefault(name, [])
            if isw:
                lst[:] = [r for r in lst if not (p0 <= r[0] and r[1] <= p1 and f0 <= r[2] and r[3] <= f1)]
            else:
                if not dma:
                    lst[:] = [r for r in lst if not ((not r[5]) and (not r[4].dma) and r[4].eng == eng
                                                     and r[0] == p0 and r[1] == p1 and r[2] == f0 and r[3] == f1)]
            lst.append([p0, p1, f0, f1, op, isw])
        self.n_ops += 1
        return op

    def barrier(self):
        targets = []
        for e in ENGS:
            if self.cpos[e] > 0:
                targets.append((("e", e, self.epoch), self.cpos[e], self.last_op[e]))
        for k, o in self.dma_last.items():
            targets.append((k, o.val, o))
        for e in ENGS:
            clk = self.clock[e]
            op = _Op()
            op.eng, op.fn, op.dma = e, None, False
            need = {}
            for k, v, o in targets:
                if clk.get(k, 0) < v:
                    need[k] = v
                    clk[k] = v
            op.waits = list(need.items())
            op.key = None
            op.know = dict(clk)
            self.streams[e].append(op)
        self.acc = {}
        self.pe_bank = {}
        if max(self.cpos.values()) > 16000:
            self.epoch += 1
            self.cpos = {e: 0 for e in ENGS}

    def _pe_bank(self, out, lhsT):
        kpos = (int(lhsT.base_partition()) if hasattr(lhsT, "base_partition") else 0, int(lhsT.partition_size()))
        prev = self.pe_bank.get(out.name)
        force = ()
        if prev is not None and prev[0] != kpos:
            force = (prev[1],)
        return kpos, force

    def mm(self, out, lhsT, rhs, start=True, stop=True):
        rd = [lhsT, rhs] + ([] if start else [out])
        kpos, force = self._pe_bank(out, lhsT)
        op = self._add("pe", lambda e: e.matmul(out, lhsT, rhs, start=start, stop=stop), rd, [out], False, force=force)
        self.pe_bank[out.name] = (kpos, op)
        return op

    def tr(self, out, in_, ident):
        kpos, force = self._pe_bank(out, in_)
        op = self._add("pe", lambda e: e.transpose(out, in_, ident), [in_, ident], [out], False, force=force)
        self.pe_bank[out.name] = (kpos, op)
        return op

    def act(self, out, in_, func, bias=None, scale=None, accum_out=None):
        rd = [in_]
        kw = {}
        if bias is not None:
            kw["bias"] = bias
            if not isinstance(bias, (int, float)):
                rd.append(bias)
        if scale is not None:
            kw["scale"] = scale
            if not isinstance(scale, (int, float)):
                rd.append(scale)
        wr = [out]
        if accum_out is not None:
            kw["accum_out"] = accum_out
            wr.append(accum_out)
        return self._add("act", lambda e: e.activation(out, in_, func, **kw), rd, wr, False)

    def tt(self, out, in0, in1, op, eng="dve"):
        return self._add(eng, lambda e: e.tensor_tensor(out, in0, in1, op), [in0, in1], [out], False)

    def ts(self, out, in0, s1, s2, op0, op1=None, eng="dve", accum_out=None):
        rd = [in0]
        if not isinstance(s1, (int, float)):
            rd.append(s1)
        if s2 is not None and not isinstance(s2, (int, float)):
            rd.append(s2)
        wr = [out]
        kw = {}
        if accum_out is not None:
            kw["accum_out"] = accum_out
            wr.append(accum_out)
        if op1 is None:
            return self._add(eng, lambda e: e.tensor_scalar(out, in0, s1, None, op0, **kw), rd, wr, False)
        return self._add(eng, lambda e: e.tensor_scalar(out, in0, s1, s2, op0, op1, **kw), rd, wr, False)

    def stt(self, out, in0, scalar, in1, op0, op1):
        rd = [in0, in1]
        if not isinstance(scalar, (int, float)):
            rd.append(scalar)
        return self._add("dve", lambda e: e.scalar_tensor_tensor(out, in0, scalar, in1, op0, op1), rd, [out], False)

    def copy(self, out, in_, eng="dve"):
        if eng == "act":
            return self._add("act", lambda e: e.copy(out, in_), [in_], [out], False)
        return self._add(eng, lambda e: e.tensor_copy(out, in_), [in_], [out], False)

    def memset(self, out, val, eng="dve"):
        return self._add(eng, lambda e: e.memset(out, val), [], [out], False)

    def reduce(self, out, in_, op, axis=AX.X, eng="dve"):
        return self._add(eng, lambda e: e.tensor_reduce(out, in_, axis, op), [in_], [out], False)

    def bn_stats(self, out, in_):
        return self._add("dve", lambda e: e.bn_stats(out, in_), [in_], [out], False)

    def bn_aggr(self, out, in_):
        return self._add("dve", lambda e: e.bn_aggr(out, in_), [in_], [out], False)

    def recip(self, out, in_):
        return self._add("dve", lambda e: e.reciprocal(out, in_), [in_], [out], False)

    def dma(self, out, in_, eng="sp", **kw):
        return self._add(eng, lambda e: e.dma_start(out=out, in_=in_, **kw), [in_], [out], True)

    def emit(self):
        nc = self.nc
        with ExitStack() as es:
            sems = {}
            for e in ENGS:
                for op in self.streams[e]:
                    if op.fn is not None and (not op.dma) and op.key not in sems:
                        sems[op.key] = es.enter_context(nc.semaphore("c_%s_%d" % (op.key[1], op.key[2])))
            for k in self.dma_uses:
                sems[k] = es.enter_context(nc.semaphore("d_%s_%d" % (k[1], k[2])))
            final = [(k, o.val) for k, o in self.dma_last.items()]
            for e in ENGS:
                if self.cpos[e] > 0:
                    final.append((("e", e, self.epoch), self.cpos[e]))
            block = es.enter_context(nc.Block())
            streams = self.streams

            def run(engname, e):
                for op in streams[engname]:
                    for k, v in op.waits:
                        e.wait_ge(sems[k], v)
                    if op.fn is not None:
                        op.fn(e).then_inc(sems[op.key], op.inc)
                if engname == "sp":
                    for k, v in final:
                        e.wait_ge(sems[k], v)

            @block.tensor
            def _(e):
                run("pe", e)

            @block.scalar
            def _(e):
                run("act", e)

            @block.vector
            def _(e):
                run("dve", e)

            @block.gpsimd
            def _(e):
                run("pool", e)

            @block.sync
            def _(e):
                run("sp", e)


DBG = {}
T = 4096
D = 1024
NT = 32
DEPTH = 2
NCOLS = 6936
GLA_OFF, RWKV_OFF, FOX_OFF, HGRN_OFF = 0, 1552, 3344, 4888
DN_ALPHA = (2.0 * DEPTH) ** 0.25
NE = 16


def make_consts():
    c = {}
    i = np.arange(128)
    same = (i[:, None] // 64) == (i[None, :] // 64)
    c["ident"] = np.eye(128, dtype=np.float32)
    c["ones"] = np.ones((128, 128), np.float32)
    c["triu"] = (i[:, None] <= i[None, :]).astype(np.float32)
    c["triu64"] = ((i[:, None] <= i[None, :]) & same).astype(np.float32)
    c["sup64"] = ((i[:, None] < i[None, :]) & same).astype(np.float32)
    c["slo64"] = ((i[:, None] > i[None, :]) & same).astype(np.float32)
    c["blk64"] = same.astype(np.float32)
    return c


CONST_NAMES = ["ident", "ones", "triu", "triu64", "sup64", "slo64", "blk64"]

PARAMS = [
    ("c", [1, D]), ("ada_w", [2, D, 6 * D]), ("ada_b", [2, 6, D]), ("w_in", [2, D, NCOLS]),
    ("gla_alpha_up", [2, 16, 256]), ("gla_alpha_b", [2, 256]), ("gla_norm_g", [2, 128]),
    ("rwkv_mu", [2, 1792]), ("rwkv_w0", [2, 512]), ("rwkv_w2", [2, 64, 512]), ("rwkv_a0", [2, 512]),
    ("rwkv_a2", [2, 64, 512]), ("rwkv_g2", [2, 128, 512]), ("rwkv_k_k", [2, 512]), ("rwkv_k_a", [2, 512]),
    ("rwkv_r_k", [2, 8, 64]), ("rwkv_ln_g", [2, 512]), ("rwkv_ln_b", [2, 512]), ("fox_f_bias", [2, 8]),
    ("hgrn_lb_logits", [2, 512]), ("hgrn_norm_g", [2, 128]), ("w_br", [2, 4, 512, D]),
    ("w_gate", [2, 4, D, D]), ("b_gate", [2, 4, D]), ("w_o", [2, D, D]), ("ln1_g", [2, D]), ("ln1_b", [2, D]),
    ("router_w", [D, NE]), ("router_b", [NE]), ("exp_w_gate", [2, NE, D, 512]), ("exp_w_up", [2, NE, D, 512]),
    ("exp_w_down", [2, NE, 512, D]), ("ln2_g", [2, D]), ("ln2_b", [2, D]),
]


class KB:
    def __init__(self, io=None):
        self.nc = bass.Bass("TRN2", target_bir_lowering=False)
        self.S = Sched(self.nc)
        self.io = io or {}
        self.d = {}
        nc = self.nc
        self.x = self.ext_in("x", [T, D], F32)
        for n, shp in PARAMS:
            self.d[n] = self.ext_in(n, shp, F32)
        self.cst_d = {n: self.ext_in("k_" + n, [128, 128], F32) for n in CONST_NAMES}
        self.psb = [nc.alloc_psum_tensor("psb%d" % i, [128, 512], F32) for i in range(8)]
        self.cst = {n: nc.alloc_sbuf_tensor("c_" + n, [128, 128], F32) for n in CONST_NAMES}
        for n in CONST_NAMES:
            self.S.dma(self.cst[n][:], self.cst_d[n])
        self.hT = None

    def ext_in(self, name, shape, dt):
        self.S.readonly.add(name)
        return self.nc.dram_tensor(name, list(shape), dt, kind="ExternalInput").ap()

    def dram(self, name, shape, dt):
        role = self.io.get(name)
        if role == "in":
            return self.nc.dram_tensor(name, list(shape), dt, kind="ExternalInput").ap()
        if role == "out" or name == "out":
            return self.nc.dram_tensor(name, list(shape), dt, kind="ExternalOutput").ap()
        return self.nc.dram_tensor(name, list(shape), dt).ap()


class Pool_:
    def __init__(self, kb):
        self.kb = kb
        self.es = ExitStack()

    _uid = [0]

    def sb(self, name, shape, dt=F32):
        Pool_._uid[0] += 1
        return self.es.enter_context(self.kb.nc.sbuf_tensor("%s_u%d" % (name, Pool_._uid[0]), list(shape), dt))

    def close(self):
        self.kb.S.barrier()
        self.es.close()


def phase_mod(kb, mod_d):
    S = kb.S
    P = Pool_(kb)
    condT = P.sb("condT", [128, 8])
    load_T(kb, P, condT[:], kb.d["c"].rearrange("o (c p) -> (o c) p", p=128), 8)
    S.act(condT[:], condT[:], AF.Silu)
    wst = [P.sb("adaw%d" % k, [128, 3072]) for k in range(2)]
    mrow = P.sb("mrow", [1, 6144])
    brow = P.sb("brow", [1, 6144])
    n = 0
    for i in range(2):
        S.dma(brow[:], kb.d["ada_b"][i:i + 1].rearrange("o j d -> o (j d)"))
        for half in range(2):
            for kc in range(8):
                w = wst[n % 2]
                n += 1
                S.dma(w[:], kb.d["ada_w"][i, kc * 128:(kc + 1) * 128, half * 3072:(half + 1) * 3072],
                      eng="sp" if n % 2 else "pool")
                for b in range(6):
                    S.mm(kb.psb[b][0:1, :], condT[:, kc:kc + 1], w[:, b * 512:(b + 1) * 512],
                         start=(kc == 0), stop=(kc == 7))
            for b in range(6):
                o = half * 3072 + b * 512
                S.tt(mrow[0:1, o:o + 512], kb.psb[b][0:1, :], brow[0:1, o:o + 512], ALU.add)
        for j in (1, 4):
            S.ts(mrow[0:1, j * 1024:(j + 1) * 1024], mrow[0:1, j * 1024:(j + 1) * 1024], 1.0, None, ALU.add)
        S.dma(mod_d[i:i + 1, :], mrow[:])
    P.close()


def rsqrt_eps(S, out, in_, eps, scale=1.0):
    S.act(out, in_, AF.Ln, bias=float(eps), scale=float(scale))
    S.act(out, out, AF.Exp, scale=-0.5)


def ln_stats(S, xin, st, mv, rstd, eps=1e-5):
    S.bn_stats(st[:, 0:6], xin[:, 0:512])
    S.bn_stats(st[:, 6:12], xin[:, 512:1024])
    S.bn_aggr(mv[:], st[:])
    rsqrt_eps(S, rstd[:], mv[:, 1:2], eps)


def load_T(kb, P, dst, src_rows, n, psum=None):
    S = kb.S
    tmp = P.sb("ldT_tmp", [n, 128])
    S.dma(tmp[:], src_rows)
    ps = kb.psb[7] if psum is None else psum
    S.mm(ps[:, 0:n], tmp[:], kb.cst["ident"][0:n, 0:n], start=True, stop=True)
    S.copy(dst, ps[:, 0:n])


def load_modT(kb, P, mod_d, layer, name):
    modT = P.sb(name, [128, 6, 8])
    load_T(kb, P, modT[:].rearrange("p j c -> p (j c)"), mod_d[layer].rearrange("(r p) -> r p", p=128), 48)
    return modT


def phase_ln_mixer(kb, x_src, mod_d, layer):
    S = kb.S
    P = Pool_(kb)
    modT = load_modT(kb, P, mod_d, layer, "modT_a")
    xb = [P.sb("lnx%d" % k, [128, 1024]) for k in range(2)]
    st = [P.sb("lnst%d" % k, [128, 12]) for k in range(2)]
    mv = [P.sb("lnmv%d" % k, [128, 2]) for k in range(2)]
    rs = [P.sb("lnrs%d" % k, [128, 1]) for k in range(2)]
    ident = kb.cst["ident"]
    for t in range(DBG.get("ln_nt", NT)):
        k = t % 2
        xin = xb[k]
        S.dma(xin[:], x_src[t * 128:(t + 1) * 128, :])
        if DBG.get("ln_lvl", 9) < 1:
            continue
        ln_stats(S, xin, st[k], mv[k], rs[k])
        S.ts(xin[:], xin[:], mv[k][:, 0:1], rs[k][:, 0:1], ALU.subtract, ALU.mult)
        if DBG.get("ln_lvl", 9) < 2:
            continue
        for c in range(8):
            pb = kb.psb[(t % 2) * 2 + c // 4]
            S.tr(pb[:, (c % 4) * 128:(c % 4 + 1) * 128], xin[:, c * 128:(c + 1) * 128], ident[:])
        if DBG.get("ln_lvl", 9) < 3:
            continue
        for c in range(DBG.get("ln_nc", 8)):
            pb = kb.psb[(t % 2) * 2 + c // 4]
            src = pb[:, (c % 4) * 128:(c % 4 + 1) * 128]
            off = DBG.get("ln_off", 1)
            dst = kb.hT[:, c, off + t * 128:off + (t + 1) * 128]
            ev = DBG.get("ln_evac", "both")
            if (c % 2 == 0 and ev == "both") or ev == "dve":
                S.ts(dst, src, modT[:, 1, c:c + 1], modT[:, 0, c:c + 1], ALU.mult, ALU.add)
            else:
                S.act(dst, src, AF.Identity, bias=modT[:, 0, c:c + 1], scale=modT[:, 1, c:c + 1])
    P.close()


def prep_cast(kb, P, jobs, stg, stb):
    S = kb.S
    n = 0
    for dst, src in jobs:
        R, N = src.shape
        for r in range(0, R, 128):
            a, b = stg[n % 2], stb[n % 2]
            n += 1
            S.dma(a[:, 0:N], src[r:r + 128, :], eng="sp")
            S.copy(b[:, 0:N], a[:, 0:N], eng="pool" if n % 2 else "act")
            S.dma(dst[r:r + 128, :], b[:, 0:N], eng="pool")


class Epi:
    def __init__(self, kb, P, mod_d, layer, gt_idx, g_name, b_name, tag, with_z=True):
        S = kb.S
        self.kb = kb
        self.gt = P.sb("epi_gt" + tag, [128, 1024])
        self.g = P.sb("epi_g" + tag, [128, 1024])
        self.b = P.sb("epi_b" + tag, [128, 1024])
        S.dma(self.gt[:], mod_d[layer:layer + 1, gt_idx * 1024:(gt_idx + 1) * 1024].partition_broadcast(128))
        S.dma(self.g[:], kb.d[g_name][layer:layer + 1, :].partition_broadcast(128))
        S.dma(self.b[:], kb.d[b_name][layer:layer + 1, :].partition_broadcast(128))
        self.xb = [P.sb("epi_x%s%d" % (tag, k), [128, 1024]) for k in range(2)]
        self.zb = [P.sb("epi_z%s%d" % (tag, k), [128, 1024]) for k in range(2)] if with_z else None
        self.st = [P.sb("epi_st%s%d" % (tag, k), [128, 12]) for k in range(2)]
        self.mv = [P.sb("epi_mv%s%d" % (tag, k), [128, 2]) for k in range(2)]
        self.rs = [P.sb("epi_rs%s%d" % (tag, k), [128, 1]) for k in range(2)]
        self.n = 0

    def prefetch_x(self, x_src, t):
        k = self.n % 2
        self.kb.S.dma(self.xb[k][:], x_src[t * 128:(t + 1) * 128, :])

    def run(self, y_halves, x_dst, t, x_src=None, z=None):
        S = self.kb.S
        k = self.n % 2
        self.n += 1
        if x_src is not None:
            S.dma(self.xb[k][:], x_src[t * 128:(t + 1) * 128, :])
        x = self.xb[k]
        if z is None:
            z = self.zb[k]
        for h in range(2):
            S.tt(z[:, h * 512:(h + 1) * 512], y_halves[h], self.gt[:, h * 512:(h + 1) * 512], ALU.mult)
        S.stt(z[:], x[:], DN_ALPHA, z[:], ALU.mult, ALU.add)
        ln_stats(S, z, self.st[k], self.mv[k], self.rs[k])
        S.ts(z[:], z[:], self.mv[k][:, 0:1], self.rs[k][:, 0:1], ALU.subtract, ALU.mult)
        S.tt(z[:], z[:], self.g[:], ALU.mult, eng="pool")
        S.tt(z[:], z[:], self.b[:], ALU.add, eng="pool")
        S.dma(x_dst[t * 128:(t + 1) * 128, :], z[:], eng="pool")


def phase_prep_merge(kb, layer, wg_d, wb_d, wo_d):
    P = Pool_(kb)
    stg = [P.sb("pst%d" % k, [128, 1024]) for k in range(2)]
    stb = [P.sb("psb%d" % k, [128, 1024], BF16) for k in range(2)]
    jobs = []
    for n in range(4):
        jobs.append((wg_d[n], kb.d["w_gate"][layer, n]))
        jobs.append((wb_d[n], kb.d["w_br"][layer, n]))
    jobs.append((wo_d, kb.d["w_o"][layer]))
    prep_cast(kb, P, jobs, stg, stb)
    P.close()


def phase_merge(kb, layer, mod_d, brT_d, wg_d, wb_d, wo_d, x_src, x_dst):
    S = kb.S
    P = Pool_(kb)
    epi = Epi(kb, P, mod_d, layer, 2, "ln1_g", "ln1_b", "m")
    bgT = P.sb("bgT", [128, 4, 8])
    load_T(kb, P, bgT[:].rearrange("p n c -> p (n c)"), kb.d["b_gate"][layer].rearrange("n (c p) -> (n c) p", p=128), 32)
    wo = P.sb("wo", [128, 8, 1024], BF16)
    S.dma(wo[:], wo_d.rearrange("(c p) n -> p c n", p=128))
    wg = [P.sb("wg%d" % k, [128, 8, 1024], BF16) for k in range(2)]
    wb = [P.sb("wb%d" % k, [128, 4, 1024], BF16) for k in range(2)]
    brt = [P.sb("brt%d" % k, [128, 4, 512], BF16) for k in range(2)]
    mT = P.sb("mT", [128, 8, 512])
    mTb = P.sb("mTb", [128, 8, 512], BF16)
    sig = [P.sb("sig%d" % k, [128, 512]) for k in range(2)]
    tmp = [P.sb("mtmp%d" % k, [128, 512]) for k in range(2)]
    cnt = 0
    q = 0
    for g in range(8):
        tok = slice(g * 512, (g + 1) * 512)
        for n in range(4):
            k = cnt % 2
            cnt += 1
            S.dma(wg[k][:], wg_d[n].rearrange("(c p) n -> p c n", p=128), eng="sp")
            S.dma(wb[k][:], wb_d[n].rearrange("(c p) n -> p c n", p=128), eng="sp")
            S.dma(brt[k][:], brT_d[n, :, :, tok].rearrange("c p t -> p c t"), eng="sp")
            for cc in range(8):
                pa = kb.psb[(q % 2) * 2]
                pb = kb.psb[(q % 2) * 2 + 1]
                for kc in range(8):
                    S.mm(pa[:], wg[k][:, kc, cc * 128:(cc + 1) * 128], kb.hT[:, kc, 1 + g * 512:1 + (g + 1) * 512],
                         start=(kc == 0), stop=(kc == 7))
                for kc in range(4):
                    S.mm(pb[:], wb[k][:, kc, cc * 128:(cc + 1) * 128], brt[k][:, kc, :],
                         start=(kc == 0), stop=(kc == 3))
                sg = sig[q % 2]
                S.act(sg[:], pa[:], AF.Sigmoid, bias=bgT[:, n, cc:cc + 1])
                if n == 0:
                    S.tt(mT[:, cc, :], sg[:], pb[:], ALU.mult)
                else:
                    tp = tmp[q % 2]
                    S.tt(tp[:], sg[:], pb[:], ALU.mult)
                    S.tt(mT[:, cc, :], mT[:, cc, :], tp[:], ALU.add, eng="pool")
                q += 1
        for cc in range(8):
            S.copy(mTb[:, cc, :], mT[:, cc, :], eng="act" if cc % 2 else "pool")
        for tt in range(4):
            t = g * 4 + tt
            epi.prefetch_x(x_src, t)
            ys = []
            for h in range(2):
                py = kb.psb[4 + (t % 2) * 2 + h]
                for kc in range(8):
                    S.mm(py[:], mTb[:, kc, tt * 128:(tt + 1) * 128], wo[:, kc, h * 512:(h + 1) * 512],
                         start=(kc == 0), stop=(kc == 7))
                ys.append(py[:])
            epi.run(ys, x_dst, t)
    P.close()


def phase_moe(kb, layer, mod_d, x_src, x_dst):
    S = kb.S
    P = Pool_(kb)
    NSG = 2
    TSG = T // NSG
    NTS = TSG // 128
    epi = Epi(kb, P, mod_d, layer, 5, "ln2_g", "ln2_b", "e", with_z=False)
    modT = load_modT(kb, P, mod_d, layer, "modT_e")
    rw = P.sb("rw", [128, 8, NE])
    S.dma(rw[:], kb.d["router_w"].rearrange("(c p) e -> p c e", p=128))
    rb = P.sb("rb", [128, NE])
    S.dma(rb[:], kb.d["router_b"].rearrange("(o e) -> o e", o=1).partition_broadcast(128))
    hT = P.sb("hTm", [128, 8, TSG + 1], BF16)
    yacc = P.sb("yacc", [128, NTS, 1024])
    comb = P.sb("comb", [128, NTS, NE])
    h32 = [P.sb("h32_%d" % k, [128, 8, 128]) for k in range(2)]
    st = [P.sb("mst%d" % k, [128, 12]) for k in range(2)]
    mv = [P.sb("mmv%d" % k, [128, 2]) for k in range(2)]
    rs = [P.sb("mrs%d" % k, [128, 1]) for k in range(2)]
    lg = P.sb("r_lg", [128, NE])
    pr = P.sb("r_pr", [128, NE])
    sel = P.sb("r_sel", [128, NE])
    sel2 = P.sb("r_sel2", [128, NE])
    eq = P.sb("r_eq", [128, NE])
    m1 = P.sb("r_m1", [128, 4])
    m2 = P.sb("r_m2", [128, 4])
    gs = P.sb("r_gs", [128, 4])
    gm = P.sb("r_gm", [128, 1])
    og = P.sb("r_og", [128, 4])
    thr = P.sb("r_thr", [128, 4])
    msk = P.sb("r_msk", [128, NE])
    sm = P.sb("r_sm", [128, 1])
    mx = P.sb("r_mx", [128, 1])
    wg = [P.sb("ewg%d" % k, [128, 8, 512], BF16) for k in range(2)]
    wu = [P.sb("ewu%d" % k, [128, 8, 512], BF16) for k in range(2)]
    wd = [P.sb("ewd%d" % k, [128, 4, 1024], BF16) for k in range(2)]
    stg = [P.sb("estg%d" % k, [128, 2, 512]) for k in range(3)]
    heT = [P.sb("heT%d" % k, [128, 4, 512], BF16) for k in range(2)]
    sl = [P.sb("esl%d" % k, [128, 512]) for k in range(2)]
    ident = kb.cst["ident"]
    nld = [0]

    def load_expert(e, k):
        for (dst, src, kcn) in ((wg[k], kb.d["exp_w_gate"][layer, e], 8), (wu[k], kb.d["exp_w_up"][layer, e], 8)):
            sv = src.rearrange("(c p) n -> p c n", p=128)
            for c2 in range(0, kcn, 2):
                sg_ = stg[nld[0] % 3]
                nld[0] += 1
                S.dma(sg_[:], sv[:, c2:c2 + 2, :], eng="sp")
                S.copy(dst[:, c2:c2 + 2, :], sg_[:], eng="pool")
        sv = kb.d["exp_w_down"][layer, e].rearrange("(c p) n -> p c n", p=128)
        for c in range(4):
            sg_ = stg[nld[0] % 3]
            nld[0] += 1
            S.dma(sg_[:].rearrange("p a b -> p (a b)"), sv[:, c, :], eng="sp")
            S.copy(wd[k][:, c, :], sg_[:].rearrange("p a b -> p (a b)"), eng="pool")

    for sgi in range(NSG):
        t0 = sgi * NTS
        for tl in range(NTS):
            t = t0 + tl
            k = tl % 2
            xin = epi.xb[k]
            S.dma(xin[:], x_src[t * 128:(t + 1) * 128, :])
            ln_stats(S, xin, st[k], mv[k], rs[k])
            S.ts(xin[:], xin[:], mv[k][:, 0:1], rs[k][:, 0:1], ALU.subtract, ALU.mult)
            for c in range(8):
                pb = kb.psb[k * 2 + c // 4]
                S.tr(pb[:, (c % 4) * 128:(c % 4 + 1) * 128], xin[:, c * 128:(c + 1) * 128], ident[:])
            for c in range(8):
                pb = kb.psb[k * 2 + c // 4]
                src = pb[:, (c % 4) * 128:(c % 4 + 1) * 128]
                if c % 2 == 0:
                    S.ts(h32[k][:, c, :], src, modT[:, 4, c:c + 1], modT[:, 3, c:c + 1], ALU.mult, ALU.add)
                else:
                    S.act(h32[k][:, c, :], src, AF.Identity, bias=modT[:, 3, c:c + 1], scale=modT[:, 4, c:c + 1])
                S.copy(hT[:, c, 1 + tl * 128:1 + (tl + 1) * 128], h32[k][:, c, :], eng="pool")
            pl = kb.psb[4 + k]
            for c in range(8):
                S.mm(pl[:, 0:NE], h32[k][:, c, :], rw[:, c, :], start=(c == 0), stop=(c == 7))
            S.copy(lg[:], pl[:, 0:NE])
            S.reduce(mx[:], lg[:], ALU.max)
            S.ts(mx[:], mx[:], -1.0, None, ALU.mult)
            S.act(pr[:], lg[:], AF.Exp, bias=mx[:, 0:1], scale=1.0, accum_out=sm[:])
            S.recip(sm[:], sm[:])
            S.ts(pr[:], pr[:], sm[:, 0:1], None, ALU.mult)
            S.tt(sel[:], pr[:], rb[:], ALU.add)
            sel3 = sel[:].rearrange("p (g e) -> p g e", g=4)
            S.reduce(m1[:], sel3, ALU.max)
            S.tt(eq[:].rearrange("p (g e) -> p g e", g=4), sel3, m1[:].unsqueeze(2).to_broadcast([128, 4, 4]), ALU.is_ge)
            S.stt(sel2[:], eq[:], -1e9, sel[:], ALU.mult, ALU.add)
            S.reduce(m2[:], sel2[:].rearrange("p (g e) -> p g e", g=4), ALU.max)
            S.tt(gs[:], m1[:], m2[:], ALU.add)
            S.reduce(gm[:], gs[:], ALU.max)
            S.ts(og[:], gs[:], gm[:, 0:1], None, ALU.is_ge)
            S.ts(thr[:], og[:], -1e9, 1e9, ALU.mult, ALU.add)
            S.tt(thr[:], thr[:], m2[:], ALU.add)
            S.tt(msk[:].rearrange("p (g e) -> p g e", g=4), sel3, thr[:].unsqueeze(2).to_broadcast([128, 4, 4]), ALU.is_ge)
            S.tt(msk[:], msk[:], pr[:], ALU.mult)
            S.reduce(sm[:], msk[:], ALU.add)
            S.recip(sm[:], sm[:])
            S.ts(comb[:, tl, :], msk[:], sm[:, 0:1], None, ALU.mult)
        if sgi == 0:
            load_expert(0, 0)
        q = 0
        for e in range(NE):
            k = (sgi * NE + e) % 2
            nxt = sgi * NE + e + 1
            if nxt < NSG * NE:
                load_expert(nxt % NE, nxt % 2)
            for gq in range(NTS // 4):
                he = heT[gq % 2]
                for fc in range(4):
                    pg = kb.psb[(q % 2) * 2]
                    pu = kb.psb[(q % 2) * 2 + 1]
                    for kc in range(8):
                        S.mm(pg[:], wg[k][:, kc, fc * 128:(fc + 1) * 128], hT[:, kc, 1 + gq * 512:1 + (gq + 1) * 512],
                             start=(kc == 0), stop=(kc == 7))
                    for kc in range(8):
                        S.mm(pu[:], wu[k][:, kc, fc * 128:(fc + 1) * 128], hT[:, kc, 1 + gq * 512:1 + (gq + 1) * 512],
                             start=(kc == 0), stop=(kc == 7))
                    s_ = sl[q % 2]
                    S.act(s_[:], pg[:], AF.Silu)
                    S.tt(he[:, fc, :], s_[:], pu[:], ALU.mult)
                    q += 1
                for tt in range(4):
                    tl = gq * 4 + tt
                    for h in range(2):
                        py = kb.psb[4 + (tl * 2 + h) % 4]
                        for fc in range(4):
                            S.mm(py[:], he[:, fc, tt * 128:(tt + 1) * 128], wd[k][:, fc, h * 512:(h + 1) * 512],
                                 start=(fc == 0), stop=(fc == 3))
                        ya = yacc[:, tl, h * 512:(h + 1) * 512]
                        if e == 0:
                            S.ts(ya, py[:], comb[:, tl, e:e + 1], None, ALU.mult)
                        else:
                            S.stt(ya, py[:], comb[:, tl, e:e + 1], ya, ALU.mult, ALU.add)
        for tl in range(NTS):
            t = t0 + tl
            epi.run([yacc[:, tl, 0:512], yacc[:, tl, 512:1024]], x_dst, t, x_src=x_src, z=yacc[:, tl, :])
    P.close()


def build(io=None, layers=(0, 1), stages=("mod", "ln", "prep", "br", "merge", "moe"), last_out=None):
    kb = KB(io)
    S = kb.S
    mod_d = kb.dram("mod_d", [2, 6144], F32)
    xa = kb.dram("xa", [T, D], F32)
    xbd = kb.dram("xbd", [T, D], F32)
    out = kb.dram("out", [T, D], F32)
    brT_d = kb.dram("brT", [4, 4, 128, T], BF16)
    wg_d = kb.dram("wg_bf", [4, D, D], BF16)
    wb_d = kb.dram("wb_bf", [4, 512, D], BF16)
    wo_d = kb.dram("wo_bf", [D, D], BF16)
    kb.lb_d = kb.dram("lb_d", [2, 512], F32)
    if "mod" in stages:
        phase_mod(kb, mod_d)
    for layer in layers:
        x_src = kb.x if layer == 0 else xbd
        x_fin = out if layer == layers[-1] else xbd
        MP = Pool_(kb)
        kb.hT = MP.sb("hT", [128, 8, T + 1], BF16)
        for c in range(8):
            S.memset(kb.hT[:, c, 0:1], 0.0, eng="pool")
        if "ln" in stages:
            phase_ln_mixer(kb, x_src, mod_d, layer)
        if "prep" in stages:
            phase_prep_merge(kb, layer, wg_d, wb_d, wo_d)
        if "br" in stages:
            phase_branches(kb, layer, brT_d)
        if "merge" in stages:
            phase_merge(kb, layer, mod_d, brT_d, wg_d, wb_d, wo_d, x_src, xa if "moe" in stages else x_fin)
        MP.close()
        if "moe" in stages:
            phase_moe(kb, layer, mod_d, xa, x_fin)
    S.emit()
    return kb


def load_w(kb, dst, src, stg, cast_eng="pool", dma_eng="sp"):
    S = kb.S
    kc = src.shape[0] // 128
    n = src.shape[1]
    sv = stg[:, 0:kc, 0:n]
    S.dma(sv, src.rearrange("(c p) n -> p c n", p=128), eng=dma_eng)
    S.copy(dst, sv, eng=cast_eng)


def tok(t0, n=128):
    return slice(1 + t0, 1 + t0 + n)


def fox_branch(kb, layer, brT_d):
    S = kb.S
    P = Pool_(kb)
    hT = kb.hT
    W = kb.d["w_in"][layer]
    o = FOX_OFF
    psb = kb.psb
    stg = P.sb("fstg", [128, 8, 520])
    wq = P.sb("fwq", [128, 8, 512], BF16)
    wk = P.sb("fwk", [128, 8, 512], BF16)
    wvf = P.sb("fwvf", [128, 8, 520], BF16)
    load_w(kb, wq[:], W[:, o:o + 512], stg)
    load_w(kb, wk[:], W[:, o + 512:o + 1024], stg)
    load_w(kb, wvf[:], W[:, o + 1024:o + 1544], stg)
    fb = P.sb("ffb", [128, 8])
    S.dma(fb[:], kb.d["fox_f_bias"][layer:layer + 1, :].partition_broadcast(128))
    maskb = P.sb("fmask", [128, 128], BF16)
    S.copy(maskb[:], kb.cst["triu"][:])
    lf = P.sb("flf", [128, 32, 8])
    tA = P.sb("ftA", [128, 32, 8])
    tB = P.sb("ftB", [128, 32, 8])
    Fs = P.sb("fFs", [128, 32, 8])
    Cs = P.sb("fCs", [128, 32, 8])
    vp = P.sb("fvp", [128, 32, 8, 65], BF16)
    S.memset(vp[:].rearrange("p a b c -> p (a b c)"), 1.0, eng="pool")
    for g in range(8):
        pb = psb[6 + g % 2]
        for tt in range(4):
            t = g * 4 + tt
            for kc in range(8):
                S.mm(pb[:, tt * 8:(tt + 1) * 8], hT[:, kc, tok(t * 128)], wvf[:, kc, 512:520], start=(kc == 0), stop=(kc == 7))
        S.tt(lf[:, g * 4:(g + 1) * 4, :], pb[:, 0:32].rearrange("p (a b) -> p a b", a=4),
             fb[:].unsqueeze(1).to_broadcast([128, 4, 8]), ALU.add)
    lf2 = lf[:].rearrange("p a b -> p (a b)")
    S.act(lf2, lf2, AF.Exp, scale=-1.0)
    S.act(lf2, lf2, AF.Ln, bias=1.0)
    S.ts(lf2, lf2, -1.0, None, ALU.mult)
    a, b = lf, tA
    d = 1
    while d < 32:
        nb = tA if b is tA else tB
        if a is lf:
            nb = tA
        S.tt(nb[:, d:32, :], a[:, d:32, :], a[:, 0:32 - d, :], ALU.add)
        S.copy(nb[:, 0:d, :], a[:, 0:d, :])
        a = nb
        b = tB if nb is tA else tA
        d *= 2
    incl = a
    excl = tB if incl is tA else tA
    S.tt(excl[:], incl[:], lf[:], ALU.subtract)
    pF, pC = psb[6], psb[7]
    S.mm(pF[:, 0:256], kb.cst["triu"][:], lf2, start=True, stop=False)
    S.mm(pF[:, 0:256], kb.cst["ones"][:], excl[:].rearrange("p a b -> p (a b)"), start=False, stop=True)
    S.mm(pC[:, 0:256], kb.cst["ones"][:], incl[:].rearrange("p a b -> p (a b)"), start=True, stop=True)
    S.copy(Fs[:].rearrange("p a b -> p (a b)"), pF[:, 0:256])
    S.copy(Cs[:].rearrange("p a b -> p (a b)"), pC[:, 0:256], eng="act")
    for t in range(32):
        pb = psb[6 + t % 2]
        for kc in range(8):
            S.mm(pb[:], hT[:, kc, tok(t * 128)], wvf[:, kc, 0:512], start=(kc == 0), stop=(kc == 7))
        src = pb[:].rearrange("p (h d) -> p h d", h=8)
        if t % 2 == 0:
            S.copy(vp[:, t, :, 0:64], src, eng="dve")
        else:
            S.copy(vp[:, t, :, 0:64], src, eng="act")
    QT = P.sb("fQT", [128, T], BF16)
    KA = P.sb("fKA", [128, T], BF16)
    KB_ = P.sb("fKB", [128, T], BF16)
    S.memset(KA[64:128, :], 0.0, eng="pool")
    S.memset(KB_[0:64, :], 0.0, eng="pool")
    otok = P.sb("fotok", [128, 32, 128])
    brs = P.sb("fbrs", [128, T], BF16)
    pts = [P.sb("fpt%d" % k, [128, 512], BF16) for k in range(4)]
    vss = [P.sb("fvs%d" % k, [128, 65], BF16) for k in range(6)]
    biases = [P.sb("fbias%d" % k, [128, 32]) for k in range(2)]
    dms = [P.sb("fdm%d" % k, [128, 32]) for k in range(2)]
    rcs = [P.sb("frc%d" % k, [128, 1]) for k in range(2)]
    q = 0
    nb_ = 0
    nv = 0
    for p in range(4):
        for g in range(8):
            pq = psb[6]
            pk = psb[7]
            for kc in range(8):
                S.mm(pq[:], wq[:, kc, p * 128:(p + 1) * 128], hT[:, kc, tok(g * 512, 512)], start=(kc == 0), stop=(kc == 7))
            for kc in range(8):
                S.mm(pk[:], wk[:, kc, p * 128:(p + 1) * 128], hT[:, kc, tok(g * 512, 512)], start=(kc == 0), stop=(kc == 7))
            S.act(QT[:, g * 512:(g + 1) * 512], pq[:], AF.Copy, scale=0.125)
            S.copy(KA[0:64, g * 512:(g + 1) * 512], pk[0:64, :])
            S.copy(KB_[64:128, g * 512:(g + 1) * 512], pk[64:128, :])
        rows = []
        for a_ in range(2):
            for i in range(32):
                rows.append((a_, i))
        batches = []
        for ri, (a_, i) in enumerate(rows):
            for jb in range(0, i + 1, 4):
                batches.append((ri, a_, i, jb, min(4, i + 1 - jb)))
        rowbuf = {}

        def front(bt):
            nonlocal q, nb_
            ri, a_, i, jb, nbt = bt
            h = 2 * p + a_
            Kh = KA if a_ == 0 else KB_
            if jb == 0:
                k2 = nb_ % 2
                nb_ += 1
                rowbuf[ri] = k2
                S.ts(biases[k2][:, 0:i + 1], Fs[:, 0:i + 1, h], Cs[:, i, h:h + 1], -1.0, ALU.subtract, ALU.mult)
                S.act(dms[k2][:, 0:i + 1], biases[k2][:, 0:i + 1], AF.Exp)
            ps_s = psb[q % 4]
            pt = pts[q % 4]
            q += 1
            for jj in range(nbt):
                j = jb + jj
                S.mm(ps_s[:, jj * 128:(jj + 1) * 128], Kh[:, j * 128:(j + 1) * 128], QT[:, i * 128:(i + 1) * 128])
            S.act(pt[:, 0:nbt * 128], ps_s[:, 0:nbt * 128], AF.Exp)
            if jb + nbt - 1 == i:
                S.tt(pt[:, (nbt - 1) * 128:nbt * 128], pt[:, (nbt - 1) * 128:nbt * 128], maskb[:], ALU.mult)
            return pt

        def back(bt, pt):
            nonlocal nv
            ri, a_, i, jb, nbt = bt
            h = 2 * p + a_
            k2 = rowbuf[ri]
            po = psb[4 + k2]
            dm = dms[k2]
            rc = rcs[k2]
            for jj in range(nbt):
                j = jb + jj
                vs = vss[nv % 6]
                nv += 1
                S.ts(vs[:], vp[:, j, h, :], dm[:, j:j + 1], None, ALU.mult)
                S.mm(po[:, 0:65], pt[:, jj * 128:(jj + 1) * 128], vs[:], start=(j == 0), stop=(j == i))
            if jb + nbt - 1 == i:
                S.recip(rc[:], po[:, 64:65])
                S.ts(otok[:, i, a_ * 64:(a_ + 1) * 64], po[:, 0:64], rc[:, 0:1], None, ALU.mult)
                if a_ == 1:
                    pt_ = psb[6 + i % 2]
                    S.tr(pt_[:, 0:128], otok[:, i, :], kb.cst["ident"][:])
                    S.copy(brs[:, i * 128:(i + 1) * 128], pt_[:, 0:128], eng="act" if i % 2 else "dve")

        pend = None
        for bt in batches:
            ptn = front(bt)
            if pend is not None:
                back(*pend)
            pend = (bt, ptn)
        back(*pend)
        S.dma(brT_d[2, p], brs[:], eng="pool")
    P.close()


def phase_branches(kb, layer, brT_d):
    which = DBG.get("branches", (0, 1, 2, 3))
    if 2 in which:
        fox_branch(kb, layer, brT_d)
    if 0 in which:
        gla_branch(kb, layer, brT_d)
    if 3 in which:
        hgrn_branch(kb, layer, brT_d)
    if 1 in which:
        rwkv_branch(kb, layer, brT_d)


def cgla_branch(kb, layer, brT_d, kind, lb_d=None):
    S = kb.S
    P = Pool_(kb)
    hT = kb.hT
    W = kb.d["w_in"][layer]
    psb = kb.psb
    cst = kb.cst
    gla = (kind == "gla")
    NU = 2 if gla else 4
    HPU = 2 if gla else 1
    DK = 64 if gla else 128
    KW = NU * 128
    o = GLA_OFF if gla else HGRN_OFF
    bidx = 0 if gla else 3
    qscale = 0.125 if gla else 1.0
    stg = P.sb("cstg", [128, 8, 528])
    if gla:
        wq = P.sb("cwq", [128, 8, 256], BF16)
        wk = P.sb("cwk", [128, 8, 256], BF16)
        wv = P.sb("cwv", [128, 8, 512], BF16)
        wg = P.sb("cwg", [128, 8, 528], BF16)
        load_w(kb, wq[:], W[:, o:o + 256], stg)
        load_w(kb, wk[:], W[:, o + 256:o + 512], stg)
        load_w(kb, wv[:], W[:, o + 512:o + 1024], stg)
        load_w(kb, wg[:], W[:, o + 1024:o + 1552], stg)
        aup = P.sb("caup", [16, 256])
        S.dma(aup[:], kb.d["gla_alpha_up"][layer])
        abr = P.sb("cabr", [1, 256])
        S.dma(abr[:], kb.d["gla_alpha_b"][layer:layer + 1, :])
        alT = P.sb("calT", [16, 512])
        Uc = P.sb("cUc", [128, 128])
        SUl = P.sb("cSUl", [128, 128])
        S.ts(Uc[:], cst["triu64"][:], -1.0 / 16.0, None, ALU.mult)
        S.ts(SUl[:], cst["slo64"][:], -1.0 / 16.0, None, ALU.mult)
        ng_src = kb.d["gla_norm_g"]
    else:
        wq = P.sb("cwq", [128, 8, 512], BF16)
        wk = P.sb("cwk", [128, 8, 512], BF16)
        wv = P.sb("cwv", [128, 8, 512], BF16)
        wg = P.sb("cwg", [128, 8, 512], BF16)
        load_w(kb, wq[:], W[:, o:o + 512], stg)
        load_w(kb, wk[:], W[:, o + 512:o + 1024], stg)
        load_w(kb, wv[:], W[:, o + 1024:o + 1536], stg)
        load_w(kb, wg[:], W[:, o + 1536:o + 2048], stg)
        Uc, SUl = cst["triu64"], cst["slo64"]
        lbB = P.sb("clbB", [128, 512])
        omlB = P.sb("comlB", [128, 512])
        S.dma(lbB[:], lb_d[0:1, :].partition_broadcast(128))
        S.ts(omlB[:], lbB[:], -1.0, 1.0, ALU.mult, ALU.add)
        lbT = P.sb("clbT", [128, 4])
        omlT = P.sb("comlT", [128, 4])
        load_T(kb, P, lbT[:], lb_d[0].rearrange("(c p) -> c p", p=128), 4)
        S.ts(omlT[:], lbT[:], -1.0, 1.0, ALU.mult, ALU.add)
        ng_src = kb.d["hgrn_norm_g"]
    ngb = P.sb("cngb", [128, 128])
    S.dma(ngb[:], ng_src[layer:layer + 1, :].partition_broadcast(128))
    qTs = [P.sb("cqTs%d" % u, [128, 512]) for u in range(NU)]
    kTs = [P.sb("ckTs%d" % u, [128, 512]) for u in range(NU)]
    brs = P.sb("cbrs", [128, 4, T], BF16)
    l_tok = P.sb("cltok", [128, KW])
    k_tok = P.sb("cktok", [128, KW])
    f_tok = P.sb("cftok", [128, KW])
    v_tok = P.sb("cvtok", [128, 512], BF16)
    sg_tok = P.sb("csgtok", [128, 512])
    br_tok = P.sb("cbrtok", [128, 512])
    bTs = P.sb("cbTs", [128, 128])
    kdec = P.sb("ckdec", [128, 128])
    khat = P.sb("ckhat", [128, 128], BF16)
    bm = P.sb("cbm", [128, 2])
    nbm = P.sb("cnbm", [128, 2])
    E1 = P.sb("cE1", [128, 128])
    E2 = P.sb("cE2", [128, 128])
    E3 = P.sb("cE3", [128, 128])
    qt = P.sb("cqt", [128, 128], BF16)
    kt = P.sb("ckt", [128, 128], BF16)
    qA = P.sb("cqA", [128, 128], BF16)
    qB = P.sb("cqB", [128, 128], BF16)
    attb = [P.sb("cattb%d" % a, [128, 128], BF16) for a in range(HPU)]
    Sf = [[P.sb("cSf%d_%d" % (u, k), [128, 128]) for k in range(2)] for u in range(NU)]
    Sb = [[P.sb("cSb%d_%d" % (u, k), [128, 128], BF16) for k in range(2)] for u in range(NU)]
    for u in range(NU):
        S.memset(Sf[u][0][:], 0.0)
        S.memset(Sb[u][0][:], 0.0)
    ss = P.sb("css", [128, 2])
    junk = P.sb("cjunk", [128, 128])
    for g in range(DBG.get('cg_ng', 8)):
        gt = tok(g * 512, 512)
        for u in range(NU):
            pq, pk = psb[0], psb[1]
            for kc in range(8):
                S.mm(pq[:], wq[:, kc, u * 128:(u + 1) * 128], hT[:, kc, gt], start=(kc == 0), stop=(kc == 7))
            for kc in range(8):
                S.mm(pk[:], wk[:, kc, u * 128:(u + 1) * 128], hT[:, kc, gt], start=(kc == 0), stop=(kc == 7))
            S.copy(qTs[u][:], pq[:], eng="act")
            if gla:
                S.copy(kTs[u][:], pk[:], eng="dve")
            else:
                S.act(kTs[u][:], pk[:], AF.Sigmoid)
                S.ts(kTs[u][:], kTs[u][:], omlT[:, u:u + 1], lbT[:, u:u + 1], ALU.mult, ALU.add)
                S.ts(kTs[u][:], kTs[u][:], -1.0, 1.0, ALU.mult, ALU.add)
        if gla:
            pa = psb[2]
            for kc in range(8):
                S.mm(pa[0:16, :], wg[:, kc, 512:528], hT[:, kc, gt], start=(kc == 0), stop=(kc == 7))
            S.copy(alT[:], pa[0:16, :])
        for tt in range(4):
            t = g * 4 + tt
            tk = tok(t * 128)
            tcol = slice(tt * 128, (tt + 1) * 128)
            pv, pg = psb[2], psb[3]
            for kc in range(8):
                S.mm(pv[:], hT[:, kc, tk], wv[:, kc, 0:512], start=(kc == 0), stop=(kc == 7))
            S.copy(v_tok[:], pv[:], eng="act")
            for kc in range(8):
                S.mm(pg[:], hT[:, kc, tk], wg[:, kc, 0:512], start=(kc == 0), stop=(kc == 7))
            S.act(sg_tok[:], pg[:], AF.Silu)
            pk2 = psb[2]
            if gla:
                for kc in range(8):
                    S.mm(pk2[:, 0:256], hT[:, kc, tk], wk[:, kc, 0:256], start=(kc == 0), stop=(kc == 7))
                S.mm(pk2[:, 256:512], alT[:, tcol], aup[:], start=True, stop=False)
                S.mm(pk2[:, 256:512], cst["ones"][0:1, :], abr[:], start=False, stop=True)
                S.copy(k_tok[:], pk2[:, 0:256])
                S.act(l_tok[:], pk2[:, 256:512], AF.Exp, scale=-1.0)
                S.act(l_tok[:], l_tok[:], AF.Ln, bias=1.0)
            else:
                for kc in range(8):
                    S.mm(pk2[:], hT[:, kc, tk], wk[:, kc, 0:512], start=(kc == 0), stop=(kc == 7))
                S.act(f_tok[:], pk2[:], AF.Sigmoid)
                S.tt(f_tok[:], f_tok[:], omlB[:], ALU.mult)
                S.tt(f_tok[:], f_tok[:], lbB[:], ALU.add)
                S.act(l_tok[:], f_tok[:], AF.Ln)
                S.ts(k_tok[:], f_tok[:], -1.0, 1.0, ALU.mult, ALU.add)
            for u in range(NU):
                cu = slice(u * 128, (u + 1) * 128)
                S0f, S1f = Sf[u][0], Sf[u][1]
                S0b, S1b = Sb[u][0], Sb[u][1]
                if DBG.get('cg_lvl', 99) < 1:
                    continue
                pX = psb[4]
                S.mm(pX[:, 0:128], l_tok[:, cu], Uc[:])
                S.mm(pX[:, 128:256], SUl[:], l_tok[:, cu])
                S.copy(bTs[:], pX[:, 0:128])
                S.act(kdec[:], pX[:, 128:256], AF.Exp)
                S.tt(khat[:], k_tok[:, cu], kdec[:], ALU.mult, eng="pool")
                if DBG.get('cg_lvl', 99) < 2:
                    continue
                mid = bTs[:].rearrange("p (c s) -> p c s", c=2)[:, :, 32]
                S.copy(bm[:], mid)
                S.ts(nbm[:], mid, -1.0, None, ALU.mult)
                for c in range(2):
                    cs = slice(c * 64, (c + 1) * 64)
                    S.act(E1[:, cs], bTs[:, cs], AF.Exp, bias=nbm[:, c:c + 1])
                    S.act(E2[:, cs], bTs[:, cs], AF.Exp, bias=bm[:, c:c + 1], scale=-1.0)
                S.act(E3[:], bTs[:], AF.Exp)
                if DBG.get('cg_sub', 9) < 1:
                    continue
                S.stt(qt[:], qTs[u][:, tcol], qscale, E1[:], ALU.mult, ALU.mult)
                if DBG.get('cg_sub', 9) < 2:
                    continue
                S.tt(kt[:], kTs[u][:, tcol], E2[:], ALU.mult, eng="pool")
                if DBG.get('cg_sub', 9) < 3:
                    continue
                S.stt(qA[:], qTs[u][:, tcol], qscale, E3[:], ALU.mult, ALU.mult)
                if DBG.get('cg_lvl', 99) < 3:
                    continue
                pAtt = psb[5]
                for a in range(HPU):
                    ra = slice(a * DK, (a + 1) * DK)
                    S.mm(pAtt[:, a * 128:(a + 1) * 128], kt[ra, :], qt[ra, :])
                for a in range(HPU):
                    am = DBG.get("att_mode", "swap")
                    if am == "tt":
                        S.tt(attb[a][:], pAtt[:, a * 128:(a + 1) * 128], cst["triu64"][:], ALU.mult)
                    elif am == "swap":
                        S.tt(attb[a][:], cst["triu64"][:], pAtt[:, a * 128:(a + 1) * 128], ALU.mult)
                    elif am == "act":
                        S.copy(junk[:], pAtt[:, a * 128:(a + 1) * 128], eng="act")
                        S.tt(attb[a][:], junk[:], cst["triu64"][:], ALU.mult, eng="pool")
                if DBG.get('cg_lvl', 99) < 4:
                    continue
                pS = psb[6]
                for c in range(2):
                    rows = slice(c * 64, (c + 1) * 64)
                    for a in range(HPU):
                        h = u * HPU + a
                        ra = slice(a * DK, (a + 1) * DK)
                        S.mm(pS[ra, c * 128:(c + 1) * 128], khat[rows, a * DK:(a + 1) * DK], v_tok[rows, h * 128:(h + 1) * 128])
                S.stt(S1f[:], S0f[:], E3[:, 63:64], pS[:, 0:128], ALU.mult, ALU.add)
                S.copy(S1b[:], S1f[:], eng="act")
                if DBG.get('cg_lvl', 99) < 5:
                    continue
                pO = psb[7]
                for a in range(HPU):
                    h = u * HPU + a
                    ra = slice(a * DK, (a + 1) * DK)
                    oc = slice(a * 128, (a + 1) * 128)
                    S.mm(pO[0:64, oc], qA[ra, 0:64], S0b[ra, :], start=True, stop=False)
                    S.mm(pO[64:128, oc], qA[ra, 64:128], S1b[ra, :], start=True, stop=False)
                    S.mm(pO[:, oc], attb[a][:], v_tok[:, h * 128:(h + 1) * 128], start=False, stop=True)
                S.stt(S0f[:], S1f[:], E3[:, 127:128], pS[:, 128:256], ALU.mult, ALU.add)
                S.copy(S0b[:], S0f[:], eng="act")
                if DBG.get('cg_lvl', 99) < 6:
                    continue
                for a in range(HPU):
                    h = u * HPU + a
                    oc = slice(a * 128, (a + 1) * 128)
                    hc = slice(h * 128, (h + 1) * 128)
                    S.act(junk[:], pO[:, oc], AF.Square, accum_out=ss[:, a:a + 1])
                    rsqrt_eps(S, ss[:, a:a + 1], ss[:, a:a + 1], 1e-6, scale=1.0 / 128.0)
                    S.stt(br_tok[:, hc], pO[:, oc], ss[:, a:a + 1], ngb[:], ALU.mult, ALU.mult)
                    S.tt(br_tok[:, hc], br_tok[:, hc], sg_tok[:, hc], ALU.mult, eng="pool")
            pT = psb[0] if tt % 2 else psb[1]
            for kc in range(4):
                S.tr(pT[:, kc * 128:(kc + 1) * 128], br_tok[:, kc * 128:(kc + 1) * 128], cst["ident"][:])
            S.copy(brs[:, :, t * 128:(t + 1) * 128], pT[:].rearrange("p (c t) -> p c t", c=4), eng="act" if t % 2 else "dve")
    S.dma(brT_d[bidx].rearrange("c p t -> p c t"), brs[:], eng="pool")
    P.close()


def gla_branch(kb, layer, brT_d):
    cgla_branch(kb, layer, brT_d, "gla")


def hgrn_branch(kb, layer, brT_d):
    S = kb.S
    P = Pool_(kb)
    lb_d = kb.lb_d
    row = P.sb("hlbrow", [1, 512])
    if layer == 0:
        S.memset(row[:], 0.0)
    else:
        r0 = P.sb("hlb0", [1, 512])
        S.dma(r0[:], kb.d["hgrn_lb_logits"][0:1, :])
        S.dma(row[:], kb.d["hgrn_lb_logits"][1:2, :])
        S.tt(row[:], row[:], r0[:], ALU.subtract)
        S.act(row[:], row[:], AF.Sigmoid)
    S.dma(lb_d[layer:layer + 1, :], row[:])
    P.close()
    cgla_branch(kb, layer, brT_d, "hgrn", lb_d=lb_d[layer:layer + 1, :])


def rwkv_branch(kb, layer, brT_d):
    S = kb.S
    hT = kb.hT
    W = kb.d["w_in"][layer]
    psb = kb.psb
    cst = kb.cst
    o = RWKV_OFF
    P = Pool_(kb)
    CW = math.exp(-0.5)
    Wr = [P.sb("rWr%d" % k, [128, 8, 512], BF16) for k in range(2)]
    Wk = [P.sb("rWk%d" % k, [128, 8, 512], BF16) for k in range(2)]
    Wv = [P.sb("rWv%d" % k, [128, 8, 512], BF16) for k in range(2)]
    Wl = [P.sb("rWl%d" % k, [128, 8, 256], BF16) for k in range(2)]
    PP = Pool_(kb)
    stg = PP.sb("rstg", [128, 8, 512])
    muB = PP.sb("rmuB", [128, 1792])
    omuB = PP.sb("romuB", [128, 1792])
    S.dma(muB[:], kb.d["rwkv_mu"][layer:layer + 1, :].partition_broadcast(128))
    S.ts(omuB[:], muB[:], -1.0, 1.0, ALU.mult, ALU.add)
    for (dst, c0, n) in ((Wr, 0, 512), (Wk, 512, 512), (Wv, 1024, 512), (Wl, 1536, 256)):
        sv = stg[:, :, 0:n]
        S.dma(sv, W[:, o + c0:o + c0 + n].rearrange("(c p) n -> p c n", p=128))
        S.tt(dst[0][:], sv, omuB[:, c0:c0 + n].unsqueeze(1).to_broadcast([128, 8, n]), ALU.mult)
        S.tt(dst[1][:], sv, muB[:, c0:c0 + n].unsqueeze(1).to_broadcast([128, 8, n]), ALU.mult, eng="pool")
    PP.close()
    lw = P.sb("rlw", [128, 512])
    S.dma(lw[0:64, :], kb.d["rwkv_w2"][layer])
    S.dma(lw[64:128, :], kb.d["rwkv_a2"][layer])
    g2f = P.sb("rg2f", [128, 512])
    g2b = P.sb("rg2b", [128, 512], BF16)
    S.dma(g2f[:], kb.d["rwkv_g2"][layer])
    S.copy(g2b[:], g2f[:])
    w0r = P.sb("rw0r", [1, 512])
    S.dma(w0r[:], kb.d["rwkv_w0"][layer:layer + 1, :])
    a0T = P.sb("ra0T", [128, 4])
    kkT_ = P.sb("rkkT", [128, 4])
    kaT = P.sb("rkaT", [128, 4])
    okaT = P.sb("rokaT", [128, 4])
    rkT_ = P.sb("rrkT", [128, 4])
    load_T(kb, P, a0T[:], kb.d["rwkv_a0"][layer].rearrange("(c p) -> c p", p=128), 4)
    load_T(kb, P, kkT_[:], kb.d["rwkv_k_k"][layer].rearrange("(c p) -> c p", p=128), 4)
    load_T(kb, P, kaT[:], kb.d["rwkv_k_a"][layer].rearrange("(c p) -> c p", p=128), 4)
    load_T(kb, P, rkT_[:], kb.d["rwkv_r_k"][layer].rearrange("(c two) d -> c (two d)", two=2), 4)
    S.ts(okaT[:], kaT[:], -1.0, 1.0, ALU.mult, ALU.add)
    lngB = P.sb("rlngB", [128, 512])
    lnbB = P.sb("rlnbB", [128, 512])
    S.dma(lngB[:], kb.d["rwkv_ln_g"][layer:layer + 1, :].partition_broadcast(128))
    S.dma(lnbB[:], kb.d["rwkv_ln_b"][layer:layer + 1, :].partition_broadcast(128))
    Uc = P.sb("rUc", [128, 128])
    Ux = P.sb("rUx", [128, 128])
    SUl = P.sb("rSUl", [128, 128])
    nsup = P.sb("rnsup", [128, 128])
    nslo = P.sb("rnslo", [128, 128])
    ntriu = P.sb("rntriu", [128, 128])
    S.ts(Uc[:], cst["triu64"][:], -CW, None, ALU.mult)
    S.ts(Ux[:], cst["sup64"][:], -CW, None, ALU.mult)
    S.ts(SUl[:], cst["slo64"][:], -CW, None, ALU.mult)
    S.ts(nsup[:], cst["sup64"][:], -1.0, None, ALU.mult)
    S.ts(nslo[:], cst["slo64"][:], -1.0, None, ALU.mult)
    S.ts(ntriu[:], cst["triu64"][:], -1.0, None, ALU.mult)
    hsel = P.sb("rhsel", [128, 2])
    S.copy(hsel[:], cst["blk64"][:].rearrange("p (a s) -> p a s", a=2)[:, :, 0])
    ident = cst["ident"]

    def f512(name):
        return P.sb(name, [128, 512])

    def f128(name, dt=F32):
        return P.sb(name, [128, 128], dt)

    lo = f512("rlo")
    sgl = P.sb("rsgl", [128, 512], BF16)
    rT, kT, aT, kaT_, bT_, kpT, tmpF, prodT = (f512("r_" + n) for n in ("rT", "kT", "aT", "kapT", "bbT", "kpT", "tmpF", "prodT"))
    def d128(name):
        return [f128("%s_%d" % (name, k)) for k in range(2)]

    l_tok = f128("rltok")
    v_tokD, g_tokD = d128("rvtok"), d128("rgtok")
    sb2D = [P.sb("rsb2_%d" % k, [128, 2]) for k in range(2)]
    gCD = [P.sb("rgC_%d" % k, [128, 2]) for k in range(2)]
    bTs, bxTs = f128("rbTs"), f128("rbxTs")
    bm, nbm, bl = P.sb("rbm", [128, 2]), P.sb("rnbm", [128, 2]), P.sb("rbl", [128, 2])
    E = {n: f128("rE_" + n) for n in ("r", "kx", "inv", "abs", "absx", "last")}
    rt, kxt, kt, bt, KhT, BhT = (f128("r_" + n) for n in ("rt", "kxt", "kt", "bt", "KhT", "BhT"))
    rbarD, kbarD, KhatD, BhatD = d128("rrbar"), d128("rkbar"), d128("rKhat"), d128("rBhat")
    Mm = [[f128("rM%d_%d" % (a, k)) for k in range(2)] for a in range(2)]
    MT = [[f128("rMT%d_%d" % (a, k)) for k in range(2)] for a in range(2)]
    PmD = [[[f128("rP%d_%d_%d" % (b_, a, k)) for k in range(2)] for a in range(2)] for b_ in range(2)]
    AkkD = [[f128("rAkk%d_%d" % (b_, a)) for a in range(2)] for b_ in range(2)]
    ArkD = [[f128("rArk%d_%d" % (b_, a)) for a in range(2)] for b_ in range(2)]
    ArbD = [[f128("rArb%d_%d" % (b_, a)) for a in range(2)] for b_ in range(2)]
    Ws, Us, ytok = f128("rWs"), f128("rUs"), f128("rytok")
    Hs = [[P.sb("rHs%d_%d" % (u, k), [128, 64]) for k in range(2)] for u in range(4)]
    for u in range(4):
        S.memset(Hs[u][0][:], 0.0)
    st6 = P.sb("rst6", [128, 2, 6])
    mv2 = P.sb("rmv2", [128, 2, 2])
    rs2 = P.sb("rrs2", [128, 2])
    yn = f128("ryn")
    brt_ = f128("rbrt")
    brb = [P.sb("rbrb%d" % k, [128, 128], BF16) for k in range(2)]

    def proj_fm(ps, W2, c0, n, gcol):
        for kc in range(8):
            S.mm(ps[0:n, :], W2[0][:, kc, c0:c0 + n], hT[:, kc, slice(1 + gcol, 1 + gcol + 512)], start=(kc == 0), stop=False)
        for kc in range(8):
            S.mm(ps[0:n, :], W2[1][:, kc, c0:c0 + n], hT[:, kc, slice(gcol, gcol + 512)], start=False, stop=(kc == 7))

    iters = []
    for g in range(DBG.get("rw_ng", 8)):
        for u in range(4):
            for tt_ in range(4):
                iters.append((g, u, tt_))

    def setup(it):
        g, u, tt_ = iters[it]
        bf = it % 2
        gcol = g * 512
        uc = slice(u * 128, (u + 1) * 128)
        v_tok, g_tok, sb2, gC = v_tokD[bf], g_tokD[bf], sb2D[bf], gCD[bf]
        rbar, kbar, Khat, Bhat = rbarD[bf], kbarD[bf], KhatD[bf], BhatD[bf]
        Pm, Akk, Ark, Arb = PmD[bf], AkkD[bf], ArkD[bf], ArbD[bf]
        if u == 0 and tt_ == 0:
            proj_fm(psb[0], Wl, 0, 128, gcol)
            S.act(lo[0:64, :], psb[0][0:64, :], AF.Tanh)
            S.copy(lo[64:128, :], psb[0][64:128, :])
            proj_fm(psb[1], Wl, 128, 128, gcol)
            S.act(sgl[:], psb[1][:], AF.Sigmoid)
            yield
        if tt_ == 0:
            proj_fm(psb[0], Wr, u * 128, 128, gcol)
            S.copy(rT[:], psb[0][:], eng="act")
            proj_fm(psb[1], Wk, u * 128, 128, gcol)
            S.copy(kT[:], psb[1][:])
            yield
            S.mm(psb[0][:], lw[64:128, uc], lo[64:128, :])
            S.act(aT[:], psb[0][:], AF.Sigmoid, bias=a0T[:, u:u + 1])
            S.ts(kaT_[:], kT[:], kkT_[:, u:u + 1], None, ALU.mult)
            S.tt(tmpF[:], kaT_[:], kaT_[:], ALU.mult)
            S.mm(psb[1][:], cst["blk64"][:], tmpF[:])
            yield
            S.act(tmpF[:], psb[1][:], AF.Ln, bias=1e-24)
            S.act(tmpF[:], tmpF[:], AF.Exp, scale=-0.5)
            S.tt(kaT_[:], kaT_[:], tmpF[:], ALU.mult)
            S.tt(bT_[:], kaT_[:], aT[:], ALU.mult)
            yield
            S.ts(tmpF[:], aT[:], kaT[:, u:u + 1], okaT[:, u:u + 1], ALU.mult, ALU.add)
            S.tt(kpT[:], kT[:], tmpF[:], ALU.mult)
            S.stt(prodT[:], rT[:], rkT_[:, u:u + 1], kpT[:], ALU.mult, ALU.mult)
            yield
        t = g * 4 + tt_
        t0 = t * 128
        tc_ = slice(tt_ * 128, (tt_ + 1) * 128)
        pt_ = psb[2]
        for kc in range(8):
            S.mm(pt_[:, 0:128], hT[:, kc, slice(1 + t0, 1 + t0 + 128)], Wv[0][:, kc, uc], start=(kc == 0), stop=False)
        for kc in range(8):
            S.mm(pt_[:, 0:128], hT[:, kc, slice(t0, t0 + 128)], Wv[1][:, kc, uc], start=False, stop=(kc == 7))
        S.mm(pt_[:, 128:256], lo[0:64, tc_], lw[0:64, uc], start=True, stop=False)
        S.mm(pt_[:, 128:256], cst["ones"][0:1, :], w0r[0:1, uc], start=False, stop=True)
        S.mm(pt_[:, 256:384], sgl[:, tc_], g2b[:, uc])
        S.mm(pt_[:, 384:386], prodT[:, tc_], hsel[:])
        yield
        S.act(l_tok[:], pt_[:, 128:256], AF.Sigmoid)
        S.copy(v_tok[:], pt_[:, 0:128])
        S.copy(g_tok[:], pt_[:, 256:384], eng="act")
        S.copy(sb2[:], pt_[:, 384:386])
        pX = psb[3]
        S.mm(pX[:, 0:128], l_tok[:], Uc[:])
        S.mm(pX[:, 128:256], l_tok[:], Ux[:])
        yield
        S.copy(bTs[:], pX[:, 0:128])
        S.copy(bxTs[:], pX[:, 128:256], eng="act")
        b3 = bTs[:].rearrange("p (c s) -> p c s", c=2)
        S.copy(bm[:], b3[:, :, 32])
        S.ts(nbm[:], b3[:, :, 32], -1.0, None, ALU.mult)
        S.copy(bl[:], b3[:, :, 63])
        yield
        for c in range(2):
            cs = slice(c * 64, (c + 1) * 64)
            S.act(E["r"][:, cs], bTs[:, cs], AF.Exp, bias=nbm[:, c:c + 1])
            S.act(E["kx"][:, cs], bxTs[:, cs], AF.Exp, bias=nbm[:, c:c + 1])
            S.act(E["inv"][:, cs], bTs[:, cs], AF.Exp, bias=bm[:, c:c + 1], scale=-1.0)
            S.act(E["last"][:, cs], bTs[:, cs], AF.Exp, bias=bl[:, c:c + 1], scale=-1.0)
        S.act(E["abs"][:], bTs[:], AF.Exp)
        S.act(E["absx"][:], bxTs[:], AF.Exp)
        yield
        S.tt(rt[:], rT[:, tc_], E["r"][:], ALU.mult)
        S.tt(kxt[:], kaT_[:, tc_], E["kx"][:], ALU.mult)
        S.tt(kt[:], kpT[:, tc_], E["inv"][:], ALU.mult)
        S.tt(bt[:], bT_[:, tc_], E["inv"][:], ALU.mult)
        yield
        S.tt(KhT[:], kpT[:, tc_], E["last"][:], ALU.mult)
        S.stt(BhT[:], bT_[:, tc_], -1.0, E["last"][:], ALU.mult, ALU.mult)
        S.tt(rbar[:], rT[:, tc_], E["abs"][:], ALU.mult)
        S.tt(kbar[:], kaT_[:, tc_], E["absx"][:], ALU.mult)
        S.copy(gC[:], E["abs"][:].rearrange("p (c s) -> p c s", c=2)[:, :, 63])
        for a in range(2):
            ra = slice(a * 64, (a + 1) * 64)
            pA = psb[4 + a]
            S.mm(pA[:, 0:128], bt[ra, :], kxt[ra, :])
            S.mm(pA[:, 128:256], kxt[ra, :], bt[ra, :])
            S.mm(pA[:, 256:384], kt[ra, :], kxt[ra, :])
            S.mm(pA[:, 384:512], kt[ra, :], rt[ra, :])
            S.mm(psb[6][:, a * 128:(a + 1) * 128], bt[ra, :], rt[ra, :])
        S.tr(pX[:, 256:384], KhT[:], ident[:])
        S.tr(pX[:, 384:512], BhT[:], ident[:])
        yield
        for a in range(2):
            pA = psb[4 + a]
            S.tt(Mm[a][0][:], nsup[:], pA[:, 0:128], ALU.mult)
            S.tt(MT[a][0][:], nslo[:], pA[:, 128:256], ALU.mult)
            S.tt(Pm[a][0][:], Mm[a][0][:], ident[:], ALU.add)
        yield
        for a in range(2):
            pA = psb[4 + a]
            S.tt(Akk[a][:], cst["sup64"][:], pA[:, 256:384], ALU.mult)
            S.tt(Ark[a][:], cst["triu64"][:], pA[:, 384:512], ALU.mult)
            S.tt(Arb[a][:], ntriu[:], psb[6][:, a * 128:(a + 1) * 128], ALU.mult)
        S.copy(Khat[:], pX[:, 256:384], eng="act")
        S.copy(Bhat[:], pX[:, 384:512], eng="act")
        cur = 0
        for lvl in range(1, 6):
            nxt = 1 - cur
            for a in range(2):
                pA = psb[4 + a]
                if lvl < 5:
                    S.mm(pA[:, 0:128], MT[a][cur][:], Mm[a][cur][:])
                S.mm(pA[:, 128:256], Mm[a][cur][:], MT[a][cur][:])
            yield
            for a in range(2):
                pA = psb[4 + a]
                if lvl < 5:
                    S.copy(Mm[a][nxt][:], pA[:, 0:128])
                S.copy(MT[a][nxt][:], pA[:, 128:256], eng="act")
            yield
            for a in range(2):
                pA = psb[4 + a]
                S.mm(pA[:, 256:384], MT[a][nxt][:], Pm[a][cur][:])
            yield
            for a in range(2):
                pA = psb[4 + a]
                S.tt(Pm[a][nxt][:], Pm[a][cur][:], pA[:, 256:384], ALU.add)
            yield
            cur = nxt
        assert cur == 1

    nbr = [0]

    def chain(it):
        g, u, tt_ = iters[it]
        bf = it % 2
        t0 = (g * 4 + tt_) * 128
        v_tok, g_tok, sb2, gC = v_tokD[bf], g_tokD[bf], sb2D[bf], gCD[bf]
        rbar, kbar, Khat, Bhat = rbarD[bf], kbarD[bf], KhatD[bf], BhatD[bf]
        Akk, Ark, Arb = AkkD[bf], ArkD[bf], ArbD[bf]
        Pf = [PmD[bf][a][1] for a in range(2)]
        H0, H1 = Hs[u][0], Hs[u][1]
        pC = psb[7]
        Hc = [H0, H1, H0]
        for c in range(2):
            rc = slice(c * 64, (c + 1) * 64)
            Hin, Hout = Hc[c], Hc[c + 1]
            for a in range(2):
                ra = slice(a * 64, (a + 1) * 64)
                S.mm(pC[rc, a * 64:(a + 1) * 64], kbar[ra, rc], Hin[ra, :], start=True, stop=False)
                S.mm(pC[rc, a * 64:(a + 1) * 64], Akk[a][rc, rc], v_tok[rc, ra], start=False, stop=True)
            yield
            S.copy(Ws[rc, :], pC[rc, 0:128])
            yield
            for a in range(2):
                ra = slice(a * 64, (a + 1) * 64)
                S.mm(pC[rc, 128 + a * 64:128 + (a + 1) * 64], Pf[a][rc, rc], Ws[rc, ra])
            yield
            S.copy(Us[rc, :], pC[rc, 128:256])
            yield
            for a in range(2):
                ra = slice(a * 64, (a + 1) * 64)
                S.mm(pC[ra, 384:448], Khat[rc, ra], v_tok[rc, ra], start=True, stop=False)
                S.mm(pC[ra, 384:448], Bhat[rc, ra], Us[rc, ra], start=False, stop=True)
            for a in range(2):
                ra = slice(a * 64, (a + 1) * 64)
                oy = slice(256 + a * 64, 256 + (a + 1) * 64)
                S.mm(pC[rc, oy], rbar[ra, rc], Hin[ra, :], start=True, stop=False)
                S.mm(pC[rc, oy], Ark[a][rc, rc], v_tok[rc, ra], start=False, stop=False)
                S.mm(pC[rc, oy], Arb[a][rc, rc], Us[rc, ra], start=False, stop=True)
            yield
            S.stt(Hout[:], Hin[:], gC[:, c:c + 1], pC[:, 384:448], ALU.mult, ALU.add)
            S.copy(ytok[rc, :], pC[rc, 256:384], eng="act")
            yield
        for a in range(2):
            S.bn_stats(st6[:, a, :], ytok[:, a * 64:(a + 1) * 64])
            S.bn_aggr(mv2[:, a, :], st6[:, a, :])
        rsqrt_eps(S, rs2[:], mv2[:, :, 1], 64e-5)
        yield
        for a in range(2):
            ra = slice(a * 64, (a + 1) * 64)
            gc = slice(u * 128 + a * 64, u * 128 + (a + 1) * 64)
            S.ts(yn[:, ra], ytok[:, ra], mv2[:, a, 0:1], rs2[:, a:a + 1], ALU.subtract, ALU.mult)
            S.tt(yn[:, ra], yn[:, ra], lngB[:, gc], ALU.mult)
            S.tt(yn[:, ra], yn[:, ra], lnbB[:, gc], ALU.add)
            S.stt(yn[:, ra], v_tok[:, ra], sb2[:, a:a + 1], yn[:, ra], ALU.mult, ALU.add)
        S.tt(brt_[:], yn[:], g_tok[:], ALU.mult)
        S.tr(pC[:, 0:128], brt_[:], ident[:])
        yield
        bb_ = brb[nbr[0] % 2]
        nbr[0] += 1
        S.copy(bb_[:], pC[:, 0:128], eng="act")
        S.dma(brT_d[1, u, :, t0:t0 + 128], bb_[:], eng="sp")

    def run_interleaved(gens):
        active = [x for x in gens if x is not None]
        while active:
            for gi in list(active):
                try:
                    next(gi)
                except StopIteration:
                    active.remove(gi)

    n_it = len(iters)
    pipelined = DBG.get("rw_pipe", True)
    if pipelined:
        for k in range(n_it + 1):
            run_interleaved([setup(k) if k < n_it else None, chain(k - 1) if k >= 1 else None])
    else:
        for k in range(n_it):
            run_interleaved([setup(k)])
            run_interleaved([chain(k)])
    P.close()


_CACHE = {}


def kernel(**inputs):
    if "kb" not in _CACHE:
        _CACHE["kb"] = build()
    kb = _CACHE["kb"]
    consts = make_consts()
    shared = {}
    for n, shp in PARAMS:
        if n == "c":
            continue
        shared[n] = np.ascontiguousarray(np.asarray(inputs[n], dtype=np.float32))
    for n, v in consts.items():
        shared["k_" + n] = v
    x = np.asarray(inputs["x"], dtype=np.float32)
    c = np.asarray(inputs["c"], dtype=np.float32)
    in_maps = []
    for b in range(8):
        m = dict(shared)
        m["x"] = np.ascontiguousarray(x[b])
        m["c"] = np.ascontiguousarray(c[b:b + 1])
        in_maps.append(m)
    res = run_bass_kernel_spmd(kb.nc, in_maps, core_ids=list(range(8)))
    return np.stack([np.asarray(r["out"], dtype=np.float32) for r in res.results], axis=0)
```

```python
import math
from contextlib import ExitStack

import numpy as np
import concourse.bass as bass
import concourse.mybir as mybir
from concourse.bass_utils import run_bass_kernel_spmd

F32 = mybir.dt.float32
BF16 = mybir.dt.bfloat16
AF = mybir.ActivationFunctionType
ALU = mybir.AluOpType
AX = mybir.AxisListType

ENGS = ("pe", "act", "dve", "pool", "sp")


class _Op:
    __slots__ = ("eng", "fn", "dma", "waits", "key", "val", "know", "inc")


def _region(ap):
    shape = list(ap.tensor.shape)
    off = int(ap.offset)
    if str(ap.space) == "DRAM":
        ext = 0
        for step, cnt in ap.ap:
            ext += (int(cnt) - 1) * abs(int(step))
        return (ap.name, 0, 1, off, off + ext + 1)
    if str(ap.space) == "PSUM":
        return (ap.name, 0, 128, 0, 1 << 30)
    ps = 1
    for s in shape[1:]:
        ps *= int(s)
    p0 = off // ps
    f0 = off % ps
    npart = 1
    ext = 0
    for step, cnt in ap.ap:
        step = int(step)
        cnt = int(cnt)
        if step == ps:
            npart = max(npart, cnt)
        elif step > ps:
            npart = max(npart, (cnt - 1) * (step // ps) + 1)
        else:
            ext += (cnt - 1) * step
    return (ap.name, p0, p0 + npart, f0, f0 + ext + 1)


class Sched:
    def __init__(self, nc, n_dma_sems=12):
        self.nc = nc
        self.streams = {e: [] for e in ENGS}
        self.clock = {e: {} for e in ENGS}
        self.cpos = {e: 0 for e in ENGS}
        self.last_op = {e: None for e in ENGS}
        self.n_dma_sems = n_dma_sems
        self.dma_uses = {}
        self.dma_next = {e: 0 for e in ENGS}
        self.dma_last = {}
        self.acc = {}
        self.readonly = set()
        self.pe_bank = {}
        self.epoch = 0
        self.n_ops = 0

    def _add(self, eng, fn, reads, writes, dma, force=()):
        op = _Op()
        op.eng, op.fn, op.dma = eng, fn, dma
        clk = self.clock[eng]
        need = {}

        def dep(d, forced=False):
            if (not forced) and (not dma) and (not d.dma) and d.eng == "pe" and eng == "pe":
                return
            if clk.get(d.key, 0) >= d.val:
                return
            if need.get(d.key, 0) < d.val:
                need[d.key] = d.val
            for k, v in d.know.items():
                if clk.get(k, 0) < v:
                    clk[k] = v

        for d_ in force:
            dep(d_, True)
        regs = []
        for ap in reads:
            if ap.name in self.readonly:
                continue
            regs.append((_region(ap), False))
        for ap in writes:
            regs.append((_region(ap), True))
        for (name, p0, p1, f0, f1), isw in regs:
            lst = self.acc.get(name)
            if lst is None:
                continue
            for r in lst:
                if r[4] is op:
                    continue
                if r[0] < p1 and p0 < r[1] and r[2] < f1 and f0 < r[3]:
                    if isw or r[5] or (f1 == (1 << 30) and r[4].eng != eng):
                        dep(r[4])
        if dma:
            slot = self.dma_next[eng]
            self.dma_next[eng] = (slot + 1) % self.n_dma_sems
            k = ("s", eng, slot)
            prev = self.dma_last.get(k)
            if prev is not None:
                dep(prev)
            cnt = self.dma_uses.get(k, 0) + 1
            self.dma_uses[k] = cnt
            op.key, op.val, op.inc = k, 16 * cnt, 16
            self.dma_last[k] = op
        else:
            self.cpos[eng] += 1
            op.key, op.val, op.inc = ("e", eng, self.epoch), self.cpos[eng], 1
        for k, v in need.items():
            if clk.get(k, 0) < v:
                clk[k] = v
        op.waits = list(need.items())
        op.know = dict(clk)
        self.streams[eng].append(op)
        self.last_op[eng] = op
        for (name, p0, p1, f0, f1), isw in regs:
            lst = self.acc.setdefault(name, [])
            if isw:
                lst[:] = [r for r in lst if not (p0 <= r[0] and r[1] <= p1 and f0 <= r[2] and r[3] <= f1)]
            else:
                if not dma:
                    lst[:] = [r for r in lst if not ((not r[5]) and (not r[4].dma) and r[4].eng == eng
                                                     and r[0] == p0 and r[1] == p1 and r[2] == f0 and r[3] == f1)]
            lst.append([p0, p1, f0, f1, op, isw])
        self.n_ops += 1
        return op

    def barrier(self):
        targets = []
        for e in ENGS:
            if self.cpos[e] > 0:
                targets.append((("e", e, self.epoch), self.cpos[e], self.last_op[e]))
        for k, o in self.dma_last.items():
            targets.append((k, o.val, o))
        for e in ENGS:
            clk = self.clock[e]
            op = _Op()
            op.eng, op.fn, op.dma = e, None, False
            need = {}
            for k, v, o in targets:
                if clk.get(k, 0) < v:
                    need[k] = v
                    clk[k] = v
            op.waits = list(need.items())
            op.key = None
            op.know = dict(clk)
            self.streams[e].append(op)
        self.acc = {}
        self.pe_bank = {}
        if max(self.cpos.values()) > 16000:
            self.epoch += 1
            self.cpos = {e: 0 for e in ENGS}

    def _pe_bank(self, out, lhsT):
        kpos = (int(lhsT.base_partition()) if hasattr(lhsT, "base_partition") else 0, int(lhsT.partition_size()))
        prev = self.pe_bank.get(out.name)
        force = ()
        if prev is not None and prev[0] != kpos:
            force = (prev[1],)
        return kpos, force

    def mm(self, out, lhsT, rhs, start=True, stop=True):
        rd = [lhsT, rhs] + ([] if start else [out])
        kpos, force = self._pe_bank(out, lhsT)
        op = self._add("pe", lambda e: e.matmul(out, lhsT, rhs, start=start, stop=stop), rd, [out], False, force=force)
        self.pe_bank[out.name] = (kpos, op)
        return op

    def tr(self, out, in_, ident):
        kpos, force = self._pe_bank(out, in_)
        op = self._add("pe", lambda e: e.transpose(out, in_, ident), [in_, ident], [out], False, force=force)
        self.pe_bank[out.name] = (kpos, op)
        return op

    def act(self, out, in_, func, bias=None, scale=None, accum_out=None):
        rd = [in_]
        kw = {}
        if bias is not None:
            kw["bias"] = bias
            if not isinstance(bias, (int, float)):
                rd.append(bias)
        if scale is not None:
            kw["scale"] = scale
            if not isinstance(scale, (int, float)):
                rd.append(scale)
        wr = [out]
        if accum_out is not None:
            kw["accum_out"] = accum_out
            wr.append(accum_out)
        return self._add("act", lambda e: e.activation(out, in_, func, **kw), rd, wr, False)

    def tt(self, out, in0, in1, op, eng="dve"):
        return self._add(eng, lambda e: e.tensor_tensor(out, in0, in1, op), [in0, in1], [out], False)

    def ts(self, out, in0, s1, s2, op0, op1=None, eng="dve", accum_out=None):
        rd = [in0]
        if not isinstance(s1, (int, float)):
            rd.append(s1)
        if s2 is not None and not isinstance(s2, (int, float)):
            rd.append(s2)
        wr = [out]
        kw = {}
        if accum_out is not None:
            kw["accum_out"] = accum_out
            wr.append(accum_out)
        if op1 is None:
            return self._add(eng, lambda e: e.tensor_scalar(out, in0, s1, None, op0, **kw), rd, wr, False)
        return self._add(eng, lambda e: e.tensor_scalar(out, in0, s1, s2, op0, op1, **kw), rd, wr, False)

    def stt(self, out, in0, scalar, in1, op0, op1):
        rd = [in0, in1]
        if not isinstance(scalar, (int, float)):
            rd.append(scalar)
        return self._add("dve", lambda e: e.scalar_tensor_tensor(out, in0, scalar, in1, op0, op1), rd, [out], False)

    def copy(self, out, in_, eng="dve"):
        if eng == "act":
            return self._add("act", lambda e: e.copy(out, in_), [in_], [out], False)
        return self._add(eng, lambda e: e.tensor_copy(out, in_), [in_], [out], False)

    def memset(self, out, val, eng="dve"):
        return self._add(eng, lambda e: e.memset(out, val), [], [out], False)

    def reduce(self, out, in_, op, axis=AX.X, eng="dve"):
        return self._add(eng, lambda e: e.tensor_reduce(out, in_, axis, op), [in_], [out], False)

    def bn_stats(self, out, in_):
        return self._add("dve", lambda e: e.bn_stats(out, in_), [in_], [out], False)

    def bn_aggr(self, out, in_):
        return self._add("dve", lambda e: e.bn_aggr(out, in_), [in_], [out], False)

    def recip(self, out, in_):
        return self._add("dve", lambda e: e.reciprocal(out, in_), [in_], [out], False)

    def dma(self, out, in_, eng="sp", **kw):
        return self._add(eng, lambda e: e.dma_start(out=out, in_=in_, **kw), [in_], [out], True)

    def emit(self):
        nc = self.nc
        with ExitStack() as es:
            sems = {}
            for e in ENGS:
                for op in self.streams[e]:
                    if op.fn is not None and (not op.dma) and op.key not in sems:
                        sems[op.key] = es.enter_context(nc.semaphore("c_%s_%d" % (op.key[1], op.key[2])))
            for k in self.dma_uses:
                sems[k] = es.enter_context(nc.semaphore("d_%s_%d" % (k[1], k[2])))
            final = [(k, o.val) for k, o in self.dma_last.items()]
            for e in ENGS:
                if self.cpos[e] > 0:
                    final.append((("e", e, self.epoch), self.cpos[e]))
            block = es.enter_context(nc.Block())
            streams = self.streams

            def run(engname, e):
                for op in streams[engname]:
                    for k, v in op.waits:
                        e.wait_ge(sems[k], v)
                    if op.fn is not None:
                        op.fn(e).then_inc(sems[op.key], op.inc)
                if engname == "sp":
                    for k, v in final:
                        e.wait_ge(sems[k], v)

            @block.tensor
            def _(e):
                run("pe", e)

            @block.scalar
            def _(e):
                run("act", e)

            @block.vector
            def _(e):
                run("dve", e)

            @block.gpsimd
            def _(e):
                run("pool", e)

            @block.sync
            def _(e):
                run("sp", e)


DBG = {}
T = 4096
D = 1024
NT = 32
DEPTH = 2
NCOLS = 6936
GLA_OFF, RWKV_OFF, FOX_OFF, HGRN_OFF = 0, 1552, 3344, 4888
DN_ALPHA = (2.0 * DEPTH) ** 0.25
NE = 16


def make_consts():
    c = {}
    i = np.arange(128)
    same = (i[:, None] // 64) == (i[None, :] // 64)
    c["ident"] = np.eye(128, dtype=np.float32)
    c["ones"] = np.ones((128, 128), np.float32)
    c["triu"] = (i[:, None] <= i[None, :]).astype(np.float32)
    c["triu64"] = ((i[:, None] <= i[None, :]) & same).astype(np.float32)
    c["sup64"] = ((i[:, None] < i[None, :]) & same).astype(np.float32)
    c["slo64"] = ((i[:, None] > i[None, :]) & same).astype(np.float32)
    c["blk64"] = same.astype(np.float32)
    return c


CONST_NAMES = ["ident", "ones", "triu", "triu64", "sup64", "slo64", "blk64"]

PARAMS = [
    ("c", [1, D]), ("ada_w", [2, D, 6 * D]), ("ada_b", [2, 6, D]), ("w_in", [2, D, NCOLS]),
    ("gla_alpha_up", [2, 16, 256]), ("gla_alpha_b", [2, 256]), ("gla_norm_g", [2, 128]),
    ("rwkv_mu", [2, 1792]), ("rwkv_w0", [2, 512]), ("rwkv_w2", [2, 64, 512]), ("rwkv_a0", [2, 512]),
    ("rwkv_a2", [2, 64, 512]), ("rwkv_g2", [2, 128, 512]), ("rwkv_k_k", [2, 512]), ("rwkv_k_a", [2, 512]),
    ("rwkv_r_k", [2, 8, 64]), ("rwkv_ln_g", [2, 512]), ("rwkv_ln_b", [2, 512]), ("fox_f_bias", [2, 8]),
    ("hgrn_lb_logits", [2, 512]), ("hgrn_norm_g", [2, 128]), ("w_br", [2, 4, 512, D]),
    ("w_gate", [2, 4, D, D]), ("b_gate", [2, 4, D]), ("w_o", [2, D, D]), ("ln1_g", [2, D]), ("ln1_b", [2, D]),
    ("router_w", [D, NE]), ("router_b", [NE]), ("exp_w_gate", [2, NE, D, 512]), ("exp_w_up", [2, NE, D, 512]),
    ("exp_w_down", [2, NE, 512, D]), ("ln2_g", [2, D]), ("ln2_b", [2, D]),
]


class KB:
    def __init__(self, io=None):
        self.nc = bass.Bass("TRN2", target_bir_lowering=False)
        self.S = Sched(self.nc)
        self.io = io or {}
        self.d = {}
        nc = self.nc
        self.x = self.ext_in("x", [T, D], F32)
        for n, shp in PARAMS:
            self.d[n] = self.ext_in(n, shp, F32)
        self.cst_d = {n: self.ext_in("k_" + n, [128, 128], F32) for n in CONST_NAMES}
        self.psb = [nc.alloc_psum_tensor("psb%d" % i, [128, 512], F32) for i in range(8)]
        self.cst = {n: nc.alloc_sbuf_tensor("c_" + n, [128, 128], F32) for n in CONST_NAMES}
        for n in CONST_NAMES:
            self.S.dma(self.cst[n][:], self.cst_d[n])
        self.hT = None

    def ext_in(self, name, shape, dt):
        self.S.readonly.add(name)
        return self.nc.dram_tensor(name, list(shape), dt, kind="ExternalInput").ap()

    def dram(self, name, shape, dt):
        role = self.io.get(name)
        if role == "in":
            return self.nc.dram_tensor(name, list(shape), dt, kind="ExternalInput").ap()
        if role == "out" or name == "out":
            return self.nc.dram_tensor(name, list(shape), dt, kind="ExternalOutput").ap()
        return self.nc.dram_tensor(name, list(shape), dt).ap()


class Pool_:
    def __init__(self, kb):
        self.kb = kb
        self.es = ExitStack()

    _uid = [0]

    def sb(self, name, shape, dt=F32):
        Pool_._uid[0] += 1
        return self.es.enter_context(self.kb.nc.sbuf_tensor("%s_u%d" % (name, Pool_._uid[0]), list(shape), dt))

    def close(self):
        self.kb.S.barrier()
        self.es.close()


def phase_mod(kb, mod_d):
    S = kb.S
    P = Pool_(kb)
    condT = P.sb("condT", [128, 8])
    load_T(kb, P, condT[:], kb.d["c"].rearrange("o (c p) -> (o c) p", p=128), 8)
    S.act(condT[:], condT[:], AF.Silu)
    wst = [P.sb("adaw%d" % k, [128, 3072]) for k in range(2)]
    mrow = P.sb("mrow", [1, 6144])
    brow = P.sb("brow", [1, 6144])
    n = 0
    for i in range(2):
        S.dma(brow[:], kb.d["ada_b"][i:i + 1].rearrange("o j d -> o (j d)"))
        for half in range(2):
            for kc in range(8):
                w = wst[n % 2]
                n += 1
                S.dma(w[:], kb.d["ada_w"][i, kc * 128:(kc + 1) * 128, half * 3072:(half + 1) * 3072],
                      eng="sp" if n % 2 else "pool")
                for b in range(6):
                    S.mm(kb.psb[b][0:1, :], condT[:, kc:kc + 1], w[:, b * 512:(b + 1) * 512],
                         start=(kc == 0), stop=(kc == 7))
            for b in range(6):
                o = half * 3072 + b * 512
                S.tt(mrow[0:1, o:o + 512], kb.psb[b][0:1, :], brow[0:1, o:o + 512], ALU.add)
        for j in (1, 4):
            S.ts(mrow[0:1, j * 1024:(j + 1) * 1024], mrow[0:1, j * 1024:(j + 1) * 1024], 1.0, None, ALU.add)
        S.dma(mod_d[i:i + 1, :], mrow[:])
    P.close()


def rsqrt_eps(S, out, in_, eps, scale=1.0):
    S.act(out, in_, AF.Ln, bias=float(eps), scale=float(scale))
    S.act(out, out, AF.Exp, scale=-0.5)


def ln_stats(S, xin, st, mv, rstd, eps=1e-5):
    S.bn_stats(st[:, 0:6], xin[:, 0:512])
    S.bn_stats(st[:, 6:12], xin[:, 512:1024])
    S.bn_aggr(mv[:], st[:])
    rsqrt_eps(S, rstd[:], mv[:, 1:2], eps)


def load_T(kb, P, dst, src_rows, n, psum=None):
    S = kb.S
    tmp = P.sb("ldT_tmp", [n, 128])
    S.dma(tmp[:], src_rows)
    ps = kb.psb[7] if psum is None else psum
    S.mm(ps[:, 0:n], tmp[:], kb.cst["ident"][0:n, 0:n], start=True, stop=True)
    S.copy(dst, ps[:, 0:n])


def load_modT(kb, P, mod_d, layer, name):
    modT = P.sb(name, [128, 6, 8])
    load_T(kb, P, modT[:].rearrange("p j c -> p (j c)"), mod_d[layer].rearrange("(r p) -> r p", p=128), 48)
    return modT


def phase_ln_mixer(kb, x_src, mod_d, layer):
    S = kb.S
    P = Pool_(kb)
    modT = load_modT(kb, P, mod_d, layer, "modT_a")
    xb = [P.sb("lnx%d" % k, [128, 1024]) for k in range(2)]
    st = [P.sb("lnst%d" % k, [128, 12]) for k in range(2)]
    mv = [P.sb("lnmv%d" % k, [128, 2]) for k in range(2)]
    rs = [P.sb("lnrs%d" % k, [128, 1]) for k in range(2)]
    ident = kb.cst["ident"]
    for t in range(DBG.get("ln_nt", NT)):
        k = t % 2
        xin = xb[k]
        S.dma(xin[:], x_src[t * 128:(t + 1) * 128, :])
        if DBG.get("ln_lvl", 9) < 1:
            continue
        ln_stats(S, xin, st[k], mv[k], rs[k])
        S.ts(xin[:], xin[:], mv[k][:, 0:1], rs[k][:, 0:1], ALU.subtract, ALU.mult)
        if DBG.get("ln_lvl", 9) < 2:
            continue
        for c in range(8):
            pb = kb.psb[(t % 2) * 2 + c // 4]
            S.tr(pb[:, (c % 4) * 128:(c % 4 + 1) * 128], xin[:, c * 128:(c + 1) * 128], ident[:])
        if DBG.get("ln_lvl", 9) < 3:
            continue
        for c in range(DBG.get("ln_nc", 8)):
            pb = kb.psb[(t % 2) * 2 + c // 4]
            src = pb[:, (c % 4) * 128:(c % 4 + 1) * 128]
            off = DBG.get("ln_off", 1)
            dst = kb.hT[:, c, off + t * 128:off + (t + 1) * 128]
            ev = DBG.get("ln_evac", "both")
            if (c % 2 == 0 and ev == "both") or ev == "dve":
                S.ts(dst, src, modT[:, 1, c:c + 1], modT[:, 0, c:c + 1], ALU.mult, ALU.add)
            else:
                S.act(dst, src, AF.Identity, bias=modT[:, 0, c:c + 1], scale=modT[:, 1, c:c + 1])
    P.close()


def prep_cast(kb, P, jobs, stg, stb):
    S = kb.S
    n = 0
    for dst, src in jobs:
        R, N = src.shape
        for r in range(0, R, 128):
            a, b = stg[n % 2], stb[n % 2]
            n += 1
            S.dma(a[:, 0:N], src[r:r + 128, :], eng="sp")
            S.copy(b[:, 0:N], a[:, 0:N], eng="pool" if n % 2 else "act")
            S.dma(dst[r:r + 128, :], b[:, 0:N], eng="pool")


class Epi:
    def __init__(self, kb, P, mod_d, layer, gt_idx, g_name, b_name, tag, with_z=True):
        S = kb.S
        self.kb = kb
        self.gt = P.sb("epi_gt" + tag, [128, 1024])
        self.g = P.sb("epi_g" + tag, [128, 1024])
        self.b = P.sb("epi_b" + tag, [128, 1024])
        S.dma(self.gt[:], mod_d[layer:layer + 1, gt_idx * 1024:(gt_idx + 1) * 1024].partition_broadcast(128))
        S.dma(self.g[:], kb.d[g_name][layer:layer + 1, :].partition_broadcast(128))
        S.dma(self.b[:], kb.d[b_name][layer:layer + 1, :].partition_broadcast(128))
        self.xb = [P.sb("epi_x%s%d" % (tag, k), [128, 1024]) for k in range(2)]
        self.zb = [P.sb("epi_z%s%d" % (tag, k), [128, 1024]) for k in range(2)] if with_z else None
        self.st = [P.sb("epi_st%s%d" % (tag, k), [128, 12]) for k in range(2)]
        self.mv = [P.sb("epi_mv%s%d" % (tag, k), [128, 2]) for k in range(2)]
        self.rs = [P.sb("epi_rs%s%d" % (tag, k), [128, 1]) for k in range(2)]
        self.n = 0

    def prefetch_x(self, x_src, t):
        k = self.n % 2
        self.kb.S.dma(self.xb[k][:], x_src[t * 128:(t + 1) * 128, :])

    def run(self, y_halves, x_dst, t, x_src=None, z=None):
        S = self.kb.S
        k = self.n % 2
        self.n += 1
        if x_src is not None:
            S.dma(self.xb[k][:], x_src[t * 128:(t + 1) * 128, :])
        x = self.xb[k]
        if z is None:
            z = self.zb[k]
        for h in range(2):
            S.tt(z[:, h * 512:(h + 1) * 512], y_halves[h], self.gt[:, h * 512:(h + 1) * 512], ALU.mult)
        S.stt(z[:], x[:], DN_ALPHA, z[:], ALU.mult, ALU.add)
        ln_stats(S, z, self.st[k], self.mv[k], self.rs[k])
        S.ts(z[:], z[:], self.mv[k][:, 0:1], self.rs[k][:, 0:1], ALU.subtract, ALU.mult)
        S.tt(z[:], z[:], self.g[:], ALU.mult, eng="pool")
        S.tt(z[:], z[:], self.b[:], ALU.add, eng="pool")
        S.dma(x_dst[t * 128:(t + 1) * 128, :], z[:], eng="pool")


def phase_prep_merge(kb, layer, wg_d, wb_d, wo_d):
    P = Pool_(kb)
    stg = [P.sb("pst%d" % k, [128, 1024]) for k in range(2)]
    stb = [P.sb("psb%d" % k, [128, 1024], BF16) for k in range(2)]
    jobs = []
    for n in range(4):
        jobs.append((wg_d[n], kb.d["w_gate"][layer, n]))
        jobs.append((wb_d[n], kb.d["w_br"][layer, n]))
    jobs.append((wo_d, kb.d["w_o"][layer]))
    prep_cast(kb, P, jobs, stg, stb)
    P.close()


def phase_merge(kb, layer, mod_d, brT_d, wg_d, wb_d, wo_d, x_src, x_dst):
    S = kb.S
    P = Pool_(kb)
    epi = Epi(kb, P, mod_d, layer, 2, "ln1_g", "ln1_b", "m")
    bgT = P.sb("bgT", [128, 4, 8])
    load_T(kb, P, bgT[:].rearrange("p n c -> p (n c)"), kb.d["b_gate"][layer].rearrange("n (c p) -> (n c) p", p=128), 32)
    wo = P.sb("wo", [128, 8, 1024], BF16)
    S.dma(wo[:], wo_d.rearrange("(c p) n -> p c n", p=128))
    wg = [P.sb("wg%d" % k, [128, 8, 1024], BF16) for k in range(2)]
    wb = [P.sb("wb%d" % k, [128, 4, 1024], BF16) for k in range(2)]
    brt = [P.sb("brt%d" % k, [128, 4, 512], BF16) for k in range(2)]
    mT = P.sb("mT", [128, 8, 512])
    mTb = P.sb("mTb", [128, 8, 512], BF16)
    sig = [P.sb("sig%d" % k, [128, 512]) for k in range(2)]
    tmp = [P.sb("mtmp%d" % k, [128, 512]) for k in range(2)]
    cnt = 0
    q = 0
    for g in range(8):
        tok = slice(g * 512, (g + 1) * 512)
        for n in range(4):
            k = cnt % 2
            cnt += 1
            S.dma(wg[k][:], wg_d[n].rearrange("(c p) n -> p c n", p=128), eng="sp")
            S.dma(wb[k][:], wb_d[n].rearrange("(c p) n -> p c n", p=128), eng="sp")
            S.dma(brt[k][:], brT_d[n, :, :, tok].rearrange("c p t -> p c t"), eng="sp")
            for cc in range(8):
                pa = kb.psb[(q % 2) * 2]
                pb = kb.psb[(q % 2) * 2 + 1]
                for kc in range(8):
                    S.mm(pa[:], wg[k][:, kc, cc * 128:(cc + 1) * 128], kb.hT[:, kc, 1 + g * 512:1 + (g + 1) * 512],
                         start=(kc == 0), stop=(kc == 7))
                for kc in range(4):
                    S.mm(pb[:], wb[k][:, kc, cc * 128:(cc + 1) * 128], brt[k][:, kc, :],
                         start=(kc == 0), stop=(kc == 3))
                sg = sig[q % 2]
                S.act(sg[:], pa[:], AF.Sigmoid, bias=bgT[:, n, cc:cc + 1])
                if n == 0:
                    S.tt(mT[:, cc, :], sg[:], pb[:], ALU.mult)
                else:
                    tp = tmp[q % 2]
                    S.tt(tp[:], sg[:], pb[:], ALU.mult)
                    S.tt(mT[:, cc, :], mT[:, cc, :], tp[:], ALU.add, eng="pool" if cc % 2 else "dve")
                q += 1
        for cc in range(8):
            S.copy(mTb[:, cc, :], mT[:, cc, :], eng="act" if cc % 2 else "pool")
        for tt in range(4):
            t = g * 4 + tt
            epi.prefetch_x(x_src, t)
            ys = []
            for h in range(2):
                py = kb.psb[4 + (t % 2) * 2 + h]
                for kc in range(8):
                    S.mm(py[:], mTb[:, kc, tt * 128:(tt + 1) * 128], wo[:, kc, h * 512:(h + 1) * 512],
                         start=(kc == 0), stop=(kc == 7))
                ys.append(py[:])
            epi.run(ys, x_dst, t)
    P.close()


def phase_moe(kb, layer, mod_d, x_src, x_dst):
    S = kb.S
    P = Pool_(kb)
    NSG = 2
    TSG = T // NSG
    NTS = TSG // 128
    epi = Epi(kb, P, mod_d, layer, 5, "ln2_g", "ln2_b", "e", with_z=False)
    modT = load_modT(kb, P, mod_d, layer, "modT_e")
    rw = P.sb("rw", [128, 8, NE])
    S.dma(rw[:], kb.d["router_w"].rearrange("(c p) e -> p c e", p=128))
    rb = P.sb("rb", [128, NE])
    S.dma(rb[:], kb.d["router_b"].rearrange("(o e) -> o e", o=1).partition_broadcast(128))
    hT = P.sb("hTm", [128, 8, TSG + 1], BF16)
    yacc = P.sb("yacc", [128, NTS, 1024])
    comb = P.sb("comb", [128, NTS, NE])
    h32 = [P.sb("h32_%d" % k, [128, 8, 128]) for k in range(2)]
    st = [P.sb("mst%d" % k, [128, 12]) for k in range(2)]
    mv = [P.sb("mmv%d" % k, [128, 2]) for k in range(2)]
    rs = [P.sb("mrs%d" % k, [128, 1]) for k in range(2)]
    lg = P.sb("r_lg", [128, NE])
    pr = P.sb("r_pr", [128, NE])
    sel = P.sb("r_sel", [128, NE])
    sel2 = P.sb("r_sel2", [128, NE])
    eq = P.sb("r_eq", [128, NE])
    m1 = P.sb("r_m1", [128, 4])
    m2 = P.sb("r_m2", [128, 4])
    gs = P.sb("r_gs", [128, 4])
    gm = P.sb("r_gm", [128, 1])
    og = P.sb("r_og", [128, 4])
    thr = P.sb("r_thr", [128, 4])
    msk = P.sb("r_msk", [128, NE])
    sm = P.sb("r_sm", [128, 1])
    mx = P.sb("r_mx", [128, 1])
    wg = [P.sb("ewg%d" % k, [128, 8, 512], BF16) for k in range(2)]
    wu = [P.sb("ewu%d" % k, [128, 8, 512], BF16) for k in range(2)]
    wd = [P.sb("ewd%d" % k, [128, 4, 1024], BF16) for k in range(2)]
    stg = [P.sb("estg%d" % k, [128, 2, 512]) for k in range(3)]
    heT = [P.sb("heT%d" % k, [128, 4, 512], BF16) for k in range(2)]
    sl = [P.sb("esl%d" % k, [128, 512]) for k in range(2)]
    ident = kb.cst["ident"]
    nld = [0]

    def load_expert(e, k):
        for (dst, src, kcn) in ((wg[k], kb.d["exp_w_gate"][layer, e], 8), (wu[k], kb.d["exp_w_up"][layer, e], 8)):
            sv = src.rearrange("(c p) n -> p c n", p=128)
            for c2 in range(0, kcn, 2):
                sg_ = stg[nld[0] % 3]
                nld[0] += 1
                S.dma(sg_[:], sv[:, c2:c2 + 2, :], eng="sp")
                S.copy(dst[:, c2:c2 + 2, :], sg_[:], eng="pool")
        sv = kb.d["exp_w_down"][layer, e].rearrange("(c p) n -> p c n", p=128)
        for c in range(4):
            sg_ = stg[nld[0] % 3]
            nld[0] += 1
            S.dma(sg_[:].rearrange("p a b -> p (a b)"), sv[:, c, :], eng="sp")
            S.copy(wd[k][:, c, :], sg_[:].rearrange("p a b -> p (a b)"), eng="pool")

    for sgi in range(NSG):
        t0 = sgi * NTS
        for tl in range(NTS):
            t = t0 + tl
            k = tl % 2
            xin = epi.xb[k]
            S.dma(xin[:], x_src[t * 128:(t + 1) * 128, :])
            ln_stats(S, xin, st[k], mv[k], rs[k])
            S.ts(xin[:], xin[:], mv[k][:, 0:1], rs[k][:, 0:1], ALU.subtract, ALU.mult)
            for c in range(8):
                pb = kb.psb[k * 2 + c // 4]
                S.tr(pb[:, (c % 4) * 128:(c % 4 + 1) * 128], xin[:, c * 128:(c + 1) * 128], ident[:])
            for c in range(8):
                pb = kb.psb[k * 2 + c // 4]
                src = pb[:, (c % 4) * 128:(c % 4 + 1) * 128]
                if c % 2 == 0:
                    S.ts(h32[k][:, c, :], src, modT[:, 4, c:c + 1], modT[:, 3, c:c + 1], ALU.mult, ALU.add)
                else:
                    S.act(h32[k][:, c, :], src, AF.Identity, bias=modT[:, 3, c:c + 1], scale=modT[:, 4, c:c + 1])
                S.copy(hT[:, c, 1 + tl * 128:1 + (tl + 1) * 128], h32[k][:, c, :], eng="pool")
            pl = kb.psb[4 + k]
            for c in range(8):
                S.mm(pl[:, 0:NE], h32[k][:, c, :], rw[:, c, :], start=(c == 0), stop=(c == 7))
            S.copy(lg[:], pl[:, 0:NE])
            S.reduce(mx[:], lg[:], ALU.max)
            S.ts(mx[:], mx[:], -1.0, None, ALU.mult)
            S.act(pr[:], lg[:], AF.Exp, bias=mx[:, 0:1], scale=1.0, accum_out=sm[:])
            S.recip(sm[:], sm[:])
            S.ts(pr[:], pr[:], sm[:, 0:1], None, ALU.mult)
            S.tt(sel[:], pr[:], rb[:], ALU.add)
            sel3 = sel[:].rearrange("p (g e) -> p g e", g=4)
            S.reduce(m1[:], sel3, ALU.max)
            S.tt(eq[:].rearrange("p (g e) -> p g e", g=4), sel3, m1[:].unsqueeze(2).to_broadcast([128, 4, 4]), ALU.is_ge)
            S.stt(sel2[:], eq[:], -1e9, sel[:], ALU.mult, ALU.add)
            S.reduce(m2[:], sel2[:].rearrange("p (g e) -> p g e", g=4), ALU.max)
            S.tt(gs[:], m1[:], m2[:], ALU.add)
            S.reduce(gm[:], gs[:], ALU.max)
            S.ts(og[:], gs[:], gm[:, 0:1], None, ALU.is_ge)
            S.ts(thr[:], og[:], -1e9, 1e9, ALU.mult, ALU.add)
            S.tt(thr[:], thr[:], m2[:], ALU.add)
            S.tt(msk[:].rearrange("p (g e) -> p g e", g=4), sel3, thr[:].unsqueeze(2).to_broadcast([128, 4, 4]), ALU.is_ge)
            S.tt(msk[:], msk[:], pr[:], ALU.mult)
            S.reduce(sm[:], msk[:], ALU.add)
            S.recip(sm[:], sm[:])
            S.ts(comb[:, tl, :], msk[:], sm[:, 0:1], None, ALU.mult)
        if sgi == 0:
            load_expert(0, 0)
        q = 0
        for e in range(NE):
            k = (sgi * NE + e) % 2
            nxt = sgi * NE + e + 1
            if nxt < NSG * NE:
                load_expert(nxt % NE, nxt % 2)
            for gq in range(NTS // 4):
                he = heT[gq % 2]
                for fc in range(4):
                    pg = kb.psb[(q % 2) * 2]
                    pu = kb.psb[(q % 2) * 2 + 1]
                    for kc in range(8):
                        S.mm(pg[:], wg[k][:, kc, fc * 128:(fc + 1) * 128], hT[:, kc, 1 + gq * 512:1 + (gq + 1) * 512],
                             start=(kc == 0), stop=(kc == 7))
                    for kc in range(8):
                        S.mm(pu[:], wu[k][:, kc, fc * 128:(fc + 1) * 128], hT[:, kc, 1 + gq * 512:1 + (gq + 1) * 512],
                             start=(kc == 0), stop=(kc == 7))
                    s_ = sl[q % 2]
                    S.act(s_[:], pg[:], AF.Silu)
                    S.tt(he[:, fc, :], s_[:], pu[:], ALU.mult)
                    q += 1
                for tt in range(4):
                    tl = gq * 4 + tt
                    for h in range(2):
                        py = kb.psb[4 + (tl * 2 + h) % 4]
                        for fc in range(4):
                            S.mm(py[:], he[:, fc, tt * 128:(tt + 1) * 128], wd[k][:, fc, h * 512:(h + 1) * 512],
                                 start=(fc == 0), stop=(fc == 3))
                        ya = yacc[:, tl, h * 512:(h + 1) * 512]
                        if e == 0:
                            S.ts(ya, py[:], comb[:, tl, e:e + 1], None, ALU.mult)
                        else:
                            S.stt(ya, py[:], comb[:, tl, e:e + 1], ya, ALU.mult, ALU.add)
        for tl in range(NTS):
            t = t0 + tl
            epi.run([yacc[:, tl, 0:512], yacc[:, tl, 512:1024]], x_dst, t, x_src=x_src, z=yacc[:, tl, :])
    P.close()


def build(io=None, layers=(0, 1), stages=("mod", "ln", "prep", "br", "merge", "moe"), last_out=None):
    kb = KB(io)
    S = kb.S
    mod_d = kb.dram("mod_d", [2, 6144], F32)
    xa = kb.dram("xa", [T, D], F32)
    xbd = kb.dram("xbd", [T, D], F32)
    out = kb.dram("out", [T, D], F32)
    brT_d = kb.dram("brT", [4, 4, 128, T], BF16)
    wg_d = kb.dram("wg_bf", [4, D, D], BF16)
    wb_d = kb.dram("wb_bf", [4, 512, D], BF16)
    wo_d = kb.dram("wo_bf", [D, D], BF16)
    kb.lb_d = kb.dram("lb_d", [2, 512], F32)
    if "mod" in stages:
        phase_mod(kb, mod_d)
    for layer in layers:
        x_src = kb.x if layer == 0 else xbd
        x_fin = out if layer == layers[-1] else xbd
        MP = Pool_(kb)
        kb.hT = MP.sb("hT", [128, 8, T + 1], BF16)
        for c in range(8):
            S.memset(kb.hT[:, c, 0:1], 0.0, eng="pool")
        if "ln" in stages:
            phase_ln_mixer(kb, x_src, mod_d, layer)
        if "prep" in stages:
            phase_prep_merge(kb, layer, wg_d, wb_d, wo_d)
        if "br" in stages:
            phase_branches(kb, layer, brT_d)
        if "merge" in stages:
            phase_merge(kb, layer, mod_d, brT_d, wg_d, wb_d, wo_d, x_src, xa if "moe" in stages else x_fin)
        MP.close()
        if "moe" in stages:
            phase_moe(kb, layer, mod_d, xa, x_fin)
    S.emit()
    return kb


def load_w(kb, dst, src, stg, cast_eng="pool", dma_eng="sp"):
    S = kb.S
    kc = src.shape[0] // 128
    n = src.shape[1]
    sv = stg[:, 0:kc, 0:n]
    S.dma(sv, src.rearrange("(c p) n -> p c n", p=128), eng=dma_eng)
    S.copy(dst, sv, eng=cast_eng)


def tok(t0, n=128):
    return slice(1 + t0, 1 + t0 + n)


def fox_branch(kb, layer, brT_d):
    S = kb.S
    P = Pool_(kb)
    hT = kb.hT
    W = kb.d["w_in"][layer]
    o = FOX_OFF
    psb = kb.psb
    stg = P.sb("fstg", [128, 8, 520])
    wq = P.sb("fwq", [128, 8, 512], BF16)
    wk = P.sb("fwk", [128, 8, 512], BF16)
    wvf = P.sb("fwvf", [128, 8, 520], BF16)
    load_w(kb, wq[:], W[:, o:o + 512], stg)
    load_w(kb, wk[:], W[:, o + 512:o + 1024], stg)
    load_w(kb, wvf[:], W[:, o + 1024:o + 1544], stg)
    fb = P.sb("ffb", [128, 8])
    S.dma(fb[:], kb.d["fox_f_bias"][layer:layer + 1, :].partition_broadcast(128))
    maskb = P.sb("fmask", [128, 128], BF16)
    S.copy(maskb[:], kb.cst["triu"][:])
    lf = P.sb("flf", [128, 32, 8])
    tA = P.sb("ftA", [128, 32, 8])
    tB = P.sb("ftB", [128, 32, 8])
    Fs = P.sb("fFs", [128, 32, 8])
    Cs = P.sb("fCs", [128, 32, 8])
    vp = P.sb("fvp", [128, 32, 8, 65], BF16)
    S.memset(vp[:].rearrange("p a b c -> p (a b c)"), 1.0, eng="pool")
    for g in range(8):
        pb = psb[6 + g % 2]
        for tt in range(4):
            t = g * 4 + tt
            for kc in range(8):
                S.mm(pb[:, tt * 8:(tt + 1) * 8], hT[:, kc, tok(t * 128)], wvf[:, kc, 512:520], start=(kc == 0), stop=(kc == 7))
        S.tt(lf[:, g * 4:(g + 1) * 4, :], pb[:, 0:32].rearrange("p (a b) -> p a b", a=4),
             fb[:].unsqueeze(1).to_broadcast([128, 4, 8]), ALU.add)
    lf2 = lf[:].rearrange("p a b -> p (a b)")
    S.act(lf2, lf2, AF.Exp, scale=-1.0)
    S.act(lf2, lf2, AF.Ln, bias=1.0)
    S.ts(lf2, lf2, -1.0, None, ALU.mult)
    a, b = lf, tA
    d = 1
    while d < 32:
        nb = tA if b is tA else tB
        if a is lf:
            nb = tA
        S.tt(nb[:, d:32, :], a[:, d:32, :], a[:, 0:32 - d, :], ALU.add)
        S.copy(nb[:, 0:d, :], a[:, 0:d, :])
        a = nb
        b = tB if nb is tA else tA
        d *= 2
    incl = a
    excl = tB if incl is tA else tA
    S.tt(excl[:], incl[:], lf[:], ALU.subtract)
    pF, pC = psb[6], psb[7]
    S.mm(pF[:, 0:256], kb.cst["triu"][:], lf2, start=True, stop=False)
    S.mm(pF[:, 0:256], kb.cst["ones"][:], excl[:].rearrange("p a b -> p (a b)"), start=False, stop=True)
    S.mm(pC[:, 0:256], kb.cst["ones"][:], incl[:].rearrange("p a b -> p (a b)"), start=True, stop=True)
    S.copy(Fs[:].rearrange("p a b -> p (a b)"), pF[:, 0:256])
    S.copy(Cs[:].rearrange("p a b -> p (a b)"), pC[:, 0:256], eng="act")
    for t in range(32):
        pb = psb[6 + t % 2]
        for kc in range(8):
            S.mm(pb[:], hT[:, kc, tok(t * 128)], wvf[:, kc, 0:512], start=(kc == 0), stop=(kc == 7))
        src = pb[:].rearrange("p (h d) -> p h d", h=8)
        if t % 2 == 0:
            S.copy(vp[:, t, :, 0:64], src, eng="dve")
        else:
            S.copy(vp[:, t, :, 0:64], src, eng="act")
    QT = P.sb("fQT", [128, T], BF16)
    KA = P.sb("fKA", [128, T], BF16)
    KB_ = P.sb("fKB", [128, T], BF16)
    S.memset(KA[64:128, :], 0.0, eng="pool")
    S.memset(KB_[0:64, :], 0.0, eng="pool")
    otok = P.sb("fotok", [128, 32, 128])
    brs = P.sb("fbrs", [128, T], BF16)
    pts = [P.sb("fpt%d" % k, [128, 512], BF16) for k in range(4)]
    vss = [P.sb("fvs%d" % k, [128, 65], BF16) for k in range(6)]
    biases = [P.sb("fbias%d" % k, [128, 32]) for k in range(2)]
    dms = [P.sb("fdm%d" % k, [128, 32]) for k in range(2)]
    rcs = [P.sb("frc%d" % k, [128, 1]) for k in range(2)]
    q = 0
    nb_ = 0
    nv = 0
    for p in range(4):
        for g in range(8):
            pq = psb[6]
            pk = psb[7]
            for kc in range(8):
                S.mm(pq[:], wq[:, kc, p * 128:(p + 1) * 128], hT[:, kc, tok(g * 512, 512)], start=(kc == 0), stop=(kc == 7))
            for kc in range(8):
                S.mm(pk[:], wk[:, kc, p * 128:(p + 1) * 128], hT[:, kc, tok(g * 512, 512)], start=(kc == 0), stop=(kc == 7))
            S.act(QT[:, g * 512:(g + 1) * 512], pq[:], AF.Copy, scale=0.125)
            S.copy(KA[0:64, g * 512:(g + 1) * 512], pk[0:64, :])
            S.copy(KB_[64:128, g * 512:(g + 1) * 512], pk[64:128, :])
        rows = []
        for a_ in range(2):
            for i in range(32):
                rows.append((a_, i))
        batches = []
        for ri, (a_, i) in enumerate(rows):
            for jb in range(0, i + 1, 4):
                batches.append((ri, a_, i, jb, min(4, i + 1 - jb)))
        rowbuf = {}

        def front(bt):
            nonlocal q, nb_
            ri, a_, i, jb, nbt = bt
            h = 2 * p + a_
            Kh = KA if a_ == 0 else KB_
            if jb == 0:
                k2 = nb_ % 2
                nb_ += 1
                rowbuf[ri] = k2
                S.ts(biases[k2][:, 0:i + 1], Fs[:, 0:i + 1, h], Cs[:, i, h:h + 1], -1.0, ALU.subtract, ALU.mult)
                S.act(dms[k2][:, 0:i + 1], biases[k2][:, 0:i + 1], AF.Exp)
            ps_s = psb[q % 4]
            pt = pts[q % 4]
            q += 1
            for jj in range(nbt):
                j = jb + jj
                S.mm(ps_s[:, jj * 128:(jj + 1) * 128], Kh[:, j * 128:(j + 1) * 128], QT[:, i * 128:(i + 1) * 128])
            S.act(pt[:, 0:nbt * 128], ps_s[:, 0:nbt * 128], AF.Exp)
            if jb + nbt - 1 == i:
                S.tt(pt[:, (nbt - 1) * 128:nbt * 128], pt[:, (nbt - 1) * 128:nbt * 128], maskb[:], ALU.mult)
            return pt

        def back(bt, pt):
            nonlocal nv
            ri, a_, i, jb, nbt = bt
            h = 2 * p + a_
            k2 = rowbuf[ri]
            po = psb[4 + k2]
            dm = dms[k2]
            rc = rcs[k2]
            for jj in range(nbt):
                j = jb + jj
                vs = vss[nv % 6]
                nv += 1
                S.ts(vs[:], vp[:, j, h, :], dm[:, j:j + 1], None, ALU.mult)
                S.mm(po[:, 0:65], pt[:, jj * 128:(jj + 1) * 128], vs[:], start=(j == 0), stop=(j == i))
            if jb + nbt - 1 == i:
                S.recip(rc[:], po[:, 64:65])
                S.ts(otok[:, i, a_ * 64:(a_ + 1) * 64], po[:, 0:64], rc[:, 0:1], None, ALU.mult)
                if a_ == 1:
                    pt_ = psb[6 + i % 2]
                    S.tr(pt_[:, 0:128], otok[:, i, :], kb.cst["ident"][:])
                    S.copy(brs[:, i * 128:(i + 1) * 128], pt_[:, 0:128], eng="act" if i % 2 else "dve")

        pend = None
        for bt in batches:
            ptn = front(bt)
            if pend is not None:
                back(*pend)
            pend = (bt, ptn)
        back(*pend)
        S.dma(brT_d[2, p], brs[:], eng="pool")
    P.close()


def phase_branches(kb, layer, brT_d):
    which = DBG.get("branches", (0, 1, 2, 3))
    if 2 in which:
        fox_branch(kb, layer, brT_d)
    if 0 in which:
        gla_branch(kb, layer, brT_d)
    if 3 in which:
        hgrn_branch(kb, layer, brT_d)
    if 1 in which:
        rwkv_branch(kb, layer, brT_d)


def cgla_branch(kb, layer, brT_d, kind, lb_d=None):
    S = kb.S
    P = Pool_(kb)
    hT = kb.hT
    W = kb.d["w_in"][layer]
    psb = kb.psb
    cst = kb.cst
    gla = (kind == "gla")
    NU = 2 if gla else 4
    HPU = 2 if gla else 1
    DK = 64 if gla else 128
    KW = NU * 128
    o = GLA_OFF if gla else HGRN_OFF
    bidx = 0 if gla else 3
    qscale = 0.125 if gla else 1.0
    stg = P.sb("cstg", [128, 8, 528])
    if gla:
        wq = P.sb("cwq", [128, 8, 256], BF16)
        wk = P.sb("cwk", [128, 8, 256], BF16)
        wv = P.sb("cwv", [128, 8, 512], BF16)
        wg = P.sb("cwg", [128, 8, 528], BF16)
        load_w(kb, wq[:], W[:, o:o + 256], stg)
        load_w(kb, wk[:], W[:, o + 256:o + 512], stg)
        load_w(kb, wv[:], W[:, o + 512:o + 1024], stg)
        load_w(kb, wg[:], W[:, o + 1024:o + 1552], stg)
        aup = P.sb("caup", [16, 256])
        S.dma(aup[:], kb.d["gla_alpha_up"][layer])
        abr = P.sb("cabr", [1, 256])
        S.dma(abr[:], kb.d["gla_alpha_b"][layer:layer + 1, :])
        alT = P.sb("calT", [16, 512])
        Uc = P.sb("cUc", [128, 128])
        SUl = P.sb("cSUl", [128, 128])
        S.ts(Uc[:], cst["triu64"][:], -1.0 / 16.0, None, ALU.mult)
        S.ts(SUl[:], cst["slo64"][:], -1.0 / 16.0, None, ALU.mult)
        ng_src = kb.d["gla_norm_g"]
    else:
        wq = P.sb("cwq", [128, 8, 512], BF16)
        wk = P.sb("cwk", [128, 8, 512], BF16)
        wv = P.sb("cwv", [128, 8, 512], BF16)
        wg = P.sb("cwg", [128, 8, 512], BF16)
        load_w(kb, wq[:], W[:, o:o + 512], stg)
        load_w(kb, wk[:], W[:, o + 512:o + 1024], stg)
        load_w(kb, wv[:], W[:, o + 1024:o + 1536], stg)
        load_w(kb, wg[:], W[:, o + 1536:o + 2048], stg)
        Uc, SUl = cst["triu64"], cst["slo64"]
        lbB = P.sb("clbB", [128, 512])
        omlB = P.sb("comlB", [128, 512])
        S.dma(lbB[:], lb_d[0:1, :].partition_broadcast(128))
        S.ts(omlB[:], lbB[:], -1.0, 1.0, ALU.mult, ALU.add)
        lbT = P.sb("clbT", [128, 4])
        omlT = P.sb("comlT", [128, 4])
        load_T(kb, P, lbT[:], lb_d[0].rearrange("(c p) -> c p", p=128), 4)
        S.ts(omlT[:], lbT[:], -1.0, 1.0, ALU.mult, ALU.add)
        ng_src = kb.d["hgrn_norm_g"]
    ngb = P.sb("cngb", [128, 128])
    S.dma(ngb[:], ng_src[layer:layer + 1, :].partition_broadcast(128))
    qTs = [P.sb("cqTs%d" % u, [128, 512]) for u in range(NU)]
    kTs = [P.sb("ckTs%d" % u, [128, 512]) for u in range(NU)]
    brs = P.sb("cbrs", [128, 4, T], BF16)
    l_tok = P.sb("cltok", [128, KW])
    k_tok = P.sb("cktok", [128, KW])
    f_tok = P.sb("cftok", [128, KW])
    v_tok = P.sb("cvtok", [128, 512], BF16)
    sg_tok = P.sb("csgtok", [128, 512])
    br_tok = P.sb("cbrtok", [128, 512])
    bTs = P.sb("cbTs", [128, 128])
    kdec = P.sb("ckdec", [128, 128])
    khat = P.sb("ckhat", [128, 128], BF16)
    bm = P.sb("cbm", [128, 2])
    nbm = P.sb("cnbm", [128, 2])
    E1 = P.sb("cE1", [128, 128])
    E2 = P.sb("cE2", [128, 128])
    E3 = P.sb("cE3", [128, 128])
    qt = P.sb("cqt", [128, 128], BF16)
    kt = P.sb("ckt", [128, 128], BF16)
    qA = P.sb("cqA", [128, 128], BF16)
    qB = P.sb("cqB", [128, 128], BF16)
    attb = [P.sb("cattb%d" % a, [128, 128], BF16) for a in range(HPU)]
    Sf = [[P.sb("cSf%d_%d" % (u, k), [128, 128]) for k in range(2)] for u in range(NU)]
    Sb = [[P.sb("cSb%d_%d" % (u, k), [128, 128], BF16) for k in range(2)] for u in range(NU)]
    for u in range(NU):
        S.memset(Sf[u][0][:], 0.0)
        S.memset(Sb[u][0][:], 0.0)
    ss = P.sb("css", [128, 2])
    junk = P.sb("cjunk", [128, 128])
    for g in range(DBG.get('cg_ng', 8)):
        gt = tok(g * 512, 512)
        for u in range(NU):
            pq, pk = psb[0], psb[1]
            for kc in range(8):
                S.mm(pq[:], wq[:, kc, u * 128:(u + 1) * 128], hT[:, kc, gt], start=(kc == 0), stop=(kc == 7))
            for kc in range(8):
                S.mm(pk[:], wk[:, kc, u * 128:(u + 1) * 128], hT[:, kc, gt], start=(kc == 0), stop=(kc == 7))
            S.copy(qTs[u][:], pq[:], eng="act")
            if gla:
                S.copy(kTs[u][:], pk[:], eng="dve")
            else:
                S.act(kTs[u][:], pk[:], AF.Sigmoid)
                S.ts(kTs[u][:], kTs[u][:], omlT[:, u:u + 1], lbT[:, u:u + 1], ALU.mult, ALU.add)
                S.ts(kTs[u][:], kTs[u][:], -1.0, 1.0, ALU.mult, ALU.add)
        if gla:
            pa = psb[2]
            for kc in range(8):
                S.mm(pa[0:16, :], wg[:, kc, 512:528], hT[:, kc, gt], start=(kc == 0), stop=(kc == 7))
            S.copy(alT[:], pa[0:16, :])
        for tt in range(4):
            t = g * 4 + tt
            tk = tok(t * 128)
            tcol = slice(tt * 128, (tt + 1) * 128)
            pv, pg = psb[2], psb[3]
            for kc in range(8):
                S.mm(pv[:], hT[:, kc, tk], wv[:, kc, 0:512], start=(kc == 0), stop=(kc == 7))
            S.copy(v_tok[:], pv[:], eng="act")
            for kc in range(8):
                S.mm(pg[:], hT[:, kc, tk], wg[:, kc, 0:512], start=(kc == 0), stop=(kc == 7))
            S.act(sg_tok[:], pg[:], AF.Silu)
            pk2 = psb[2]
            if gla:
                for kc in range(8):
                    S.mm(pk2[:, 0:256], hT[:, kc, tk], wk[:, kc, 0:256], start=(kc == 0), stop=(kc == 7))
                S.mm(pk2[:, 256:512], alT[:, tcol], aup[:], start=True, stop=False)
                S.mm(pk2[:, 256:512], cst["ones"][0:1, :], abr[:], start=False, stop=True)
                S.copy(k_tok[:], pk2[:, 0:256])
                S.act(l_tok[:], pk2[:, 256:512], AF.Exp, scale=-1.0)
                S.act(l_tok[:], l_tok[:], AF.Ln, bias=1.0)
            else:
                for kc in range(8):
                    S.mm(pk2[:], hT[:, kc, tk], wk[:, kc, 0:512], start=(kc == 0), stop=(kc == 7))
                S.act(f_tok[:], pk2[:], AF.Sigmoid)
                S.tt(f_tok[:], f_tok[:], omlB[:], ALU.mult)
                S.tt(f_tok[:], f_tok[:], lbB[:], ALU.add)
                S.act(l_tok[:], f_tok[:], AF.Ln)
                S.ts(k_tok[:], f_tok[:], -1.0, 1.0, ALU.mult, ALU.add)
            for u in range(NU):
                cu = slice(u * 128, (u + 1) * 128)
                S0f, S1f = Sf[u][0], Sf[u][1]
                S0b, S1b = Sb[u][0], Sb[u][1]
                if DBG.get('cg_lvl', 99) < 1:
                    continue
                pX = psb[4]
                S.mm(pX[:, 0:128], l_tok[:, cu], Uc[:])
                S.mm(pX[:, 128:256], SUl[:], l_tok[:, cu])
                S.copy(bTs[:], pX[:, 0:128])
                S.act(kdec[:], pX[:, 128:256], AF.Exp)
                S.tt(khat[:], k_tok[:, cu], kdec[:], ALU.mult, eng="pool")
                if DBG.get('cg_lvl', 99) < 2:
                    continue
                mid = bTs[:].rearrange("p (c s) -> p c s", c=2)[:, :, 32]
                S.copy(bm[:], mid)
                S.ts(nbm[:], mid, -1.0, None, ALU.mult)
                for c in range(2):
                    cs = slice(c * 64, (c + 1) * 64)
                    S.act(E1[:, cs], bTs[:, cs], AF.Exp, bias=nbm[:, c:c + 1])
                    S.act(E2[:, cs], bTs[:, cs], AF.Exp, bias=bm[:, c:c + 1], scale=-1.0)
                S.act(E3[:], bTs[:], AF.Exp)
                if DBG.get('cg_sub', 9) < 1:
                    continue
                S.stt(qt[:], qTs[u][:, tcol], qscale, E1[:], ALU.mult, ALU.mult)
                if DBG.get('cg_sub', 9) < 2:
                    continue
                S.tt(kt[:], kTs[u][:, tcol], E2[:], ALU.mult, eng="pool")
                if DBG.get('cg_sub', 9) < 3:
                    continue
                S.stt(qA[:], qTs[u][:, tcol], qscale, E3[:], ALU.mult, ALU.mult)
                if DBG.get('cg_lvl', 99) < 3:
                    continue
                pAtt = psb[5]
                for a in range(HPU):
                    ra = slice(a * DK, (a + 1) * DK)
                    S.mm(pAtt[:, a * 128:(a + 1) * 128], kt[ra, :], qt[ra, :])
                for a in range(HPU):
                    am = DBG.get("att_mode", "swap")
                    if am == "tt":
                        S.tt(attb[a][:], pAtt[:, a * 128:(a + 1) * 128], cst["triu64"][:], ALU.mult)
                    elif am == "swap":
                        S.tt(attb[a][:], cst["triu64"][:], pAtt[:, a * 128:(a + 1) * 128], ALU.mult)
                    elif am == "act":
                        S.copy(junk[:], pAtt[:, a * 128:(a + 1) * 128], eng="act")
                        S.tt(attb[a][:], junk[:], cst["triu64"][:], ALU.mult, eng="pool")
                if DBG.get('cg_lvl', 99) < 4:
                    continue
                pS = psb[6]
                for c in range(2):
                    rows = slice(c * 64, (c + 1) * 64)
                    for a in range(HPU):
                        h = u * HPU + a
                        ra = slice(a * DK, (a + 1) * DK)
                        S.mm(pS[ra, c * 128:(c + 1) * 128], khat[rows, a * DK:(a + 1) * DK], v_tok[rows, h * 128:(h + 1) * 128])
                S.stt(S1f[:], S0f[:], E3[:, 63:64], pS[:, 0:128], ALU.mult, ALU.add)
                S.copy(S1b[:], S1f[:], eng="act")
                if DBG.get('cg_lvl', 99) < 5:
                    continue
                pO = psb[7]
                for a in range(HPU):
                    h = u * HPU + a
                    ra = slice(a * DK, (a + 1) * DK)
                    oc = slice(a * 128, (a + 1) * 128)
                    S.mm(pO[0:64, oc], qA[ra, 0:64], S0b[ra, :], start=True, stop=False)
                    S.mm(pO[64:128, oc], qA[ra, 64:128], S1b[ra, :], start=True, stop=False)
                    S.mm(pO[:, oc], attb[a][:], v_tok[:, h * 128:(h + 1) * 128], start=False, stop=True)
                S.stt(S0f[:], S1f[:], E3[:, 127:128], pS[:, 128:256], ALU.mult, ALU.add)
                S.copy(S0b[:], S0f[:], eng="act")
                if DBG.get('cg_lvl', 99) < 6:
                    continue
                for a in range(HPU):
                    h = u * HPU + a
                    oc = slice(a * 128, (a + 1) * 128)
                    hc = slice(h * 128, (h + 1) * 128)
                    S.act(junk[:], pO[:, oc], AF.Square, accum_out=ss[:, a:a + 1])
                    rsqrt_eps(S, ss[:, a:a + 1], ss[:, a:a + 1], 1e-6, scale=1.0 / 128.0)
                    S.stt(br_tok[:, hc], pO[:, oc], ss[:, a:a + 1], ngb[:], ALU.mult, ALU.mult)
                    S.tt(br_tok[:, hc], br_tok[:, hc], sg_tok[:, hc], ALU.mult, eng="pool")
            pT = psb[0] if tt % 2 else psb[1]
            for kc in range(4):
                S.tr(pT[:, kc * 128:(kc + 1) * 128], br_tok[:, kc * 128:(kc + 1) * 128], cst["ident"][:])
            S.copy(brs[:, :, t * 128:(t + 1) * 128], pT[:].rearrange("p (c t) -> p c t", c=4), eng="act" if t % 2 else "dve")
    S.dma(brT_d[bidx].rearrange("c p t -> p c t"), brs[:], eng="pool")
    P.close()


def gla_branch(kb, layer, brT_d):
    cgla_branch(kb, layer, brT_d, "gla")


def hgrn_branch(kb, layer, brT_d):
    S = kb.S
    P = Pool_(kb)
    lb_d = kb.lb_d
    row = P.sb("hlbrow", [1, 512])
    if layer == 0:
        S.memset(row[:], 0.0)
    else:
        r0 = P.sb("hlb0", [1, 512])
        S.dma(r0[:], kb.d["hgrn_lb_logits"][0:1, :])
        S.dma(row[:], kb.d["hgrn_lb_logits"][1:2, :])
        S.tt(row[:], row[:], r0[:], ALU.subtract)
        S.act(row[:], row[:], AF.Sigmoid)
    S.dma(lb_d[layer:layer + 1, :], row[:])
    P.close()
    cgla_branch(kb, layer, brT_d, "hgrn", lb_d=lb_d[layer:layer + 1, :])


def rwkv_branch(kb, layer, brT_d):
    S = kb.S
    hT = kb.hT
    W = kb.d["w_in"][layer]
    psb = kb.psb
    cst = kb.cst
    o = RWKV_OFF
    P = Pool_(kb)
    CW = math.exp(-0.5)
    Wr = [P.sb("rWr%d" % k, [128, 8, 512], BF16) for k in range(2)]
    Wk = [P.sb("rWk%d" % k, [128, 8, 512], BF16) for k in range(2)]
    Wv = [P.sb("rWv%d" % k, [128, 8, 512], BF16) for k in range(2)]
    Wl = [P.sb("rWl%d" % k, [128, 8, 256], BF16) for k in range(2)]
    PP = Pool_(kb)
    stg = PP.sb("rstg", [128, 8, 512])
    muB = PP.sb("rmuB", [128, 1792])
    omuB = PP.sb("romuB", [128, 1792])
    S.dma(muB[:], kb.d["rwkv_mu"][layer:layer + 1, :].partition_broadcast(128))
    S.ts(omuB[:], muB[:], -1.0, 1.0, ALU.mult, ALU.add)
    for (dst, c0, n) in ((Wr, 0, 512), (Wk, 512, 512), (Wv, 1024, 512), (Wl, 1536, 256)):
        sv = stg[:, :, 0:n]
        S.dma(sv, W[:, o + c0:o + c0 + n].rearrange("(c p) n -> p c n", p=128))
        S.tt(dst[0][:], sv, omuB[:, c0:c0 + n].unsqueeze(1).to_broadcast([128, 8, n]), ALU.mult)
        S.tt(dst[1][:], sv, muB[:, c0:c0 + n].unsqueeze(1).to_broadcast([128, 8, n]), ALU.mult, eng="pool")
    PP.close()
    lw = P.sb("rlw", [128, 512])
    S.dma(lw[0:64, :], kb.d["rwkv_w2"][layer])
    S.dma(lw[64:128, :], kb.d["rwkv_a2"][layer])
    g2f = P.sb("rg2f", [128, 512])
    g2b = P.sb("rg2b", [128, 512], BF16)
    S.dma(g2f[:], kb.d["rwkv_g2"][layer])
    S.copy(g2b[:], g2f[:])
    w0r = P.sb("rw0r", [1, 512])
    S.dma(w0r[:], kb.d["rwkv_w0"][layer:layer + 1, :])
    a0T = P.sb("ra0T", [128, 4])
    kkT_ = P.sb("rkkT", [128, 4])
    kaT = P.sb("rkaT", [128, 4])
    okaT = P.sb("rokaT", [128, 4])
    rkT_ = P.sb("rrkT", [128, 4])
    load_T(kb, P, a0T[:], kb.d["rwkv_a0"][layer].rearrange("(c p) -> c p", p=128), 4)
    load_T(kb, P, kkT_[:], kb.d["rwkv_k_k"][layer].rearrange("(c p) -> c p", p=128), 4)
    load_T(kb, P, kaT[:], kb.d["rwkv_k_a"][layer].rearrange("(c p) -> c p", p=128), 4)
    load_T(kb, P, rkT_[:], kb.d["rwkv_r_k"][layer].rearrange("(c two) d -> c (two d)", two=2), 4)
    S.ts(okaT[:], kaT[:], -1.0, 1.0, ALU.mult, ALU.add)
    lngB = P.sb("rlngB", [128, 512])
    lnbB = P.sb("rlnbB", [128, 512])
    S.dma(lngB[:], kb.d["rwkv_ln_g"][layer:layer + 1, :].partition_broadcast(128))
    S.dma(lnbB[:], kb.d["rwkv_ln_b"][layer:layer + 1, :].partition_broadcast(128))
    Uc = P.sb("rUc", [128, 128])
    Ux = P.sb("rUx", [128, 128])
    SUl = P.sb("rSUl", [128, 128])
    nsup = P.sb("rnsup", [128, 128])
    nslo = P.sb("rnslo", [128, 128])
    ntriu = P.sb("rntriu", [128, 128])
    S.ts(Uc[:], cst["triu64"][:], -CW, None, ALU.mult)
    S.ts(Ux[:], cst["sup64"][:], -CW, None, ALU.mult)
    S.ts(SUl[:], cst["slo64"][:], -CW, None, ALU.mult)
    S.ts(nsup[:], cst["sup64"][:], -1.0, None, ALU.mult)
    S.ts(nslo[:], cst["slo64"][:], -1.0, None, ALU.mult)
    S.ts(ntriu[:], cst["triu64"][:], -1.0, None, ALU.mult)
    hsel = P.sb("rhsel", [128, 2])
    S.copy(hsel[:], cst["blk64"][:].rearrange("p (a s) -> p a s", a=2)[:, :, 0])
    ident = cst["ident"]

    def f512(name):
        return P.sb(name, [128, 512])

    def f128(name, dt=F32):
        return P.sb(name, [128, 128], dt)

    lo = f512("rlo")
    sgl = P.sb("rsgl", [128, 512], BF16)
    rT, kT, aT, kaT_, bT_, kpT, tmpF, prodT = (f512("r_" + n) for n in ("rT", "kT", "aT", "kapT", "bbT", "kpT", "tmpF", "prodT"))
    def d128(name):
        return [f128("%s_%d" % (name, k)) for k in range(2)]

    l_tok = f128("rltok")
    v_tokD, g_tokD = d128("rvtok"), d128("rgtok")
    sb2D = [P.sb("rsb2_%d" % k, [128, 2]) for k in range(2)]
    gCD = [P.sb("rgC_%d" % k, [128, 2]) for k in range(2)]
    bTs, bxTs = f128("rbTs"), f128("rbxTs")
    bm, nbm, bl = P.sb("rbm", [128, 2]), P.sb("rnbm", [128, 2]), P.sb("rbl", [128, 2])
    E = {n: f128("rE_" + n) for n in ("r", "kx", "inv", "abs", "absx", "last")}
    RB = BF16 if DBG.get("rw_bf16", True) else F32
    rt, kxt, kt, bt = (f128("r_" + n, RB) for n in ("rt", "kxt", "kt", "bt"))
    KhT, BhT = (f128("r_" + n) for n in ("KhT", "BhT"))
    rbarD, kbarD, KhatD, BhatD = d128("rrbar"), d128("rkbar"), d128("rKhat"), d128("rBhat")
    Mm = [[f128("rM%d_%d" % (a, k), RB) for k in range(2)] for a in range(2)]
    MT = [[f128("rMT%d_%d" % (a, k), RB) for k in range(2)] for a in range(2)]
    Pb = [[f128("rPb%d_%d" % (a, k), RB) for k in range(2)] for a in range(2)]
    PmD = [[[f128("rP%d_%d_%d" % (b_, a, k)) for k in range(2)] for a in range(2)] for b_ in range(2)]
    AkkD = [[f128("rAkk%d_%d" % (b_, a)) for a in range(2)] for b_ in range(2)]
    ArkD = [[f128("rArk%d_%d" % (b_, a)) for a in range(2)] for b_ in range(2)]
    ArbD = [[f128("rArb%d_%d" % (b_, a)) for a in range(2)] for b_ in range(2)]
    Ws, Us, ytok = f128("rWs"), f128("rUs"), f128("rytok")
    Hs = [[P.sb("rHs%d_%d" % (u, k), [128, 64]) for k in range(2)] for u in range(4)]
    for u in range(4):
        S.memset(Hs[u][0][:], 0.0)
    st6 = P.sb("rst6", [128, 2, 6])
    mv2 = P.sb("rmv2", [128, 2, 2])
    rs2 = P.sb("rrs2", [128, 2])
    yn = f128("ryn")
    brt_ = f128("rbrt")
    brb = [P.sb("rbrb%d" % k, [128, 128], BF16) for k in range(2)]

    def proj_fm(ps, W2, c0, n, gcol):
        for kc in range(8):
            S.mm(ps[0:n, :], W2[0][:, kc, c0:c0 + n], hT[:, kc, slice(1 + gcol, 1 + gcol + 512)], start=(kc == 0), stop=False)
        for kc in range(8):
            S.mm(ps[0:n, :], W2[1][:, kc, c0:c0 + n], hT[:, kc, slice(gcol, gcol + 512)], start=False, stop=(kc == 7))

    iters = []
    for g in range(DBG.get("rw_ng", 8)):
        for u in range(4):
            for tt_ in range(4):
                iters.append((g, u, tt_))

    def setup(it):
        g, u, tt_ = iters[it]
        bf = it % 2
        gcol = g * 512
        uc = slice(u * 128, (u + 1) * 128)
        v_tok, g_tok, sb2, gC = v_tokD[bf], g_tokD[bf], sb2D[bf], gCD[bf]
        rbar, kbar, Khat, Bhat = rbarD[bf], kbarD[bf], KhatD[bf], BhatD[bf]
        Pm, Akk, Ark, Arb = PmD[bf], AkkD[bf], ArkD[bf], ArbD[bf]
        if u == 0 and tt_ == 0:
            proj_fm(psb[0], Wl, 0, 128, gcol)
            S.act(lo[0:64, :], psb[0][0:64, :], AF.Tanh)
            S.copy(lo[64:128, :], psb[0][64:128, :])
            proj_fm(psb[1], Wl, 128, 128, gcol)
            S.act(sgl[:], psb[1][:], AF.Sigmoid)
            yield
        if tt_ == 0:
            proj_fm(psb[0], Wr, u * 128, 128, gcol)
            S.copy(rT[:], psb[0][:], eng="act")
            proj_fm(psb[1], Wk, u * 128, 128, gcol)
            S.copy(kT[:], psb[1][:])
            yield
            S.mm(psb[0][:], lw[64:128, uc], lo[64:128, :])
            S.act(aT[:], psb[0][:], AF.Sigmoid, bias=a0T[:, u:u + 1])
            S.ts(kaT_[:], kT[:], kkT_[:, u:u + 1], None, ALU.mult)
            S.tt(tmpF[:], kaT_[:], kaT_[:], ALU.mult)
            S.mm(psb[1][:], cst["blk64"][:], tmpF[:])
            yield
            S.act(tmpF[:], psb[1][:], AF.Ln, bias=1e-24)
            S.act(tmpF[:], tmpF[:], AF.Exp, scale=-0.5)
            S.tt(kaT_[:], kaT_[:], tmpF[:], ALU.mult)
            S.tt(bT_[:], kaT_[:], aT[:], ALU.mult)
            yield
            S.ts(tmpF[:], aT[:], kaT[:, u:u + 1], okaT[:, u:u + 1], ALU.mult, ALU.add)
            S.tt(kpT[:], kT[:], tmpF[:], ALU.mult)
            S.stt(prodT[:], rT[:], rkT_[:, u:u + 1], kpT[:], ALU.mult, ALU.mult)
            yield
        t = g * 4 + tt_
        t0 = t * 128
        tc_ = slice(tt_ * 128, (tt_ + 1) * 128)
        pt_ = psb[2]
        for kc in range(8):
            S.mm(pt_[:, 0:128], hT[:, kc, slice(1 + t0, 1 + t0 + 128)], Wv[0][:, kc, uc], start=(kc == 0), stop=False)
        for kc in range(8):
            S.mm(pt_[:, 0:128], hT[:, kc, slice(t0, t0 + 128)], Wv[1][:, kc, uc], start=False, stop=(kc == 7))
        S.mm(pt_[:, 128:256], lo[0:64, tc_], lw[0:64, uc], start=True, stop=False)
        S.mm(pt_[:, 128:256], cst["ones"][0:1, :], w0r[0:1, uc], start=False, stop=True)
        S.mm(pt_[:, 256:384], sgl[:, tc_], g2b[:, uc])
        S.mm(pt_[:, 384:386], prodT[:, tc_], hsel[:])
        yield
        S.act(l_tok[:], pt_[:, 128:256], AF.Sigmoid)
        S.copy(v_tok[:], pt_[:, 0:128])
        S.copy(g_tok[:], pt_[:, 256:384], eng="act")
        S.copy(sb2[:], pt_[:, 384:386])
        pX = psb[3]
        S.mm(pX[:, 0:128], l_tok[:], Uc[:])
        S.mm(pX[:, 128:256], l_tok[:], Ux[:])
        yield
        S.copy(bTs[:], pX[:, 0:128])
        S.copy(bxTs[:], pX[:, 128:256], eng="act")
        b3 = bTs[:].rearrange("p (c s) -> p c s", c=2)
        S.copy(bm[:], b3[:, :, 32])
        S.ts(nbm[:], b3[:, :, 32], -1.0, None, ALU.mult)
        S.copy(bl[:], b3[:, :, 63])
        yield
        for c in range(2):
            cs = slice(c * 64, (c + 1) * 64)
            S.act(E["r"][:, cs], bTs[:, cs], AF.Exp, bias=nbm[:, c:c + 1])
            S.act(E["kx"][:, cs], bxTs[:, cs], AF.Exp, bias=nbm[:, c:c + 1])
            S.act(E["inv"][:, cs], bTs[:, cs], AF.Exp, bias=bm[:, c:c + 1], scale=-1.0)
            S.act(E["last"][:, cs], bTs[:, cs], AF.Exp, bias=bl[:, c:c + 1], scale=-1.0)
        S.act(E["abs"][:], bTs[:], AF.Exp)
        S.act(E["absx"][:], bxTs[:], AF.Exp)
        yield
        S.tt(rt[:], rT[:, tc_], E["r"][:], ALU.mult)
        S.tt(kxt[:], kaT_[:, tc_], E["kx"][:], ALU.mult)
        S.tt(kt[:], kpT[:, tc_], E["inv"][:], ALU.mult)
        S.tt(bt[:], bT_[:, tc_], E["inv"][:], ALU.mult)
        yield
        S.tt(KhT[:], kpT[:, tc_], E["last"][:], ALU.mult)
        S.stt(BhT[:], bT_[:, tc_], -1.0, E["last"][:], ALU.mult, ALU.mult)
        S.tt(rbar[:], rT[:, tc_], E["abs"][:], ALU.mult)
        S.tt(kbar[:], kaT_[:, tc_], E["absx"][:], ALU.mult)
        S.copy(gC[:], E["abs"][:].rearrange("p (c s) -> p c s", c=2)[:, :, 63])
        for a in range(2):
            ra = slice(a * 64, (a + 1) * 64)
            pA = psb[4 + a]
            S.mm(pA[:, 0:128], bt[ra, :], kxt[ra, :])
            S.mm(pA[:, 128:256], kxt[ra, :], bt[ra, :])
            S.mm(pA[:, 256:384], kt[ra, :], kxt[ra, :])
            S.mm(pA[:, 384:512], kt[ra, :], rt[ra, :])
            S.mm(psb[6][:, a * 128:(a + 1) * 128], bt[ra, :], rt[ra, :])
        S.tr(pX[:, 256:384], KhT[:], ident[:])
        S.tr(pX[:, 384:512], BhT[:], ident[:])
        yield
        for a in range(2):
            pA = psb[4 + a]
            S.tt(Mm[a][0][:], nsup[:], pA[:, 0:128], ALU.mult)
            S.tt(MT[a][0][:], nslo[:], pA[:, 128:256], ALU.mult)
            S.tt(Pm[a][0][:], Mm[a][0][:], ident[:], ALU.add)
            S.copy(Pb[a][0][:], Pm[a][0][:], eng="act")
        yield
        for a in range(2):
            pA = psb[4 + a]
            S.tt(Akk[a][:], cst["sup64"][:], pA[:, 256:384], ALU.mult)
            S.tt(Ark[a][:], cst["triu64"][:], pA[:, 384:512], ALU.mult)
            S.tt(Arb[a][:], ntriu[:], psb[6][:, a * 128:(a + 1) * 128], ALU.mult)
        S.copy(Khat[:], pX[:, 256:384], eng="act")
        S.copy(Bhat[:], pX[:, 384:512], eng="act")
        cur = 0

        def stA(lvl, cur):
            for a in range(2):
                pA = psb[4 + a]
                if lvl < 5:
                    S.mm(pA[:, 0:128], MT[a][cur][:], Mm[a][cur][:])
                S.mm(pA[:, 128:256], Mm[a][cur][:], MT[a][cur][:])

        def stB(lvl, cur):
            nxt = 1 - cur
            for a in range(2):
                pA = psb[4 + a]
                eng = "dve" if a == 0 else "act"
                if lvl < 5:
                    S.copy(Mm[a][nxt][:], pA[:, 0:128], eng=eng)
                S.copy(MT[a][nxt][:], pA[:, 128:256], eng=eng)

        def stC(lvl, cur):
            nxt = 1 - cur
            for a in range(2):
                pA = psb[4 + a]
                S.mm(pA[:, 256:384], MT[a][nxt][:], Pb[a][cur][:])

        def stD(lvl, cur):
            nxt = 1 - cur
            for a in range(2):
                pA = psb[4 + a]
                S.tt(Pm[a][nxt][:], Pm[a][cur][:], pA[:, 256:384], ALU.add)
                if lvl < 5:
                    S.copy(Pb[a][nxt][:], Pm[a][nxt][:], eng="act")

        stA(1, 0)
        yield
        stB(1, 0)
        yield
        for lvl in range(2, 6):
            c_prev = (lvl - 2) % 2
            c_cur = (lvl - 1) % 2
            stC(lvl - 1, c_prev)
            stA(lvl, c_cur)
            yield
            stD(lvl - 1, c_prev)
            stB(lvl, c_cur)
            yield
        stC(5, 0)
        yield
        stD(5, 0)
        yield
        cur = 1
        assert cur == 1

    nbr = [0]

    def chain(it):
        g, u, tt_ = iters[it]
        bf = it % 2
        t0 = (g * 4 + tt_) * 128
        v_tok, g_tok, sb2, gC = v_tokD[bf], g_tokD[bf], sb2D[bf], gCD[bf]
        rbar, kbar, Khat, Bhat = rbarD[bf], kbarD[bf], KhatD[bf], BhatD[bf]
        Akk, Ark, Arb = AkkD[bf], ArkD[bf], ArbD[bf]
        Pf = [PmD[bf][a][1] for a in range(2)]
        H0, H1 = Hs[u][0], Hs[u][1]
        pC = psb[7]
        Hc = [H0, H1, H0]
        for c in range(2):
            rc = slice(c * 64, (c + 1) * 64)
            Hin, Hout = Hc[c], Hc[c + 1]
            for a in range(2):
                ra = slice(a * 64, (a + 1) * 64)
                S.mm(pC[rc, a * 64:(a + 1) * 64], kbar[ra, rc], Hin[ra, :], start=True, stop=False)
                S.mm(pC[rc, a * 64:(a + 1) * 64], Akk[a][rc, rc], v_tok[rc, ra], start=False, stop=True)
            yield
            S.copy(Ws[rc, :], pC[rc, 0:128])
            yield
            for a in range(2):
                ra = slice(a * 64, (a + 1) * 64)
                S.mm(pC[rc, 128 + a * 64:128 + (a + 1) * 64], Pf[a][rc, rc], Ws[rc, ra])
            yield
            S.copy(Us[rc, :], pC[rc, 128:256])
            yield
            for a in range(2):
                ra = slice(a * 64, (a + 1) * 64)
                S.mm(pC[ra, 384:448], Khat[rc, ra], v_tok[rc, ra], start=True, stop=False)
                S.mm(pC[ra, 384:448], Bhat[rc, ra], Us[rc, ra], start=False, stop=True)
            for a in range(2):
                ra = slice(a * 64, (a + 1) * 64)
                oy = slice(256 + a * 64, 256 + (a + 1) * 64)
                S.mm(pC[rc, oy], rbar[ra, rc], Hin[ra, :], start=True, stop=False)
                S.mm(pC[rc, oy], Ark[a][rc, rc], v_tok[rc, ra], start=False, stop=False)
                S.mm(pC[rc, oy], Arb[a][rc, rc], Us[rc, ra], start=False, stop=True)
            yield
            S.stt(Hout[:], Hin[:], gC[:, c:c + 1], pC[:, 384:448], ALU.mult, ALU.add)
            S.copy(ytok[rc, :], pC[rc, 256:384], eng="act")
            yield
        for a in range(2):
            S.bn_stats(st6[:, a, :], ytok[:, a * 64:(a + 1) * 64])
            S.bn_aggr(mv2[:, a, :], st6[:, a, :])
        rsqrt_eps(S, rs2[:], mv2[:, :, 1], 64e-5)
        yield
        for a in range(2):
            ra = slice(a * 64, (a + 1) * 64)
            gc = slice(u * 128 + a * 64, u * 128 + (a + 1) * 64)
            S.ts(yn[:, ra], ytok[:, ra], mv2[:, a, 0:1], rs2[:, a:a + 1], ALU.subtract, ALU.mult)
            S.tt(yn[:, ra], yn[:, ra], lngB[:, gc], ALU.mult)
            S.tt(yn[:, ra], yn[:, ra], lnbB[:, gc], ALU.add)
            S.stt(yn[:, ra], v_tok[:, ra], sb2[:, a:a + 1], yn[:, ra], ALU.mult, ALU.add)
        S.tt(brt_[:], yn[:], g_tok[:], ALU.mult)
        S.tr(pC[:, 0:128], brt_[:], ident[:])
        yield
        bb_ = brb[nbr[0] % 2]
        nbr[0] += 1
        S.copy(bb_[:], pC[:, 0:128], eng="act")
        S.dma(brT_d[1, u, :, t0:t0 + 128], bb_[:], eng="sp")

    def run_interleaved(gens):
        active = [x for x in gens if x is not None]
        while active:
            for gi in list(active):
                try:
                    next(gi)
                except StopIteration:
                    active.remove(gi)

    n_it = len(iters)
    pipelined = DBG.get("rw_pipe", True)
    if pipelined:
        for k in range(n_it + 1):
            run_interleaved([setup(k) if k < n_it else None, chain(k - 1) if k >= 1 else None])
    else:
        for k in range(n_it):
            run_interleaved([setup(k)])
            run_interleaved([chain(k)])
    P.close()


_CACHE = {}


def kernel(**inputs):
    if "kb" not in _CACHE:
        _CACHE["kb"] = build()
    kb = _CACHE["kb"]
    consts = make_consts()
    shared = {}
    for n, shp in PARAMS:
        if n == "c":
            continue
        shared[n] = np.ascontiguousarray(np.asarray(inputs[n], dtype=np.float32))
    for n, v in consts.items():
        shared["k_" + n] = v
    x = np.asarray(inputs["x"], dtype=np.float32)
    c = np.asarray(inputs["c"], dtype=np.float32)
    in_maps = []
    for b in range(8):
        m = dict(shared)
        m["x"] = np.ascontiguousarray(x[b])
        m["c"] = np.ascontiguousarray(c[b:b + 1])
        in_maps.append(m)
    res = run_bass_kernel_spmd(kb.nc, in_maps, core_ids=list(range(8)))
    return np.stack([np.asarray(r["out"], dtype=np.float32) for r in res.results], axis=0)
```

```python
import math
from contextlib import ExitStack

import numpy as np
import concourse.bass as bass
import concourse.mybir as mybir
from concourse.bass_utils import run_bass_kernel_spmd

F32 = mybir.dt.float32
BF16 = mybir.dt.bfloat16
AF = mybir.ActivationFunctionType
ALU = mybir.AluOpType
AX = mybir.AxisListType

ENGS = ("pe", "act", "dve", "pool", "sp")


class _Op:
    __slots__ = ("eng", "fn", "dma", "waits", "key", "val", "know", "inc")


def _region(ap):
    shape = list(ap.tensor.shape)
    off = int(ap.offset)
    if str(ap.space) == "DRAM":
        ext = 0
        for step, cnt in ap.ap:
            ext += (int(cnt) - 1) * abs(int(step))
        return (ap.name, 0, 1, off, off + ext + 1)
    if str(ap.space) == "PSUM":
        return (ap.name, 0, 128, 0, 1 << 30)
    ps = 1
    for s in shape[1:]:
        ps *= int(s)
    p0 = off // ps
    f0 = off % ps
    npart = 1
    ext = 0
    for step, cnt in ap.ap:
        step = int(step)
        cnt = int(cnt)
        if step == ps:
            npart = max(npart, cnt)
        elif step > ps:
            npart = max(npart, (cnt - 1) * (step // ps) + 1)
        else:
            ext += (cnt - 1) * step
    return (ap.name, p0, p0 + npart, f0, f0 + ext + 1)


class Sched:
    def __init__(self, nc, n_dma_sems=12):
        self.nc = nc
        self.streams = {e: [] for e in ENGS}
        self.clock = {e: {} for e in ENGS}
        self.cpos = {e: 0 for e in ENGS}
        self.last_op = {e: None for e in ENGS}
        self.n_dma_sems = n_dma_sems
        self.dma_uses = {}
        self.dma_next = {e: 0 for e in ENGS}
        self.dma_last = {}
        self.acc = {}
        self.readonly = set()
        self.pe_bank = {}
        self.epoch = 0
        self.n_ops = 0

    def _add(self, eng, fn, reads, writes, dma, force=()):
        op = _Op()
        op.eng, op.fn, op.dma = eng, fn, dma
        clk = self.clock[eng]
        need = {}

        def dep(d, forced=False):
            if (not forced) and (not dma) and (not d.dma) and d.eng == "pe" and eng == "pe":
                return
            if clk.get(d.key, 0) >= d.val:
                return
            if need.get(d.key, 0) < d.val:
                need[d.key] = d.val
            for k, v in d.know.items():
                if clk.get(k, 0) < v:
                    clk[k] = v

        for d_ in force:
            dep(d_, True)
        regs = []
        for ap in reads:
            if ap.name in self.readonly:
                continue
            regs.append((_region(ap), False))
        for ap in writes:
            regs.append((_region(ap), True))
        for (name, p0, p1, f0, f1), isw in regs:
            lst = self.acc.get(name)
            if lst is None:
                continue
            for r in lst:
                if r[4] is op:
                    continue
                if r[0] < p1 and p0 < r[1] and r[2] < f1 and f0 < r[3]:
                    if isw or r[5] or (f1 == (1 << 30) and r[4].eng != eng):
                        dep(r[4])
        if dma:
            slot = self.dma_next[eng]
            self.dma_next[eng] = (slot + 1) % self.n_dma_sems
            k = ("s", eng, slot)
            prev = self.dma_last.get(k)
            if prev is not None:
                dep(prev)
            cnt = self.dma_uses.get(k, 0) + 1
            self.dma_uses[k] = cnt
            op.key, op.val, op.inc = k, 16 * cnt, 16
            self.dma_last[k] = op
        else:
            self.cpos[eng] += 1
            op.key, op.val, op.inc = ("e", eng, self.epoch), self.cpos[eng], 1
        for k, v in need.items():
            if clk.get(k, 0) < v:
                clk[k] = v
        op.waits = list(need.items())
        op.know = dict(clk)
        self.streams[eng].append(op)
        self.last_op[eng] = op
        for (name, p0, p1, f0, f1), isw in regs:
            lst = self.acc.setdefault(name, [])
            if isw:
                lst[:] = [r for r in lst if not (p0 <= r[0] and r[1] <= p1 and f0 <= r[2] and r[3] <= f1)]
            else:
                if not dma:
                    lst[:] = [r for r in lst if not ((not r[5]) and (not r[4].dma) and r[4].eng == eng
                                                     and r[0] == p0 and r[1] == p1 and r[2] == f0 and r[3] == f1)]
            lst.append([p0, p1, f0, f1, op, isw])
        self.n_ops += 1
        return op

    def barrier(self):
        targets = []
        for e in ENGS:
            if self.cpos[e] > 0:
                targets.append((("e", e, self.epoch), self.cpos[e], self.last_op[e]))
        for k, o in self.dma_last.items():
            targets.append((k, o.val, o))
        for e in ENGS:
            clk = self.clock[e]
            op = _Op()
            op.eng, op.fn, op.dma = e, None, False
            need = {}
            for k, v, o in targets:
                if clk.get(k, 0) < v:
                    need[k] = v
                    clk[k] = v
            op.waits = list(need.items())
            op.key = None
            op.know = dict(clk)
            self.streams[e].append(op)
        self.acc = {}
        self.pe_bank = {}
        if max(self.cpos.values()) > 16000:
            self.epoch += 1
            self.cpos = {e: 0 for e in ENGS}

    def _pe_bank(self, out, lhsT):
        kpos = (int(lhsT.base_partition()), int(lhsT.partition_size()),
                int(out.base_partition()), int(out.partition_size()))
        prev = self.pe_bank.get(out.name)
        force = ()
        if prev is not None and prev[0] != kpos:
            force = (prev[1],)
        return kpos, force

    def mm(self, out, lhsT, rhs, start=True, stop=True):
        rd = [lhsT, rhs] + ([] if start else [out])
        kpos, force = self._pe_bank(out, lhsT)
        op = self._add("pe", lambda e: e.matmul(out, lhsT, rhs, start=start, stop=stop), rd, [out], False, force=force)
        self.pe_bank[out.name] = (kpos, op)
        return op

    def tr(self, out, in_, ident):
        kpos, force = self._pe_bank(out, in_)
        op = self._add("pe", lambda e: e.transpose(out, in_, ident), [in_, ident], [out], False, force=force)
        self.pe_bank[out.name] = (kpos, op)
        return op

    def act(self, out, in_, func, bias=None, scale=None, accum_out=None):
        rd = [in_]
        kw = {}
        if bias is not None:
            kw["bias"] = bias
            if not isinstance(bias, (int, float)):
                rd.append(bias)
        if scale is not None:
            kw["scale"] = scale
            if not isinstance(scale, (int, float)):
                rd.append(scale)
        wr = [out]
        if accum_out is not None:
            kw["accum_out"] = accum_out
            wr.append(accum_out)
        return self._add("act", lambda e: e.activation(out, in_, func, **kw), rd, wr, False)

    def tt(self, out, in0, in1, op, eng="dve"):
        return self._add(eng, lambda e: e.tensor_tensor(out, in0, in1, op), [in0, in1], [out], False)

    def ts(self, out, in0, s1, s2, op0, op1=None, eng="dve", accum_out=None):
        rd = [in0]
        if not isinstance(s1, (int, float)):
            rd.append(s1)
        if s2 is not None and not isinstance(s2, (int, float)):
            rd.append(s2)
        wr = [out]
        kw = {}
        if accum_out is not None:
            kw["accum_out"] = accum_out
            wr.append(accum_out)
        if op1 is None:
            return self._add(eng, lambda e: e.tensor_scalar(out, in0, s1, None, op0, **kw), rd, wr, False)
        return self._add(eng, lambda e: e.tensor_scalar(out, in0, s1, s2, op0, op1, **kw), rd, wr, False)

    def stt(self, out, in0, scalar, in1, op0, op1):
        rd = [in0, in1]
        if not isinstance(scalar, (int, float)):
            rd.append(scalar)
        return self._add("dve", lambda e: e.scalar_tensor_tensor(out, in0, scalar, in1, op0, op1), rd, [out], False)

    def copy(self, out, in_, eng="dve"):
        if eng == "act":
            return self._add("act", lambda e: e.copy(out, in_), [in_], [out], False)
        return self._add(eng, lambda e: e.tensor_copy(out, in_), [in_], [out], False)

    def memset(self, out, val, eng="dve"):
        return self._add(eng, lambda e: e.memset(out, val), [], [out], False)

    def reduce(self, out, in_, op, axis=AX.X, eng="dve"):
        return self._add(eng, lambda e: e.tensor_reduce(out, in_, axis, op), [in_], [out], False)

    def bn_stats(self, out, in_):
        return self._add("dve", lambda e: e.bn_stats(out, in_), [in_], [out], False)

    def bn_aggr(self, out, in_):
        return self._add("dve", lambda e: e.bn_aggr(out, in_), [in_], [out], False)

    def recip(self, out, in_):
        return self._add("dve", lambda e: e.reciprocal(out, in_), [in_], [out], False)

    def dma(self, out, in_, eng="sp", **kw):
        return self._add(eng, lambda e: e.dma_start(out=out, in_=in_, **kw), [in_], [out], True)

    def emit(self):
        nc = self.nc
        with ExitStack() as es:
            sems = {}
            for e in ENGS:
                for op in self.streams[e]:
                    if op.fn is not None and (not op.dma) and op.key not in sems:
                        sems[op.key] = es.enter_context(nc.semaphore("c_%s_%d" % (op.key[1], op.key[2])))
            for k in self.dma_uses:
                sems[k] = es.enter_context(nc.semaphore("d_%s_%d" % (k[1], k[2])))
            final = [(k, o.val) for k, o in self.dma_last.items()]
            for e in ENGS:
                if self.cpos[e] > 0:
                    final.append((("e", e, self.epoch), self.cpos[e]))
            block = es.enter_context(nc.Block())
            streams = self.streams

            def run(engname, e):
                for op in streams[engname]:
                    for k, v in op.waits:
                        e.wait_ge(sems[k], v)
                    if op.fn is not None:
                        op.fn(e).then_inc(sems[op.key], op.inc)
                if engname == "sp":
                    for k, v in final:
                        e.wait_ge(sems[k], v)

            @block.tensor
            def _(e):
                run("pe", e)

            @block.scalar
            def _(e):
                run("act", e)

            @block.vector
            def _(e):
                run("dve", e)

            @block.gpsimd
            def _(e):
                run("pool", e)

            @block.sync
            def _(e):
                run("sp", e)


DBG = {}
T = 4096
D = 1024
NT = 32
DEPTH = 2
NCOLS = 6936
GLA_OFF, RWKV_OFF, FOX_OFF, HGRN_OFF = 0, 1552, 3344, 4888
DN_ALPHA = (2.0 * DEPTH) ** 0.25
NE = 16


def make_consts():
    c = {}
    i = np.arange(128)
    same = (i[:, None] // 64) == (i[None, :] // 64)
    c["ident"] = np.eye(128, dtype=np.float32)
    c["ones"] = np.ones((128, 128), np.float32)
    c["triu"] = (i[:, None] <= i[None, :]).astype(np.float32)
    c["triu64"] = ((i[:, None] <= i[None, :]) & same).astype(np.float32)
    c["sup64"] = ((i[:, None] < i[None, :]) & same).astype(np.float32)
    c["slo64"] = ((i[:, None] > i[None, :]) & same).astype(np.float32)
    c["blk64"] = same.astype(np.float32)
    return c


CONST_NAMES = ["ident", "ones", "triu", "triu64", "sup64", "slo64", "blk64"]

PARAMS = [
    ("c", [1, D]), ("ada_w", [2, D, 6 * D]), ("ada_b", [2, 6, D]), ("w_in", [2, D, NCOLS]),
    ("gla_alpha_up", [2, 16, 256]), ("gla_alpha_b", [2, 256]), ("gla_norm_g", [2, 128]),
    ("rwkv_mu", [2, 1792]), ("rwkv_w0", [2, 512]), ("rwkv_w2", [2, 64, 512]), ("rwkv_a0", [2, 512]),
    ("rwkv_a2", [2, 64, 512]), ("rwkv_g2", [2, 128, 512]), ("rwkv_k_k", [2, 512]), ("rwkv_k_a", [2, 512]),
    ("rwkv_r_k", [2, 8, 64]), ("rwkv_ln_g", [2, 512]), ("rwkv_ln_b", [2, 512]), ("fox_f_bias", [2, 8]),
    ("hgrn_lb_logits", [2, 512]), ("hgrn_norm_g", [2, 128]), ("w_br", [2, 4, 512, D]),
    ("w_gate", [2, 4, D, D]), ("b_gate", [2, 4, D]), ("w_o", [2, D, D]), ("ln1_g", [2, D]), ("ln1_b", [2, D]),
    ("router_w", [D, NE]), ("router_b", [NE]), ("exp_w_gate", [2, NE, D, 512]), ("exp_w_up", [2, NE, D, 512]),
    ("exp_w_down", [2, NE, 512, D]), ("ln2_g", [2, D]), ("ln2_b", [2, D]),
]


class KB:
    def __init__(self, io=None):
        self.nc = bass.Bass("TRN2", target_bir_lowering=False)
        self.S = Sched(self.nc)
        self.io = io or {}
        self.d = {}
        nc = self.nc
        self.x = self.ext_in("x", [T, D], F32)
        for n, shp in PARAMS:
            self.d[n] = self.ext_in(n, shp, F32)
        self.cst_d = {n: self.ext_in("k_" + n, [128, 128], F32) for n in CONST_NAMES}
        self.psb = [nc.alloc_psum_tensor("psb%d" % i, [128, 512], F32) for i in range(8)]
        self.cst = {n: nc.alloc_sbuf_tensor("c_" + n, [128, 128], F32) for n in CONST_NAMES}
        for n in CONST_NAMES:
            self.S.dma(self.cst[n][:], self.cst_d[n])
        self.hT = None

    def ext_in(self, name, shape, dt):
        self.S.readonly.add(name)
        return self.nc.dram_tensor(name, list(shape), dt, kind="ExternalInput").ap()

    def dram(self, name, shape, dt):
        role = self.io.get(name)
        if role == "in":
            return self.nc.dram_tensor(name, list(shape), dt, kind="ExternalInput").ap()
        if role == "out" or name == "out":
            return self.nc.dram_tensor(name, list(shape), dt, kind="ExternalOutput").ap()
        return self.nc.dram_tensor(name, list(shape), dt).ap()


class Pool_:
    def __init__(self, kb):
        self.kb = kb
        self.es = ExitStack()

    _uid = [0]

    def sb(self, name, shape, dt=F32):
        Pool_._uid[0] += 1
        return self.es.enter_context(self.kb.nc.sbuf_tensor("%s_u%d" % (name, Pool_._uid[0]), list(shape), dt))

    def close(self):
        self.kb.S.barrier()
        self.es.close()


def phase_mod(kb, mod_d):
    S = kb.S
    P = Pool_(kb)
    condT = P.sb("condT", [128, 8])
    load_T(kb, P, condT[:], kb.d["c"].rearrange("o (c p) -> (o c) p", p=128), 8)
    S.act(condT[:], condT[:], AF.Silu)
    wst = [P.sb("adaw%d" % k, [128, 3072]) for k in range(2)]
    mrow = P.sb("mrow", [1, 6144])
    brow = P.sb("brow", [1, 6144])
    n = 0
    for i in range(2):
        S.dma(brow[:], kb.d["ada_b"][i:i + 1].rearrange("o j d -> o (j d)"))
        for half in range(2):
            for kc in range(8):
                w = wst[n % 2]
                n += 1
                S.dma(w[:], kb.d["ada_w"][i, kc * 128:(kc + 1) * 128, half * 3072:(half + 1) * 3072],
                      eng="sp" if n % 2 else "pool")
                for b in range(6):
                    S.mm(kb.psb[b][0:1, :], condT[:, kc:kc + 1], w[:, b * 512:(b + 1) * 512],
                         start=(kc == 0), stop=(kc == 7))
            for b in range(6):
                o = half * 3072 + b * 512
                S.tt(mrow[0:1, o:o + 512], kb.psb[b][0:1, :], brow[0:1, o:o + 512], ALU.add)
        for j in (1, 4):
            S.ts(mrow[0:1, j * 1024:(j + 1) * 1024], mrow[0:1, j * 1024:(j + 1) * 1024], 1.0, None, ALU.add)
        S.dma(mod_d[i:i + 1, :], mrow[:])
    P.close()


def rsqrt_eps(S, out, in_, eps, scale=1.0):
    S.act(out, in_, AF.Ln, bias=float(eps), scale=float(scale))
    S.act(out, out, AF.Exp, scale=-0.5)


def ln_stats(S, xin, st, mv, rstd, eps=1e-5):
    S.bn_stats(st[:, 0:6], xin[:, 0:512])
    S.bn_stats(st[:, 6:12], xin[:, 512:1024])
    S.bn_aggr(mv[:], st[:])
    rsqrt_eps(S, rstd[:], mv[:, 1:2], eps)


def load_T(kb, P, dst, src_rows, n, psum=None):
    S = kb.S
    tmp = P.sb("ldT_tmp", [n, 128])
    S.dma(tmp[:], src_rows)
    ps = kb.psb[7] if psum is None else psum
    S.mm(ps[:, 0:n], tmp[:], kb.cst["ident"][0:n, 0:n], start=True, stop=True)
    S.copy(dst, ps[:, 0:n])


def load_modT(kb, P, mod_d, layer, name):
    modT = P.sb(name, [128, 6, 8])
    load_T(kb, P, modT[:].rearrange("p j c -> p (j c)"), mod_d[layer].rearrange("(r p) -> r p", p=128), 48)
    return modT


def phase_ln_mixer(kb, x_src, mod_d, layer):
    S = kb.S
    P = Pool_(kb)
    modT = load_modT(kb, P, mod_d, layer, "modT_a")
    xb = [P.sb("lnx%d" % k, [128, 1024]) for k in range(2)]
    st = [P.sb("lnst%d" % k, [128, 12]) for k in range(2)]
    mv = [P.sb("lnmv%d" % k, [128, 2]) for k in range(2)]
    rs = [P.sb("lnrs%d" % k, [128, 1]) for k in range(2)]
    ident = kb.cst["ident"]
    for t in range(DBG.get("ln_nt", NT)):
        k = t % 2
        xin = xb[k]
        S.dma(xin[:], x_src[t * 128:(t + 1) * 128, :])
        if DBG.get("ln_lvl", 9) < 1:
            continue
        ln_stats(S, xin, st[k], mv[k], rs[k])
        S.ts(xin[:], xin[:], mv[k][:, 0:1], rs[k][:, 0:1], ALU.subtract, ALU.mult)
        if DBG.get("ln_lvl", 9) < 2:
            continue
        for c in range(8):
            pb = kb.psb[(t % 2) * 2 + c // 4]
            S.tr(pb[:, (c % 4) * 128:(c % 4 + 1) * 128], xin[:, c * 128:(c + 1) * 128], ident[:])
        if DBG.get("ln_lvl", 9) < 3:
            continue
        for c in range(DBG.get("ln_nc", 8)):
            pb = kb.psb[(t % 2) * 2 + c // 4]
            src = pb[:, (c % 4) * 128:(c % 4 + 1) * 128]
            off = DBG.get("ln_off", 1)
            dst = kb.hT[:, c, off + t * 128:off + (t + 1) * 128]
            ev = DBG.get("ln_evac", "both")
            if (c % 2 == 0 and ev == "both") or ev == "dve":
                S.ts(dst, src, modT[:, 1, c:c + 1], modT[:, 0, c:c + 1], ALU.mult, ALU.add)
            else:
                S.act(dst, src, AF.Identity, bias=modT[:, 0, c:c + 1], scale=modT[:, 1, c:c + 1])
    P.close()


def prep_cast(kb, P, jobs, stg, stb):
    S = kb.S
    n = 0
    for dst, src in jobs:
        R, N = src.shape
        for r in range(0, R, 128):
            a, b = stg[n % 2], stb[n % 2]
            n += 1
            S.dma(a[:, 0:N], src[r:r + 128, :], eng="sp")
            S.copy(b[:, 0:N], a[:, 0:N], eng="pool" if n % 2 else "act")
            S.dma(dst[r:r + 128, :], b[:, 0:N], eng="pool")


class Epi:
    def __init__(self, kb, P, mod_d, layer, gt_idx, g_name, b_name, tag, with_z=True):
        S = kb.S
        self.kb = kb
        self.gt = P.sb("epi_gt" + tag, [128, 1024])
        self.g = P.sb("epi_g" + tag, [128, 1024])
        self.b = P.sb("epi_b" + tag, [128, 1024])
        S.dma(self.gt[:], mod_d[layer:layer + 1, gt_idx * 1024:(gt_idx + 1) * 1024].partition_broadcast(128))
        S.dma(self.g[:], kb.d[g_name][layer:layer + 1, :].partition_broadcast(128))
        S.dma(self.b[:], kb.d[b_name][layer:layer + 1, :].partition_broadcast(128))
        self.xb = [P.sb("epi_x%s%d" % (tag, k), [128, 1024]) for k in range(2)]
        self.zb = [P.sb("epi_z%s%d" % (tag, k), [128, 1024]) for k in range(2)] if with_z else None
        self.st = [P.sb("epi_st%s%d" % (tag, k), [128, 12]) for k in range(2)]
        self.mv = [P.sb("epi_mv%s%d" % (tag, k), [128, 2]) for k in range(2)]
        self.rs = [P.sb("epi_rs%s%d" % (tag, k), [128, 1]) for k in range(2)]
        self.n = 0

    def prefetch_x(self, x_src, t):
        k = self.n % 2
        self.kb.S.dma(self.xb[k][:], x_src[t * 128:(t + 1) * 128, :])

    def run(self, y_halves, x_dst, t, x_src=None, z=None):
        S = self.kb.S
        k = self.n % 2
        self.n += 1
        if x_src is not None:
            S.dma(self.xb[k][:], x_src[t * 128:(t + 1) * 128, :])
        x = self.xb[k]
        if z is None:
            z = self.zb[k]
        for h in range(2):
            S.tt(z[:, h * 512:(h + 1) * 512], y_halves[h], self.gt[:, h * 512:(h + 1) * 512], ALU.mult)
        S.stt(z[:], x[:], DN_ALPHA, z[:], ALU.mult, ALU.add)
        ln_stats(S, z, self.st[k], self.mv[k], self.rs[k])
        S.ts(z[:], z[:], self.mv[k][:, 0:1], self.rs[k][:, 0:1], ALU.subtract, ALU.mult)
        S.tt(z[:], z[:], self.g[:], ALU.mult, eng="pool")
        S.tt(z[:], z[:], self.b[:], ALU.add, eng="pool")
        S.dma(x_dst[t * 128:(t + 1) * 128, :], z[:], eng="pool")


def phase_prep_merge(kb, layer, wg_d, wb_d, wo_d):
    P = Pool_(kb)
    stg = [P.sb("pst%d" % k, [128, 1024]) for k in range(2)]
    stb = [P.sb("psb%d" % k, [128, 1024], BF16) for k in range(2)]
    jobs = []
    for n in range(4):
        jobs.append((wg_d[n], kb.d["w_gate"][layer, n]))
        jobs.append((wb_d[n], kb.d["w_br"][layer, n]))
    jobs.append((wo_d, kb.d["w_o"][layer]))
    prep_cast(kb, P, jobs, stg, stb)
    P.close()


def phase_merge(kb, layer, mod_d, brT_d, wg_d, wb_d, wo_d, x_src, x_dst):
    S = kb.S
    P = Pool_(kb)
    epi = Epi(kb, P, mod_d, layer, 2, "ln1_g", "ln1_b", "m")
    bgT = P.sb("bgT", [128, 4, 8])
    load_T(kb, P, bgT[:].rearrange("p n c -> p (n c)"), kb.d["b_gate"][layer].rearrange("n (c p) -> (n c) p", p=128), 32)
    wo = P.sb("wo", [128, 8, 1024], BF16)
    S.dma(wo[:], wo_d.rearrange("(c p) n -> p c n", p=128))
    wg = [P.sb("wg%d" % k, [128, 8, 1024], BF16) for k in range(2)]
    wb = [P.sb("wb%d" % k, [128, 4, 1024], BF16) for k in range(2)]
    brt = [P.sb("brt%d" % k, [128, 4, 512], BF16) for k in range(2)]
    mT = P.sb("mT", [128, 8, 512])
    mTb = P.sb("mTb", [128, 8, 512], BF16)
    sig = [P.sb("sig%d" % k, [128, 512]) for k in range(2)]
    tmp = [P.sb("mtmp%d" % k, [128, 512]) for k in range(2)]
    cnt = 0
    q = 0
    for g in range(8):
        tok = slice(g * 512, (g + 1) * 512)
        for n in range(4):
            k = cnt % 2
            cnt += 1
            S.dma(wg[k][:], wg_d[n].rearrange("(c p) n -> p c n", p=128), eng="sp")
            S.dma(wb[k][:], wb_d[n].rearrange("(c p) n -> p c n", p=128), eng="sp")
            S.dma(brt[k][:], brT_d[n, :, :, tok].rearrange("c p t -> p c t"), eng="sp")
            for cc in range(8):
                pa = kb.psb[(q % 2) * 2]
                pb = kb.psb[(q % 2) * 2 + 1]
                for kc in range(8):
                    S.mm(pa[:], wg[k][:, kc, cc * 128:(cc + 1) * 128], kb.hT[:, kc, 1 + g * 512:1 + (g + 1) * 512],
                         start=(kc == 0), stop=(kc == 7))
                for kc in range(4):
                    S.mm(pb[:], wb[k][:, kc, cc * 128:(cc + 1) * 128], brt[k][:, kc, :],
                         start=(kc == 0), stop=(kc == 3))
                sg = sig[q % 2]
                S.act(sg[:], pa[:], AF.Sigmoid, bias=bgT[:, n, cc:cc + 1])
                if n == 0:
                    S.tt(mT[:, cc, :], sg[:], pb[:], ALU.mult)
                else:
                    tp = tmp[q % 2]
                    S.tt(tp[:], sg[:], pb[:], ALU.mult)
                    S.tt(mT[:, cc, :], mT[:, cc, :], tp[:], ALU.add, eng="pool" if cc % 2 else "dve")
                q += 1
        for cc in range(8):
            S.copy(mTb[:, cc, :], mT[:, cc, :], eng="act" if cc % 2 else "pool")
        for tt in range(4):
            t = g * 4 + tt
            epi.prefetch_x(x_src, t)
            ys = []
            for h in range(2):
                py = kb.psb[4 + (t % 2) * 2 + h]
                for kc in range(8):
                    S.mm(py[:], mTb[:, kc, tt * 128:(tt + 1) * 128], wo[:, kc, h * 512:(h + 1) * 512],
                         start=(kc == 0), stop=(kc == 7))
                ys.append(py[:])
            epi.run(ys, x_dst, t)
    P.close()


def phase_moe(kb, layer, mod_d, x_src, x_dst):
    S = kb.S
    P = Pool_(kb)
    NSG = 2
    TSG = T // NSG
    NTS = TSG // 128
    epi = Epi(kb, P, mod_d, layer, 5, "ln2_g", "ln2_b", "e", with_z=False)
    modT = load_modT(kb, P, mod_d, layer, "modT_e")
    rw = P.sb("rw", [128, 8, NE])
    S.dma(rw[:], kb.d["router_w"].rearrange("(c p) e -> p c e", p=128))
    rb = P.sb("rb", [128, NE])
    S.dma(rb[:], kb.d["router_b"].rearrange("(o e) -> o e", o=1).partition_broadcast(128))
    hT = P.sb("hTm", [128, 8, TSG + 1], BF16)
    yacc = P.sb("yacc", [128, NTS, 1024])
    comb = P.sb("comb", [128, NTS, NE])
    h32 = [P.sb("h32_%d" % k, [128, 8, 128]) for k in range(2)]
    st = [P.sb("mst%d" % k, [128, 12]) for k in range(2)]
    mv = [P.sb("mmv%d" % k, [128, 2]) for k in range(2)]
    rs = [P.sb("mrs%d" % k, [128, 1]) for k in range(2)]
    lg = P.sb("r_lg", [128, NE])
    pr = P.sb("r_pr", [128, NE])
    sel = P.sb("r_sel", [128, NE])
    sel2 = P.sb("r_sel2", [128, NE])
    eq = P.sb("r_eq", [128, NE])
    m1 = P.sb("r_m1", [128, 4])
    m2 = P.sb("r_m2", [128, 4])
    gs = P.sb("r_gs", [128, 4])
    gm = P.sb("r_gm", [128, 1])
    og = P.sb("r_og", [128, 4])
    thr = P.sb("r_thr", [128, 4])
    msk = P.sb("r_msk", [128, NE])
    sm = P.sb("r_sm", [128, 1])
    mx = P.sb("r_mx", [128, 1])
    wg = [P.sb("ewg%d" % k, [128, 8, 512], BF16) for k in range(2)]
    wu = [P.sb("ewu%d" % k, [128, 8, 512], BF16) for k in range(2)]
    wd = [P.sb("ewd%d" % k, [128, 4, 1024], BF16) for k in range(2)]
    stg = [P.sb("estg%d" % k, [128, 2, 512]) for k in range(3)]
    heT = [P.sb("heT%d" % k, [128, 4, 512], BF16) for k in range(2)]
    sl = [P.sb("esl%d" % k, [128, 512]) for k in range(2)]
    ident = kb.cst["ident"]
    nld = [0]

    def load_expert(e, k):
        for (dst, src, kcn) in ((wg[k], kb.d["exp_w_gate"][layer, e], 8), (wu[k], kb.d["exp_w_up"][layer, e], 8)):
            sv = src.rearrange("(c p) n -> p c n", p=128)
            for c2 in range(0, kcn, 2):
                sg_ = stg[nld[0] % 3]
                nld[0] += 1
                S.dma(sg_[:], sv[:, c2:c2 + 2, :], eng="sp")
                S.copy(dst[:, c2:c2 + 2, :], sg_[:], eng="pool")
        sv = kb.d["exp_w_down"][layer, e].rearrange("(c p) n -> p c n", p=128)
        for c in range(4):
            sg_ = stg[nld[0] % 3]
            nld[0] += 1
            S.dma(sg_[:].rearrange("p a b -> p (a b)"), sv[:, c, :], eng="sp")
            S.copy(wd[k][:, c, :], sg_[:].rearrange("p a b -> p (a b)"), eng="pool")

    for sgi in range(NSG):
        t0 = sgi * NTS
        for tl in range(NTS):
            t = t0 + tl
            k = tl % 2
            xin = epi.xb[k]
            S.dma(xin[:], x_src[t * 128:(t + 1) * 128, :])
            ln_stats(S, xin, st[k], mv[k], rs[k])
            S.ts(xin[:], xin[:], mv[k][:, 0:1], rs[k][:, 0:1], ALU.subtract, ALU.mult)
            for c in range(8):
                pb = kb.psb[k * 2 + c // 4]
                S.tr(pb[:, (c % 4) * 128:(c % 4 + 1) * 128], xin[:, c * 128:(c + 1) * 128], ident[:])
            for c in range(8):
                pb = kb.psb[k * 2 + c // 4]
                src = pb[:, (c % 4) * 128:(c % 4 + 1) * 128]
                if c % 2 == 0:
                    S.ts(h32[k][:, c, :], src, modT[:, 4, c:c + 1], modT[:, 3, c:c + 1], ALU.mult, ALU.add)
                else:
                    S.act(h32[k][:, c, :], src, AF.Identity, bias=modT[:, 3, c:c + 1], scale=modT[:, 4, c:c + 1])
                S.copy(hT[:, c, 1 + tl * 128:1 + (tl + 1) * 128], h32[k][:, c, :], eng="pool")
            pl = kb.psb[4 + k]
            for c in range(8):
                S.mm(pl[:, 0:NE], h32[k][:, c, :], rw[:, c, :], start=(c == 0), stop=(c == 7))
            S.copy(lg[:], pl[:, 0:NE])
            S.reduce(mx[:], lg[:], ALU.max)
            S.ts(mx[:], mx[:], -1.0, None, ALU.mult)
            S.act(pr[:], lg[:], AF.Exp, bias=mx[:, 0:1], scale=1.0, accum_out=sm[:])
            S.recip(sm[:], sm[:])
            S.ts(pr[:], pr[:], sm[:, 0:1], None, ALU.mult)
            S.tt(sel[:], pr[:], rb[:], ALU.add)
            sel3 = sel[:].rearrange("p (g e) -> p g e", g=4)
            S.reduce(m1[:], sel3, ALU.max)
            S.tt(eq[:].rearrange("p (g e) -> p g e", g=4), sel3, m1[:].unsqueeze(2).to_broadcast([128, 4, 4]), ALU.is_ge)
            S.stt(sel2[:], eq[:], -1e9, sel[:], ALU.mult, ALU.add)
            S.reduce(m2[:], sel2[:].rearrange("p (g e) -> p g e", g=4), ALU.max)
            S.tt(gs[:], m1[:], m2[:], ALU.add)
            S.reduce(gm[:], gs[:], ALU.max)
            S.ts(og[:], gs[:], gm[:, 0:1], None, ALU.is_ge)
            S.ts(thr[:], og[:], -1e9, 1e9, ALU.mult, ALU.add)
            S.tt(thr[:], thr[:], m2[:], ALU.add)
            S.tt(msk[:].rearrange("p (g e) -> p g e", g=4), sel3, thr[:].unsqueeze(2).to_broadcast([128, 4, 4]), ALU.is_ge)
            S.tt(msk[:], msk[:], pr[:], ALU.mult)
            S.reduce(sm[:], msk[:], ALU.add)
            S.recip(sm[:], sm[:])
            S.ts(comb[:, tl, :], msk[:], sm[:, 0:1], None, ALU.mult)
        if sgi == 0:
            load_expert(0, 0)
        q = 0
        for e in range(NE):
            k = (sgi * NE + e) % 2
            nxt = sgi * NE + e + 1
            if nxt < NSG * NE:
                load_expert(nxt % NE, nxt % 2)
            for gq in range(NTS // 4):
                he = heT[gq % 2]
                for fc in range(4):
                    pg = kb.psb[(q % 2) * 2]
                    pu = kb.psb[(q % 2) * 2 + 1]
                    for kc in range(8):
                        S.mm(pg[:], wg[k][:, kc, fc * 128:(fc + 1) * 128], hT[:, kc, 1 + gq * 512:1 + (gq + 1) * 512],
                             start=(kc == 0), stop=(kc == 7))
                    for kc in range(8):
                        S.mm(pu[:], wu[k][:, kc, fc * 128:(fc + 1) * 128], hT[:, kc, 1 + gq * 512:1 + (gq + 1) * 512],
                             start=(kc == 0), stop=(kc == 7))
                    s_ = sl[q % 2]
                    S.act(s_[:], pg[:], AF.Silu)
                    S.tt(he[:, fc, :], s_[:], pu[:], ALU.mult)
                    q += 1
                for tt in range(4):
                    tl = gq * 4 + tt
                    for h in range(2):
                        py = kb.psb[4 + (tl * 2 + h) % 4]
                        for fc in range(4):
                            S.mm(py[:], he[:, fc, tt * 128:(tt + 1) * 128], wd[k][:, fc, h * 512:(h + 1) * 512],
                                 start=(fc == 0), stop=(fc == 3))
                        ya = yacc[:, tl, h * 512:(h + 1) * 512]
                        if e == 0:
                            S.ts(ya, py[:], comb[:, tl, e:e + 1], None, ALU.mult)
                        else:
                            S.stt(ya, py[:], comb[:, tl, e:e + 1], ya, ALU.mult, ALU.add)
        for tl in range(NTS):
            t = t0 + tl
            epi.run([yacc[:, tl, 0:512], yacc[:, tl, 512:1024]], x_dst, t, x_src=x_src, z=yacc[:, tl, :])
    P.close()


def build(io=None, layers=(0, 1), stages=("mod", "ln", "prep", "br", "merge", "moe"), last_out=None):
    kb = KB(io)
    S = kb.S
    mod_d = kb.dram("mod_d", [2, 6144], F32)
    xa = kb.dram("xa", [T, D], F32)
    xbd = kb.dram("xbd", [T, D], F32)
    out = kb.dram("out", [T, D], F32)
    brT_d = kb.dram("brT", [4, 4, 128, T], BF16)
    wg_d = kb.dram("wg_bf", [4, D, D], BF16)
    wb_d = kb.dram("wb_bf", [4, 512, D], BF16)
    wo_d = kb.dram("wo_bf", [D, D], BF16)
    kb.lb_d = kb.dram("lb_d", [2, 512], F32)
    if "mod" in stages:
        phase_mod(kb, mod_d)
    for layer in layers:
        x_src = kb.x if layer == 0 else xbd
        x_fin = out if layer == layers[-1] else xbd
        MP = Pool_(kb)
        kb.hT = MP.sb("hT", [128, 8, T + 1], BF16)
        for c in range(8):
            S.memset(kb.hT[:, c, 0:1], 0.0, eng="pool")
        if "ln" in stages:
            phase_ln_mixer(kb, x_src, mod_d, layer)
        if "prep" in stages:
            phase_prep_merge(kb, layer, wg_d, wb_d, wo_d)
        if "br" in stages:
            phase_branches(kb, layer, brT_d)
        if "merge" in stages:
            phase_merge(kb, layer, mod_d, brT_d, wg_d, wb_d, wo_d, x_src, xa if "moe" in stages else x_fin)
        MP.close()
        if "moe" in stages:
            phase_moe(kb, layer, mod_d, xa, x_fin)
    S.emit()
    return kb


def load_w(kb, dst, src, stg, cast_eng="pool", dma_eng="sp"):
    S = kb.S
    kc = src.shape[0] // 128
    n = src.shape[1]
    sv = stg[:, 0:kc, 0:n]
    S.dma(sv, src.rearrange("(c p) n -> p c n", p=128), eng=dma_eng)
    S.copy(dst, sv, eng=cast_eng)


def tok(t0, n=128):
    return slice(1 + t0, 1 + t0 + n)


def fox_branch(kb, layer, brT_d):
    S = kb.S
    P = Pool_(kb)
    hT = kb.hT
    W = kb.d["w_in"][layer]
    o = FOX_OFF
    psb = kb.psb
    stg = P.sb("fstg", [128, 8, 520])
    wq = P.sb("fwq", [128, 8, 512], BF16)
    wk = P.sb("fwk", [128, 8, 512], BF16)
    wvf = P.sb("fwvf", [128, 8, 520], BF16)
    load_w(kb, wq[:], W[:, o:o + 512], stg)
    load_w(kb, wk[:], W[:, o + 512:o + 1024], stg)
    load_w(kb, wvf[:], W[:, o + 1024:o + 1544], stg)
    fb = P.sb("ffb", [128, 8])
    S.dma(fb[:], kb.d["fox_f_bias"][layer:layer + 1, :].partition_broadcast(128))
    maskb = P.sb("fmask", [128, 128], BF16)
    S.copy(maskb[:], kb.cst["triu"][:])
    lf = P.sb("flf", [128, 32, 8])
    tA = P.sb("ftA", [128, 32, 8])
    tB = P.sb("ftB", [128, 32, 8])
    Fs = P.sb("fFs", [128, 32, 8])
    Cs = P.sb("fCs", [128, 32, 8])
    vp = P.sb("fvp", [128, 32, 8, 65], BF16)
    S.memset(vp[:].rearrange("p a b c -> p (a b c)"), 1.0, eng="pool")
    for g in range(8):
        pb = psb[6 + g % 2]
        for tt in range(4):
            t = g * 4 + tt
            for kc in range(8):
                S.mm(pb[:, tt * 8:(tt + 1) * 8], hT[:, kc, tok(t * 128)], wvf[:, kc, 512:520], start=(kc == 0), stop=(kc == 7))
        S.tt(lf[:, g * 4:(g + 1) * 4, :], pb[:, 0:32].rearrange("p (a b) -> p a b", a=4),
             fb[:].unsqueeze(1).to_broadcast([128, 4, 8]), ALU.add)
    lf2 = lf[:].rearrange("p a b -> p (a b)")
    S.act(lf2, lf2, AF.Exp, scale=-1.0)
    S.act(lf2, lf2, AF.Ln, bias=1.0)
    S.ts(lf2, lf2, -1.0, None, ALU.mult)
    a, b = lf, tA
    d = 1
    while d < 32:
        nb = tA if b is tA else tB
        if a is lf:
            nb = tA
        S.tt(nb[:, d:32, :], a[:, d:32, :], a[:, 0:32 - d, :], ALU.add)
        S.copy(nb[:, 0:d, :], a[:, 0:d, :])
        a = nb
        b = tB if nb is tA else tA
        d *= 2
    incl = a
    excl = tB if incl is tA else tA
    S.tt(excl[:], incl[:], lf[:], ALU.subtract)
    pF, pC = psb[6], psb[7]
    S.mm(pF[:, 0:256], kb.cst["triu"][:], lf2, start=True, stop=False)
    S.mm(pF[:, 0:256], kb.cst["ones"][:], excl[:].rearrange("p a b -> p (a b)"), start=False, stop=True)
    S.mm(pC[:, 0:256], kb.cst["ones"][:], incl[:].rearrange("p a b -> p (a b)"), start=True, stop=True)
    S.copy(Fs[:].rearrange("p a b -> p (a b)"), pF[:, 0:256])
    S.copy(Cs[:].rearrange("p a b -> p (a b)"), pC[:, 0:256], eng="act")
    for t in range(32):
        pb = psb[6 + t % 2]
        for kc in range(8):
            S.mm(pb[:], hT[:, kc, tok(t * 128)], wvf[:, kc, 0:512], start=(kc == 0), stop=(kc == 7))
        src = pb[:].rearrange("p (h d) -> p h d", h=8)
        if t % 2 == 0:
            S.copy(vp[:, t, :, 0:64], src, eng="dve")
        else:
            S.copy(vp[:, t, :, 0:64], src, eng="act")
    QT = P.sb("fQT", [128, T], BF16)
    KA = P.sb("fKA", [128, T], BF16)
    KB_ = P.sb("fKB", [128, T], BF16)
    S.memset(KA[64:128, :], 0.0, eng="pool")
    S.memset(KB_[0:64, :], 0.0, eng="pool")
    otok = P.sb("fotok", [128, 32, 128])
    brs = P.sb("fbrs", [128, T], BF16)
    pts = [P.sb("fpt%d" % k, [128, 512], BF16) for k in range(4)]
    vss = [P.sb("fvs%d" % k, [128, 65], BF16) for k in range(6)]
    biases = [P.sb("fbias%d" % k, [128, 32]) for k in range(2)]
    dms = [P.sb("fdm%d" % k, [128, 32]) for k in range(2)]
    rcs = [P.sb("frc%d" % k, [128, 1]) for k in range(2)]
    q = 0
    nb_ = 0
    nv = 0
    for p in range(4):
        for g in range(8):
            pq = psb[6]
            pk = psb[7]
            for kc in range(8):
                S.mm(pq[:], wq[:, kc, p * 128:(p + 1) * 128], hT[:, kc, tok(g * 512, 512)], start=(kc == 0), stop=(kc == 7))
            for kc in range(8):
                S.mm(pk[:], wk[:, kc, p * 128:(p + 1) * 128], hT[:, kc, tok(g * 512, 512)], start=(kc == 0), stop=(kc == 7))
            S.act(QT[:, g * 512:(g + 1) * 512], pq[:], AF.Copy, scale=0.125)
            S.copy(KA[0:64, g * 512:(g + 1) * 512], pk[0:64, :])
            S.copy(KB_[64:128, g * 512:(g + 1) * 512], pk[64:128, :])
        rows = []
        for a_ in range(2):
            for i in range(32):
                rows.append((a_, i))
        batches = []
        for ri, (a_, i) in enumerate(rows):
            for jb in range(0, i + 1, 4):
                batches.append((ri, a_, i, jb, min(4, i + 1 - jb)))
        rowbuf = {}

        def front(bt):
            nonlocal q, nb_
            ri, a_, i, jb, nbt = bt
            h = 2 * p + a_
            Kh = KA if a_ == 0 else KB_
            if jb == 0:
                k2 = nb_ % 2
                nb_ += 1
                rowbuf[ri] = k2
                S.ts(biases[k2][:, 0:i + 1], Fs[:, 0:i + 1, h], Cs[:, i, h:h + 1], -1.0, ALU.subtract, ALU.mult)
                S.act(dms[k2][:, 0:i + 1], biases[k2][:, 0:i + 1], AF.Exp)
            ps_s = psb[q % 4]
            pt = pts[q % 4]
            q += 1
            for jj in range(nbt):
                j = jb + jj
                S.mm(ps_s[:, jj * 128:(jj + 1) * 128], Kh[:, j * 128:(j + 1) * 128], QT[:, i * 128:(i + 1) * 128])
            S.act(pt[:, 0:nbt * 128], ps_s[:, 0:nbt * 128], AF.Exp)
            if jb + nbt - 1 == i:
                S.tt(pt[:, (nbt - 1) * 128:nbt * 128], pt[:, (nbt - 1) * 128:nbt * 128], maskb[:], ALU.mult)
            return pt

        def back(bt, pt):
            nonlocal nv
            ri, a_, i, jb, nbt = bt
            h = 2 * p + a_
            k2 = rowbuf[ri]
            po = psb[4 + k2]
            dm = dms[k2]
            rc = rcs[k2]
            for jj in range(nbt):
                j = jb + jj
                vs = vss[nv % 6]
                nv += 1
                S.ts(vs[:], vp[:, j, h, :], dm[:, j:j + 1], None, ALU.mult)
                S.mm(po[:, 0:65], pt[:, jj * 128:(jj + 1) * 128], vs[:], start=(j == 0), stop=(j == i))
            if jb + nbt - 1 == i:
                S.recip(rc[:], po[:, 64:65])
                S.ts(otok[:, i, a_ * 64:(a_ + 1) * 64], po[:, 0:64], rc[:, 0:1], None, ALU.mult)
                if a_ == 1:
                    pt_ = psb[6 + i % 2]
                    S.tr(pt_[:, 0:128], otok[:, i, :], kb.cst["ident"][:])
                    S.copy(brs[:, i * 128:(i + 1) * 128], pt_[:, 0:128], eng="act" if i % 2 else "dve")

        pend = None
        for bt in batches:
            ptn = front(bt)
            if pend is not None:
                back(*pend)
            pend = (bt, ptn)
        back(*pend)
        S.dma(brT_d[2, p], brs[:], eng="pool")
    P.close()


def phase_branches(kb, layer, brT_d):
    which = DBG.get("branches", (0, 1, 2, 3))
    if 2 in which:
        fox_branch(kb, layer, brT_d)
    if 0 in which:
        gla_branch(kb, layer, brT_d)
    if 3 in which:
        hgrn_branch(kb, layer, brT_d)
    if 1 in which:
        rwkv_branch(kb, layer, brT_d)


def cgla_branch(kb, layer, brT_d, kind, lb_d=None):
    S = kb.S
    P = Pool_(kb)
    hT = kb.hT
    W = kb.d["w_in"][layer]
    psb = kb.psb
    cst = kb.cst
    gla = (kind == "gla")
    NU = 2 if gla else 4
    HPU = 2 if gla else 1
    DK = 64 if gla else 128
    KW = NU * 128
    o = GLA_OFF if gla else HGRN_OFF
    bidx = 0 if gla else 3
    qscale = 0.125 if gla else 1.0
    stg = P.sb("cstg", [128, 8, 528])
    if gla:
        wq = P.sb("cwq", [128, 8, 256], BF16)
        wk = P.sb("cwk", [128, 8, 256], BF16)
        wv = P.sb("cwv", [128, 8, 512], BF16)
        wg = P.sb("cwg", [128, 8, 528], BF16)
        load_w(kb, wq[:], W[:, o:o + 256], stg)
        load_w(kb, wk[:], W[:, o + 256:o + 512], stg)
        load_w(kb, wv[:], W[:, o + 512:o + 1024], stg)
        load_w(kb, wg[:], W[:, o + 1024:o + 1552], stg)
        aup = P.sb("caup", [16, 256])
        S.dma(aup[:], kb.d["gla_alpha_up"][layer])
        abr = P.sb("cabr", [1, 256])
        S.dma(abr[:], kb.d["gla_alpha_b"][layer:layer + 1, :])
        alT = P.sb("calT", [16, 512])
        Uc = P.sb("cUc", [128, 128])
        SUl = P.sb("cSUl", [128, 128])
        S.ts(Uc[:], cst["triu64"][:], -1.0 / 16.0, None, ALU.mult)
        S.ts(SUl[:], cst["slo64"][:], -1.0 / 16.0, None, ALU.mult)
        ng_src = kb.d["gla_norm_g"]
    else:
        wq = P.sb("cwq", [128, 8, 512], BF16)
        wk = P.sb("cwk", [128, 8, 512], BF16)
        wv = P.sb("cwv", [128, 8, 512], BF16)
        wg = P.sb("cwg", [128, 8, 512], BF16)
        load_w(kb, wq[:], W[:, o:o + 512], stg)
        load_w(kb, wk[:], W[:, o + 512:o + 1024], stg)
        load_w(kb, wv[:], W[:, o + 1024:o + 1536], stg)
        load_w(kb, wg[:], W[:, o + 1536:o + 2048], stg)
        Uc, SUl = cst["triu64"], cst["slo64"]
        lbB = P.sb("clbB", [128, 512])
        omlB = P.sb("comlB", [128, 512])
        S.dma(lbB[:], lb_d[0:1, :].partition_broadcast(128))
        S.ts(omlB[:], lbB[:], -1.0, 1.0, ALU.mult, ALU.add)
        lbT = P.sb("clbT", [128, 4])
        omlT = P.sb("comlT", [128, 4])
        load_T(kb, P, lbT[:], lb_d[0].rearrange("(c p) -> c p", p=128), 4)
        S.ts(omlT[:], lbT[:], -1.0, 1.0, ALU.mult, ALU.add)
        ng_src = kb.d["hgrn_norm_g"]
    ngb = P.sb("cngb", [128, 128])
    S.dma(ngb[:], ng_src[layer:layer + 1, :].partition_broadcast(128))
    qTs = [P.sb("cqTs%d" % u, [128, 512]) for u in range(NU)]
    kTs = [P.sb("ckTs%d" % u, [128, 512]) for u in range(NU)]
    brs = P.sb("cbrs", [128, 4, T], BF16)
    l_tok = P.sb("cltok", [128, KW])
    k_tok = P.sb("cktok", [128, KW])
    f_tok = P.sb("cftok", [128, KW])
    v_tok = P.sb("cvtok", [128, 512], BF16)
    sg_tok = P.sb("csgtok", [128, 512])
    br_tok = P.sb("cbrtok", [128, 512])
    bTs = P.sb("cbTs", [128, 128])
    kdec = P.sb("ckdec", [128, 128])
    khat = P.sb("ckhat", [128, 128], BF16)
    bm = P.sb("cbm", [128, 2])
    nbm = P.sb("cnbm", [128, 2])
    E1 = P.sb("cE1", [128, 128])
    E2 = P.sb("cE2", [128, 128])
    E3 = P.sb("cE3", [128, 128])
    qt = P.sb("cqt", [128, 128], BF16)
    kt = P.sb("ckt", [128, 128], BF16)
    qA = P.sb("cqA", [128, 128], BF16)
    qB = P.sb("cqB", [128, 128], BF16)
    attb = [P.sb("cattb%d" % a, [128, 128], BF16) for a in range(HPU)]
    Sf = [[P.sb("cSf%d_%d" % (u, k), [128, 128]) for k in range(2)] for u in range(NU)]
    Sb = [[P.sb("cSb%d_%d" % (u, k), [128, 128], BF16) for k in range(2)] for u in range(NU)]
    for u in range(NU):
        S.memset(Sf[u][0][:], 0.0)
        S.memset(Sb[u][0][:], 0.0)
    ss = P.sb("css", [128, 2])
    junk = P.sb("cjunk", [128, 128])
    v_tokD = [v_tok, P.sb("cvtok2", [128, 512], BF16)]
    sg_tokD = [sg_tok, P.sb("csgtok2", [128, 512])]
    br_tokD = [br_tok, P.sb("cbrtok2", [128, 512])]
    khatD = [khat, P.sb("ckhat2", [128, 128], BF16)]
    qAD = [qA, P.sb("cqA2", [128, 128], BF16)]
    attbD = [attb, [P.sb("cattb2_%d" % a, [128, 128], BF16) for a in range(HPU)]]
    eblD = [P.sb("cebl%d" % k, [128, 2]) for k in range(2)]
    iters = []
    for g in range(DBG.get('cg_ng', 8)):
        for tt in range(4):
            for u in range(NU):
                iters.append((g, tt, u))

    def setup(it):
        g, tt, u = iters[it]
        bf = it % 2
        t = g * 4 + tt
        tp = t % 2
        gt = tok(g * 512, 512)
        tk = tok(t * 128)
        tcol = slice(tt * 128, (tt + 1) * 128)
        v_tok, sg_tok = v_tokD[tp], sg_tokD[tp]
        khat, qA, attb, ebl = khatD[bf], qAD[bf], attbD[bf], eblD[bf]
        if tt == 0 and u == 0:
            for u2 in range(NU):
                pq, pk = psb[0], psb[1]
                for kc in range(8):
                    S.mm(pq[:], wq[:, kc, u2 * 128:(u2 + 1) * 128], hT[:, kc, gt], start=(kc == 0), stop=(kc == 7))
                for kc in range(8):
                    S.mm(pk[:], wk[:, kc, u2 * 128:(u2 + 1) * 128], hT[:, kc, gt], start=(kc == 0), stop=(kc == 7))
                yield
                S.copy(qTs[u2][:], pq[:], eng="act")
                if gla:
                    S.copy(kTs[u2][:], pk[:], eng="dve")
                else:
                    S.act(kTs[u2][:], pk[:], AF.Sigmoid)
                    S.ts(kTs[u2][:], kTs[u2][:], omlT[:, u2:u2 + 1], lbT[:, u2:u2 + 1], ALU.mult, ALU.add)
                    S.ts(kTs[u2][:], kTs[u2][:], -1.0, 1.0, ALU.mult, ALU.add)
                yield
            if gla:
                pa = psb[2]
                for kc in range(8):
                    S.mm(pa[0:16, :], wg[:, kc, 512:528], hT[:, kc, gt], start=(kc == 0), stop=(kc == 7))
                S.copy(alT[:], pa[0:16, :])
                yield
        if u == 0:
            pv, pg = psb[2], psb[3]
            for kc in range(8):
                S.mm(pv[:], hT[:, kc, tk], wv[:, kc, 0:512], start=(kc == 0), stop=(kc == 7))
            for kc in range(8):
                S.mm(pg[:], hT[:, kc, tk], wg[:, kc, 0:512], start=(kc == 0), stop=(kc == 7))
            yield
            S.copy(v_tok[:], pv[:], eng="act")
            S.act(sg_tok[:], pg[:], AF.Silu)
            pk2 = psb[2]
            if gla:
                for kc in range(8):
                    S.mm(pk2[:, 0:256], hT[:, kc, tk], wk[:, kc, 0:256], start=(kc == 0), stop=(kc == 7))
                S.mm(pk2[:, 256:512], alT[:, tcol], aup[:], start=True, stop=False)
                S.mm(pk2[:, 256:512], cst["ones"][0:1, :], abr[:], start=False, stop=True)
                yield
                S.copy(k_tok[:], pk2[:, 0:256])
                S.act(l_tok[:], pk2[:, 256:512], AF.Exp, scale=-1.0)
                S.act(l_tok[:], l_tok[:], AF.Ln, bias=1.0)
            else:
                for kc in range(8):
                    S.mm(pk2[:], hT[:, kc, tk], wk[:, kc, 0:512], start=(kc == 0), stop=(kc == 7))
                yield
                S.act(f_tok[:], pk2[:], AF.Sigmoid)
                S.tt(f_tok[:], f_tok[:], omlB[:], ALU.mult)
                S.tt(f_tok[:], f_tok[:], lbB[:], ALU.add)
                S.act(l_tok[:], f_tok[:], AF.Ln)
                S.ts(k_tok[:], f_tok[:], -1.0, 1.0, ALU.mult, ALU.add)
            yield
        cu = slice(u * 128, (u + 1) * 128)
        pX = psb[4]
        S.mm(pX[:, 0:128], l_tok[:, cu], Uc[:])
        S.mm(pX[:, 128:256], SUl[:], l_tok[:, cu])
        yield
        S.copy(bTs[:], pX[:, 0:128])
        S.act(kdec[:], pX[:, 128:256], AF.Exp)
        S.tt(khat[:], k_tok[:, cu], kdec[:], ALU.mult)
        mid = bTs[:].rearrange("p (c s) -> p c s", c=2)[:, :, 32]
        S.copy(bm[:], mid)
        S.ts(nbm[:], mid, -1.0, None, ALU.mult)
        yield
        for c in range(2):
            cs = slice(c * 64, (c + 1) * 64)
            S.act(E1[:, cs], bTs[:, cs], AF.Exp, bias=nbm[:, c:c + 1])
            S.act(E2[:, cs], bTs[:, cs], AF.Exp, bias=bm[:, c:c + 1], scale=-1.0)
        S.act(E3[:], bTs[:], AF.Exp)
        yield
        S.stt(qt[:], qTs[u][:, tcol], qscale, E1[:], ALU.mult, ALU.mult)
        S.tt(kt[:], kTs[u][:, tcol], E2[:], ALU.mult)
        S.stt(qA[:], qTs[u][:, tcol], qscale, E3[:], ALU.mult, ALU.mult)
        S.copy(ebl[:], E3[:].rearrange("p (c s) -> p c s", c=2)[:, :, 63])
        pAtt = psb[5]
        for a in range(HPU):
            ra = slice(a * DK, (a + 1) * DK)
            S.mm(pAtt[:, a * 128:(a + 1) * 128], kt[ra, :], qt[ra, :])
        yield
        for a in range(HPU):
            S.tt(attb[a][:], cst["triu64"][:], pAtt[:, a * 128:(a + 1) * 128], ALU.mult)
        yield

    def chain(it):
        g, tt, u = iters[it]
        bf = it % 2
        t = g * 4 + tt
        tp = t % 2
        v_tok, sg_tok, br_tok = v_tokD[tp], sg_tokD[tp], br_tokD[tp]
        khat, qA, attb, ebl = khatD[bf], qAD[bf], attbD[bf], eblD[bf]
        S0f, S1f = Sf[u][0], Sf[u][1]
        S0b, S1b = Sb[u][0], Sb[u][1]
        pS = psb[6]
        for c in range(2):
            rows = slice(c * 64, (c + 1) * 64)
            for a in range(HPU):
                h = u * HPU + a
                ra = slice(a * DK, (a + 1) * DK)
                S.mm(pS[ra, c * 128:(c + 1) * 128], khat[rows, a * DK:(a + 1) * DK], v_tok[rows, h * 128:(h + 1) * 128])
        yield
        S.stt(S1f[:], S0f[:], ebl[:, 0:1], pS[:, 0:128], ALU.mult, ALU.add)
        S.copy(S1b[:], S1f[:], eng="act")
        yield
        pO = psb[7]
        for a in range(HPU):
            h = u * HPU + a
            ra = slice(a * DK, (a + 1) * DK)
            oc = slice(a * 128, (a + 1) * 128)
            S.mm(pO[0:64, oc], qA[ra, 0:64], S0b[ra, :], start=True, stop=False)
            S.mm(pO[64:128, oc], qA[ra, 64:128], S1b[ra, :], start=True, stop=False)
            S.mm(pO[:, oc], attb[a][:], v_tok[:, h * 128:(h + 1) * 128], start=False, stop=True)
        yield
        S.stt(S0f[:], S1f[:], ebl[:, 1:2], pS[:, 128:256], ALU.mult, ALU.add)
        S.copy(S0b[:], S0f[:], eng="act")
        for a in range(HPU):
            oc = slice(a * 128, (a + 1) * 128)
            S.act(junk[:], pO[:, oc], AF.Square, accum_out=ss[:, a:a + 1])
        yield
        for a in range(HPU):
            rsqrt_eps(S, ss[:, a:a + 1], ss[:, a:a + 1], 1e-6, scale=1.0 / 128.0)
        yield
        for a in range(HPU):
            h = u * HPU + a
            oc = slice(a * 128, (a + 1) * 128)
            hc = slice(h * 128, (h + 1) * 128)
            S.stt(br_tok[:, hc], pO[:, oc], ss[:, a:a + 1], ngb[:], ALU.mult, ALU.mult)
            S.tt(br_tok[:, hc], br_tok[:, hc], sg_tok[:, hc], ALU.mult)
        yield
        if u == NU - 1:
            for kc in range(4):
                pT = pO if kc < 2 else pS
                S.tr(pT[:, 256 + (kc % 2) * 128:256 + (kc % 2 + 1) * 128], br_tok[:, kc * 128:(kc + 1) * 128], cst["ident"][:])
            yield
            S.copy(brs[:, 0:2, t * 128:(t + 1) * 128], pO[:, 256:512].rearrange("p (c t) -> p c t", c=2), eng="act")
            S.copy(brs[:, 2:4, t * 128:(t + 1) * 128], pS[:, 256:512].rearrange("p (c t) -> p c t", c=2), eng="dve")
            yield

    def run_interleaved(gens):
        active = [x for x in gens if x is not None]
        while active:
            for gi in list(active):
                try:
                    next(gi)
                except StopIteration:
                    active.remove(gi)

    n_it = len(iters)
    for k in range(n_it + 1):
        run_interleaved([setup(k) if k < n_it else None, chain(k - 1) if k >= 1 else None])
    S.dma(brT_d[bidx].rearrange("c p t -> p c t"), brs[:], eng="pool")
    P.close()


def gla_branch(kb, layer, brT_d):
    cgla_branch(kb, layer, brT_d, "gla")


def hgrn_branch(kb, layer, brT_d):
    S = kb.S
    P = Pool_(kb)
    lb_d = kb.lb_d
    row = P.sb("hlbrow", [1, 512])
    if layer == 0:
        S.memset(row[:], 0.0)
    else:
        r0 = P.sb("hlb0", [1, 512])
        S.dma(r0[:], kb.d["hgrn_lb_logits"][0:1, :])
        S.dma(row[:], kb.d["hgrn_lb_logits"][1:2, :])
        S.tt(row[:], row[:], r0[:], ALU.subtract)
        S.act(row[:], row[:], AF.Sigmoid)
    S.dma(lb_d[layer:layer + 1, :], row[:])
    P.close()
    cgla_branch(kb, layer, brT_d, "hgrn", lb_d=lb_d[layer:layer + 1, :])


def rwkv_branch(kb, layer, brT_d):
    S = kb.S
    hT = kb.hT
    W = kb.d["w_in"][layer]
    psb = kb.psb
    cst = kb.cst
    o = RWKV_OFF
    P = Pool_(kb)
    CW = math.exp(-0.5)
    Wr = [P.sb("rWr%d" % k, [128, 8, 512], BF16) for k in range(2)]
    Wk = [P.sb("rWk%d" % k, [128, 8, 512], BF16) for k in range(2)]
    Wv = [P.sb("rWv%d" % k, [128, 8, 512], BF16) for k in range(2)]
    Wl = [P.sb("rWl%d" % k, [128, 8, 256], BF16) for k in range(2)]
    PP = Pool_(kb)
    stg = PP.sb("rstg", [128, 8, 512])
    muB = PP.sb("rmuB", [128, 1792])
    omuB = PP.sb("romuB", [128, 1792])
    S.dma(muB[:], kb.d["rwkv_mu"][layer:layer + 1, :].partition_broadcast(128))
    S.ts(omuB[:], muB[:], -1.0, 1.0, ALU.mult, ALU.add)
    for (dst, c0, n) in ((Wr, 0, 512), (Wk, 512, 512), (Wv, 1024, 512), (Wl, 1536, 256)):
        sv = stg[:, :, 0:n]
        S.dma(sv, W[:, o + c0:o + c0 + n].rearrange("(c p) n -> p c n", p=128))
        S.tt(dst[0][:], sv, omuB[:, c0:c0 + n].unsqueeze(1).to_broadcast([128, 8, n]), ALU.mult)
        S.tt(dst[1][:], sv, muB[:, c0:c0 + n].unsqueeze(1).to_broadcast([128, 8, n]), ALU.mult, eng="pool")
    PP.close()
    lw = P.sb("rlw", [128, 512])
    S.dma(lw[0:64, :], kb.d["rwkv_w2"][layer])
    S.dma(lw[64:128, :], kb.d["rwkv_a2"][layer])
    g2f = P.sb("rg2f", [128, 512])
    g2b = P.sb("rg2b", [128, 512], BF16)
    S.dma(g2f[:], kb.d["rwkv_g2"][layer])
    S.copy(g2b[:], g2f[:])
    w0r = P.sb("rw0r", [1, 512])
    S.dma(w0r[:], kb.d["rwkv_w0"][layer:layer + 1, :])
    a0T = P.sb("ra0T", [128, 4])
    kkT_ = P.sb("rkkT", [128, 4])
    kaT = P.sb("rkaT", [128, 4])
    okaT = P.sb("rokaT", [128, 4])
    rkT_ = P.sb("rrkT", [128, 4])
    load_T(kb, P, a0T[:], kb.d["rwkv_a0"][layer].rearrange("(c p) -> c p", p=128), 4)
    load_T(kb, P, kkT_[:], kb.d["rwkv_k_k"][layer].rearrange("(c p) -> c p", p=128), 4)
    load_T(kb, P, kaT[:], kb.d["rwkv_k_a"][layer].rearrange("(c p) -> c p", p=128), 4)
    load_T(kb, P, rkT_[:], kb.d["rwkv_r_k"][layer].rearrange("(c two) d -> c (two d)", two=2), 4)
    S.ts(okaT[:], kaT[:], -1.0, 1.0, ALU.mult, ALU.add)
    lngB = P.sb("rlngB", [128, 512])
    lnbB = P.sb("rlnbB", [128, 512])
    S.dma(lngB[:], kb.d["rwkv_ln_g"][layer:layer + 1, :].partition_broadcast(128))
    S.dma(lnbB[:], kb.d["rwkv_ln_b"][layer:layer + 1, :].partition_broadcast(128))
    Uc = P.sb("rUc", [128, 128])
    Ux = P.sb("rUx", [128, 128])
    SUl = P.sb("rSUl", [128, 128])
    nsup = P.sb("rnsup", [128, 128])
    nslo = P.sb("rnslo", [128, 128])
    ntriu = P.sb("rntriu", [128, 128])
    S.ts(Uc[:], cst["triu64"][:], -CW, None, ALU.mult)
    S.ts(Ux[:], cst["sup64"][:], -CW, None, ALU.mult)
    S.ts(SUl[:], cst["slo64"][:], -CW, None, ALU.mult)
    S.ts(nsup[:], cst["sup64"][:], -1.0, None, ALU.mult)
    S.ts(nslo[:], cst["slo64"][:], -1.0, None, ALU.mult)
    S.ts(ntriu[:], cst["triu64"][:], -1.0, None, ALU.mult)
    hsel = P.sb("rhsel", [128, 2])
    S.copy(hsel[:], cst["blk64"][:].rearrange("p (a s) -> p a s", a=2)[:, :, 0])
    ident = cst["ident"]

    def f512(name):
        return P.sb(name, [128, 512])

    def f128(name, dt=F32):
        return P.sb(name, [128, 128], dt)

    lo = f512("rlo")
    sgl = P.sb("rsgl", [128, 512], BF16)
    rT, kT, aT, kaT_, bT_, kpT, tmpF, prodT = (f512("r_" + n) for n in ("rT", "kT", "aT", "kapT", "bbT", "kpT", "tmpF", "prodT"))
    def d128(name):
        return [f128("%s_%d" % (name, k)) for k in range(2)]

    l_tok = f128("rltok")
    v_tokD, g_tokD = d128("rvtok"), d128("rgtok")
    sb2D = [P.sb("rsb2_%d" % k, [128, 2]) for k in range(2)]
    gCD = [P.sb("rgC_%d" % k, [128, 2]) for k in range(2)]
    bTs, bxTs = f128("rbTs"), f128("rbxTs")
    bm, nbm, bl = P.sb("rbm", [128, 2]), P.sb("rnbm", [128, 2]), P.sb("rbl", [128, 2])
    E = {n: f128("rE_" + n) for n in ("r", "kx", "inv", "abs", "absx", "last")}
    RB = BF16 if DBG.get("rw_bf16", True) else F32
    rt, kxt, kt, bt = (f128("r_" + n, RB) for n in ("rt", "kxt", "kt", "bt"))
    KhT, BhT = (f128("r_" + n) for n in ("KhT", "BhT"))
    rbarD, kbarD, KhatD, BhatD = d128("rrbar"), d128("rkbar"), d128("rKhat"), d128("rBhat")
    Mm = [[f128("rM%d_%d" % (a, k), RB) for k in range(2)] for a in range(2)]
    MT = [[f128("rMT%d_%d" % (a, k), RB) for k in range(2)] for a in range(2)]
    Pb = [[f128("rPb%d_%d" % (a, k), RB) for k in range(2)] for a in range(2)]
    PmD = [[[f128("rP%d_%d_%d" % (b_, a, k)) for k in range(2)] for a in range(2)] for b_ in range(2)]
    AkkD = [[f128("rAkk%d_%d" % (b_, a)) for a in range(2)] for b_ in range(2)]
    ArkD = [[f128("rArk%d_%d" % (b_, a)) for a in range(2)] for b_ in range(2)]
    ArbD = [[f128("rArb%d_%d" % (b_, a)) for a in range(2)] for b_ in range(2)]
    Ws, Us, ytok = f128("rWs"), f128("rUs"), f128("rytok")
    Hs = [[P.sb("rHs%d_%d" % (u, k), [128, 64]) for k in range(2)] for u in range(4)]
    for u in range(4):
        S.memset(Hs[u][0][:], 0.0)
    st6 = P.sb("rst6", [128, 2, 6])
    mv2 = P.sb("rmv2", [128, 2, 2])
    rs2 = P.sb("rrs2", [128, 2])
    yn = f128("ryn")
    brt_ = f128("rbrt")
    brb = [P.sb("rbrb%d" % k, [128, 128], BF16) for k in range(2)]

    def proj_fm(ps, W2, c0, n, gcol):
        for kc in range(8):
            S.mm(ps[0:n, :], W2[0][:, kc, c0:c0 + n], hT[:, kc, slice(1 + gcol, 1 + gcol + 512)], start=(kc == 0), stop=False)
        for kc in range(8):
            S.mm(ps[0:n, :], W2[1][:, kc, c0:c0 + n], hT[:, kc, slice(gcol, gcol + 512)], start=False, stop=(kc == 7))

    iters = []
    for g in range(DBG.get("rw_ng", 8)):
        for u in range(4):
            for tt_ in range(4):
                iters.append((g, u, tt_))

    def setup(it):
        g, u, tt_ = iters[it]
        bf = it % 2
        gcol = g * 512
        uc = slice(u * 128, (u + 1) * 128)
        v_tok, g_tok, sb2, gC = v_tokD[bf], g_tokD[bf], sb2D[bf], gCD[bf]
        rbar, kbar, Khat, Bhat = rbarD[bf], kbarD[bf], KhatD[bf], BhatD[bf]
        Pm, Akk, Ark, Arb = PmD[bf], AkkD[bf], ArkD[bf], ArbD[bf]
        if u == 0 and tt_ == 0:
            proj_fm(psb[0], Wl, 0, 128, gcol)
            S.act(lo[0:64, :], psb[0][0:64, :], AF.Tanh)
            S.copy(lo[64:128, :], psb[0][64:128, :])
            proj_fm(psb[1], Wl, 128, 128, gcol)
            S.act(sgl[:], psb[1][:], AF.Sigmoid)
            yield
        if tt_ == 0:
            proj_fm(psb[0], Wr, u * 128, 128, gcol)
            S.copy(rT[:], psb[0][:], eng="act")
            proj_fm(psb[1], Wk, u * 128, 128, gcol)
            S.copy(kT[:], psb[1][:])
            yield
            S.mm(psb[0][:], lw[64:128, uc], lo[64:128, :])
            S.act(aT[:], psb[0][:], AF.Sigmoid, bias=a0T[:, u:u + 1])
            S.ts(kaT_[:], kT[:], kkT_[:, u:u + 1], None, ALU.mult)
            S.tt(tmpF[:], kaT_[:], kaT_[:], ALU.mult)
            S.mm(psb[1][:], cst["blk64"][:], tmpF[:])
            yield
            S.act(tmpF[:], psb[1][:], AF.Ln, bias=1e-24)
            S.act(tmpF[:], tmpF[:], AF.Exp, scale=-0.5)
            S.tt(kaT_[:], kaT_[:], tmpF[:], ALU.mult)
            S.tt(bT_[:], kaT_[:], aT[:], ALU.mult)
            yield
            S.ts(tmpF[:], aT[:], kaT[:, u:u + 1], okaT[:, u:u + 1], ALU.mult, ALU.add)
            S.tt(kpT[:], kT[:], tmpF[:], ALU.mult)
            S.stt(prodT[:], rT[:], rkT_[:, u:u + 1], kpT[:], ALU.mult, ALU.mult)
            yield
        t = g * 4 + tt_
        t0 = t * 128
        tc_ = slice(tt_ * 128, (tt_ + 1) * 128)
        pt_ = psb[2]
        for kc in range(8):
            S.mm(pt_[:, 0:128], hT[:, kc, slice(1 + t0, 1 + t0 + 128)], Wv[0][:, kc, uc], start=(kc == 0), stop=False)
        for kc in range(8):
            S.mm(pt_[:, 0:128], hT[:, kc, slice(t0, t0 + 128)], Wv[1][:, kc, uc], start=False, stop=(kc == 7))
        S.mm(pt_[:, 128:256], lo[0:64, tc_], lw[0:64, uc], start=True, stop=False)
        S.mm(pt_[:, 128:256], cst["ones"][0:1, :], w0r[0:1, uc], start=False, stop=True)
        S.mm(pt_[:, 256:384], sgl[:, tc_], g2b[:, uc])
        S.mm(pt_[:, 384:386], prodT[:, tc_], hsel[:])
        yield
        S.act(l_tok[:], pt_[:, 128:256], AF.Sigmoid)
        S.copy(v_tok[:], pt_[:, 0:128])
        S.copy(g_tok[:], pt_[:, 256:384], eng="act")
        S.copy(sb2[:], pt_[:, 384:386])
        pX = psb[3]
        S.mm(pX[:, 0:128], l_tok[:], Uc[:])
        S.mm(pX[:, 128:256], l_tok[:], Ux[:])
        yield
        S.copy(bTs[:], pX[:, 0:128])
        S.copy(bxTs[:], pX[:, 128:256], eng="act")
        b3 = bTs[:].rearrange("p (c s) -> p c s", c=2)
        S.copy(bm[:], b3[:, :, 32])
        S.ts(nbm[:], b3[:, :, 32], -1.0, None, ALU.mult)
        S.copy(bl[:], b3[:, :, 63])
        yield
        for c in range(2):
            cs = slice(c * 64, (c + 1) * 64)
            S.act(E["r"][:, cs], bTs[:, cs], AF.Exp, bias=nbm[:, c:c + 1])
            S.act(E["kx"][:, cs], bxTs[:, cs], AF.Exp, bias=nbm[:, c:c + 1])
            S.act(E["inv"][:, cs], bTs[:, cs], AF.Exp, bias=bm[:, c:c + 1], scale=-1.0)
            S.act(E["last"][:, cs], bTs[:, cs], AF.Exp, bias=bl[:, c:c + 1], scale=-1.0)
        S.act(E["abs"][:], bTs[:], AF.Exp)
        S.act(E["absx"][:], bxTs[:], AF.Exp)
        yield
        S.tt(rt[:], rT[:, tc_], E["r"][:], ALU.mult)
        S.tt(kxt[:], kaT_[:, tc_], E["kx"][:], ALU.mult)
        S.tt(kt[:], kpT[:, tc_], E["inv"][:], ALU.mult)
        S.tt(bt[:], bT_[:, tc_], E["inv"][:], ALU.mult)
        yield
        S.tt(KhT[:], kpT[:, tc_], E["last"][:], ALU.mult)
        S.stt(BhT[:], bT_[:, tc_], -1.0, E["last"][:], ALU.mult, ALU.mult)
        S.tt(rbar[:], rT[:, tc_], E["abs"][:], ALU.mult)
        S.tt(kbar[:], kaT_[:, tc_], E["absx"][:], ALU.mult)
        S.copy(gC[:], E["abs"][:].rearrange("p (c s) -> p c s", c=2)[:, :, 63])
        for a in range(2):
            ra = slice(a * 64, (a + 1) * 64)
            pA = psb[4 + a]
            S.mm(pA[:, 0:128], bt[ra, :], kxt[ra, :])
            S.mm(pA[:, 128:256], kxt[ra, :], bt[ra, :])
            S.mm(pA[:, 256:384], kt[ra, :], kxt[ra, :])
            S.mm(pA[:, 384:512], kt[ra, :], rt[ra, :])
            S.mm(psb[6][:, a * 128:(a + 1) * 128], bt[ra, :], rt[ra, :])
        S.tr(pX[:, 256:384], KhT[:], ident[:])
        S.tr(pX[:, 384:512], BhT[:], ident[:])
        yield
        for a in range(2):
            pA = psb[4 + a]
            S.tt(Mm[a][0][:], nsup[:], pA[:, 0:128], ALU.mult)
            S.tt(MT[a][0][:], nslo[:], pA[:, 128:256], ALU.mult)
            S.tt(Pm[a][0][:], Mm[a][0][:], ident[:], ALU.add)
            S.copy(Pb[a][0][:], Pm[a][0][:], eng="act")
        yield
        for a in range(2):
            pA = psb[4 + a]
            S.tt(Akk[a][:], cst["sup64"][:], pA[:, 256:384], ALU.mult)
            S.tt(Ark[a][:], cst["triu64"][:], pA[:, 384:512], ALU.mult)
            S.tt(Arb[a][:], ntriu[:], psb[6][:, a * 128:(a + 1) * 128], ALU.mult)
        S.copy(Khat[:], pX[:, 256:384], eng="act")
        S.copy(Bhat[:], pX[:, 384:512], eng="act")
        cur = 0

        def stA(lvl, cur):
            for a in range(2):
                pA = psb[4 + a]
                if lvl < 5:
                    S.mm(pA[:, 0:128], MT[a][cur][:], Mm[a][cur][:])
                S.mm(pA[:, 128:256], Mm[a][cur][:], MT[a][cur][:])

        def stB(lvl, cur):
            nxt = 1 - cur
            for a in range(2):
                pA = psb[4 + a]
                eng = "dve" if a == 0 else "act"
                if lvl < 5:
                    S.copy(Mm[a][nxt][:], pA[:, 0:128], eng=eng)
                S.copy(MT[a][nxt][:], pA[:, 128:256], eng=eng)

        def stC(lvl, cur):
            nxt = 1 - cur
            for a in range(2):
                pA = psb[4 + a]
                S.mm(pA[:, 256:384], MT[a][nxt][:], Pb[a][cur][:])

        def stD(lvl, cur):
            nxt = 1 - cur
            for a in range(2):
                pA = psb[4 + a]
                S.tt(Pm[a][nxt][:], Pm[a][cur][:], pA[:, 256:384], ALU.add)
                if lvl < 5:
                    S.copy(Pb[a][nxt][:], Pm[a][nxt][:], eng="act")

        stA(1, 0)
        yield
        stB(1, 0)
        yield
        for lvl in range(2, 6):
            c_prev = (lvl - 2) % 2
            c_cur = (lvl - 1) % 2
            stC(lvl - 1, c_prev)
            stA(lvl, c_cur)
            yield
            stD(lvl - 1, c_prev)
            stB(lvl, c_cur)
            yield
        stC(5, 0)
        yield
        stD(5, 0)
        yield
        cur = 1
        assert cur == 1

    nbr = [0]

    def chain(it):
        g, u, tt_ = iters[it]
        bf = it % 2
        t0 = (g * 4 + tt_) * 128
        v_tok, g_tok, sb2, gC = v_tokD[bf], g_tokD[bf], sb2D[bf], gCD[bf]
        rbar, kbar, Khat, Bhat = rbarD[bf], kbarD[bf], KhatD[bf], BhatD[bf]
        Akk, Ark, Arb = AkkD[bf], ArkD[bf], ArbD[bf]
        Pf = [PmD[bf][a][1] for a in range(2)]
        H0, H1 = Hs[u][0], Hs[u][1]
        pC = psb[7]
        Hc = [H0, H1, H0]
        for c in range(2):
            rc = slice(c * 64, (c + 1) * 64)
            Hin, Hout = Hc[c], Hc[c + 1]
            for a in range(2):
                ra = slice(a * 64, (a + 1) * 64)
                S.mm(pC[rc, a * 64:(a + 1) * 64], kbar[ra, rc], Hin[ra, :], start=True, stop=False)
                S.mm(pC[rc, a * 64:(a + 1) * 64], Akk[a][rc, rc], v_tok[rc, ra], start=False, stop=True)
            yield
            S.copy(Ws[rc, :], pC[rc, 0:128])
            yield
            for a in range(2):
                ra = slice(a * 64, (a + 1) * 64)
                S.mm(pC[rc, 128 + a * 64:128 + (a + 1) * 64], Pf[a][rc, rc], Ws[rc, ra])
            yield
            S.copy(Us[rc, :], pC[rc, 128:256])
            yield
            for a in range(2):
                ra = slice(a * 64, (a + 1) * 64)
                S.mm(pC[ra, 384:448], Khat[rc, ra], v_tok[rc, ra], start=True, stop=False)
                S.mm(pC[ra, 384:448], Bhat[rc, ra], Us[rc, ra], start=False, stop=True)
            for a in range(2):
                ra = slice(a * 64, (a + 1) * 64)
                oy = slice(256 + a * 64, 256 + (a + 1) * 64)
                S.mm(pC[rc, oy], rbar[ra, rc], Hin[ra, :], start=True, stop=False)
                S.mm(pC[rc, oy], Ark[a][rc, rc], v_tok[rc, ra], start=False, stop=False)
                S.mm(pC[rc, oy], Arb[a][rc, rc], Us[rc, ra], start=False, stop=True)
            yield
            S.stt(Hout[:], Hin[:], gC[:, c:c + 1], pC[:, 384:448], ALU.mult, ALU.add)
            S.copy(ytok[rc, :], pC[rc, 256:384], eng="act")
            yield
        for a in range(2):
            S.bn_stats(st6[:, a, :], ytok[:, a * 64:(a + 1) * 64])
            S.bn_aggr(mv2[:, a, :], st6[:, a, :])
        rsqrt_eps(S, rs2[:], mv2[:, :, 1], 64e-5)
        yield
        for a in range(2):
            ra = slice(a * 64, (a + 1) * 64)
            gc = slice(u * 128 + a * 64, u * 128 + (a + 1) * 64)
            S.ts(yn[:, ra], ytok[:, ra], mv2[:, a, 0:1], rs2[:, a:a + 1], ALU.subtract, ALU.mult)
            S.tt(yn[:, ra], yn[:, ra], lngB[:, gc], ALU.mult)
            S.tt(yn[:, ra], yn[:, ra], lnbB[:, gc], ALU.add)
            S.stt(yn[:, ra], v_tok[:, ra], sb2[:, a:a + 1], yn[:, ra], ALU.mult, ALU.add)
        S.tt(brt_[:], yn[:], g_tok[:], ALU.mult)
        S.tr(pC[:, 0:128], brt_[:], ident[:])
        yield
        bb_ = brb[nbr[0] % 2]
        nbr[0] += 1
        S.copy(bb_[:], pC[:, 0:128], eng="act")
        S.dma(brT_d[1, u, :, t0:t0 + 128], bb_[:], eng="sp")

    def run_interleaved(gens):
        active = [x for x in gens if x is not None]
        while active:
            for gi in list(active):
                try:
                    next(gi)
                except StopIteration:
                    active.remove(gi)

    n_it = len(iters)
    pipelined = DBG.get("rw_pipe", True)
    if pipelined:
        for k in range(n_it + 1):
            run_interleaved([setup(k) if k < n_it else None, chain(k - 1) if k >= 1 else None])
    else:
        for k in range(n_it):
            run_interleaved([setup(k)])
            run_interleaved([chain(k)])
    P.close()


_CACHE = {}


def kernel(**inputs):
    if "kb" not in _CACHE:
        _CACHE["kb"] = build()
    kb = _CACHE["kb"]
    consts = make_consts()
    shared = {}
    for n, shp in PARAMS:
        if n == "c":
            continue
        shared[n] = np.ascontiguousarray(np.asarray(inputs[n], dtype=np.float32))
    for n, v in consts.items():
        shared["k_" + n] = v
    x = np.asarray(inputs["x"], dtype=np.float32)
    c = np.asarray(inputs["c"], dtype=np.float32)
    in_maps = []
    for b in range(8):
        m = dict(shared)
        m["x"] = np.ascontiguousarray(x[b])
        m["c"] = np.ascontiguousarray(c[b:b + 1])
        in_maps.append(m)
    res = run_bass_kernel_spmd(kb.nc, in_maps, core_ids=list(range(8)))
    return np.stack([np.asarray(r["out"], dtype=np.float32) for r in res.results], axis=0)
```

```python
import math
from contextlib import ExitStack

import numpy as np
import concourse.bass as bass
import concourse.mybir as mybir
from concourse.bass_utils import run_bass_kernel_spmd

F32 = mybir.dt.float32
BF16 = mybir.dt.bfloat16
AF = mybir.ActivationFunctionType
ALU = mybir.AluOpType
AX = mybir.AxisListType

ENGS = ("pe", "act", "dve", "pool", "sp")


class _Op:
    __slots__ = ("eng", "fn", "dma", "waits", "key", "val", "know", "inc")


def _region(ap):
    shape = list(ap.tensor.shape)
    off = int(ap.offset)
    if str(ap.space) == "DRAM":
        ext = 0
        for step, cnt in ap.ap:
            ext += (int(cnt) - 1) * abs(int(step))
        return (ap.name, 0, 1, off, off + ext + 1)
    if str(ap.space) == "PSUM":
        return (ap.name, 0, 128, 0, 1 << 30)
    ps = 1
    for s in shape[1:]:
        ps *= int(s)
    p0 = off // ps
    f0 = off % ps
    npart = 1
    ext = 0
    for step, cnt in ap.ap:
        step = int(step)
        cnt = int(cnt)
        if step == ps:
            npart = max(npart, cnt)
        elif step > ps:
            npart = max(npart, (cnt - 1) * (step // ps) + 1)
        else:
            ext += (cnt - 1) * step
    return (ap.name, p0, p0 + npart, f0, f0 + ext + 1)


class Sched:
    def __init__(self, nc, n_dma_sems=12):
        self.nc = nc
        self.streams = {e: [] for e in ENGS}
        self.clock = {e: {} for e in ENGS}
        self.cpos = {e: 0 for e in ENGS}
        self.last_op = {e: None for e in ENGS}
        self.n_dma_sems = n_dma_sems
        self.dma_uses = {}
        self.dma_next = {e: 0 for e in ENGS}
        self.dma_last = {}
        self.acc = {}
        self.readonly = set()
        self.pe_bank = {}
        self.epoch = 0
        self.n_ops = 0

    def _add(self, eng, fn, reads, writes, dma, force=()):
        op = _Op()
        op.eng, op.fn, op.dma = eng, fn, dma
        clk = self.clock[eng]
        need = {}

        def dep(d, forced=False):
            if (not forced) and (not dma) and (not d.dma) and d.eng == "pe" and eng == "pe":
                return
            if clk.get(d.key, 0) >= d.val:
                return
            if need.get(d.key, 0) < d.val:
                need[d.key] = d.val
            for k, v in d.know.items():
                if clk.get(k, 0) < v:
                    clk[k] = v

        for d_ in force:
            dep(d_, True)
        regs = []
        for ap in reads:
            if ap.name in self.readonly:
                continue
            regs.append((_region(ap), False))
        for ap in writes:
            regs.append((_region(ap), True))
        for (name, p0, p1, f0, f1), isw in regs:
            lst = self.acc.get(name)
            if lst is None:
                continue
            for r in lst:
                if r[4] is op:
                    continue
                if r[0] < p1 and p0 < r[1] and r[2] < f1 and f0 < r[3]:
                    if isw or r[5] or (f1 == (1 << 30) and r[4].eng != eng):
                        dep(r[4])
        if dma:
            slot = self.dma_next[eng]
            self.dma_next[eng] = (slot + 1) % self.n_dma_sems
            k = ("s", eng, slot)
            prev = self.dma_last.get(k)
            if prev is not None:
                dep(prev)
            cnt = self.dma_uses.get(k, 0) + 1
            self.dma_uses[k] = cnt
            op.key, op.val, op.inc = k, 16 * cnt, 16
            self.dma_last[k] = op
        else:
            self.cpos[eng] += 1
            op.key, op.val, op.inc = ("e", eng, self.epoch), self.cpos[eng], 1
        for k, v in need.items():
            if clk.get(k, 0) < v:
                clk[k] = v
        op.waits = list(need.items())
        op.know = dict(clk)
        self.streams[eng].append(op)
        self.last_op[eng] = op
        for (name, p0, p1, f0, f1), isw in regs:
            lst = self.acc.setdefault(name, [])
            if isw:
                lst[:] = [r for r in lst if not (p0 <= r[0] and r[1] <= p1 and f0 <= r[2] and r[3] <= f1)]
            else:
                if not dma:
                    lst[:] = [r for r in lst if not ((not r[5]) and (not r[4].dma) and r[4].eng == eng
                                                     and r[0] == p0 and r[1] == p1 and r[2] == f0 and r[3] == f1)]
            lst.append([p0, p1, f0, f1, op, isw])
        self.n_ops += 1
        return op

    def barrier(self):
        targets = []
        for e in ENGS:
            if self.cpos[e] > 0:
                targets.append((("e", e, self.epoch), self.cpos[e], self.last_op[e]))
        for k, o in self.dma_last.items():
            targets.append((k, o.val, o))
        for e in ENGS:
            clk = self.clock[e]
            op = _Op()
            op.eng, op.fn, op.dma = e, None, False
            need = {}
            for k, v, o in targets:
                if clk.get(k, 0) < v:
                    need[k] = v
                    clk[k] = v
            op.waits = list(need.items())
            op.key = None
            op.know = dict(clk)
            self.streams[e].append(op)
        self.acc = {}
        self.pe_bank = {}
        if max(self.cpos.values()) > 16000:
            self.epoch += 1
            self.cpos = {e: 0 for e in ENGS}

    def _pe_bank(self, out, lhsT):
        kpos = (int(lhsT.base_partition()), int(lhsT.partition_size()),
                int(out.base_partition()), int(out.partition_size()))
        prev = self.pe_bank.get(out.name)
        force = ()
        if prev is not None and prev[0] != kpos:
            force = (prev[1],)
        return kpos, force

    def mm(self, out, lhsT, rhs, start=True, stop=True):
        rd = [lhsT, rhs] + ([] if start else [out])
        kpos, force = self._pe_bank(out, lhsT)
        op = self._add("pe", lambda e: e.matmul(out, lhsT, rhs, start=start, stop=stop), rd, [out], False, force=force)
        self.pe_bank[out.name] = (kpos, op)
        return op

    def tr(self, out, in_, ident):
        kpos, force = self._pe_bank(out, in_)
        op = self._add("pe", lambda e: e.transpose(out, in_, ident), [in_, ident], [out], False, force=force)
        self.pe_bank[out.name] = (kpos, op)
        return op

    def act(self, out, in_, func, bias=None, scale=None, accum_out=None):
        rd = [in_]
        kw = {}
        if bias is not None:
            kw["bias"] = bias
            if not isinstance(bias, (int, float)):
                rd.append(bias)
        if scale is not None:
            kw["scale"] = scale
            if not isinstance(scale, (int, float)):
                rd.append(scale)
        wr = [out]
        if accum_out is not None:
            kw["accum_out"] = accum_out
            wr.append(accum_out)
        return self._add("act", lambda e: e.activation(out, in_, func, **kw), rd, wr, False)

    def tt(self, out, in0, in1, op, eng="dve"):
        return self._add(eng, lambda e: e.tensor_tensor(out, in0, in1, op), [in0, in1], [out], False)

    def ts(self, out, in0, s1, s2, op0, op1=None, eng="dve", accum_out=None):
        rd = [in0]
        if not isinstance(s1, (int, float)):
            rd.append(s1)
        if s2 is not None and not isinstance(s2, (int, float)):
            rd.append(s2)
        wr = [out]
        kw = {}
        if accum_out is not None:
            kw["accum_out"] = accum_out
            wr.append(accum_out)
        if op1 is None:
            return self._add(eng, lambda e: e.tensor_scalar(out, in0, s1, None, op0, **kw), rd, wr, False)
        return self._add(eng, lambda e: e.tensor_scalar(out, in0, s1, s2, op0, op1, **kw), rd, wr, False)

    def stt(self, out, in0, scalar, in1, op0, op1):
        rd = [in0, in1]
        if not isinstance(scalar, (int, float)):
            rd.append(scalar)
        return self._add("dve", lambda e: e.scalar_tensor_tensor(out, in0, scalar, in1, op0, op1), rd, [out], False)

    def copy(self, out, in_, eng="dve"):
        if eng == "act":
            return self._add("act", lambda e: e.copy(out, in_), [in_], [out], False)
        return self._add(eng, lambda e: e.tensor_copy(out, in_), [in_], [out], False)

    def memset(self, out, val, eng="dve"):
        return self._add(eng, lambda e: e.memset(out, val), [], [out], False)

    def reduce(self, out, in_, op, axis=AX.X, eng="dve"):
        return self._add(eng, lambda e: e.tensor_reduce(out, in_, axis, op), [in_], [out], False)

    def bn_stats(self, out, in_):
        return self._add("dve", lambda e: e.bn_stats(out, in_), [in_], [out], False)

    def bn_aggr(self, out, in_):
        return self._add("dve", lambda e: e.bn_aggr(out, in_), [in_], [out], False)

    def recip(self, out, in_):
        return self._add("dve", lambda e: e.reciprocal(out, in_), [in_], [out], False)

    def dma(self, out, in_, eng="sp", **kw):
        return self._add(eng, lambda e: e.dma_start(out=out, in_=in_, **kw), [in_], [out], True)

    def emit(self):
        nc = self.nc
        with ExitStack() as es:
            sems = {}
            for e in ENGS:
                for op in self.streams[e]:
                    if op.fn is not None and (not op.dma) and op.key not in sems:
                        sems[op.key] = es.enter_context(nc.semaphore("c_%s_%d" % (op.key[1], op.key[2])))
            for k in self.dma_uses:
                sems[k] = es.enter_context(nc.semaphore("d_%s_%d" % (k[1], k[2])))
            final = [(k, o.val) for k, o in self.dma_last.items()]
            for e in ENGS:
                if self.cpos[e] > 0:
                    final.append((("e", e, self.epoch), self.cpos[e]))
            block = es.enter_context(nc.Block())
            streams = self.streams

            def run(engname, e):
                for op in streams[engname]:
                    for k, v in op.waits:
                        e.wait_ge(sems[k], v)
                    if op.fn is not None:
                        op.fn(e).then_inc(sems[op.key], op.inc)
                if engname == "sp":
                    for k, v in final:
                        e.wait_ge(sems[k], v)

            @block.tensor
            def _(e):
                run("pe", e)

            @block.scalar
            def _(e):
                run("act", e)

            @block.vector
            def _(e):
                run("dve", e)

            @block.gpsimd
            def _(e):
                run("pool", e)

            @block.sync
            def _(e):
                run("sp", e)


DBG = {}
T = 4096
D = 1024
NT = 32
DEPTH = 2
NCOLS = 6936
GLA_OFF, RWKV_OFF, FOX_OFF, HGRN_OFF = 0, 1552, 3344, 4888
DN_ALPHA = (2.0 * DEPTH) ** 0.25
NE = 16


def make_consts():
    c = {}
    i = np.arange(128)
    same = (i[:, None] // 64) == (i[None, :] // 64)
    c["ident"] = np.eye(128, dtype=np.float32)
    c["ones"] = np.ones((128, 128), np.float32)
    c["triu"] = (i[:, None] <= i[None, :]).astype(np.float32)
    c["triu64"] = ((i[:, None] <= i[None, :]) & same).astype(np.float32)
    c["sup64"] = ((i[:, None] < i[None, :]) & same).astype(np.float32)
    c["slo64"] = ((i[:, None] > i[None, :]) & same).astype(np.float32)
    c["blk64"] = same.astype(np.float32)
    return c


CONST_NAMES = ["ident", "ones", "triu", "triu64", "sup64", "slo64", "blk64"]

PARAMS = [
    ("c", [1, D]), ("ada_w", [2, D, 6 * D]), ("ada_b", [2, 6, D]), ("w_in", [2, D, NCOLS]),
    ("gla_alpha_up", [2, 16, 256]), ("gla_alpha_b", [2, 256]), ("gla_norm_g", [2, 128]),
    ("rwkv_mu", [2, 1792]), ("rwkv_w0", [2, 512]), ("rwkv_w2", [2, 64, 512]), ("rwkv_a0", [2, 512]),
    ("rwkv_a2", [2, 64, 512]), ("rwkv_g2", [2, 128, 512]), ("rwkv_k_k", [2, 512]), ("rwkv_k_a", [2, 512]),
    ("rwkv_r_k", [2, 8, 64]), ("rwkv_ln_g", [2, 512]), ("rwkv_ln_b", [2, 512]), ("fox_f_bias", [2, 8]),
    ("hgrn_lb_logits", [2, 512]), ("hgrn_norm_g", [2, 128]), ("w_br", [2, 4, 512, D]),
    ("w_gate", [2, 4, D, D]), ("b_gate", [2, 4, D]), ("w_o", [2, D, D]), ("ln1_g", [2, D]), ("ln1_b", [2, D]),
    ("router_w", [D, NE]), ("router_b", [NE]), ("exp_w_gate", [2, NE, D, 512]), ("exp_w_up", [2, NE, D, 512]),
    ("exp_w_down", [2, NE, 512, D]), ("ln2_g", [2, D]), ("ln2_b", [2, D]),
]


class KB:
    def __init__(self, io=None):
        self.nc = bass.Bass("TRN2", target_bir_lowering=False)
        self.S = Sched(self.nc)
        self.io = io or {}
        self.d = {}
        nc = self.nc
        self.x = self.ext_in("x", [T, D], F32)
        for n, shp in PARAMS:
            self.d[n] = self.ext_in(n, shp, F32)
        self.cst_d = {n: self.ext_in("k_" + n, [128, 128], F32) for n in CONST_NAMES}
        self.psb = [nc.alloc_psum_tensor("psb%d" % i, [128, 512], F32) for i in range(8)]
        self.cst = {n: nc.alloc_sbuf_tensor("c_" + n, [128, 128], F32) for n in CONST_NAMES}
        for n in CONST_NAMES:
            self.S.dma(self.cst[n][:], self.cst_d[n])
        self.hT = None

    def ext_in(self, name, shape, dt):
        self.S.readonly.add(name)
        return self.nc.dram_tensor(name, list(shape), dt, kind="ExternalInput").ap()

    def dram(self, name, shape, dt):
        role = self.io.get(name)
        if role == "in":
            return self.nc.dram_tensor(name, list(shape), dt, kind="ExternalInput").ap()
        if role == "out" or name == "out":
            return self.nc.dram_tensor(name, list(shape), dt, kind="ExternalOutput").ap()
        return self.nc.dram_tensor(name, list(shape), dt).ap()


class Pool_:
    def __init__(self, kb):
        self.kb = kb
        self.es = ExitStack()

    _uid = [0]

    def sb(self, name, shape, dt=F32):
        Pool_._uid[0] += 1
        return self.es.enter_context(self.kb.nc.sbuf_tensor("%s_u%d" % (name, Pool_._uid[0]), list(shape), dt))

    def close(self):
        self.kb.S.barrier()
        self.es.close()


def phase_mod(kb, mod_d):
    S = kb.S
    P = Pool_(kb)
    condT = P.sb("condT", [128, 8])
    load_T(kb, P, condT[:], kb.d["c"].rearrange("o (c p) -> (o c) p", p=128), 8)
    S.act(condT[:], condT[:], AF.Silu)
    wst = [P.sb("adaw%d" % k, [128, 3072]) for k in range(2)]
    mrow = P.sb("mrow", [1, 6144])
    brow = P.sb("brow", [1, 6144])
    n = 0
    for i in range(2):
        S.dma(brow[:], kb.d["ada_b"][i:i + 1].rearrange("o j d -> o (j d)"))
        for half in range(2):
            for kc in range(8):
                w = wst[n % 2]
                n += 1
                S.dma(w[:], kb.d["ada_w"][i, kc * 128:(kc + 1) * 128, half * 3072:(half + 1) * 3072],
                      eng="sp" if n % 2 else "pool")
                for b in range(6):
                    S.mm(kb.psb[b][0:1, :], condT[:, kc:kc + 1], w[:, b * 512:(b + 1) * 512],
                         start=(kc == 0), stop=(kc == 7))
            for b in range(6):
                o = half * 3072 + b * 512
                S.tt(mrow[0:1, o:o + 512], kb.psb[b][0:1, :], brow[0:1, o:o + 512], ALU.add)
        for j in (1, 4):
            S.ts(mrow[0:1, j * 1024:(j + 1) * 1024], mrow[0:1, j * 1024:(j + 1) * 1024], 1.0, None, ALU.add)
        S.dma(mod_d[i:i + 1, :], mrow[:])
    P.close()


def rsqrt_eps(S, out, in_, eps, scale=1.0):
    S.act(out, in_, AF.Ln, bias=float(eps), scale=float(scale))
    S.act(out, out, AF.Exp, scale=-0.5)


def ln_stats(S, xin, st, mv, rstd, eps=1e-5):
    S.bn_stats(st[:, 0:6], xin[:, 0:512])
    S.bn_stats(st[:, 6:12], xin[:, 512:1024])
    S.bn_aggr(mv[:], st[:])
    rsqrt_eps(S, rstd[:], mv[:, 1:2], eps)


def load_T(kb, P, dst, src_rows, n, psum=None):
    S = kb.S
    tmp = P.sb("ldT_tmp", [n, 128])
    S.dma(tmp[:], src_rows)
    ps = kb.psb[7] if psum is None else psum
    S.mm(ps[:, 0:n], tmp[:], kb.cst["ident"][0:n, 0:n], start=True, stop=True)
    S.copy(dst, ps[:, 0:n])


def load_modT(kb, P, mod_d, layer, name):
    modT = P.sb(name, [128, 6, 8])
    load_T(kb, P, modT[:].rearrange("p j c -> p (j c)"), mod_d[layer].rearrange("(r p) -> r p", p=128), 48)
    return modT


def phase_ln_mixer(kb, x_src, mod_d, layer):
    S = kb.S
    P = Pool_(kb)
    modT = load_modT(kb, P, mod_d, layer, "modT_a")
    xb = [P.sb("lnx%d" % k, [128, 1024]) for k in range(2)]
    st = [P.sb("lnst%d" % k, [128, 12]) for k in range(2)]
    mv = [P.sb("lnmv%d" % k, [128, 2]) for k in range(2)]
    rs = [P.sb("lnrs%d" % k, [128, 1]) for k in range(2)]
    ident = kb.cst["ident"]
    for t in range(DBG.get("ln_nt", NT)):
        k = t % 2
        xin = xb[k]
        S.dma(xin[:], x_src[t * 128:(t + 1) * 128, :])
        if DBG.get("ln_lvl", 9) < 1:
            continue
        ln_stats(S, xin, st[k], mv[k], rs[k])
        S.ts(xin[:], xin[:], mv[k][:, 0:1], rs[k][:, 0:1], ALU.subtract, ALU.mult)
        if DBG.get("ln_lvl", 9) < 2:
            continue
        for c in range(8):
            pb = kb.psb[(t % 2) * 2 + c // 4]
            S.tr(pb[:, (c % 4) * 128:(c % 4 + 1) * 128], xin[:, c * 128:(c + 1) * 128], ident[:])
        if DBG.get("ln_lvl", 9) < 3:
            continue
        for c in range(DBG.get("ln_nc", 8)):
            pb = kb.psb[(t % 2) * 2 + c // 4]
            src = pb[:, (c % 4) * 128:(c % 4 + 1) * 128]
            off = DBG.get("ln_off", 1)
            dst = kb.hT[:, c, off + t * 128:off + (t + 1) * 128]
            ev = DBG.get("ln_evac", "both")
            if (c % 2 == 0 and ev == "both") or ev == "dve":
                S.ts(dst, src, modT[:, 1, c:c + 1], modT[:, 0, c:c + 1], ALU.mult, ALU.add)
            else:
                S.act(dst, src, AF.Identity, bias=modT[:, 0, c:c + 1], scale=modT[:, 1, c:c + 1])
    P.close()


def prep_cast(kb, P, jobs, stg, stb):
    S = kb.S
    n = 0
    for dst, src in jobs:
        R, N = src.shape
        for r in range(0, R, 128):
            a, b = stg[n % 2], stb[n % 2]
            n += 1
            S.dma(a[:, 0:N], src[r:r + 128, :], eng="sp")
            S.copy(b[:, 0:N], a[:, 0:N], eng="pool" if n % 2 else "act")
            S.dma(dst[r:r + 128, :], b[:, 0:N], eng="pool")


class Epi:
    def __init__(self, kb, P, mod_d, layer, gt_idx, g_name, b_name, tag, with_z=True):
        S = kb.S
        self.kb = kb
        self.gt = P.sb("epi_gt" + tag, [128, 1024])
        self.g = P.sb("epi_g" + tag, [128, 1024])
        self.b = P.sb("epi_b" + tag, [128, 1024])
        S.dma(self.gt[:], mod_d[layer:layer + 1, gt_idx * 1024:(gt_idx + 1) * 1024].partition_broadcast(128))
        S.dma(self.g[:], kb.d[g_name][layer:layer + 1, :].partition_broadcast(128))
        S.dma(self.b[:], kb.d[b_name][layer:layer + 1, :].partition_broadcast(128))
        self.xb = [P.sb("epi_x%s%d" % (tag, k), [128, 1024]) for k in range(2)]
        self.zb = [P.sb("epi_z%s%d" % (tag, k), [128, 1024]) for k in range(2)] if with_z else None
        self.st = [P.sb("epi_st%s%d" % (tag, k), [128, 12]) for k in range(2)]
        self.mv = [P.sb("epi_mv%s%d" % (tag, k), [128, 2]) for k in range(2)]
        self.rs = [P.sb("epi_rs%s%d" % (tag, k), [128, 1]) for k in range(2)]
        self.n = 0

    def prefetch_x(self, x_src, t):
        k = self.n % 2
        self.kb.S.dma(self.xb[k][:], x_src[t * 128:(t + 1) * 128, :])

    def run(self, y_halves, x_dst, t, x_src=None, z=None):
        S = self.kb.S
        k = self.n % 2
        self.n += 1
        if x_src is not None:
            S.dma(self.xb[k][:], x_src[t * 128:(t + 1) * 128, :])
        x = self.xb[k]
        if z is None:
            z = self.zb[k]
        for h in range(2):
            S.tt(z[:, h * 512:(h + 1) * 512], y_halves[h], self.gt[:, h * 512:(h + 1) * 512], ALU.mult)
        S.stt(z[:], x[:], DN_ALPHA, z[:], ALU.mult, ALU.add)
        ln_stats(S, z, self.st[k], self.mv[k], self.rs[k])
        S.ts(z[:], z[:], self.mv[k][:, 0:1], self.rs[k][:, 0:1], ALU.subtract, ALU.mult)
        S.tt(z[:], z[:], self.g[:], ALU.mult, eng="pool")
        S.tt(z[:], z[:], self.b[:], ALU.add, eng="pool")
        S.dma(x_dst[t * 128:(t + 1) * 128, :], z[:], eng="pool")


def phase_prep_merge(kb, layer, wg_d, wb_d, wo_d):
    P = Pool_(kb)
    stg = [P.sb("pst%d" % k, [128, 1024]) for k in range(2)]
    stb = [P.sb("psb%d" % k, [128, 1024], BF16) for k in range(2)]
    jobs = []
    for n in range(4):
        jobs.append((wg_d[n], kb.d["w_gate"][layer, n]))
        jobs.append((wb_d[n], kb.d["w_br"][layer, n]))
    jobs.append((wo_d, kb.d["w_o"][layer]))
    prep_cast(kb, P, jobs, stg, stb)
    P.close()


def phase_merge(kb, layer, mod_d, brT_d, wg_d, wb_d, wo_d, x_src, x_dst):
    S = kb.S
    P = Pool_(kb)
    epi = Epi(kb, P, mod_d, layer, 2, "ln1_g", "ln1_b", "m")
    bgT = P.sb("bgT", [128, 4, 8])
    load_T(kb, P, bgT[:].rearrange("p n c -> p (n c)"), kb.d["b_gate"][layer].rearrange("n (c p) -> (n c) p", p=128), 32)
    wo = P.sb("wo", [128, 8, 1024], BF16)
    S.dma(wo[:], wo_d.rearrange("(c p) n -> p c n", p=128))
    wg = [P.sb("wg%d" % k, [128, 8, 1024], BF16) for k in range(2)]
    wb = [P.sb("wb%d" % k, [128, 4, 1024], BF16) for k in range(2)]
    brt = [P.sb("brt%d" % k, [128, 4, 512], BF16) for k in range(2)]
    mT = P.sb("mT", [128, 8, 512])
    mTb = P.sb("mTb", [128, 8, 512], BF16)
    sig = [P.sb("sig%d" % k, [128, 512]) for k in range(2)]
    tmp = [P.sb("mtmp%d" % k, [128, 512]) for k in range(2)]
    cnt = 0
    q = 0
    for g in range(8):
        tok = slice(g * 512, (g + 1) * 512)
        for n in range(4):
            k = cnt % 2
            cnt += 1
            S.dma(wg[k][:], wg_d[n].rearrange("(c p) n -> p c n", p=128), eng="sp")
            S.dma(wb[k][:], wb_d[n].rearrange("(c p) n -> p c n", p=128), eng="sp")
            S.dma(brt[k][:], brT_d[n, :, :, tok].rearrange("c p t -> p c t"), eng="sp")
            for cc in range(8):
                pa = kb.psb[(q % 2) * 2]
                pb = kb.psb[(q % 2) * 2 + 1]
                for kc in range(8):
                    S.mm(pa[:], wg[k][:, kc, cc * 128:(cc + 1) * 128], kb.hT[:, kc, 1 + g * 512:1 + (g + 1) * 512],
                         start=(kc == 0), stop=(kc == 7))
                for kc in range(4):
                    S.mm(pb[:], wb[k][:, kc, cc * 128:(cc + 1) * 128], brt[k][:, kc, :],
                         start=(kc == 0), stop=(kc == 3))
                sg = sig[q % 2]
                S.act(sg[:], pa[:], AF.Sigmoid, bias=bgT[:, n, cc:cc + 1])
                if n == 0:
                    S.tt(mT[:, cc, :], sg[:], pb[:], ALU.mult)
                else:
                    tp = tmp[q % 2]
                    S.tt(tp[:], sg[:], pb[:], ALU.mult)
                    S.tt(mT[:, cc, :], mT[:, cc, :], tp[:], ALU.add, eng="pool" if cc % 2 else "dve")
                q += 1
        for cc in range(8):
            S.copy(mTb[:, cc, :], mT[:, cc, :], eng="act" if cc % 2 else "pool")
        for tt in range(4):
            t = g * 4 + tt
            epi.prefetch_x(x_src, t)
            ys = []
            for h in range(2):
                py = kb.psb[4 + (t % 2) * 2 + h]
                for kc in range(8):
                    S.mm(py[:], mTb[:, kc, tt * 128:(tt + 1) * 128], wo[:, kc, h * 512:(h + 1) * 512],
                         start=(kc == 0), stop=(kc == 7))
                ys.append(py[:])
            epi.run(ys, x_dst, t)
    P.close()


def phase_moe(kb, layer, mod_d, x_src, x_dst):
    S = kb.S
    P = Pool_(kb)
    NSG = 2
    TSG = T // NSG
    NTS = TSG // 128
    epi = Epi(kb, P, mod_d, layer, 5, "ln2_g", "ln2_b", "e", with_z=False)
    modT = load_modT(kb, P, mod_d, layer, "modT_e")
    rw = P.sb("rw", [128, 8, NE])
    S.dma(rw[:], kb.d["router_w"].rearrange("(c p) e -> p c e", p=128))
    rb = P.sb("rb", [128, NE])
    S.dma(rb[:], kb.d["router_b"].rearrange("(o e) -> o e", o=1).partition_broadcast(128))
    hT = P.sb("hTm", [128, 8, TSG + 1], BF16)
    yacc = P.sb("yacc", [128, NTS, 1024])
    comb = P.sb("comb", [128, NTS, NE])
    h32 = [P.sb("h32_%d" % k, [128, 8, 128]) for k in range(2)]
    st = [P.sb("mst%d" % k, [128, 12]) for k in range(2)]
    mv = [P.sb("mmv%d" % k, [128, 2]) for k in range(2)]
    rs = [P.sb("mrs%d" % k, [128, 1]) for k in range(2)]
    lg = P.sb("r_lg", [128, NE])
    pr = P.sb("r_pr", [128, NE])
    sel = P.sb("r_sel", [128, NE])
    sel2 = P.sb("r_sel2", [128, NE])
    eq = P.sb("r_eq", [128, NE])
    m1 = P.sb("r_m1", [128, 4])
    m2 = P.sb("r_m2", [128, 4])
    gs = P.sb("r_gs", [128, 4])
    gm = P.sb("r_gm", [128, 1])
    og = P.sb("r_og", [128, 4])
    thr = P.sb("r_thr", [128, 4])
    msk = P.sb("r_msk", [128, NE])
    sm = P.sb("r_sm", [128, 1])
    mx = P.sb("r_mx", [128, 1])
    wg = [P.sb("ewg%d" % k, [128, 8, 512], BF16) for k in range(2)]
    wu = [P.sb("ewu%d" % k, [128, 8, 512], BF16) for k in range(2)]
    wd = [P.sb("ewd%d" % k, [128, 4, 1024], BF16) for k in range(2)]
    stg = [P.sb("estg%d" % k, [128, 2, 512]) for k in range(3)]
    heT = [P.sb("heT%d" % k, [128, 4, 512], BF16) for k in range(2)]
    sl = [P.sb("esl%d" % k, [128, 512]) for k in range(2)]
    ident = kb.cst["ident"]
    nld = [0]

    def load_expert(e, k):
        for (dst, src, kcn) in ((wg[k], kb.d["exp_w_gate"][layer, e], 8), (wu[k], kb.d["exp_w_up"][layer, e], 8)):
            sv = src.rearrange("(c p) n -> p c n", p=128)
            for c2 in range(0, kcn, 2):
                sg_ = stg[nld[0] % 3]
                nld[0] += 1
                S.dma(sg_[:], sv[:, c2:c2 + 2, :], eng="sp")
                S.copy(dst[:, c2:c2 + 2, :], sg_[:], eng="pool")
        sv = kb.d["exp_w_down"][layer, e].rearrange("(c p) n -> p c n", p=128)
        for c in range(4):
            sg_ = stg[nld[0] % 3]
            nld[0] += 1
            S.dma(sg_[:].rearrange("p a b -> p (a b)"), sv[:, c, :], eng="sp")
            S.copy(wd[k][:, c, :], sg_[:].rearrange("p a b -> p (a b)"), eng="pool")

    for sgi in range(NSG):
        t0 = sgi * NTS
        for tl in range(NTS):
            t = t0 + tl
            k = tl % 2
            xin = epi.xb[k]
            S.dma(xin[:], x_src[t * 128:(t + 1) * 128, :])
            ln_stats(S, xin, st[k], mv[k], rs[k])
            S.ts(xin[:], xin[:], mv[k][:, 0:1], rs[k][:, 0:1], ALU.subtract, ALU.mult)
            for c in range(8):
                pb = kb.psb[k * 2 + c // 4]
                S.tr(pb[:, (c % 4) * 128:(c % 4 + 1) * 128], xin[:, c * 128:(c + 1) * 128], ident[:])
            for c in range(8):
                pb = kb.psb[k * 2 + c // 4]
                src = pb[:, (c % 4) * 128:(c % 4 + 1) * 128]
                if c % 2 == 0:
                    S.ts(h32[k][:, c, :], src, modT[:, 4, c:c + 1], modT[:, 3, c:c + 1], ALU.mult, ALU.add)
                else:
                    S.act(h32[k][:, c, :], src, AF.Identity, bias=modT[:, 3, c:c + 1], scale=modT[:, 4, c:c + 1])
                S.copy(hT[:, c, 1 + tl * 128:1 + (tl + 1) * 128], h32[k][:, c, :], eng="pool")
            pl = kb.psb[4 + k]
            for c in range(8):
                S.mm(pl[:, 0:NE], h32[k][:, c, :], rw[:, c, :], start=(c == 0), stop=(c == 7))
            S.copy(lg[:], pl[:, 0:NE])
            S.reduce(mx[:], lg[:], ALU.max)
            S.ts(mx[:], mx[:], -1.0, None, ALU.mult)
            S.act(pr[:], lg[:], AF.Exp, bias=mx[:, 0:1], scale=1.0, accum_out=sm[:])
            S.recip(sm[:], sm[:])
            S.ts(pr[:], pr[:], sm[:, 0:1], None, ALU.mult)
            S.tt(sel[:], pr[:], rb[:], ALU.add)
            sel3 = sel[:].rearrange("p (g e) -> p g e", g=4)
            S.reduce(m1[:], sel3, ALU.max)
            S.tt(eq[:].rearrange("p (g e) -> p g e", g=4), sel3, m1[:].unsqueeze(2).to_broadcast([128, 4, 4]), ALU.is_ge)
            S.stt(sel2[:], eq[:], -1e9, sel[:], ALU.mult, ALU.add)
            S.reduce(m2[:], sel2[:].rearrange("p (g e) -> p g e", g=4), ALU.max)
            S.tt(gs[:], m1[:], m2[:], ALU.add)
            S.reduce(gm[:], gs[:], ALU.max)
            S.ts(og[:], gs[:], gm[:, 0:1], None, ALU.is_ge)
            S.ts(thr[:], og[:], -1e9, 1e9, ALU.mult, ALU.add)
            S.tt(thr[:], thr[:], m2[:], ALU.add)
            S.tt(msk[:].rearrange("p (g e) -> p g e", g=4), sel3, thr[:].unsqueeze(2).to_broadcast([128, 4, 4]), ALU.is_ge)
            S.tt(msk[:], msk[:], pr[:], ALU.mult)
            S.reduce(sm[:], msk[:], ALU.add)
            S.recip(sm[:], sm[:])
            S.ts(comb[:, tl, :], msk[:], sm[:, 0:1], None, ALU.mult)
        if sgi == 0:
            load_expert(0, 0)
        q = 0
        for e in range(NE):
            k = (sgi * NE + e) % 2
            nxt = sgi * NE + e + 1
            if nxt < NSG * NE:
                load_expert(nxt % NE, nxt % 2)
            for gq in range(NTS // 4):
                he = heT[gq % 2]
                for fc in range(4):
                    pg = kb.psb[(q % 2) * 2]
                    pu = kb.psb[(q % 2) * 2 + 1]
                    for kc in range(8):
                        S.mm(pg[:], wg[k][:, kc, fc * 128:(fc + 1) * 128], hT[:, kc, 1 + gq * 512:1 + (gq + 1) * 512],
                             start=(kc == 0), stop=(kc == 7))
                    for kc in range(8):
                        S.mm(pu[:], wu[k][:, kc, fc * 128:(fc + 1) * 128], hT[:, kc, 1 + gq * 512:1 + (gq + 1) * 512],
                             start=(kc == 0), stop=(kc == 7))
                    s_ = sl[q % 2]
                    S.act(s_[:], pg[:], AF.Silu)
                    S.tt(he[:, fc, :], s_[:], pu[:], ALU.mult)
                    q += 1
                for tt in range(4):
                    tl = gq * 4 + tt
                    for h in range(2):
                        py = kb.psb[4 + (tl * 2 + h) % 4]
                        for fc in range(4):
                            S.mm(py[:], he[:, fc, tt * 128:(tt + 1) * 128], wd[k][:, fc, h * 512:(h + 1) * 512],
                                 start=(fc == 0), stop=(fc == 3))
                        ya = yacc[:, tl, h * 512:(h + 1) * 512]
                        if e == 0:
                            S.ts(ya, py[:], comb[:, tl, e:e + 1], None, ALU.mult)
                        else:
                            S.stt(ya, py[:], comb[:, tl, e:e + 1], ya, ALU.mult, ALU.add)
        for tl in range(NTS):
            t = t0 + tl
            epi.run([yacc[:, tl, 0:512], yacc[:, tl, 512:1024]], x_dst, t, x_src=x_src, z=yacc[:, tl, :])
    P.close()


def build(io=None, layers=(0, 1), stages=("mod", "ln", "prep", "br", "merge", "moe"), last_out=None):
    kb = KB(io)
    S = kb.S
    mod_d = kb.dram("mod_d", [2, 6144], F32)
    xa = kb.dram("xa", [T, D], F32)
    xbd = kb.dram("xbd", [T, D], F32)
    out = kb.dram("out", [T, D], F32)
    brT_d = kb.dram("brT", [4, 4, 128, T], BF16)
    wg_d = kb.dram("wg_bf", [4, D, D], BF16)
    wb_d = kb.dram("wb_bf", [4, 512, D], BF16)
    wo_d = kb.dram("wo_bf", [D, D], BF16)
    kb.lb_d = kb.dram("lb_d", [2, 512], F32)
    if "mod" in stages:
        phase_mod(kb, mod_d)
    for layer in layers:
        x_src = kb.x if layer == 0 else xbd
        x_fin = out if layer == layers[-1] else xbd
        MP = Pool_(kb)
        kb.hT = MP.sb("hT", [128, 8, T + 1], BF16)
        for c in range(8):
            S.memset(kb.hT[:, c, 0:1], 0.0, eng="pool")
        if "ln" in stages:
            phase_ln_mixer(kb, x_src, mod_d, layer)
        if "prep" in stages:
            phase_prep_merge(kb, layer, wg_d, wb_d, wo_d)
        if "br" in stages:
            phase_branches(kb, layer, brT_d)
        if "merge" in stages:
            phase_merge(kb, layer, mod_d, brT_d, wg_d, wb_d, wo_d, x_src, xa if "moe" in stages else x_fin)
        MP.close()
        if "moe" in stages:
            phase_moe(kb, layer, mod_d, xa, x_fin)
    S.emit()
    return kb


def load_w(kb, dst, src, stg, cast_eng="pool", dma_eng="sp"):
    S = kb.S
    kc = src.shape[0] // 128
    n = src.shape[1]
    sv = stg[:, 0:kc, 0:n]
    S.dma(sv, src.rearrange("(c p) n -> p c n", p=128), eng=dma_eng)
    S.copy(dst, sv, eng=cast_eng)


def tok(t0, n=128):
    return slice(1 + t0, 1 + t0 + n)


def fox_branch(kb, layer, brT_d):
    S = kb.S
    P = Pool_(kb)
    hT = kb.hT
    W = kb.d["w_in"][layer]
    o = FOX_OFF
    psb = kb.psb
    stg = P.sb("fstg", [128, 8, 520])
    wq = P.sb("fwq", [128, 8, 512], BF16)
    wk = P.sb("fwk", [128, 8, 512], BF16)
    wvf = P.sb("fwvf", [128, 8, 520], BF16)
    load_w(kb, wq[:], W[:, o:o + 512], stg)
    load_w(kb, wk[:], W[:, o + 512:o + 1024], stg)
    load_w(kb, wvf[:], W[:, o + 1024:o + 1544], stg)
    fb = P.sb("ffb", [128, 8])
    S.dma(fb[:], kb.d["fox_f_bias"][layer:layer + 1, :].partition_broadcast(128))
    maskb = P.sb("fmask", [128, 128], BF16)
    S.copy(maskb[:], kb.cst["triu"][:])
    lf = P.sb("flf", [128, 32, 8])
    tA = P.sb("ftA", [128, 32, 8])
    tB = P.sb("ftB", [128, 32, 8])
    Fs = P.sb("fFs", [128, 32, 8])
    Cs = P.sb("fCs", [128, 32, 8])
    vp = P.sb("fvp", [128, 32, 8, 65], BF16)
    S.memset(vp[:].rearrange("p a b c -> p (a b c)"), 1.0, eng="pool")
    for g in range(8):
        pb = psb[6 + g % 2]
        for tt in range(4):
            t = g * 4 + tt
            for kc in range(8):
                S.mm(pb[:, tt * 8:(tt + 1) * 8], hT[:, kc, tok(t * 128)], wvf[:, kc, 512:520], start=(kc == 0), stop=(kc == 7))
        S.tt(lf[:, g * 4:(g + 1) * 4, :], pb[:, 0:32].rearrange("p (a b) -> p a b", a=4),
             fb[:].unsqueeze(1).to_broadcast([128, 4, 8]), ALU.add)
    lf2 = lf[:].rearrange("p a b -> p (a b)")
    S.act(lf2, lf2, AF.Exp, scale=-1.0)
    S.act(lf2, lf2, AF.Ln, bias=1.0)
    S.ts(lf2, lf2, -1.0, None, ALU.mult)
    a, b = lf, tA
    d = 1
    while d < 32:
        nb = tA if b is tA else tB
        if a is lf:
            nb = tA
        S.tt(nb[:, d:32, :], a[:, d:32, :], a[:, 0:32 - d, :], ALU.add)
        S.copy(nb[:, 0:d, :], a[:, 0:d, :])
        a = nb
        b = tB if nb is tA else tA
        d *= 2
    incl = a
    excl = tB if incl is tA else tA
    S.tt(excl[:], incl[:], lf[:], ALU.subtract)
    pF, pC = psb[6], psb[7]
    S.mm(pF[:, 0:256], kb.cst["triu"][:], lf2, start=True, stop=False)
    S.mm(pF[:, 0:256], kb.cst["ones"][:], excl[:].rearrange("p a b -> p (a b)"), start=False, stop=True)
    S.mm(pC[:, 0:256], kb.cst["ones"][:], incl[:].rearrange("p a b -> p (a b)"), start=True, stop=True)
    S.copy(Fs[:].rearrange("p a b -> p (a b)"), pF[:, 0:256])
    S.copy(Cs[:].rearrange("p a b -> p (a b)"), pC[:, 0:256], eng="act")
    for t in range(32):
        pb = psb[6 + t % 2]
        for kc in range(8):
            S.mm(pb[:], hT[:, kc, tok(t * 128)], wvf[:, kc, 0:512], start=(kc == 0), stop=(kc == 7))
        src = pb[:].rearrange("p (h d) -> p h d", h=8)
        if t % 2 == 0:
            S.copy(vp[:, t, :, 0:64], src, eng="dve")
        else:
            S.copy(vp[:, t, :, 0:64], src, eng="act")
    QT = P.sb("fQT", [128, T], BF16)
    KA = P.sb("fKA", [128, T], BF16)
    KB_ = P.sb("fKB", [128, T], BF16)
    S.memset(KA[64:128, :], 0.0, eng="pool")
    S.memset(KB_[0:64, :], 0.0, eng="pool")
    otok = P.sb("fotok", [128, 32, 128])
    brs = P.sb("fbrs", [128, T], BF16)
    pts = [P.sb("fpt%d" % k, [128, 512], BF16) for k in range(4)]
    vss = [P.sb("fvs%d" % k, [128, 65], BF16) for k in range(6)]
    biases = [P.sb("fbias%d" % k, [128, 32]) for k in range(4)]
    dms = [P.sb("fdm%d" % k, [128, 32]) for k in range(4)]
    rcs = [P.sb("frc%d" % k, [128, 1]) for k in range(4)]
    q = 0
    nb_ = 0
    nv = 0
    for p in range(4):
        for g in range(8):
            pq = psb[6]
            pk = psb[7]
            for kc in range(8):
                S.mm(pq[:], wq[:, kc, p * 128:(p + 1) * 128], hT[:, kc, tok(g * 512, 512)], start=(kc == 0), stop=(kc == 7))
            for kc in range(8):
                S.mm(pk[:], wk[:, kc, p * 128:(p + 1) * 128], hT[:, kc, tok(g * 512, 512)], start=(kc == 0), stop=(kc == 7))
            S.act(QT[:, g * 512:(g + 1) * 512], pq[:], AF.Copy, scale=0.125)
            S.copy(KA[0:64, g * 512:(g + 1) * 512], pk[0:64, :])
            S.copy(KB_[64:128, g * 512:(g + 1) * 512], pk[64:128, :])
        rows = []
        for a_ in range(2):
            for i in range(32):
                rows.append((a_, i))
        batches = []
        for ri, (a_, i) in enumerate(rows):
            for jb in range(0, i + 1, 4):
                batches.append((ri, a_, i, jb, min(4, i + 1 - jb)))
        rowbuf = {}

        def front(bt):
            nonlocal q, nb_
            ri, a_, i, jb, nbt = bt
            h = 2 * p + a_
            Kh = KA if a_ == 0 else KB_
            if jb == 0:
                k2 = nb_ % 4
                nb_ += 1
                rowbuf[ri] = k2
                S.ts(biases[k2][:, 0:i + 1], Fs[:, 0:i + 1, h], Cs[:, i, h:h + 1], -1.0, ALU.subtract, ALU.mult)
                S.act(dms[k2][:, 0:i + 1], biases[k2][:, 0:i + 1], AF.Exp)
            ps_s = psb[q % 4]
            pt = pts[q % 4]
            q += 1
            for jj in range(nbt):
                j = jb + jj
                S.mm(ps_s[:, jj * 128:(jj + 1) * 128], Kh[:, j * 128:(j + 1) * 128], QT[:, i * 128:(i + 1) * 128])
            S.act(pt[:, 0:nbt * 128], ps_s[:, 0:nbt * 128], AF.Exp)
            if jb + nbt - 1 == i:
                S.tt(pt[:, (nbt - 1) * 128:nbt * 128], pt[:, (nbt - 1) * 128:nbt * 128], maskb[:], ALU.mult)
            return pt

        def back(bt, pt):
            nonlocal nv
            ri, a_, i, jb, nbt = bt
            h = 2 * p + a_
            k2 = rowbuf[ri]
            po = psb[4 + ri % 2]
            dm = dms[k2]
            rc = rcs[k2]
            for jj in range(nbt):
                j = jb + jj
                vs = vss[nv % 6]
                nv += 1
                S.ts(vs[:], vp[:, j, h, :], dm[:, j:j + 1], None, ALU.mult)
                S.mm(po[:, 0:65], pt[:, jj * 128:(jj + 1) * 128], vs[:], start=(j == 0), stop=(j == i))
            if jb + nbt - 1 == i:
                S.recip(rc[:], po[:, 64:65])
                S.ts(otok[:, i, a_ * 64:(a_ + 1) * 64], po[:, 0:64], rc[:, 0:1], None, ALU.mult)
                if a_ == 1:
                    pt_ = psb[6 + i % 2]
                    S.tr(pt_[:, 0:128], otok[:, i, :], kb.cst["ident"][:])
                    S.copy(brs[:, i * 128:(i + 1) * 128], pt_[:, 0:128], eng="act" if i % 2 else "dve")

        LOOK = DBG.get("fox_look", 2)
        pend = []
        for bt in batches:
            ptn = front(bt)
            pend.append((bt, ptn))
            if len(pend) > LOOK:
                back(*pend.pop(0))
        while pend:
            back(*pend.pop(0))
        S.dma(brT_d[2, p], brs[:], eng="pool")
    P.close()


def phase_branches(kb, layer, brT_d):
    which = DBG.get("branches", (0, 1, 2, 3))
    if 2 in which:
        fox_branch(kb, layer, brT_d)
    if 0 in which:
        gla_branch(kb, layer, brT_d)
    if 3 in which:
        hgrn_branch(kb, layer, brT_d)
    if 1 in which:
        rwkv_branch(kb, layer, brT_d)


def cgla_branch(kb, layer, brT_d, kind, lb_d=None):
    S = kb.S
    P = Pool_(kb)
    hT = kb.hT
    W = kb.d["w_in"][layer]
    psb = kb.psb
    cst = kb.cst
    gla = (kind == "gla")
    NU = 2 if gla else 4
    HPU = 2 if gla else 1
    DK = 64 if gla else 128
    KW = NU * 128
    o = GLA_OFF if gla else HGRN_OFF
    bidx = 0 if gla else 3
    qscale = 0.125 if gla else 1.0
    stg = P.sb("cstg", [128, 8, 528])
    if gla:
        wq = P.sb("cwq", [128, 8, 256], BF16)
        wk = P.sb("cwk", [128, 8, 256], BF16)
        wv = P.sb("cwv", [128, 8, 512], BF16)
        wg = P.sb("cwg", [128, 8, 528], BF16)
        load_w(kb, wq[:], W[:, o:o + 256], stg)
        load_w(kb, wk[:], W[:, o + 256:o + 512], stg)
        load_w(kb, wv[:], W[:, o + 512:o + 1024], stg)
        load_w(kb, wg[:], W[:, o + 1024:o + 1552], stg)
        aup = P.sb("caup", [16, 256])
        S.dma(aup[:], kb.d["gla_alpha_up"][layer])
        abr = P.sb("cabr", [1, 256])
        S.dma(abr[:], kb.d["gla_alpha_b"][layer:layer + 1, :])
        alT = P.sb("calT", [16, 512])
        Uc = P.sb("cUc", [128, 128])
        SUl = P.sb("cSUl", [128, 128])
        S.ts(Uc[:], cst["triu64"][:], -1.0 / 16.0, None, ALU.mult)
        S.ts(SUl[:], cst["slo64"][:], -1.0 / 16.0, None, ALU.mult)
        ng_src = kb.d["gla_norm_g"]
    else:
        wq = P.sb("cwq", [128, 8, 512], BF16)
        wk = P.sb("cwk", [128, 8, 512], BF16)
        wv = P.sb("cwv", [128, 8, 512], BF16)
        wg = P.sb("cwg", [128, 8, 512], BF16)
        load_w(kb, wq[:], W[:, o:o + 512], stg)
        load_w(kb, wk[:], W[:, o + 512:o + 1024], stg)
        load_w(kb, wv[:], W[:, o + 1024:o + 1536], stg)
        load_w(kb, wg[:], W[:, o + 1536:o + 2048], stg)
        Uc, SUl = cst["triu64"], cst["slo64"]
        lbB = P.sb("clbB", [128, 512])
        omlB = P.sb("comlB", [128, 512])
        S.dma(lbB[:], lb_d[0:1, :].partition_broadcast(128))
        S.ts(omlB[:], lbB[:], -1.0, 1.0, ALU.mult, ALU.add)
        lbT = P.sb("clbT", [128, 4])
        omlT = P.sb("comlT", [128, 4])
        load_T(kb, P, lbT[:], lb_d[0].rearrange("(c p) -> c p", p=128), 4)
        S.ts(omlT[:], lbT[:], -1.0, 1.0, ALU.mult, ALU.add)
        ng_src = kb.d["hgrn_norm_g"]
    ngb = P.sb("cngb", [128, 128])
    S.dma(ngb[:], ng_src[layer:layer + 1, :].partition_broadcast(128))
    qTs = [P.sb("cqTs%d" % u, [128, 512]) for u in range(NU)]
    kTs = [P.sb("ckTs%d" % u, [128, 512]) for u in range(NU)]
    brs = P.sb("cbrs", [128, 4, T], BF16)
    l_tok = P.sb("cltok", [128, KW])
    k_tok = P.sb("cktok", [128, KW])
    f_tok = P.sb("cftok", [128, KW])
    v_tok = P.sb("cvtok", [128, 512], BF16)
    sg_tok = P.sb("csgtok", [128, 512])
    br_tok = P.sb("cbrtok", [128, 512])
    bTs = P.sb("cbTs", [128, 128])
    kdec = P.sb("ckdec", [128, 128])
    khat = P.sb("ckhat", [128, 128], BF16)
    bm = P.sb("cbm", [128, 2])
    nbm = P.sb("cnbm", [128, 2])
    E1 = P.sb("cE1", [128, 128])
    E2 = P.sb("cE2", [128, 128])
    E3 = P.sb("cE3", [128, 128])
    qt = P.sb("cqt", [128, 128], BF16)
    kt = P.sb("ckt", [128, 128], BF16)
    qA = P.sb("cqA", [128, 128], BF16)
    qB = P.sb("cqB", [128, 128], BF16)
    attb = [P.sb("cattb%d" % a, [128, 128], BF16) for a in range(HPU)]
    Sf = [[P.sb("cSf%d_%d" % (u, k), [128, 128]) for k in range(2)] for u in range(NU)]
    Sb = [[P.sb("cSb%d_%d" % (u, k), [128, 128], BF16) for k in range(2)] for u in range(NU)]
    for u in range(NU):
        S.memset(Sf[u][0][:], 0.0)
        S.memset(Sb[u][0][:], 0.0)
    ss = P.sb("css", [128, 2])
    junk = P.sb("cjunk", [128, 128])
    v_tokD = [v_tok, P.sb("cvtok2", [128, 512], BF16)]
    sg_tokD = [sg_tok, P.sb("csgtok2", [128, 512])]
    br_tokD = [br_tok, P.sb("cbrtok2", [128, 512])]
    khatD = [khat, P.sb("ckhat2", [128, 128], BF16)]
    qAD = [qA, P.sb("cqA2", [128, 128], BF16)]
    attbD = [attb, [P.sb("cattb2_%d" % a, [128, 128], BF16) for a in range(HPU)]]
    eblD = [P.sb("cebl%d" % k, [128, 2]) for k in range(2)]
    iters = []
    for g in range(DBG.get('cg_ng', 8)):
        for tt in range(4):
            for u in range(NU):
                iters.append((g, tt, u))

    def setup(it):
        g, tt, u = iters[it]
        bf = it % 2
        t = g * 4 + tt
        tp = t % 2
        gt = tok(g * 512, 512)
        tk = tok(t * 128)
        tcol = slice(tt * 128, (tt + 1) * 128)
        v_tok, sg_tok = v_tokD[tp], sg_tokD[tp]
        khat, qA, attb, ebl = khatD[bf], qAD[bf], attbD[bf], eblD[bf]
        if tt == 0 and u == 0:
            for u2 in range(NU):
                pq, pk = psb[0], psb[1]
                for kc in range(8):
                    S.mm(pq[:], wq[:, kc, u2 * 128:(u2 + 1) * 128], hT[:, kc, gt], start=(kc == 0), stop=(kc == 7))
                for kc in range(8):
                    S.mm(pk[:], wk[:, kc, u2 * 128:(u2 + 1) * 128], hT[:, kc, gt], start=(kc == 0), stop=(kc == 7))
                yield
                S.copy(qTs[u2][:], pq[:], eng="act")
                if gla:
                    S.copy(kTs[u2][:], pk[:], eng="dve")
                else:
                    S.act(kTs[u2][:], pk[:], AF.Sigmoid)
                    S.ts(kTs[u2][:], kTs[u2][:], omlT[:, u2:u2 + 1], lbT[:, u2:u2 + 1], ALU.mult, ALU.add)
                    S.ts(kTs[u2][:], kTs[u2][:], -1.0, 1.0, ALU.mult, ALU.add)
                yield
            if gla:
                pa = psb[2]
                for kc in range(8):
                    S.mm(pa[0:16, :], wg[:, kc, 512:528], hT[:, kc, gt], start=(kc == 0), stop=(kc == 7))
                S.copy(alT[:], pa[0:16, :])
                yield
        if u == 0:
            pv, pg = psb[2], psb[3]
            for kc in range(8):
                S.mm(pv[:], hT[:, kc, tk], wv[:, kc, 0:512], start=(kc == 0), stop=(kc == 7))
            for kc in range(8):
                S.mm(pg[:], hT[:, kc, tk], wg[:, kc, 0:512], start=(kc == 0), stop=(kc == 7))
            yield
            S.copy(v_tok[:], pv[:], eng="act")
            S.act(sg_tok[:], pg[:], AF.Silu)
            pk2 = psb[2]
            if gla:
                for kc in range(8):
                    S.mm(pk2[:, 0:256], hT[:, kc, tk], wk[:, kc, 0:256], start=(kc == 0), stop=(kc == 7))
                S.mm(pk2[:, 256:512], alT[:, tcol], aup[:], start=True, stop=False)
                S.mm(pk2[:, 256:512], cst["ones"][0:1, :], abr[:], start=False, stop=True)
                yield
                S.copy(k_tok[:], pk2[:, 0:256])
                S.act(l_tok[:], pk2[:, 256:512], AF.Exp, scale=-1.0)
                S.act(l_tok[:], l_tok[:], AF.Ln, bias=1.0)
            else:
                for kc in range(8):
                    S.mm(pk2[:], hT[:, kc, tk], wk[:, kc, 0:512], start=(kc == 0), stop=(kc == 7))
                yield
                S.act(f_tok[:], pk2[:], AF.Sigmoid)
                S.tt(f_tok[:], f_tok[:], omlB[:], ALU.mult)
                S.tt(f_tok[:], f_tok[:], lbB[:], ALU.add)
                S.act(l_tok[:], f_tok[:], AF.Ln)
                S.ts(k_tok[:], f_tok[:], -1.0, 1.0, ALU.mult, ALU.add)
            yield
        cu = slice(u * 128, (u + 1) * 128)
        pX = psb[4]
        S.mm(pX[:, 0:128], l_tok[:, cu], Uc[:])
        S.mm(pX[:, 128:256], SUl[:], l_tok[:, cu])
        yield
        S.copy(bTs[:], pX[:, 0:128])
        S.act(kdec[:], pX[:, 128:256], AF.Exp)
        S.tt(khat[:], k_tok[:, cu], kdec[:], ALU.mult)
        mid = bTs[:].rearrange("p (c s) -> p c s", c=2)[:, :, 32]
        S.copy(bm[:], mid)
        S.ts(nbm[:], mid, -1.0, None, ALU.mult)
        yield
        for c in range(2):
            cs = slice(c * 64, (c + 1) * 64)
            S.act(E1[:, cs], bTs[:, cs], AF.Exp, bias=nbm[:, c:c + 1])
            S.act(E2[:, cs], bTs[:, cs], AF.Exp, bias=bm[:, c:c + 1], scale=-1.0)
        S.act(E3[:], bTs[:], AF.Exp)
        yield
        S.stt(qt[:], qTs[u][:, tcol], qscale, E1[:], ALU.mult, ALU.mult)
        S.tt(kt[:], kTs[u][:, tcol], E2[:], ALU.mult)
        S.stt(qA[:], qTs[u][:, tcol], qscale, E3[:], ALU.mult, ALU.mult)
        S.copy(ebl[:], E3[:].rearrange("p (c s) -> p c s", c=2)[:, :, 63])
        pAtt = psb[5]
        for a in range(HPU):
            ra = slice(a * DK, (a + 1) * DK)
            S.mm(pAtt[:, a * 128:(a + 1) * 128], kt[ra, :], qt[ra, :])
        yield
        for a in range(HPU):
            S.tt(attb[a][:], cst["triu64"][:], pAtt[:, a * 128:(a + 1) * 128], ALU.mult)
        yield

    def chain(it):
        g, tt, u = iters[it]
        bf = it % 2
        t = g * 4 + tt
        tp = t % 2
        v_tok, sg_tok, br_tok = v_tokD[tp], sg_tokD[tp], br_tokD[tp]
        khat, qA, attb, ebl = khatD[bf], qAD[bf], attbD[bf], eblD[bf]
        S0f, S1f = Sf[u][0], Sf[u][1]
        S0b, S1b = Sb[u][0], Sb[u][1]
        pS = psb[6]
        for c in range(2):
            rows = slice(c * 64, (c + 1) * 64)
            for a in range(HPU):
                h = u * HPU + a
                ra = slice(a * DK, (a + 1) * DK)
                S.mm(pS[ra, c * 128:(c + 1) * 128], khat[rows, a * DK:(a + 1) * DK], v_tok[rows, h * 128:(h + 1) * 128])
        yield
        S.stt(S1f[:], S0f[:], ebl[:, 0:1], pS[:, 0:128], ALU.mult, ALU.add)
        S.copy(S1b[:], S1f[:], eng="act")
        yield
        pO = psb[7]
        for a in range(HPU):
            h = u * HPU + a
            ra = slice(a * DK, (a + 1) * DK)
            oc = slice(a * 128, (a + 1) * 128)
            S.mm(pO[0:64, oc], qA[ra, 0:64], S0b[ra, :], start=True, stop=False)
            S.mm(pO[64:128, oc], qA[ra, 64:128], S1b[ra, :], start=True, stop=False)
            S.mm(pO[:, oc], attb[a][:], v_tok[:, h * 128:(h + 1) * 128], start=False, stop=True)
        yield
        S.stt(S0f[:], S1f[:], ebl[:, 1:2], pS[:, 128:256], ALU.mult, ALU.add)
        S.copy(S0b[:], S0f[:], eng="act")
        for a in range(HPU):
            oc = slice(a * 128, (a + 1) * 128)
            S.act(junk[:], pO[:, oc], AF.Square, accum_out=ss[:, a:a + 1])
        yield
        for a in range(HPU):
            rsqrt_eps(S, ss[:, a:a + 1], ss[:, a:a + 1], 1e-6, scale=1.0 / 128.0)
        yield
        for a in range(HPU):
            h = u * HPU + a
            oc = slice(a * 128, (a + 1) * 128)
            hc = slice(h * 128, (h + 1) * 128)
            S.stt(br_tok[:, hc], pO[:, oc], ss[:, a:a + 1], ngb[:], ALU.mult, ALU.mult)
            S.tt(br_tok[:, hc], br_tok[:, hc], sg_tok[:, hc], ALU.mult)
        yield
        if u == NU - 1:
            for kc in range(4):
                pT = pO if kc < 2 else pS
                S.tr(pT[:, 256 + (kc % 2) * 128:256 + (kc % 2 + 1) * 128], br_tok[:, kc * 128:(kc + 1) * 128], cst["ident"][:])
            yield
            S.copy(brs[:, 0:2, t * 128:(t + 1) * 128], pO[:, 256:512].rearrange("p (c t) -> p c t", c=2), eng="act")
            S.copy(brs[:, 2:4, t * 128:(t + 1) * 128], pS[:, 256:512].rearrange("p (c t) -> p c t", c=2), eng="dve")
            yield

    def run_interleaved(gens):
        active = [x for x in gens if x is not None]
        while active:
            for gi in list(active):
                try:
                    next(gi)
                except StopIteration:
                    active.remove(gi)

    n_it = len(iters)
    for k in range(n_it + 1):
        run_interleaved([setup(k) if k < n_it else None, chain(k - 1) if k >= 1 else None])
    S.dma(brT_d[bidx].rearrange("c p t -> p c t"), brs[:], eng="pool")
    P.close()


def gla_branch(kb, layer, brT_d):
    cgla_branch(kb, layer, brT_d, "gla")


def hgrn_branch(kb, layer, brT_d):
    S = kb.S
    P = Pool_(kb)
    lb_d = kb.lb_d
    row = P.sb("hlbrow", [1, 512])
    if layer == 0:
        S.memset(row[:], 0.0)
    else:
        r0 = P.sb("hlb0", [1, 512])
        S.dma(r0[:], kb.d["hgrn_lb_logits"][0:1, :])
        S.dma(row[:], kb.d["hgrn_lb_logits"][1:2, :])
        S.tt(row[:], row[:], r0[:], ALU.subtract)
        S.act(row[:], row[:], AF.Sigmoid)
    S.dma(lb_d[layer:layer + 1, :], row[:])
    P.close()
    cgla_branch(kb, layer, brT_d, "hgrn", lb_d=lb_d[layer:layer + 1, :])


def rwkv_branch(kb, layer, brT_d):
    S = kb.S
    hT = kb.hT
    W = kb.d["w_in"][layer]
    psb = kb.psb
    cst = kb.cst
    o = RWKV_OFF
    P = Pool_(kb)
    CW = math.exp(-0.5)
    Wr = [P.sb("rWr%d" % k, [128, 8, 512], BF16) for k in range(2)]
    Wk = [P.sb("rWk%d" % k, [128, 8, 512], BF16) for k in range(2)]
    Wv = [P.sb("rWv%d" % k, [128, 8, 512], BF16) for k in range(2)]
    Wl = [P.sb("rWl%d" % k, [128, 8, 256], BF16) for k in range(2)]
    PP = Pool_(kb)
    stg = PP.sb("rstg", [128, 8, 512])
    muB = PP.sb("rmuB", [128, 1792])
    omuB = PP.sb("romuB", [128, 1792])
    S.dma(muB[:], kb.d["rwkv_mu"][layer:layer + 1, :].partition_broadcast(128))
    S.ts(omuB[:], muB[:], -1.0, 1.0, ALU.mult, ALU.add)
    for (dst, c0, n) in ((Wr, 0, 512), (Wk, 512, 512), (Wv, 1024, 512), (Wl, 1536, 256)):
        sv = stg[:, :, 0:n]
        S.dma(sv, W[:, o + c0:o + c0 + n].rearrange("(c p) n -> p c n", p=128))
        S.tt(dst[0][:], sv, omuB[:, c0:c0 + n].unsqueeze(1).to_broadcast([128, 8, n]), ALU.mult)
        S.tt(dst[1][:], sv, muB[:, c0:c0 + n].unsqueeze(1).to_broadcast([128, 8, n]), ALU.mult, eng="pool")
    PP.close()
    lw = P.sb("rlw", [128, 512])
    S.dma(lw[0:64, :], kb.d["rwkv_w2"][layer])
    S.dma(lw[64:128, :], kb.d["rwkv_a2"][layer])
    g2f = P.sb("rg2f", [128, 512])
    g2b = P.sb("rg2b", [128, 512], BF16)
    S.dma(g2f[:], kb.d["rwkv_g2"][layer])
    S.copy(g2b[:], g2f[:])
    w0r = P.sb("rw0r", [1, 512])
    S.dma(w0r[:], kb.d["rwkv_w0"][layer:layer + 1, :])
    a0T = P.sb("ra0T", [128, 4])
    kkT_ = P.sb("rkkT", [128, 4])
    kaT = P.sb("rkaT", [128, 4])
    okaT = P.sb("rokaT", [128, 4])
    rkT_ = P.sb("rrkT", [128, 4])
    load_T(kb, P, a0T[:], kb.d["rwkv_a0"][layer].rearrange("(c p) -> c p", p=128), 4)
    load_T(kb, P, kkT_[:], kb.d["rwkv_k_k"][layer].rearrange("(c p) -> c p", p=128), 4)
    load_T(kb, P, kaT[:], kb.d["rwkv_k_a"][layer].rearrange("(c p) -> c p", p=128), 4)
    load_T(kb, P, rkT_[:], kb.d["rwkv_r_k"][layer].rearrange("(c two) d -> c (two d)", two=2), 4)
    S.ts(okaT[:], kaT[:], -1.0, 1.0, ALU.mult, ALU.add)
    lngB = P.sb("rlngB", [128, 512])
    lnbB = P.sb("rlnbB", [128, 512])
    S.dma(lngB[:], kb.d["rwkv_ln_g"][layer:layer + 1, :].partition_broadcast(128))
    S.dma(lnbB[:], kb.d["rwkv_ln_b"][layer:layer + 1, :].partition_broadcast(128))
    Uc = P.sb("rUc", [128, 128])
    Ux = P.sb("rUx", [128, 128])
    SUl = P.sb("rSUl", [128, 128])
    nsup = P.sb("rnsup", [128, 128])
    nslo = P.sb("rnslo", [128, 128])
    ntriu = P.sb("rntriu", [128, 128])
    S.ts(Uc[:], cst["triu64"][:], -CW, None, ALU.mult)
    S.ts(Ux[:], cst["sup64"][:], -CW, None, ALU.mult)
    S.ts(SUl[:], cst["slo64"][:], -CW, None, ALU.mult)
    S.ts(nsup[:], cst["sup64"][:], -1.0, None, ALU.mult)
    S.ts(nslo[:], cst["slo64"][:], -1.0, None, ALU.mult)
    S.ts(ntriu[:], cst["triu64"][:], -1.0, None, ALU.mult)
    hsel = P.sb("rhsel", [128, 2])
    S.copy(hsel[:], cst["blk64"][:].rearrange("p (a s) -> p a s", a=2)[:, :, 0])
    ident = cst["ident"]

    def f512(name):
        return P.sb(name, [128, 512])

    def f128(name, dt=F32):
        return P.sb(name, [128, 128], dt)

    lo = f512("rlo")
    sgl = P.sb("rsgl", [128, 512], BF16)
    rT, kT, aT, kaT_, bT_, kpT, tmpF, prodT = (f512("r_" + n) for n in ("rT", "kT", "aT", "kapT", "bbT", "kpT", "tmpF", "prodT"))
    def d128(name):
        return [f128("%s_%d" % (name, k)) for k in range(2)]

    l_tok = f128("rltok")
    v_tokD, g_tokD = d128("rvtok"), d128("rgtok")
    sb2D = [P.sb("rsb2_%d" % k, [128, 2]) for k in range(2)]
    gCD = [P.sb("rgC_%d" % k, [128, 2]) for k in range(2)]
    bTs, bxTs = f128("rbTs"), f128("rbxTs")
    bm, nbm, bl = P.sb("rbm", [128, 2]), P.sb("rnbm", [128, 2]), P.sb("rbl", [128, 2])
    E = {n: f128("rE_" + n) for n in ("r", "kx", "inv", "abs", "absx", "last")}
    RB = BF16 if DBG.get("rw_bf16", True) else F32
    rt, kxt, kt, bt = (f128("r_" + n, RB) for n in ("rt", "kxt", "kt", "bt"))
    KhT, BhT = (f128("r_" + n) for n in ("KhT", "BhT"))
    rbarD, kbarD, KhatD, BhatD = d128("rrbar"), d128("rkbar"), d128("rKhat"), d128("rBhat")
    Mm = [[f128("rM%d_%d" % (a, k), RB) for k in range(2)] for a in range(2)]
    MT = [[f128("rMT%d_%d" % (a, k), RB) for k in range(2)] for a in range(2)]
    Pb = [[f128("rPb%d_%d" % (a, k), RB) for k in range(2)] for a in range(2)]
    PmD = [[[f128("rP%d_%d_%d" % (b_, a, k)) for k in range(2)] for a in range(2)] for b_ in range(2)]
    AkkD = [[f128("rAkk%d_%d" % (b_, a)) for a in range(2)] for b_ in range(2)]
    ArkD = [[f128("rArk%d_%d" % (b_, a)) for a in range(2)] for b_ in range(2)]
    ArbD = [[f128("rArb%d_%d" % (b_, a)) for a in range(2)] for b_ in range(2)]
    Ws, Us, ytok = f128("rWs"), f128("rUs"), f128("rytok")
    Hs = [[P.sb("rHs%d_%d" % (u, k), [128, 64]) for k in range(2)] for u in range(4)]
    for u in range(4):
        S.memset(Hs[u][0][:], 0.0)
    st6 = P.sb("rst6", [128, 2, 6])
    mv2 = P.sb("rmv2", [128, 2, 2])
    rs2 = P.sb("rrs2", [128, 2])
    yn = f128("ryn")
    brt_ = f128("rbrt")
    brb = [P.sb("rbrb%d" % k, [128, 128], BF16) for k in range(2)]

    def proj_fm(ps, W2, c0, n, gcol):
        for kc in range(8):
            S.mm(ps[0:n, :], W2[0][:, kc, c0:c0 + n], hT[:, kc, slice(1 + gcol, 1 + gcol + 512)], start=(kc == 0), stop=False)
        for kc in range(8):
            S.mm(ps[0:n, :], W2[1][:, kc, c0:c0 + n], hT[:, kc, slice(gcol, gcol + 512)], start=False, stop=(kc == 7))

    iters = []
    for g in range(DBG.get("rw_ng", 8)):
        for u in range(4):
            for tt_ in range(4):
                iters.append((g, u, tt_))

    def setup(it):
        g, u, tt_ = iters[it]
        bf = it % 2
        gcol = g * 512
        uc = slice(u * 128, (u + 1) * 128)
        v_tok, g_tok, sb2, gC = v_tokD[bf], g_tokD[bf], sb2D[bf], gCD[bf]
        rbar, kbar, Khat, Bhat = rbarD[bf], kbarD[bf], KhatD[bf], BhatD[bf]
        Pm, Akk, Ark, Arb = PmD[bf], AkkD[bf], ArkD[bf], ArbD[bf]
        if u == 0 and tt_ == 0:
            proj_fm(psb[0], Wl, 0, 128, gcol)
            S.act(lo[0:64, :], psb[0][0:64, :], AF.Tanh)
            S.copy(lo[64:128, :], psb[0][64:128, :])
            proj_fm(psb[1], Wl, 128, 128, gcol)
            S.act(sgl[:], psb[1][:], AF.Sigmoid)
            yield
        if tt_ == 0:
            proj_fm(psb[0], Wr, u * 128, 128, gcol)
            S.copy(rT[:], psb[0][:], eng="act")
            proj_fm(psb[1], Wk, u * 128, 128, gcol)
            S.copy(kT[:], psb[1][:])
            yield
            S.mm(psb[0][:], lw[64:128, uc], lo[64:128, :])
            S.act(aT[:], psb[0][:], AF.Sigmoid, bias=a0T[:, u:u + 1])
            S.ts(kaT_[:], kT[:], kkT_[:, u:u + 1], None, ALU.mult)
            S.tt(tmpF[:], kaT_[:], kaT_[:], ALU.mult)
            S.mm(psb[1][:], cst["blk64"][:], tmpF[:])
            yield
            S.act(tmpF[:], psb[1][:], AF.Ln, bias=1e-24)
            S.act(tmpF[:], tmpF[:], AF.Exp, scale=-0.5)
            S.tt(kaT_[:], kaT_[:], tmpF[:], ALU.mult)
            S.tt(bT_[:], kaT_[:], aT[:], ALU.mult)
            yield
            S.ts(tmpF[:], aT[:], kaT[:, u:u + 1], okaT[:, u:u + 1], ALU.mult, ALU.add)
            S.tt(kpT[:], kT[:], tmpF[:], ALU.mult)
            S.stt(prodT[:], rT[:], rkT_[:, u:u + 1], kpT[:], ALU.mult, ALU.mult)
            yield
        t = g * 4 + tt_
        t0 = t * 128
        tc_ = slice(tt_ * 128, (tt_ + 1) * 128)
        pt_ = psb[2]
        for kc in range(8):
            S.mm(pt_[:, 0:128], hT[:, kc, slice(1 + t0, 1 + t0 + 128)], Wv[0][:, kc, uc], start=(kc == 0), stop=False)
        for kc in range(8):
            S.mm(pt_[:, 0:128], hT[:, kc, slice(t0, t0 + 128)], Wv[1][:, kc, uc], start=False, stop=(kc == 7))
        S.mm(pt_[:, 128:256], lo[0:64, tc_], lw[0:64, uc], start=True, stop=False)
        S.mm(pt_[:, 128:256], cst["ones"][0:1, :], w0r[0:1, uc], start=False, stop=True)
        S.mm(pt_[:, 256:384], sgl[:, tc_], g2b[:, uc])
        S.mm(pt_[:, 384:386], prodT[:, tc_], hsel[:])
        yield
        S.act(l_tok[:], pt_[:, 128:256], AF.Sigmoid)
        S.copy(v_tok[:], pt_[:, 0:128])
        S.copy(g_tok[:], pt_[:, 256:384], eng="act")
        S.copy(sb2[:], pt_[:, 384:386])
        pX = psb[3]
        S.mm(pX[:, 0:128], l_tok[:], Uc[:])
        S.mm(pX[:, 128:256], l_tok[:], Ux[:])
        yield
        S.copy(bTs[:], pX[:, 0:128])
        S.copy(bxTs[:], pX[:, 128:256], eng="act")
        b3 = bTs[:].rearrange("p (c s) -> p c s", c=2)
        S.copy(bm[:], b3[:, :, 32])
        S.ts(nbm[:], b3[:, :, 32], -1.0, None, ALU.mult)
        S.copy(bl[:], b3[:, :, 63])
        yield
        for c in range(2):
            cs = slice(c * 64, (c + 1) * 64)
            S.act(E["r"][:, cs], bTs[:, cs], AF.Exp, bias=nbm[:, c:c + 1])
            S.act(E["kx"][:, cs], bxTs[:, cs], AF.Exp, bias=nbm[:, c:c + 1])
            S.act(E["inv"][:, cs], bTs[:, cs], AF.Exp, bias=bm[:, c:c + 1], scale=-1.0)
            S.act(E["last"][:, cs], bTs[:, cs], AF.Exp, bias=bl[:, c:c + 1], scale=-1.0)
        S.act(E["abs"][:], bTs[:], AF.Exp)
        S.act(E["absx"][:], bxTs[:], AF.Exp)
        yield
        S.tt(rt[:], rT[:, tc_], E["r"][:], ALU.mult)
        S.tt(kxt[:], kaT_[:, tc_], E["kx"][:], ALU.mult)
        S.tt(kt[:], kpT[:, tc_], E["inv"][:], ALU.mult)
        S.tt(bt[:], bT_[:, tc_], E["inv"][:], ALU.mult)
        yield
        S.tt(KhT[:], kpT[:, tc_], E["last"][:], ALU.mult)
        S.stt(BhT[:], bT_[:, tc_], -1.0, E["last"][:], ALU.mult, ALU.mult)
        S.tt(rbar[:], rT[:, tc_], E["abs"][:], ALU.mult)
        S.tt(kbar[:], kaT_[:, tc_], E["absx"][:], ALU.mult)
        S.copy(gC[:], E["abs"][:].rearrange("p (c s) -> p c s", c=2)[:, :, 63])
        for a in range(2):
            ra = slice(a * 64, (a + 1) * 64)
            pA = psb[4 + a]
            S.mm(pA[:, 0:128], bt[ra, :], kxt[ra, :])
            S.mm(pA[:, 128:256], kxt[ra, :], bt[ra, :])
            S.mm(pA[:, 256:384], kt[ra, :], kxt[ra, :])
            S.mm(pA[:, 384:512], kt[ra, :], rt[ra, :])
            S.mm(psb[6][:, a * 128:(a + 1) * 128], bt[ra, :], rt[ra, :])
        S.tr(pX[:, 256:384], KhT[:], ident[:])
        S.tr(pX[:, 384:512], BhT[:], ident[:])
        yield
        for a in range(2):
            pA = psb[4 + a]
            S.tt(Mm[a][0][:], nsup[:], pA[:, 0:128], ALU.mult)
            S.tt(MT[a][0][:], nslo[:], pA[:, 128:256], ALU.mult)
            S.tt(Pm[a][0][:], Mm[a][0][:], ident[:], ALU.add)
            S.copy(Pb[a][0][:], Pm[a][0][:], eng="act")
        yield
        for a in range(2):
            pA = psb[4 + a]
            S.tt(Akk[a][:], cst["sup64"][:], pA[:, 256:384], ALU.mult)
            S.tt(Ark[a][:], cst["triu64"][:], pA[:, 384:512], ALU.mult)
            S.tt(Arb[a][:], ntriu[:], psb[6][:, a * 128:(a + 1) * 128], ALU.mult)
        S.copy(Khat[:], pX[:, 256:384], eng="act")
        S.copy(Bhat[:], pX[:, 384:512], eng="act")
        cur = 0

        def stA(lvl, cur):
            for a in range(2):
                pA = psb[4 + a]
                if lvl < 5:
                    S.mm(pA[:, 0:128], MT[a][cur][:], Mm[a][cur][:])
                S.mm(pA[:, 128:256], Mm[a][cur][:], MT[a][cur][:])

        def stB(lvl, cur):
            nxt = 1 - cur
            for a in range(2):
                pA = psb[4 + a]
                eng = "dve" if a == 0 else "act"
                if lvl < 5:
                    S.copy(Mm[a][nxt][:], pA[:, 0:128], eng=eng)
                S.copy(MT[a][nxt][:], pA[:, 128:256], eng=eng)

        def stC(lvl, cur):
            nxt = 1 - cur
            for a in range(2):
                pA = psb[4 + a]
                S.mm(pA[:, 256:384], MT[a][nxt][:], Pb[a][cur][:])

        def stD(lvl, cur):
            nxt = 1 - cur
            for a in range(2):
                pA = psb[4 + a]
                S.tt(Pm[a][nxt][:], Pm[a][cur][:], pA[:, 256:384], ALU.add)
                if lvl < 5:
                    S.copy(Pb[a][nxt][:], Pm[a][nxt][:], eng="act")

        stA(1, 0)
        yield
        stB(1, 0)
        yield
        for lvl in range(2, 6):
            c_prev = (lvl - 2) % 2
            c_cur = (lvl - 1) % 2
            stC(lvl - 1, c_prev)
            stA(lvl, c_cur)
            yield
            stD(lvl - 1, c_prev)
            stB(lvl, c_cur)
            yield
        stC(5, 0)
        yield
        stD(5, 0)
        yield
        cur = 1
        assert cur == 1

    nbr = [0]

    def chain(it):
        g, u, tt_ = iters[it]
        bf = it % 2
        t0 = (g * 4 + tt_) * 128
        v_tok, g_tok, sb2, gC = v_tokD[bf], g_tokD[bf], sb2D[bf], gCD[bf]
        rbar, kbar, Khat, Bhat = rbarD[bf], kbarD[bf], KhatD[bf], BhatD[bf]
        Akk, Ark, Arb = AkkD[bf], ArkD[bf], ArbD[bf]
        Pf = [PmD[bf][a][1] for a in range(2)]
        H0, H1 = Hs[u][0], Hs[u][1]
        pC = psb[7]
        Hc = [H0, H1, H0]
        for c in range(2):
            rc = slice(c * 64, (c + 1) * 64)
            Hin, Hout = Hc[c], Hc[c + 1]
            for a in range(2):
                ra = slice(a * 64, (a + 1) * 64)
                S.mm(pC[rc, a * 64:(a + 1) * 64], kbar[ra, rc], Hin[ra, :], start=True, stop=False)
                S.mm(pC[rc, a * 64:(a + 1) * 64], Akk[a][rc, rc], v_tok[rc, ra], start=False, stop=True)
            yield
            S.copy(Ws[rc, :], pC[rc, 0:128])
            yield
            for a in range(2):
                ra = slice(a * 64, (a + 1) * 64)
                S.mm(pC[rc, 128 + a * 64:128 + (a + 1) * 64], Pf[a][rc, rc], Ws[rc, ra])
            yield
            S.copy(Us[rc, :], pC[rc, 128:256])
            yield
            for a in range(2):
                ra = slice(a * 64, (a + 1) * 64)
                S.mm(pC[ra, 384:448], Khat[rc, ra], v_tok[rc, ra], start=True, stop=False)
                S.mm(pC[ra, 384:448], Bhat[rc, ra], Us[rc, ra], start=False, stop=True)
            for a in range(2):
                ra = slice(a * 64, (a + 1) * 64)
                oy = slice(256 + a * 64, 256 + (a + 1) * 64)
                S.mm(pC[rc, oy], rbar[ra, rc], Hin[ra, :], start=True, stop=False)
                S.mm(pC[rc, oy], Ark[a][rc, rc], v_tok[rc, ra], start=False, stop=False)
                S.mm(pC[rc, oy], Arb[a][rc, rc], Us[rc, ra], start=False, stop=True)
            yield
            S.stt(Hout[:], Hin[:], gC[:, c:c + 1], pC[:, 384:448], ALU.mult, ALU.add)
            S.copy(ytok[rc, :], pC[rc, 256:384], eng="act")
            yield
        for a in range(2):
            S.bn_stats(st6[:, a, :], ytok[:, a * 64:(a + 1) * 64])
            S.bn_aggr(mv2[:, a, :], st6[:, a, :])
        rsqrt_eps(S, rs2[:], mv2[:, :, 1], 64e-5)
        yield
        for a in range(2):
            ra = slice(a * 64, (a + 1) * 64)
            gc = slice(u * 128 + a * 64, u * 128 + (a + 1) * 64)
            S.ts(yn[:, ra], ytok[:, ra], mv2[:, a, 0:1], rs2[:, a:a + 1], ALU.subtract, ALU.mult)
            S.tt(yn[:, ra], yn[:, ra], lngB[:, gc], ALU.mult)
            S.tt(yn[:, ra], yn[:, ra], lnbB[:, gc], ALU.add)
            S.stt(yn[:, ra], v_tok[:, ra], sb2[:, a:a + 1], yn[:, ra], ALU.mult, ALU.add)
        S.tt(brt_[:], yn[:], g_tok[:], ALU.mult)
        S.tr(pC[:, 0:128], brt_[:], ident[:])
        yield
        bb_ = brb[nbr[0] % 2]
        nbr[0] += 1
        S.copy(bb_[:], pC[:, 0:128], eng="act")
        S.dma(brT_d[1, u, :, t0:t0 + 128], bb_[:], eng="sp")

    def run_interleaved(gens):
        active = [x for x in gens if x is not None]
        while active:
            for gi in list(active):
                try:
                    next(gi)
                except StopIteration:
                    active.remove(gi)

    def run_weighted(ga, gb, wa):
        a_live, b_live = ga is not None, gb is not None
        while a_live or b_live:
            if a_live:
                for _ in range(wa):
                    try:
                        next(ga)
                    except StopIteration:
                        a_live = False
                        break
            if b_live:
                try:
                    next(gb)
                except StopIteration:
                    b_live = False

    n_it = len(iters)
    pipelined = DBG.get("rw_pipe", True)
    if pipelined:
        for k in range(n_it + 1):
            if DBG.get("rw_chain_first", False):
                run_weighted(chain(k - 1) if k >= 1 else None, setup(k) if k < n_it else None, DBG.get("rw_ratio", 2))
            else:
                run_weighted(setup(k) if k < n_it else None, chain(k - 1) if k >= 1 else None, DBG.get("rw_ratio", 1))
    else:
        for k in range(n_it):
            run_interleaved([setup(k)])
            run_interleaved([chain(k)])
    P.close()


_CACHE = {}


def kernel(**inputs):
    if "kb" not in _CACHE:
        _CACHE["kb"] = build()
    kb = _CACHE["kb"]
    consts = make_consts()
    shared = {}
    for n, shp in PARAMS:
        if n == "c":
            continue
        shared[n] = np.ascontiguousarray(np.asarray(inputs[n], dtype=np.float32))
    for n, v in consts.items():
        shared["k_" + n] = v
    x = np.asarray(inputs["x"], dtype=np.float32)
    c = np.asarray(inputs["c"], dtype=np.float32)
    in_maps = []
    for b in range(8):
        m = dict(shared)
        m["x"] = np.ascontiguousarray(x[b])
        m["c"] = np.ascontiguousarray(c[b:b + 1])
        in_maps.append(m)
    res = run_bass_kernel_spmd(kb.nc, in_maps, core_ids=list(range(8)))
    return np.stack([np.asarray(r["out"], dtype=np.float32) for r in res.results], axis=0)
```

```python
import math
from contextlib import ExitStack

import numpy as np
import concourse.bass as bass
import concourse.mybir as mybir
from concourse.bass_utils import run_bass_kernel_spmd

F32 = mybir.dt.float32
BF16 = mybir.dt.bfloat16
AF = mybir.ActivationFunctionType
ALU = mybir.AluOpType
AX = mybir.AxisListType

ENGS = ("pe", "act", "dve", "pool", "sp")


class _Op:
    __slots__ = ("eng", "fn", "dma", "waits", "key", "val", "know", "inc")


def _region(ap):
    shape = list(ap.tensor.shape)
    off = int(ap.offset)
    if str(ap.space) == "DRAM":
        ext = 0
        for step, cnt in ap.ap:
            ext += (int(cnt) - 1) * abs(int(step))
        return (ap.name, 0, 1, off, off + ext + 1)
    if str(ap.space) == "PSUM":
        return (ap.name, 0, 128, 0, 1 << 30)
    ps = 1
    for s in shape[1:]:
        ps *= int(s)
    p0 = off // ps
    f0 = off % ps
    npart = 1
    ext = 0
    for step, cnt in ap.ap:
        step = int(step)
        cnt = int(cnt)
        if step == ps:
            npart = max(npart, cnt)
        elif step > ps:
            npart = max(npart, (cnt - 1) * (step // ps) + 1)
        else:
            ext += (cnt - 1) * step
    return (ap.name, p0, p0 + npart, f0, f0 + ext + 1)


class Sched:
    def __init__(self, nc, n_dma_sems=12):
        self.nc = nc
        self.streams = {e: [] for e in ENGS}
        self.clock = {e: {} for e in ENGS}
        self.cpos = {e: 0 for e in ENGS}
        self.last_op = {e: None for e in ENGS}
        self.n_dma_sems = n_dma_sems
        self.dma_uses = {}
        self.dma_next = {e: 0 for e in ENGS}
        self.dma_last = {}
        self.acc = {}
        self.readonly = set()
        self.pe_bank = {}
        self.epoch = 0
        self.n_ops = 0

    def _add(self, eng, fn, reads, writes, dma, force=()):
        op = _Op()
        op.eng, op.fn, op.dma = eng, fn, dma
        clk = self.clock[eng]
        need = {}

        def dep(d, forced=False):
            if (not forced) and (not dma) and (not d.dma) and d.eng == "pe" and eng == "pe":
                return
            if clk.get(d.key, 0) >= d.val:
                return
            if need.get(d.key, 0) < d.val:
                need[d.key] = d.val
            for k, v in d.know.items():
                if clk.get(k, 0) < v:
                    clk[k] = v

        for d_ in force:
            dep(d_, True)
        regs = []
        for ap in reads:
            if ap.name in self.readonly:
                continue
            regs.append((_region(ap), False))
        for ap in writes:
            regs.append((_region(ap), True))
        for (name, p0, p1, f0, f1), isw in regs:
            lst = self.acc.get(name)
            if lst is None:
                continue
            for r in lst:
                if r[4] is op:
                    continue
                if r[0] < p1 and p0 < r[1] and r[2] < f1 and f0 < r[3]:
                    if isw or r[5] or (f1 == (1 << 30) and r[4].eng != eng):
                        dep(r[4])
        if dma:
            slot = self.dma_next[eng]
            self.dma_next[eng] = (slot + 1) % self.n_dma_sems
            k = ("s", eng, slot)
            prev = self.dma_last.get(k)
            if prev is not None:
                dep(prev)
            cnt = self.dma_uses.get(k, 0) + 1
            self.dma_uses[k] = cnt
            op.key, op.val, op.inc = k, 16 * cnt, 16
            self.dma_last[k] = op
        else:
            self.cpos[eng] += 1
            op.key, op.val, op.inc = ("e", eng, self.epoch), self.cpos[eng], 1
        for k, v in need.items():
            if clk.get(k, 0) < v:
                clk[k] = v
        op.waits = list(need.items())
        op.know = dict(clk)
        self.streams[eng].append(op)
        self.last_op[eng] = op
        for (name, p0, p1, f0, f1), isw in regs:
            lst = self.acc.setdefault(name, [])
            if isw:
                lst[:] = [r for r in lst if not (p0 <= r[0] and r[1] <= p1 and f0 <= r[2] and r[3] <= f1)]
            else:
                if not dma:
                    lst[:] = [r for r in lst if not ((not r[5]) and (not r[4].dma) and r[4].eng == eng
                                                     and r[0] == p0 and r[1] == p1 and r[2] == f0 and r[3] == f1)]
            lst.append([p0, p1, f0, f1, op, isw])
        self.n_ops += 1
        return op

    def barrier(self):
        targets = []
        for e in ENGS:
            if self.cpos[e] > 0:
                targets.append((("e", e, self.epoch), self.cpos[e], self.last_op[e]))
        for k, o in self.dma_last.items():
            targets.append((k, o.val, o))
        for e in ENGS:
            clk = self.clock[e]
            op = _Op()
            op.eng, op.fn, op.dma = e, None, False
            need = {}
            for k, v, o in targets:
                if clk.get(k, 0) < v:
                    need[k] = v
                    clk[k] = v
            op.waits = list(need.items())
            op.key = None
            op.know = dict(clk)
            self.streams[e].append(op)
        self.acc = {}
        self.pe_bank = {}
        if max(self.cpos.values()) > 16000:
            self.epoch += 1
            self.cpos = {e: 0 for e in ENGS}

    def _pe_bank(self, out, lhsT):
        kpos = (int(lhsT.base_partition()), int(lhsT.partition_size()),
                int(out.base_partition()), int(out.partition_size()))
        prev = self.pe_bank.get(out.name)
        force = ()
        if prev is not None and prev[0] != kpos:
            force = (prev[1],)
        return kpos, force

    def mm(self, out, lhsT, rhs, start=True, stop=True):
        rd = [lhsT, rhs] + ([] if start else [out])
        kpos, force = self._pe_bank(out, lhsT)
        op = self._add("pe", lambda e: e.matmul(out, lhsT, rhs, start=start, stop=stop), rd, [out], False, force=force)
        self.pe_bank[out.name] = (kpos, op)
        return op

    def tr(self, out, in_, ident):
        kpos, force = self._pe_bank(out, in_)
        op = self._add("pe", lambda e: e.transpose(out, in_, ident), [in_, ident], [out], False, force=force)
        self.pe_bank[out.name] = (kpos, op)
        return op

    def act(self, out, in_, func, bias=None, scale=None, accum_out=None):
        rd = [in_]
        kw = {}
        if bias is not None:
            kw["bias"] = bias
            if not isinstance(bias, (int, float)):
                rd.append(bias)
        if scale is not None:
            kw["scale"] = scale
            if not isinstance(scale, (int, float)):
                rd.append(scale)
        wr = [out]
        if accum_out is not None:
            kw["accum_out"] = accum_out
            wr.append(accum_out)
        return self._add("act", lambda e: e.activation(out, in_, func, **kw), rd, wr, False)

    def tt(self, out, in0, in1, op, eng="dve"):
        return self._add(eng, lambda e: e.tensor_tensor(out, in0, in1, op), [in0, in1], [out], False)

    def ts(self, out, in0, s1, s2, op0, op1=None, eng="dve", accum_out=None):
        rd = [in0]
        if not isinstance(s1, (int, float)):
            rd.append(s1)
        if s2 is not None and not isinstance(s2, (int, float)):
            rd.append(s2)
        wr = [out]
        kw = {}
        if accum_out is not None:
            kw["accum_out"] = accum_out
            wr.append(accum_out)
        if op1 is None:
            return self._add(eng, lambda e: e.tensor_scalar(out, in0, s1, None, op0, **kw), rd, wr, False)
        return self._add(eng, lambda e: e.tensor_scalar(out, in0, s1, s2, op0, op1, **kw), rd, wr, False)

    def stt(self, out, in0, scalar, in1, op0, op1):
        rd = [in0, in1]
        if not isinstance(scalar, (int, float)):
            rd.append(scalar)
        return self._add("dve", lambda e: e.scalar_tensor_tensor(out, in0, scalar, in1, op0, op1), rd, [out], False)

    def copy(self, out, in_, eng="dve"):
        if eng == "act":
            return self._add("act", lambda e: e.copy(out, in_), [in_], [out], False)
        return self._add(eng, lambda e: e.tensor_copy(out, in_), [in_], [out], False)

    def memset(self, out, val, eng="dve"):
        return self._add(eng, lambda e: e.memset(out, val), [], [out], False)

    def reduce(self, out, in_, op, axis=AX.X, eng="dve"):
        return self._add(eng, lambda e: e.tensor_reduce(out, in_, axis, op), [in_], [out], False)

    def bn_stats(self, out, in_):
        return self._add("dve", lambda e: e.bn_stats(out, in_), [in_], [out], False)

    def bn_aggr(self, out, in_):
        return self._add("dve", lambda e: e.bn_aggr(out, in_), [in_], [out], False)

    def recip(self, out, in_):
        return self._add("dve", lambda e: e.reciprocal(out, in_), [in_], [out], False)

    def dma(self, out, in_, eng="sp", **kw):
        return self._add(eng, lambda e: e.dma_start(out=out, in_=in_, **kw), [in_], [out], True)

    def emit(self):
        nc = self.nc
        with ExitStack() as es:
            sems = {}
            for e in ENGS:
                for op in self.streams[e]:
                    if op.fn is not None and (not op.dma) and op.key not in sems:
                        sems[op.key] = es.enter_context(nc.semaphore("c_%s_%d" % (op.key[1], op.key[2])))
            for k in self.dma_uses:
                sems[k] = es.enter_context(nc.semaphore("d_%s_%d" % (k[1], k[2])))
            final = [(k, o.val) for k, o in self.dma_last.items()]
            for e in ENGS:
                if self.cpos[e] > 0:
                    final.append((("e", e, self.epoch), self.cpos[e]))
            block = es.enter_context(nc.Block())
            streams = self.streams

            def run(engname, e):
                for op in streams[engname]:
                    for k, v in op.waits:
                        e.wait_ge(sems[k], v)
                    if op.fn is not None:
                        op.fn(e).then_inc(sems[op.key], op.inc)
                if engname == "sp":
                    for k, v in final:
                        e.wait_ge(sems[k], v)

            @block.tensor
            def _(e):
                run("pe", e)

            @block.scalar
            def _(e):
                run("act", e)

            @block.vector
            def _(e):
                run("dve", e)

            @block.gpsimd
            def _(e):
                run("pool", e)

            @block.sync
            def _(e):
                run("sp", e)


DBG = {}
T = 4096
D = 1024
NT = 32
DEPTH = 2
NCOLS = 6936
GLA_OFF, RWKV_OFF, FOX_OFF, HGRN_OFF = 0, 1552, 3344, 4888
DN_ALPHA = (2.0 * DEPTH) ** 0.25
NE = 16


def make_consts():
    c = {}
    i = np.arange(128)
    same = (i[:, None] // 64) == (i[None, :] // 64)
    c["ident"] = np.eye(128, dtype=np.float32)
    c["ones"] = np.ones((128, 128), np.float32)
    c["triu"] = (i[:, None] <= i[None, :]).astype(np.float32)
    c["triu64"] = ((i[:, None] <= i[None, :]) & same).astype(np.float32)
    c["sup64"] = ((i[:, None] < i[None, :]) & same).astype(np.float32)
    c["slo64"] = ((i[:, None] > i[None, :]) & same).astype(np.float32)
    c["blk64"] = same.astype(np.float32)
    return c


CONST_NAMES = ["ident", "ones", "triu", "triu64", "sup64", "slo64", "blk64"]

PARAMS = [
    ("c", [1, D]), ("ada_w", [2, D, 6 * D]), ("ada_b", [2, 6, D]), ("w_in", [2, D, NCOLS]),
    ("gla_alpha_up", [2, 16, 256]), ("gla_alpha_b", [2, 256]), ("gla_norm_g", [2, 128]),
    ("rwkv_mu", [2, 1792]), ("rwkv_w0", [2, 512]), ("rwkv_w2", [2, 64, 512]), ("rwkv_a0", [2, 512]),
    ("rwkv_a2", [2, 64, 512]), ("rwkv_g2", [2, 128, 512]), ("rwkv_k_k", [2, 512]), ("rwkv_k_a", [2, 512]),
    ("rwkv_r_k", [2, 8, 64]), ("rwkv_ln_g", [2, 512]), ("rwkv_ln_b", [2, 512]), ("fox_f_bias", [2, 8]),
    ("hgrn_lb_logits", [2, 512]), ("hgrn_norm_g", [2, 128]), ("w_br", [2, 4, 512, D]),
    ("w_gate", [2, 4, D, D]), ("b_gate", [2, 4, D]), ("w_o", [2, D, D]), ("ln1_g", [2, D]), ("ln1_b", [2, D]),
    ("router_w", [D, NE]), ("router_b", [NE]), ("exp_w_gate", [2, NE, D, 512]), ("exp_w_up", [2, NE, D, 512]),
    ("exp_w_down", [2, NE, 512, D]), ("ln2_g", [2, D]), ("ln2_b", [2, D]),
]


class KB:
    def __init__(self, io=None):
        self.nc = bass.Bass("TRN2", target_bir_lowering=False)
        self.S = Sched(self.nc)
        self.io = io or {}
        self.d = {}
        nc = self.nc
        self.x = self.ext_in("x", [T, D], F32)
        for n, shp in PARAMS:
            self.d[n] = self.ext_in(n, shp, F32)
        self.cst_d = {n: self.ext_in("k_" + n, [128, 128], F32) for n in CONST_NAMES}
        self.psb = [nc.alloc_psum_tensor("psb%d" % i, [128, 512], F32) for i in range(8)]
        self.cst = {n: nc.alloc_sbuf_tensor("c_" + n, [128, 128], F32) for n in CONST_NAMES}
        for n in CONST_NAMES:
            self.S.dma(self.cst[n][:], self.cst_d[n])
        self.hT = None

    def ext_in(self, name, shape, dt):
        self.S.readonly.add(name)
        return self.nc.dram_tensor(name, list(shape), dt, kind="ExternalInput").ap()

    def dram(self, name, shape, dt):
        role = self.io.get(name)
        if role == "in":
            return self.nc.dram_tensor(name, list(shape), dt, kind="ExternalInput").ap()
        if role == "out" or name == "out":
            return self.nc.dram_tensor(name, list(shape), dt, kind="ExternalOutput").ap()
        return self.nc.dram_tensor(name, list(shape), dt).ap()


class Pool_:
    def __init__(self, kb):
        self.kb = kb
        self.es = ExitStack()

    _uid = [0]

    def sb(self, name, shape, dt=F32):
        Pool_._uid[0] += 1
        return self.es.enter_context(self.kb.nc.sbuf_tensor("%s_u%d" % (name, Pool_._uid[0]), list(shape), dt))

    def close(self):
        self.kb.S.barrier()
        self.es.close()


def phase_mod(kb, mod_d):
    S = kb.S
    P = Pool_(kb)
    condT = P.sb("condT", [128, 8])
    load_T(kb, P, condT[:], kb.d["c"].rearrange("o (c p) -> (o c) p", p=128), 8)
    S.act(condT[:], condT[:], AF.Silu)
    wst = [P.sb("adaw%d" % k, [128, 3072]) for k in range(2)]
    mrow = P.sb("mrow", [1, 6144])
    brow = P.sb("brow", [1, 6144])
    n = 0
    for i in range(2):
        S.dma(brow[:], kb.d["ada_b"][i:i + 1].rearrange("o j d -> o (j d)"))
        for half in range(2):
            for kc in range(8):
                w = wst[n % 2]
                n += 1
                S.dma(w[:], kb.d["ada_w"][i, kc * 128:(kc + 1) * 128, half * 3072:(half + 1) * 3072],
                      eng="sp" if n % 2 else "pool")
                for b in range(6):
                    S.mm(kb.psb[b][0:1, :], condT[:, kc:kc + 1], w[:, b * 512:(b + 1) * 512],
                         start=(kc == 0), stop=(kc == 7))
            for b in range(6):
                o = half * 3072 + b * 512
                S.tt(mrow[0:1, o:o + 512], kb.psb[b][0:1, :], brow[0:1, o:o + 512], ALU.add)
        for j in (1, 4):
            S.ts(mrow[0:1, j * 1024:(j + 1) * 1024], mrow[0:1, j * 1024:(j + 1) * 1024], 1.0, None, ALU.add)
        S.dma(mod_d[i:i + 1, :], mrow[:])
    P.close()


def rsqrt_eps(S, out, in_, eps, scale=1.0):
    S.act(out, in_, AF.Ln, bias=float(eps), scale=float(scale))
    S.act(out, out, AF.Exp, scale=-0.5)


def ln_stats(S, xin, st, mv, rstd, eps=1e-5):
    S.bn_stats(st[:, 0:6], xin[:, 0:512])
    S.bn_stats(st[:, 6:12], xin[:, 512:1024])
    S.bn_aggr(mv[:], st[:])
    rsqrt_eps(S, rstd[:], mv[:, 1:2], eps)


def load_T(kb, P, dst, src_rows, n, psum=None):
    S = kb.S
    tmp = P.sb("ldT_tmp", [n, 128])
    S.dma(tmp[:], src_rows)
    ps = kb.psb[7] if psum is None else psum
    S.mm(ps[:, 0:n], tmp[:], kb.cst["ident"][0:n, 0:n], start=True, stop=True)
    S.copy(dst, ps[:, 0:n])


def load_modT(kb, P, mod_d, layer, name):
    modT = P.sb(name, [128, 6, 8])
    load_T(kb, P, modT[:].rearrange("p j c -> p (j c)"), mod_d[layer].rearrange("(r p) -> r p", p=128), 48)
    return modT


def phase_ln_mixer(kb, x_src, mod_d, layer, prep=None):
    S = kb.S
    P = Pool_(kb)
    pgen = None
    if prep is not None:
        wg_d, wb_d, wo_d = prep
        pstg = [P.sb("pst%d" % k, [128, 1024]) for k in range(2)]
        pstb = [P.sb("pstb%d" % k, [128, 1024], BF16) for k in range(2)]
        jobs = []
        for n_ in range(4):
            jobs.append((wg_d[n_], kb.d["w_gate"][layer, n_]))
            jobs.append((wb_d[n_], kb.d["w_br"][layer, n_]))
        jobs.append((wo_d, kb.d["w_o"][layer]))
        pgen = prep_cast_gen(kb, P, jobs, pstg, pstb)
    modT = load_modT(kb, P, mod_d, layer, "modT_a")
    xb = [P.sb("lnx%d" % k, [128, 1024]) for k in range(2)]
    st = [P.sb("lnst%d" % k, [128, 12]) for k in range(2)]
    mv = [P.sb("lnmv%d" % k, [128, 2]) for k in range(2)]
    rs = [P.sb("lnrs%d" % k, [128, 1]) for k in range(2)]
    ident = kb.cst["ident"]
    for t in range(DBG.get("ln_nt", NT)):
        k = t % 2
        xin = xb[k]
        S.dma(xin[:], x_src[t * 128:(t + 1) * 128, :])
        if DBG.get("ln_lvl", 9) < 1:
            continue
        ln_stats(S, xin, st[k], mv[k], rs[k])
        S.ts(xin[:], xin[:], mv[k][:, 0:1], rs[k][:, 0:1], ALU.subtract, ALU.mult)
        if DBG.get("ln_lvl", 9) < 2:
            continue
        for c in range(8):
            pb = kb.psb[(t % 2) * 2 + c // 4]
            S.tr(pb[:, (c % 4) * 128:(c % 4 + 1) * 128], xin[:, c * 128:(c + 1) * 128], ident[:])
        if DBG.get("ln_lvl", 9) < 3:
            continue
        for c in range(DBG.get("ln_nc", 8)):
            pb = kb.psb[(t % 2) * 2 + c // 4]
            src = pb[:, (c % 4) * 128:(c % 4 + 1) * 128]
            off = DBG.get("ln_off", 1)
            dst = kb.hT[:, c, off + t * 128:off + (t + 1) * 128]
            ev = DBG.get("ln_evac", "both")
            if (c % 2 == 0 and ev == "both") or ev == "dve":
                S.ts(dst, src, modT[:, 1, c:c + 1], modT[:, 0, c:c + 1], ALU.mult, ALU.add)
            else:
                S.act(dst, src, AF.Identity, bias=modT[:, 0, c:c + 1], scale=modT[:, 1, c:c + 1])
        if pgen is not None:
            for _ in range(2):
                next(pgen, None)
    if pgen is not None:
        for _ in pgen:
            pass
    P.close()


def prep_cast_gen(kb, P, jobs, stg, stb):
    S = kb.S
    n = 0
    for dst, src in jobs:
        R, N = src.shape
        for r in range(0, R, 128):
            a, b = stg[n % 2], stb[n % 2]
            n += 1
            S.dma(a[:, 0:N], src[r:r + 128, :], eng="sp")
            S.copy(b[:, 0:N], a[:, 0:N], eng="pool" if n % 2 else "act")
            S.dma(dst[r:r + 128, :], b[:, 0:N], eng="pool")
            yield


def prep_cast(kb, P, jobs, stg, stb):
    for _ in prep_cast_gen(kb, P, jobs, stg, stb):
        pass


class Epi:
    def __init__(self, kb, P, mod_d, layer, gt_idx, g_name, b_name, tag, with_z=True):
        S = kb.S
        self.kb = kb
        self.gt = P.sb("epi_gt" + tag, [128, 1024])
        self.g = P.sb("epi_g" + tag, [128, 1024])
        self.b = P.sb("epi_b" + tag, [128, 1024])
        S.dma(self.gt[:], mod_d[layer:layer + 1, gt_idx * 1024:(gt_idx + 1) * 1024].partition_broadcast(128))
        S.dma(self.g[:], kb.d[g_name][layer:layer + 1, :].partition_broadcast(128))
        S.dma(self.b[:], kb.d[b_name][layer:layer + 1, :].partition_broadcast(128))
        self.xb = [P.sb("epi_x%s%d" % (tag, k), [128, 1024]) for k in range(2)]
        self.zb = [P.sb("epi_z%s%d" % (tag, k), [128, 1024]) for k in range(2)] if with_z else None
        self.st = [P.sb("epi_st%s%d" % (tag, k), [128, 12]) for k in range(2)]
        self.mv = [P.sb("epi_mv%s%d" % (tag, k), [128, 2]) for k in range(2)]
        self.rs = [P.sb("epi_rs%s%d" % (tag, k), [128, 1]) for k in range(2)]
        self.n = 0

    def prefetch_x(self, x_src, t):
        k = self.n % 2
        self.kb.S.dma(self.xb[k][:], x_src[t * 128:(t + 1) * 128, :])

    def run(self, y_halves, x_dst, t, x_src=None, z=None):
        S = self.kb.S
        k = self.n % 2
        self.n += 1
        if x_src is not None:
            S.dma(self.xb[k][:], x_src[t * 128:(t + 1) * 128, :])
        x = self.xb[k]
        if z is None:
            z = self.zb[k]
        for h in range(2):
            S.tt(z[:, h * 512:(h + 1) * 512], y_halves[h], self.gt[:, h * 512:(h + 1) * 512], ALU.mult)
        S.stt(z[:], x[:], DN_ALPHA, z[:], ALU.mult, ALU.add)
        ln_stats(S, z, self.st[k], self.mv[k], self.rs[k])
        S.ts(z[:], z[:], self.mv[k][:, 0:1], self.rs[k][:, 0:1], ALU.subtract, ALU.mult)
        S.tt(z[:], z[:], self.g[:], ALU.mult, eng="pool")
        S.tt(z[:], z[:], self.b[:], ALU.add, eng="pool")
        S.dma(x_dst[t * 128:(t + 1) * 128, :], z[:], eng="pool")


def phase_prep_merge(kb, layer, wg_d, wb_d, wo_d):
    P = Pool_(kb)
    stg = [P.sb("pst%d" % k, [128, 1024]) for k in range(2)]
    stb = [P.sb("psb%d" % k, [128, 1024], BF16) for k in range(2)]
    jobs = []
    for n in range(4):
        jobs.append((wg_d[n], kb.d["w_gate"][layer, n]))
        jobs.append((wb_d[n], kb.d["w_br"][layer, n]))
    jobs.append((wo_d, kb.d["w_o"][layer]))
    prep_cast(kb, P, jobs, stg, stb)
    P.close()


def phase_merge(kb, layer, mod_d, brT_d, wg_d, wb_d, wo_d, x_src, x_dst):
    S = kb.S
    P = Pool_(kb)
    epi = Epi(kb, P, mod_d, layer, 2, "ln1_g", "ln1_b", "m")
    bgT = P.sb("bgT", [128, 4, 8])
    load_T(kb, P, bgT[:].rearrange("p n c -> p (n c)"), kb.d["b_gate"][layer].rearrange("n (c p) -> (n c) p", p=128), 32)
    wo = P.sb("wo", [128, 8, 1024], BF16)
    S.dma(wo[:], wo_d.rearrange("(c p) n -> p c n", p=128))
    wg = [P.sb("wg%d" % k, [128, 8, 1024], BF16) for k in range(2)]
    wb = [P.sb("wb%d" % k, [128, 4, 1024], BF16) for k in range(2)]
    brt = [P.sb("brt%d" % k, [128, 4, 512], BF16) for k in range(2)]
    mT = P.sb("mT", [128, 8, 512])
    mTb = P.sb("mTb", [128, 8, 512], BF16)
    sig = [P.sb("sig%d" % k, [128, 512]) for k in range(2)]
    tmp = [P.sb("mtmp%d" % k, [128, 512]) for k in range(2)]
    cnt = 0
    q = 0
    for g in range(8):
        tok = slice(g * 512, (g + 1) * 512)
        for n in range(4):
            k = cnt % 2
            cnt += 1
            S.dma(wg[k][:], wg_d[n].rearrange("(c p) n -> p c n", p=128), eng="sp")
            S.dma(wb[k][:], wb_d[n].rearrange("(c p) n -> p c n", p=128), eng="sp")
            S.dma(brt[k][:], brT_d[n, :, :, tok].rearrange("c p t -> p c t"), eng="sp")
            for cc in range(8):
                pa = kb.psb[(q % 2) * 2]
                pb = kb.psb[(q % 2) * 2 + 1]
                for kc in range(8):
                    S.mm(pa[:], wg[k][:, kc, cc * 128:(cc + 1) * 128], kb.hT[:, kc, 1 + g * 512:1 + (g + 1) * 512],
                         start=(kc == 0), stop=(kc == 7))
                for kc in range(4):
                    S.mm(pb[:], wb[k][:, kc, cc * 128:(cc + 1) * 128], brt[k][:, kc, :],
                         start=(kc == 0), stop=(kc == 3))
                sg = sig[q % 2]
                S.act(sg[:], pa[:], AF.Sigmoid, bias=bgT[:, n, cc:cc + 1])
                if n == 0:
                    S.tt(mT[:, cc, :], sg[:], pb[:], ALU.mult)
                else:
                    tp = tmp[q % 2]
                    S.tt(tp[:], sg[:], pb[:], ALU.mult)
                    S.tt(mT[:, cc, :], mT[:, cc, :], tp[:], ALU.add, eng="pool" if cc % 2 else "dve")
                q += 1
        for cc in range(8):
            S.copy(mTb[:, cc, :], mT[:, cc, :], eng="act" if cc % 2 else "pool")
        for tt in range(4):
            t = g * 4 + tt
            epi.prefetch_x(x_src, t)
            ys = []
            for h in range(2):
                py = kb.psb[4 + (t % 2) * 2 + h]
                for kc in range(8):
                    S.mm(py[:], mTb[:, kc, tt * 128:(tt + 1) * 128], wo[:, kc, h * 512:(h + 1) * 512],
                         start=(kc == 0), stop=(kc == 7))
                ys.append(py[:])
            epi.run(ys, x_dst, t)
    P.close()


def phase_moe(kb, layer, mod_d, x_src, x_dst):
    S = kb.S
    P = Pool_(kb)
    NSG = 2
    TSG = T // NSG
    NTS = TSG // 128
    epi = Epi(kb, P, mod_d, layer, 5, "ln2_g", "ln2_b", "e", with_z=False)
    modT = load_modT(kb, P, mod_d, layer, "modT_e")
    rw = P.sb("rw", [128, 8, NE])
    S.dma(rw[:], kb.d["router_w"].rearrange("(c p) e -> p c e", p=128))
    rb = P.sb("rb", [128, NE])
    S.dma(rb[:], kb.d["router_b"].rearrange("(o e) -> o e", o=1).partition_broadcast(128))
    hT = P.sb("hTm", [128, 8, TSG + 1], BF16)
    yacc = P.sb("yacc", [128, NTS, 1024])
    comb = P.sb("comb", [128, NTS, NE])
    h32 = [P.sb("h32_%d" % k, [128, 8, 128]) for k in range(2)]
    st = [P.sb("mst%d" % k, [128, 12]) for k in range(2)]
    mv = [P.sb("mmv%d" % k, [128, 2]) for k in range(2)]
    rs = [P.sb("mrs%d" % k, [128, 1]) for k in range(2)]
    lg = P.sb("r_lg", [128, NE])
    pr = P.sb("r_pr", [128, NE])
    sel = P.sb("r_sel", [128, NE])
    sel2 = P.sb("r_sel2", [128, NE])
    eq = P.sb("r_eq", [128, NE])
    m1 = P.sb("r_m1", [128, 4])
    m2 = P.sb("r_m2", [128, 4])
    gs = P.sb("r_gs", [128, 4])
    gm = P.sb("r_gm", [128, 1])
    og = P.sb("r_og", [128, 4])
    thr = P.sb("r_thr", [128, 4])
    msk = P.sb("r_msk", [128, NE])
    sm = P.sb("r_sm", [128, 1])
    mx = P.sb("r_mx", [128, 1])
    wg = [P.sb("ewg%d" % k, [128, 8, 512], BF16) for k in range(2)]
    wu = [P.sb("ewu%d" % k, [128, 8, 512], BF16) for k in range(2)]
    wd = [P.sb("ewd%d" % k, [128, 4, 1024], BF16) for k in range(2)]
    stg = [P.sb("estg%d" % k, [128, 2, 512]) for k in range(3)]
    heT = [P.sb("heT%d" % k, [128, 4, 512], BF16) for k in range(2)]
    sl = [P.sb("esl%d" % k, [128, 512]) for k in range(2)]
    ident = kb.cst["ident"]
    nld = [0]

    def load_expert(e, k):
        for (dst, src, kcn) in ((wg[k], kb.d["exp_w_gate"][layer, e], 8), (wu[k], kb.d["exp_w_up"][layer, e], 8)):
            sv = src.rearrange("(c p) n -> p c n", p=128)
            for c2 in range(0, kcn, 2):
                sg_ = stg[nld[0] % 3]
                nld[0] += 1
                S.dma(sg_[:], sv[:, c2:c2 + 2, :], eng="sp")
                S.copy(dst[:, c2:c2 + 2, :], sg_[:], eng="pool")
        sv = kb.d["exp_w_down"][layer, e].rearrange("(c p) n -> p c n", p=128)
        for c in range(4):
            sg_ = stg[nld[0] % 3]
            nld[0] += 1
            S.dma(sg_[:].rearrange("p a b -> p (a b)"), sv[:, c, :], eng="sp")
            S.copy(wd[k][:, c, :], sg_[:].rearrange("p a b -> p (a b)"), eng="pool")

    for sgi in range(NSG):
        t0 = sgi * NTS
        for tl in range(NTS):
            t = t0 + tl
            k = tl % 2
            xin = epi.xb[k]
            S.dma(xin[:], x_src[t * 128:(t + 1) * 128, :])
            ln_stats(S, xin, st[k], mv[k], rs[k])
            S.ts(xin[:], xin[:], mv[k][:, 0:1], rs[k][:, 0:1], ALU.subtract, ALU.mult)
            for c in range(8):
                pb = kb.psb[k * 2 + c // 4]
                S.tr(pb[:, (c % 4) * 128:(c % 4 + 1) * 128], xin[:, c * 128:(c + 1) * 128], ident[:])
            for c in range(8):
                pb = kb.psb[k * 2 + c // 4]
                src = pb[:, (c % 4) * 128:(c % 4 + 1) * 128]
                if c % 2 == 0:
                    S.ts(h32[k][:, c, :], src, modT[:, 4, c:c + 1], modT[:, 3, c:c + 1], ALU.mult, ALU.add)
                else:
                    S.act(h32[k][:, c, :], src, AF.Identity, bias=modT[:, 3, c:c + 1], scale=modT[:, 4, c:c + 1])
                S.copy(hT[:, c, 1 + tl * 128:1 + (tl + 1) * 128], h32[k][:, c, :], eng="pool")
            pl = kb.psb[4 + k]
            for c in range(8):
                S.mm(pl[:, 0:NE], h32[k][:, c, :], rw[:, c, :], start=(c == 0), stop=(c == 7))
            S.copy(lg[:], pl[:, 0:NE])
            S.reduce(mx[:], lg[:], ALU.max)
            S.ts(mx[:], mx[:], -1.0, None, ALU.mult)
            S.act(pr[:], lg[:], AF.Exp, bias=mx[:, 0:1], scale=1.0, accum_out=sm[:])
            S.recip(sm[:], sm[:])
            S.ts(pr[:], pr[:], sm[:, 0:1], None, ALU.mult)
            S.tt(sel[:], pr[:], rb[:], ALU.add)
            sel3 = sel[:].rearrange("p (g e) -> p g e", g=4)
            S.reduce(m1[:], sel3, ALU.max)
            S.tt(eq[:].rearrange("p (g e) -> p g e", g=4), sel3, m1[:].unsqueeze(2).to_broadcast([128, 4, 4]), ALU.is_ge)
            S.stt(sel2[:], eq[:], -1e9, sel[:], ALU.mult, ALU.add)
            S.reduce(m2[:], sel2[:].rearrange("p (g e) -> p g e", g=4), ALU.max)
            S.tt(gs[:], m1[:], m2[:], ALU.add)
            S.reduce(gm[:], gs[:], ALU.max)
            S.ts(og[:], gs[:], gm[:, 0:1], None, ALU.is_ge)
            S.ts(thr[:], og[:], -1e9, 1e9, ALU.mult, ALU.add)
            S.tt(thr[:], thr[:], m2[:], ALU.add)
            S.tt(msk[:].rearrange("p (g e) -> p g e", g=4), sel3, thr[:].unsqueeze(2).to_broadcast([128, 4, 4]), ALU.is_ge)
            S.tt(msk[:], msk[:], pr[:], ALU.mult)
            S.reduce(sm[:], msk[:], ALU.add)
            S.recip(sm[:], sm[:])
            S.ts(comb[:, tl, :], msk[:], sm[:, 0:1], None, ALU.mult)
        if sgi == 0:
            load_expert(0, 0)
        q = 0
        for e in range(NE):
            k = (sgi * NE + e) % 2
            nxt = sgi * NE + e + 1
            if nxt < NSG * NE:
                load_expert(nxt % NE, nxt % 2)
            for gq in range(NTS // 4):
                he = heT[gq % 2]
                for fc in range(4):
                    pg = kb.psb[(q % 2) * 2]
                    pu = kb.psb[(q % 2) * 2 + 1]
                    for kc in range(8):
                        S.mm(pg[:], wg[k][:, kc, fc * 128:(fc + 1) * 128], hT[:, kc, 1 + gq * 512:1 + (gq + 1) * 512],
                             start=(kc == 0), stop=(kc == 7))
                    for kc in range(8):
                        S.mm(pu[:], wu[k][:, kc, fc * 128:(fc + 1) * 128], hT[:, kc, 1 + gq * 512:1 + (gq + 1) * 512],
                             start=(kc == 0), stop=(kc == 7))
                    s_ = sl[q % 2]
                    S.act(s_[:], pg[:], AF.Silu)
                    S.tt(he[:, fc, :], s_[:], pu[:], ALU.mult)
                    q += 1
                for tt in range(4):
                    tl = gq * 4 + tt
                    for h in range(2):
                        py = kb.psb[4 + (tl * 2 + h) % 4]
                        for fc in range(4):
                            S.mm(py[:], he[:, fc, tt * 128:(tt + 1) * 128], wd[k][:, fc, h * 512:(h + 1) * 512],
                                 start=(fc == 0), stop=(fc == 3))
                        ya = yacc[:, tl, h * 512:(h + 1) * 512]
                        if e == 0:
                            S.ts(ya, py[:], comb[:, tl, e:e + 1], None, ALU.mult)
                        else:
                            S.stt(ya, py[:], comb[:, tl, e:e + 1], ya, ALU.mult, ALU.add)
        for tl in range(NTS):
            t = t0 + tl
            epi.run([yacc[:, tl, 0:512], yacc[:, tl, 512:1024]], x_dst, t, x_src=x_src, z=yacc[:, tl, :])
    P.close()


def build(io=None, layers=(0, 1), stages=("mod", "ln", "prep", "br", "merge", "moe"), last_out=None):
    kb = KB(io)
    S = kb.S
    mod_d = kb.dram("mod_d", [2, 6144], F32)
    xa = kb.dram("xa", [T, D], F32)
    xbd = kb.dram("xbd", [T, D], F32)
    out = kb.dram("out", [T, D], F32)
    brT_d = kb.dram("brT", [4, 4, 128, T], BF16)
    wg_d = kb.dram("wg_bf", [4, D, D], BF16)
    wb_d = kb.dram("wb_bf", [4, 512, D], BF16)
    wo_d = kb.dram("wo_bf", [D, D], BF16)
    kb.lb_d = kb.dram("lb_d", [2, 512], F32)
    if "mod" in stages:
        phase_mod(kb, mod_d)
    for layer in layers:
        x_src = kb.x if layer == 0 else xbd
        x_fin = out if layer == layers[-1] else xbd
        MP = Pool_(kb)
        kb.hT = MP.sb("hT", [128, 8, T + 1], BF16)
        for c in range(8):
            S.memset(kb.hT[:, c, 0:1], 0.0, eng="pool")
        fold = ("ln" in stages) and ("prep" in stages)
        if "ln" in stages:
            phase_ln_mixer(kb, x_src, mod_d, layer, prep=(wg_d, wb_d, wo_d) if fold else None)
        if "prep" in stages and not fold:
            phase_prep_merge(kb, layer, wg_d, wb_d, wo_d)
        if "br" in stages:
            phase_branches(kb, layer, brT_d)
        if "merge" in stages:
            phase_merge(kb, layer, mod_d, brT_d, wg_d, wb_d, wo_d, x_src, xa if "moe" in stages else x_fin)
        MP.close()
        if "moe" in stages:
            phase_moe(kb, layer, mod_d, xa, x_fin)
    S.emit()
    return kb


def load_w(kb, dst, src, stg, cast_eng="pool", dma_eng="sp"):
    S = kb.S
    kc = src.shape[0] // 128
    n = src.shape[1]
    sv = stg[:, 0:kc, 0:n]
    S.dma(sv, src.rearrange("(c p) n -> p c n", p=128), eng=dma_eng)
    S.copy(dst, sv, eng=cast_eng)


def tok(t0, n=128):
    return slice(1 + t0, 1 + t0 + n)


def fox_branch(kb, layer, brT_d):
    S = kb.S
    P = Pool_(kb)
    hT = kb.hT
    W = kb.d["w_in"][layer]
    o = FOX_OFF
    psb = kb.psb
    stg = P.sb("fstg", [128, 8, 520])
    wq = P.sb("fwq", [128, 8, 512], BF16)
    wk = P.sb("fwk", [128, 8, 512], BF16)
    wvf = P.sb("fwvf", [128, 8, 520], BF16)
    load_w(kb, wq[:], W[:, o:o + 512], stg)
    load_w(kb, wk[:], W[:, o + 512:o + 1024], stg)
    load_w(kb, wvf[:], W[:, o + 1024:o + 1544], stg)
    fb = P.sb("ffb", [128, 8])
    S.dma(fb[:], kb.d["fox_f_bias"][layer:layer + 1, :].partition_broadcast(128))
    maskb = P.sb("fmask", [128, 128], BF16)
    S.copy(maskb[:], kb.cst["triu"][:])
    lf = P.sb("flf", [128, 32, 8])
    tA = P.sb("ftA", [128, 32, 8])
    tB = P.sb("ftB", [128, 32, 8])
    Fs = P.sb("fFs", [128, 32, 8])
    Cs = P.sb("fCs", [128, 32, 8])
    vp = P.sb("fvp", [128, 32, 8, 65], BF16)
    S.memset(vp[:].rearrange("p a b c -> p (a b c)"), 1.0, eng="pool")
    for g in range(8):
        pb = psb[6 + g % 2]
        for tt in range(4):
            t = g * 4 + tt
            for kc in range(8):
                S.mm(pb[:, tt * 8:(tt + 1) * 8], hT[:, kc, tok(t * 128)], wvf[:, kc, 512:520], start=(kc == 0), stop=(kc == 7))
        S.tt(lf[:, g * 4:(g + 1) * 4, :], pb[:, 0:32].rearrange("p (a b) -> p a b", a=4),
             fb[:].unsqueeze(1).to_broadcast([128, 4, 8]), ALU.add)
    lf2 = lf[:].rearrange("p a b -> p (a b)")
    S.act(lf2, lf2, AF.Exp, scale=-1.0)
    S.act(lf2, lf2, AF.Ln, bias=1.0)
    S.ts(lf2, lf2, -1.0, None, ALU.mult)
    a, b = lf, tA
    d = 1
    while d < 32:
        nb = tA if b is tA else tB
        if a is lf:
            nb = tA
        S.tt(nb[:, d:32, :], a[:, d:32, :], a[:, 0:32 - d, :], ALU.add)
        S.copy(nb[:, 0:d, :], a[:, 0:d, :])
        a = nb
        b = tB if nb is tA else tA
        d *= 2
    incl = a
    excl = tB if incl is tA else tA
    S.tt(excl[:], incl[:], lf[:], ALU.subtract)
    pF, pC = psb[6], psb[7]
    S.mm(pF[:, 0:256], kb.cst["triu"][:], lf2, start=True, stop=False)
    S.mm(pF[:, 0:256], kb.cst["ones"][:], excl[:].rearrange("p a b -> p (a b)"), start=False, stop=True)
    S.mm(pC[:, 0:256], kb.cst["ones"][:], incl[:].rearrange("p a b -> p (a b)"), start=True, stop=True)
    S.copy(Fs[:].rearrange("p a b -> p (a b)"), pF[:, 0:256])
    S.copy(Cs[:].rearrange("p a b -> p (a b)"), pC[:, 0:256], eng="act")
    for t in range(32):
        pb = psb[6 + t % 2]
        for kc in range(8):
            S.mm(pb[:], hT[:, kc, tok(t * 128)], wvf[:, kc, 0:512], start=(kc == 0), stop=(kc == 7))
        src = pb[:].rearrange("p (h d) -> p h d", h=8)
        if t % 2 == 0:
            S.copy(vp[:, t, :, 0:64], src, eng="dve")
        else:
            S.copy(vp[:, t, :, 0:64], src, eng="act")
    QT = P.sb("fQT", [128, T], BF16)
    KA = P.sb("fKA", [128, T], BF16)
    KB_ = P.sb("fKB", [128, T], BF16)
    S.memset(KA[64:128, :], 0.0, eng="pool")
    S.memset(KB_[0:64, :], 0.0, eng="pool")
    otok = P.sb("fotok", [128, 32, 128])
    brs = P.sb("fbrs", [128, T], BF16)
    pts = [P.sb("fpt%d" % k, [128, 512], BF16) for k in range(4)]
    vss = [P.sb("fvs%d" % k, [128, 65], BF16) for k in range(6)]
    biases = [P.sb("fbias%d" % k, [128, 32]) for k in range(4)]
    dms = [P.sb("fdm%d" % k, [128, 32]) for k in range(4)]
    rcs = [P.sb("frc%d" % k, [128, 1]) for k in range(4)]
    q = 0
    nb_ = 0
    nv = 0
    for p in range(4):
        for g in range(8):
            pq = psb[6]
            pk = psb[7]
            for kc in range(8):
                S.mm(pq[:], wq[:, kc, p * 128:(p + 1) * 128], hT[:, kc, tok(g * 512, 512)], start=(kc == 0), stop=(kc == 7))
            for kc in range(8):
                S.mm(pk[:], wk[:, kc, p * 128:(p + 1) * 128], hT[:, kc, tok(g * 512, 512)], start=(kc == 0), stop=(kc == 7))
            S.act(QT[:, g * 512:(g + 1) * 512], pq[:], AF.Copy, scale=0.125)
            S.copy(KA[0:64, g * 512:(g + 1) * 512], pk[0:64, :])
            S.copy(KB_[64:128, g * 512:(g + 1) * 512], pk[64:128, :])
        rows = []
        for a_ in range(2):
            for i in range(32):
                rows.append((a_, i))
        batches = []
        for ri, (a_, i) in enumerate(rows):
            for jb in range(0, i + 1, 4):
                batches.append((ri, a_, i, jb, min(4, i + 1 - jb)))
        rowbuf = {}

        def front(bt):
            nonlocal q, nb_
            ri, a_, i, jb, nbt = bt
            h = 2 * p + a_
            Kh = KA if a_ == 0 else KB_
            if jb == 0:
                k2 = nb_ % 4
                nb_ += 1
                rowbuf[ri] = k2
                S.ts(biases[k2][:, 0:i + 1], Fs[:, 0:i + 1, h], Cs[:, i, h:h + 1], -1.0, ALU.subtract, ALU.mult)
                S.act(dms[k2][:, 0:i + 1], biases[k2][:, 0:i + 1], AF.Exp)
            ps_s = psb[q % 4]
            pt = pts[q % 4]
            q += 1
            for jj in range(nbt):
                j = jb + jj
                S.mm(ps_s[:, jj * 128:(jj + 1) * 128], Kh[:, j * 128:(j + 1) * 128], QT[:, i * 128:(i + 1) * 128])
            S.act(pt[:, 0:nbt * 128], ps_s[:, 0:nbt * 128], AF.Exp)
            if jb + nbt - 1 == i:
                S.tt(pt[:, (nbt - 1) * 128:nbt * 128], pt[:, (nbt - 1) * 128:nbt * 128], maskb[:], ALU.mult)
            return pt

        def back(bt, pt):
            nonlocal nv
            ri, a_, i, jb, nbt = bt
            h = 2 * p + a_
            k2 = rowbuf[ri]
            po = psb[4 + ri % 2]
            dm = dms[k2]
            rc = rcs[k2]
            for jj in range(nbt):
                j = jb + jj
                vs = vss[nv % 6]
                nv += 1
                S.ts(vs[:], vp[:, j, h, :], dm[:, j:j + 1], None, ALU.mult)
                S.mm(po[:, 0:65], pt[:, jj * 128:(jj + 1) * 128], vs[:], start=(j == 0), stop=(j == i))
            if jb + nbt - 1 == i:
                S.recip(rc[:], po[:, 64:65])
                S.ts(otok[:, i, a_ * 64:(a_ + 1) * 64], po[:, 0:64], rc[:, 0:1], None, ALU.mult)
                if a_ == 1:
                    pt_ = psb[6 + i % 2]
                    S.tr(pt_[:, 0:128], otok[:, i, :], kb.cst["ident"][:])
                    S.copy(brs[:, i * 128:(i + 1) * 128], pt_[:, 0:128], eng="act" if i % 2 else "dve")

        LOOK = DBG.get("fox_look", 2)
        pend = []
        for bt in batches:
            ptn = front(bt)
            pend.append((bt, ptn))
            if len(pend) > LOOK:
                back(*pend.pop(0))
        while pend:
            back(*pend.pop(0))
        S.dma(brT_d[2, p], brs[:], eng="pool")
    P.close()


def phase_branches(kb, layer, brT_d):
    which = DBG.get("branches", (0, 1, 2, 3))
    if 2 in which:
        fox_branch(kb, layer, brT_d)
    if 0 in which:
        gla_branch(kb, layer, brT_d)
    if 3 in which:
        hgrn_branch(kb, layer, brT_d)
    if 1 in which:
        rwkv_branch(kb, layer, brT_d)


def cgla_branch(kb, layer, brT_d, kind, lb_d=None):
    S = kb.S
    P = Pool_(kb)
    hT = kb.hT
    W = kb.d["w_in"][layer]
    psb = kb.psb
    cst = kb.cst
    gla = (kind == "gla")
    NU = 2 if gla else 4
    HPU = 2 if gla else 1
    DK = 64 if gla else 128
    KW = NU * 128
    o = GLA_OFF if gla else HGRN_OFF
    bidx = 0 if gla else 3
    qscale = 0.125 if gla else 1.0
    stg = P.sb("cstg", [128, 8, 528])
    if gla:
        wq = P.sb("cwq", [128, 8, 256], BF16)
        wk = P.sb("cwk", [128, 8, 256], BF16)
        wv = P.sb("cwv", [128, 8, 512], BF16)
        wg = P.sb("cwg", [128, 8, 528], BF16)
        load_w(kb, wq[:], W[:, o:o + 256], stg)
        load_w(kb, wk[:], W[:, o + 256:o + 512], stg)
        load_w(kb, wv[:], W[:, o + 512:o + 1024], stg)
        load_w(kb, wg[:], W[:, o + 1024:o + 1552], stg)
        aup = P.sb("caup", [16, 256])
        S.dma(aup[:], kb.d["gla_alpha_up"][layer])
        abr = P.sb("cabr", [1, 256])
        S.dma(abr[:], kb.d["gla_alpha_b"][layer:layer + 1, :])
        alT = P.sb("calT", [16, 512])
        Uc = P.sb("cUc", [128, 128])
        SUl = P.sb("cSUl", [128, 128])
        S.ts(Uc[:], cst["triu64"][:], -1.0 / 16.0, None, ALU.mult)
        S.ts(SUl[:], cst["slo64"][:], -1.0 / 16.0, None, ALU.mult)
        ng_src = kb.d["gla_norm_g"]
    else:
        wq = P.sb("cwq", [128, 8, 512], BF16)
        wk = P.sb("cwk", [128, 8, 512], BF16)
        wv = P.sb("cwv", [128, 8, 512], BF16)
        wg = P.sb("cwg", [128, 8, 512], BF16)
        load_w(kb, wq[:], W[:, o:o + 512], stg)
        load_w(kb, wk[:], W[:, o + 512:o + 1024], stg)
        load_w(kb, wv[:], W[:, o + 1024:o + 1536], stg)
        load_w(kb, wg[:], W[:, o + 1536:o + 2048], stg)
        Uc, SUl = cst["triu64"], cst["slo64"]
        lbB = P.sb("clbB", [128, 512])
        omlB = P.sb("comlB", [128, 512])
        S.dma(lbB[:], lb_d[0:1, :].partition_broadcast(128))
        S.ts(omlB[:], lbB[:], -1.0, 1.0, ALU.mult, ALU.add)
        lbT = P.sb("clbT", [128, 4])
        omlT = P.sb("comlT", [128, 4])
        load_T(kb, P, lbT[:], lb_d[0].rearrange("(c p) -> c p", p=128), 4)
        S.ts(omlT[:], lbT[:], -1.0, 1.0, ALU.mult, ALU.add)
        ng_src = kb.d["hgrn_norm_g"]
    ngb = P.sb("cngb", [128, 128])
    S.dma(ngb[:], ng_src[layer:layer + 1, :].partition_broadcast(128))
    qTs = [P.sb("cqTs%d" % u, [128, 512]) for u in range(NU)]
    kTs = [P.sb("ckTs%d" % u, [128, 512]) for u in range(NU)]
    brs = P.sb("cbrs", [128, 4, T], BF16)
    l_tok = P.sb("cltok", [128, KW])
    k_tok = P.sb("cktok", [128, KW])
    f_tok = P.sb("cftok", [128, KW])
    v_tok = P.sb("cvtok", [128, 512], BF16)
    sg_tok = P.sb("csgtok", [128, 512])
    br_tok = P.sb("cbrtok", [128, 512])
    bTs = P.sb("cbTs", [128, 128])
    kdec = P.sb("ckdec", [128, 128])
    khat = P.sb("ckhat", [128, 128], BF16)
    bm = P.sb("cbm", [128, 2])
    nbm = P.sb("cnbm", [128, 2])
    E1 = P.sb("cE1", [128, 128])
    E2 = P.sb("cE2", [128, 128])
    E3 = P.sb("cE3", [128, 128])
    qt = P.sb("cqt", [128, 128], BF16)
    kt = P.sb("ckt", [128, 128], BF16)
    qA = P.sb("cqA", [128, 128], BF16)
    qB = P.sb("cqB", [128, 128], BF16)
    attb = [P.sb("cattb%d" % a, [128, 128], BF16) for a in range(HPU)]
    Sf = [[P.sb("cSf%d_%d" % (u, k), [128, 128]) for k in range(2)] for u in range(NU)]
    Sb = [[P.sb("cSb%d_%d" % (u, k), [128, 128], BF16) for k in range(2)] for u in range(NU)]
    for u in range(NU):
        S.memset(Sf[u][0][:], 0.0)
        S.memset(Sb[u][0][:], 0.0)
    ss = P.sb("css", [128, 2])
    junk = P.sb("cjunk", [128, 128])
    v_tokD = [v_tok, P.sb("cvtok2", [128, 512], BF16)]
    sg_tokD = [sg_tok, P.sb("csgtok2", [128, 512])]
    br_tokD = [br_tok, P.sb("cbrtok2", [128, 512])]
    khatD = [khat, P.sb("ckhat2", [128, 128], BF16)]
    qAD = [qA, P.sb("cqA2", [128, 128], BF16)]
    attbD = [attb, [P.sb("cattb2_%d" % a, [128, 128], BF16) for a in range(HPU)]]
    eblD = [P.sb("cebl%d" % k, [128, 2]) for k in range(2)]
    iters = []
    for g in range(DBG.get('cg_ng', 8)):
        for tt in range(4):
            for u in range(NU):
                iters.append((g, tt, u))

    def setup(it):
        g, tt, u = iters[it]
        bf = it % 2
        t = g * 4 + tt
        tp = t % 2
        gt = tok(g * 512, 512)
        tk = tok(t * 128)
        tcol = slice(tt * 128, (tt + 1) * 128)
        v_tok, sg_tok = v_tokD[tp], sg_tokD[tp]
        khat, qA, attb, ebl = khatD[bf], qAD[bf], attbD[bf], eblD[bf]
        if tt == 0 and u == 0:
            for u2 in range(NU):
                pq, pk = psb[0], psb[1]
                for kc in range(8):
                    S.mm(pq[:], wq[:, kc, u2 * 128:(u2 + 1) * 128], hT[:, kc, gt], start=(kc == 0), stop=(kc == 7))
                for kc in range(8):
                    S.mm(pk[:], wk[:, kc, u2 * 128:(u2 + 1) * 128], hT[:, kc, gt], start=(kc == 0), stop=(kc == 7))
                yield
                S.copy(qTs[u2][:], pq[:], eng="act")
                if gla:
                    S.copy(kTs[u2][:], pk[:], eng="dve")
                else:
                    S.act(kTs[u2][:], pk[:], AF.Sigmoid)
                    S.ts(kTs[u2][:], kTs[u2][:], omlT[:, u2:u2 + 1], lbT[:, u2:u2 + 1], ALU.mult, ALU.add)
                    S.ts(kTs[u2][:], kTs[u2][:], -1.0, 1.0, ALU.mult, ALU.add)
                yield
            if gla:
                pa = psb[2]
                for kc in range(8):
                    S.mm(pa[0:16, :], wg[:, kc, 512:528], hT[:, kc, gt], start=(kc == 0), stop=(kc == 7))
                S.copy(alT[:], pa[0:16, :])
                yield
        if u == 0:
            pv, pg = psb[2], psb[3]
            for kc in range(8):
                S.mm(pv[:], hT[:, kc, tk], wv[:, kc, 0:512], start=(kc == 0), stop=(kc == 7))
            for kc in range(8):
                S.mm(pg[:], hT[:, kc, tk], wg[:, kc, 0:512], start=(kc == 0), stop=(kc == 7))
            yield
            S.copy(v_tok[:], pv[:], eng="act")
            S.act(sg_tok[:], pg[:], AF.Silu)
            pk2 = psb[2]
            if gla:
                for kc in range(8):
                    S.mm(pk2[:, 0:256], hT[:, kc, tk], wk[:, kc, 0:256], start=(kc == 0), stop=(kc == 7))
                S.mm(pk2[:, 256:512], alT[:, tcol], aup[:], start=True, stop=False)
                S.mm(pk2[:, 256:512], cst["ones"][0:1, :], abr[:], start=False, stop=True)
                yield
                S.copy(k_tok[:], pk2[:, 0:256])
                S.act(l_tok[:], pk2[:, 256:512], AF.Exp, scale=-1.0)
                S.act(l_tok[:], l_tok[:], AF.Ln, bias=1.0)
            else:
                for kc in range(8):
                    S.mm(pk2[:], hT[:, kc, tk], wk[:, kc, 0:512], start=(kc == 0), stop=(kc == 7))
                yield
                S.act(f_tok[:], pk2[:], AF.Sigmoid)
                S.tt(f_tok[:], f_tok[:], omlB[:], ALU.mult)
                S.tt(f_tok[:], f_tok[:], lbB[:], ALU.add)
                S.act(l_tok[:], f_tok[:], AF.Ln)
                S.ts(k_tok[:], f_tok[:], -1.0, 1.0, ALU.mult, ALU.add)
            yield
        cu = slice(u * 128, (u + 1) * 128)
        pX = psb[4]
        S.mm(pX[:, 0:128], l_tok[:, cu], Uc[:])
        S.mm(pX[:, 128:256], SUl[:], l_tok[:, cu])
        yield
        S.copy(bTs[:], pX[:, 0:128])
        S.act(kdec[:], pX[:, 128:256], AF.Exp)
        S.tt(khat[:], k_tok[:, cu], kdec[:], ALU.mult)
        mid = bTs[:].rearrange("p (c s) -> p c s", c=2)[:, :, 32]
        S.copy(bm[:], mid)
        S.ts(nbm[:], mid, -1.0, None, ALU.mult)
        yield
        for c in range(2):
            cs = slice(c * 64, (c + 1) * 64)
            S.act(E1[:, cs], bTs[:, cs], AF.Exp, bias=nbm[:, c:c + 1])
            S.act(E2[:, cs], bTs[:, cs], AF.Exp, bias=bm[:, c:c + 1], scale=-1.0)
        S.act(E3[:], bTs[:], AF.Exp)
        yield
        S.stt(qt[:], qTs[u][:, tcol], qscale, E1[:], ALU.mult, ALU.mult)
        S.tt(kt[:], kTs[u][:, tcol], E2[:], ALU.mult)
        S.stt(qA[:], qTs[u][:, tcol], qscale, E3[:], ALU.mult, ALU.mult)
        S.copy(ebl[:], E3[:].rearrange("p (c s) -> p c s", c=2)[:, :, 63])
        pAtt = psb[5]
        for a in range(HPU):
            ra = slice(a * DK, (a + 1) * DK)
            S.mm(pAtt[:, a * 128:(a + 1) * 128], kt[ra, :], qt[ra, :])
        yield
        for a in range(HPU):
            S.tt(attb[a][:], cst["triu64"][:], pAtt[:, a * 128:(a + 1) * 128], ALU.mult)
        yield

    def chain(it):
        g, tt, u = iters[it]
        bf = it % 2
        t = g * 4 + tt
        tp = t % 2
        v_tok, sg_tok, br_tok = v_tokD[tp], sg_tokD[tp], br_tokD[tp]
        khat, qA, attb, ebl = khatD[bf], qAD[bf], attbD[bf], eblD[bf]
        S0f, S1f = Sf[u][0], Sf[u][1]
        S0b, S1b = Sb[u][0], Sb[u][1]
        pS = psb[6]
        for c in range(2):
            rows = slice(c * 64, (c + 1) * 64)
            for a in range(HPU):
                h = u * HPU + a
                ra = slice(a * DK, (a + 1) * DK)
                S.mm(pS[ra, c * 128:(c + 1) * 128], khat[rows, a * DK:(a + 1) * DK], v_tok[rows, h * 128:(h + 1) * 128])
        yield
        S.stt(S1f[:], S0f[:], ebl[:, 0:1], pS[:, 0:128], ALU.mult, ALU.add)
        S.copy(S1b[:], S1f[:], eng="act")
        yield
        pO = psb[7]
        for a in range(HPU):
            h = u * HPU + a
            ra = slice(a * DK, (a + 1) * DK)
            oc = slice(a * 128, (a + 1) * 128)
            S.mm(pO[0:64, oc], qA[ra, 0:64], S0b[ra, :], start=True, stop=False)
            S.mm(pO[64:128, oc], qA[ra, 64:128], S1b[ra, :], start=True, stop=False)
            S.mm(pO[:, oc], attb[a][:], v_tok[:, h * 128:(h + 1) * 128], start=False, stop=True)
        yield
        S.stt(S0f[:], S1f[:], ebl[:, 1:2], pS[:, 128:256], ALU.mult, ALU.add)
        S.copy(S0b[:], S0f[:], eng="act")
        for a in range(HPU):
            oc = slice(a * 128, (a + 1) * 128)
            S.act(junk[:], pO[:, oc], AF.Square, accum_out=ss[:, a:a + 1])
        yield
        for a in range(HPU):
            rsqrt_eps(S, ss[:, a:a + 1], ss[:, a:a + 1], 1e-6, scale=1.0 / 128.0)
        yield
        for a in range(HPU):
            h = u * HPU + a
            oc = slice(a * 128, (a + 1) * 128)
            hc = slice(h * 128, (h + 1) * 128)
            S.stt(br_tok[:, hc], pO[:, oc], ss[:, a:a + 1], ngb[:], ALU.mult, ALU.mult)
            S.tt(br_tok[:, hc], br_tok[:, hc], sg_tok[:, hc], ALU.mult)
        yield
        if u == NU - 1:
            for kc in range(4):
                pT = pO if kc < 2 else pS
                S.tr(pT[:, 256 + (kc % 2) * 128:256 + (kc % 2 + 1) * 128], br_tok[:, kc * 128:(kc + 1) * 128], cst["ident"][:])
            yield
            S.copy(brs[:, 0:2, t * 128:(t + 1) * 128], pO[:, 256:512].rearrange("p (c t) -> p c t", c=2), eng="act")
            S.copy(brs[:, 2:4, t * 128:(t + 1) * 128], pS[:, 256:512].rearrange("p (c t) -> p c t", c=2), eng="dve")
            yield

    def run_interleaved(gens):
        active = [x for x in gens if x is not None]
        while active:
            for gi in list(active):
                try:
                    next(gi)
                except StopIteration:
                    active.remove(gi)

    n_it = len(iters)
    for k in range(n_it + 1):
        run_interleaved([setup(k) if k < n_it else None, chain(k - 1) if k >= 1 else None])
    S.dma(brT_d[bidx].rearrange("c p t -> p c t"), brs[:], eng="pool")
    P.close()


def gla_branch(kb, layer, brT_d):
    cgla_branch(kb, layer, brT_d, "gla")


def hgrn_branch(kb, layer, brT_d):
    S = kb.S
    P = Pool_(kb)
    lb_d = kb.lb_d
    row = P.sb("hlbrow", [1, 512])
    if layer == 0:
        S.memset(row[:], 0.0)
    else:
        r0 = P.sb("hlb0", [1, 512])
        S.dma(r0[:], kb.d["hgrn_lb_logits"][0:1, :])
        S.dma(row[:], kb.d["hgrn_lb_logits"][1:2, :])
        S.tt(row[:], row[:], r0[:], ALU.subtract)
        S.act(row[:], row[:], AF.Sigmoid)
    S.dma(lb_d[layer:layer + 1, :], row[:])
    P.close()
    cgla_branch(kb, layer, brT_d, "hgrn", lb_d=lb_d[layer:layer + 1, :])


def rwkv_branch(kb, layer, brT_d):
    S = kb.S
    hT = kb.hT
    W = kb.d["w_in"][layer]
    psb = kb.psb
    cst = kb.cst
    o = RWKV_OFF
    P = Pool_(kb)
    CW = math.exp(-0.5)
    Wr = [P.sb("rWr%d" % k, [128, 8, 512], BF16) for k in range(2)]
    Wk = [P.sb("rWk%d" % k, [128, 8, 512], BF16) for k in range(2)]
    Wv = [P.sb("rWv%d" % k, [128, 8, 512], BF16) for k in range(2)]
    Wl = [P.sb("rWl%d" % k, [128, 8, 256], BF16) for k in range(2)]
    PP = Pool_(kb)
    stg = PP.sb("rstg", [128, 8, 512])
    muB = PP.sb("rmuB", [128, 1792])
    omuB = PP.sb("romuB", [128, 1792])
    S.dma(muB[:], kb.d["rwkv_mu"][layer:layer + 1, :].partition_broadcast(128))
    S.ts(omuB[:], muB[:], -1.0, 1.0, ALU.mult, ALU.add)
    for (dst, c0, n) in ((Wr, 0, 512), (Wk, 512, 512), (Wv, 1024, 512), (Wl, 1536, 256)):
        sv = stg[:, :, 0:n]
        S.dma(sv, W[:, o + c0:o + c0 + n].rearrange("(c p) n -> p c n", p=128))
        S.tt(dst[0][:], sv, omuB[:, c0:c0 + n].unsqueeze(1).to_broadcast([128, 8, n]), ALU.mult)
        S.tt(dst[1][:], sv, muB[:, c0:c0 + n].unsqueeze(1).to_broadcast([128, 8, n]), ALU.mult, eng="pool")
    PP.close()
    lw = P.sb("rlw", [128, 512])
    S.dma(lw[0:64, :], kb.d["rwkv_w2"][layer])
    S.dma(lw[64:128, :], kb.d["rwkv_a2"][layer])
    g2f = P.sb("rg2f", [128, 512])
    g2b = P.sb("rg2b", [128, 512], BF16)
    S.dma(g2f[:], kb.d["rwkv_g2"][layer])
    S.copy(g2b[:], g2f[:])
    w0r = P.sb("rw0r", [1, 512])
    S.dma(w0r[:], kb.d["rwkv_w0"][layer:layer + 1, :])
    a0T = P.sb("ra0T", [128, 4])
    kkT_ = P.sb("rkkT", [128, 4])
    kaT = P.sb("rkaT", [128, 4])
    okaT = P.sb("rokaT", [128, 4])
    rkT_ = P.sb("rrkT", [128, 4])
    load_T(kb, P, a0T[:], kb.d["rwkv_a0"][layer].rearrange("(c p) -> c p", p=128), 4)
    load_T(kb, P, kkT_[:], kb.d["rwkv_k_k"][layer].rearrange("(c p) -> c p", p=128), 4)
    load_T(kb, P, kaT[:], kb.d["rwkv_k_a"][layer].rearrange("(c p) -> c p", p=128), 4)
    load_T(kb, P, rkT_[:], kb.d["rwkv_r_k"][layer].rearrange("(c two) d -> c (two d)", two=2), 4)
    S.ts(okaT[:], kaT[:], -1.0, 1.0, ALU.mult, ALU.add)
    lngB = P.sb("rlngB", [128, 512])
    lnbB = P.sb("rlnbB", [128, 512])
    S.dma(lngB[:], kb.d["rwkv_ln_g"][layer:layer + 1, :].partition_broadcast(128))
    S.dma(lnbB[:], kb.d["rwkv_ln_b"][layer:layer + 1, :].partition_broadcast(128))
    Uc = P.sb("rUc", [128, 128])
    Ux = P.sb("rUx", [128, 128])
    SUl = P.sb("rSUl", [128, 128])
    nsup = P.sb("rnsup", [128, 128])
    nslo = P.sb("rnslo", [128, 128])
    ntriu = P.sb("rntriu", [128, 128])
    S.ts(Uc[:], cst["triu64"][:], -CW, None, ALU.mult)
    S.ts(Ux[:], cst["sup64"][:], -CW, None, ALU.mult)
    S.ts(SUl[:], cst["slo64"][:], -CW, None, ALU.mult)
    S.ts(nsup[:], cst["sup64"][:], -1.0, None, ALU.mult)
    S.ts(nslo[:], cst["slo64"][:], -1.0, None, ALU.mult)
    S.ts(ntriu[:], cst["triu64"][:], -1.0, None, ALU.mult)
    hsel = P.sb("rhsel", [128, 2])
    S.copy(hsel[:], cst["blk64"][:].rearrange("p (a s) -> p a s", a=2)[:, :, 0])
    ident = cst["ident"]

    def f512(name):
        return P.sb(name, [128, 512])

    def f128(name, dt=F32):
        return P.sb(name, [128, 128], dt)

    lo = f512("rlo")
    sgl = P.sb("rsgl", [128, 512], BF16)
    rT, kT, aT, kaT_, bT_, kpT, tmpF, prodT = (f512("r_" + n) for n in ("rT", "kT", "aT", "kapT", "bbT", "kpT", "tmpF", "prodT"))
    def d128(name):
        return [f128("%s_%d" % (name, k)) for k in range(2)]

    l_tok = f128("rltok")
    v_tokD, g_tokD = d128("rvtok"), d128("rgtok")
    sb2D = [P.sb("rsb2_%d" % k, [128, 2]) for k in range(2)]
    gCD = [P.sb("rgC_%d" % k, [128, 2]) for k in range(2)]
    bTs, bxTs = f128("rbTs"), f128("rbxTs")
    bm, nbm, bl = P.sb("rbm", [128, 2]), P.sb("rnbm", [128, 2]), P.sb("rbl", [128, 2])
    E = {n: f128("rE_" + n) for n in ("r", "kx", "inv", "abs", "absx", "last")}
    RB = BF16 if DBG.get("rw_bf16", True) else F32
    rt, kxt, kt, bt = (f128("r_" + n, RB) for n in ("rt", "kxt", "kt", "bt"))
    KhT, BhT = (f128("r_" + n) for n in ("KhT", "BhT"))
    rbarD, kbarD, KhatD, BhatD = d128("rrbar"), d128("rkbar"), d128("rKhat"), d128("rBhat")
    Mm = [[f128("rM%d_%d" % (a, k), RB) for k in range(2)] for a in range(2)]
    MT = [[f128("rMT%d_%d" % (a, k), RB) for k in range(2)] for a in range(2)]
    Pb = [[f128("rPb%d_%d" % (a, k), RB) for k in range(2)] for a in range(2)]
    PmD = [[[f128("rP%d_%d_%d" % (b_, a, k)) for k in range(2)] for a in range(2)] for b_ in range(2)]
    AkkD = [[f128("rAkk%d_%d" % (b_, a)) for a in range(2)] for b_ in range(2)]
    ArkD = [[f128("rArk%d_%d" % (b_, a)) for a in range(2)] for b_ in range(2)]
    ArbD = [[f128("rArb%d_%d" % (b_, a)) for a in range(2)] for b_ in range(2)]
    Ws, Us, ytok = f128("rWs"), f128("rUs"), f128("rytok")
    Hs = [[P.sb("rHs%d_%d" % (u, k), [128, 64]) for k in range(2)] for u in range(4)]
    for u in range(4):
        S.memset(Hs[u][0][:], 0.0)
    st6 = P.sb("rst6", [128, 2, 6])
    mv2 = P.sb("rmv2", [128, 2, 2])
    rs2 = P.sb("rrs2", [128, 2])
    yn = f128("ryn")
    brt_ = f128("rbrt")
    brb = [P.sb("rbrb%d" % k, [128, 128], BF16) for k in range(2)]

    def proj_fm(ps, W2, c0, n, gcol):
        for kc in range(8):
            S.mm(ps[0:n, :], W2[0][:, kc, c0:c0 + n], hT[:, kc, slice(1 + gcol, 1 + gcol + 512)], start=(kc == 0), stop=False)
        for kc in range(8):
            S.mm(ps[0:n, :], W2[1][:, kc, c0:c0 + n], hT[:, kc, slice(gcol, gcol + 512)], start=False, stop=(kc == 7))

    iters = []
    for g in range(DBG.get("rw_ng", 8)):
        for u in range(4):
            for tt_ in range(4):
                iters.append((g, u, tt_))

    def setup(it):
        g, u, tt_ = iters[it]
        bf = it % 2
        gcol = g * 512
        uc = slice(u * 128, (u + 1) * 128)
        v_tok, g_tok, sb2, gC = v_tokD[bf], g_tokD[bf], sb2D[bf], gCD[bf]
        rbar, kbar, Khat, Bhat = rbarD[bf], kbarD[bf], KhatD[bf], BhatD[bf]
        Pm, Akk, Ark, Arb = PmD[bf], AkkD[bf], ArkD[bf], ArbD[bf]
        if u == 0 and tt_ == 0:
            proj_fm(psb[0], Wl, 0, 128, gcol)
            S.act(lo[0:64, :], psb[0][0:64, :], AF.Tanh)
            S.copy(lo[64:128, :], psb[0][64:128, :])
            proj_fm(psb[1], Wl, 128, 128, gcol)
            S.act(sgl[:], psb[1][:], AF.Sigmoid)
            yield
        if tt_ == 0:
            proj_fm(psb[0], Wr, u * 128, 128, gcol)
            S.copy(rT[:], psb[0][:], eng="act")
            proj_fm(psb[1], Wk, u * 128, 128, gcol)
            S.copy(kT[:], psb[1][:])
            yield
            S.mm(psb[0][:], lw[64:128, uc], lo[64:128, :])
            S.act(aT[:], psb[0][:], AF.Sigmoid, bias=a0T[:, u:u + 1])
            S.ts(kaT_[:], kT[:], kkT_[:, u:u + 1], None, ALU.mult)
            S.tt(tmpF[:], kaT_[:], kaT_[:], ALU.mult)
            S.mm(psb[1][:], cst["blk64"][:], tmpF[:])
            yield
            S.act(tmpF[:], psb[1][:], AF.Ln, bias=1e-24)
            S.act(tmpF[:], tmpF[:], AF.Exp, scale=-0.5)
            S.tt(kaT_[:], kaT_[:], tmpF[:], ALU.mult)
            S.tt(bT_[:], kaT_[:], aT[:], ALU.mult)
            yield
            S.ts(tmpF[:], aT[:], kaT[:, u:u + 1], okaT[:, u:u + 1], ALU.mult, ALU.add)
            S.tt(kpT[:], kT[:], tmpF[:], ALU.mult)
            S.stt(prodT[:], rT[:], rkT_[:, u:u + 1], kpT[:], ALU.mult, ALU.mult)
            yield
        t = g * 4 + tt_
        t0 = t * 128
        tc_ = slice(tt_ * 128, (tt_ + 1) * 128)
        pt_ = psb[2]
        for kc in range(8):
            S.mm(pt_[:, 0:128], hT[:, kc, slice(1 + t0, 1 + t0 + 128)], Wv[0][:, kc, uc], start=(kc == 0), stop=False)
        for kc in range(8):
            S.mm(pt_[:, 0:128], hT[:, kc, slice(t0, t0 + 128)], Wv[1][:, kc, uc], start=False, stop=(kc == 7))
        S.mm(pt_[:, 128:256], lo[0:64, tc_], lw[0:64, uc], start=True, stop=False)
        S.mm(pt_[:, 128:256], cst["ones"][0:1, :], w0r[0:1, uc], start=False, stop=True)
        S.mm(pt_[:, 256:384], sgl[:, tc_], g2b[:, uc])
        S.mm(pt_[:, 384:386], prodT[:, tc_], hsel[:])
        yield
        S.act(l_tok[:], pt_[:, 128:256], AF.Sigmoid)
        S.copy(v_tok[:], pt_[:, 0:128])
        S.copy(g_tok[:], pt_[:, 256:384], eng="act")
        S.copy(sb2[:], pt_[:, 384:386])
        pX = psb[3]
        S.mm(pX[:, 0:128], l_tok[:], Uc[:])
        S.mm(pX[:, 128:256], l_tok[:], Ux[:])
        yield
        S.copy(bTs[:], pX[:, 0:128])
        S.copy(bxTs[:], pX[:, 128:256], eng="act")
        b3 = bTs[:].rearrange("p (c s) -> p c s", c=2)
        S.copy(bm[:], b3[:, :, 32])
        S.ts(nbm[:], b3[:, :, 32], -1.0, None, ALU.mult)
        S.copy(bl[:], b3[:, :, 63])
        yield
        for c in range(2):
            cs = slice(c * 64, (c + 1) * 64)
            S.act(E["r"][:, cs], bTs[:, cs], AF.Exp, bias=nbm[:, c:c + 1])
            S.act(E["kx"][:, cs], bxTs[:, cs], AF.Exp, bias=nbm[:, c:c + 1])
            S.act(E["inv"][:, cs], bTs[:, cs], AF.Exp, bias=bm[:, c:c + 1], scale=-1.0)
            S.act(E["last"][:, cs], bTs[:, cs], AF.Exp, bias=bl[:, c:c + 1], scale=-1.0)
        S.act(E["abs"][:], bTs[:], AF.Exp)
        S.act(E["absx"][:], bxTs[:], AF.Exp)
        yield
        S.tt(rt[:], rT[:, tc_], E["r"][:], ALU.mult)
        S.tt(kxt[:], kaT_[:, tc_], E["kx"][:], ALU.mult)
        S.tt(kt[:], kpT[:, tc_], E["inv"][:], ALU.mult)
        S.tt(bt[:], bT_[:, tc_], E["inv"][:], ALU.mult)
        yield
        S.tt(KhT[:], kpT[:, tc_], E["last"][:], ALU.mult)
        S.stt(BhT[:], bT_[:, tc_], -1.0, E["last"][:], ALU.mult, ALU.mult)
        S.tt(rbar[:], rT[:, tc_], E["abs"][:], ALU.mult)
        S.tt(kbar[:], kaT_[:, tc_], E["absx"][:], ALU.mult)
        S.copy(gC[:], E["abs"][:].rearrange("p (c s) -> p c s", c=2)[:, :, 63])
        for a in range(2):
            ra = slice(a * 64, (a + 1) * 64)
            pA = psb[4 + a]
            S.mm(pA[:, 0:128], bt[ra, :], kxt[ra, :])
            S.mm(pA[:, 128:256], kxt[ra, :], bt[ra, :])
            S.mm(pA[:, 256:384], kt[ra, :], kxt[ra, :])
            S.mm(pA[:, 384:512], kt[ra, :], rt[ra, :])
            S.mm(psb[6][:, a * 128:(a + 1) * 128], bt[ra, :], rt[ra, :])
        S.tr(pX[:, 256:384], KhT[:], ident[:])
        S.tr(pX[:, 384:512], BhT[:], ident[:])
        yield
        for a in range(2):
            pA = psb[4 + a]
            S.tt(Mm[a][0][:], nsup[:], pA[:, 0:128], ALU.mult)
            S.tt(MT[a][0][:], nslo[:], pA[:, 128:256], ALU.mult)
            S.tt(Pm[a][0][:], Mm[a][0][:], ident[:], ALU.add)
            S.copy(Pb[a][0][:], Pm[a][0][:], eng="act")
        yield
        for a in range(2):
            pA = psb[4 + a]
            S.tt(Akk[a][:], cst["sup64"][:], pA[:, 256:384], ALU.mult)
            S.tt(Ark[a][:], cst["triu64"][:], pA[:, 384:512], ALU.mult)
            S.tt(Arb[a][:], ntriu[:], psb[6][:, a * 128:(a + 1) * 128], ALU.mult)
        S.copy(Khat[:], pX[:, 256:384], eng="act")
        S.copy(Bhat[:], pX[:, 384:512], eng="act")
        cur = 0

        def stA(lvl, cur):
            for a in range(2):
                pA = psb[4 + a]
                if lvl < 5:
                    S.mm(pA[:, 0:128], MT[a][cur][:], Mm[a][cur][:])
                S.mm(pA[:, 128:256], Mm[a][cur][:], MT[a][cur][:])

        def stB(lvl, cur):
            nxt = 1 - cur
            for a in range(2):
                pA = psb[4 + a]
                eng = "dve" if a == 0 else "act"
                if lvl < 5:
                    S.copy(Mm[a][nxt][:], pA[:, 0:128], eng=eng)
                S.copy(MT[a][nxt][:], pA[:, 128:256], eng=eng)

        def stC(lvl, cur):
            nxt = 1 - cur
            for a in range(2):
                pA = psb[4 + a]
                S.mm(pA[:, 256:384], MT[a][nxt][:], Pb[a][cur][:])

        def stD(lvl, cur):
            nxt = 1 - cur
            for a in range(2):
                pA = psb[4 + a]
                S.tt(Pm[a][nxt][:], Pm[a][cur][:], pA[:, 256:384], ALU.add)
                if lvl < 5:
                    S.copy(Pb[a][nxt][:], Pm[a][nxt][:], eng="act")

        stA(1, 0)
        yield
        stB(1, 0)
        yield
        for lvl in range(2, 6):
            c_prev = (lvl - 2) % 2
            c_cur = (lvl - 1) % 2
            stC(lvl - 1, c_prev)
            stA(lvl, c_cur)
            yield
            stD(lvl - 1, c_prev)
            stB(lvl, c_cur)
            yield
        stC(5, 0)
        yield
        stD(5, 0)
        yield
        cur = 1
        assert cur == 1

    nbr = [0]

    def chain(it):
        g, u, tt_ = iters[it]
        bf = it % 2
        t0 = (g * 4 + tt_) * 128
        v_tok, g_tok, sb2, gC = v_tokD[bf], g_tokD[bf], sb2D[bf], gCD[bf]
        rbar, kbar, Khat, Bhat = rbarD[bf], kbarD[bf], KhatD[bf], BhatD[bf]
        Akk, Ark, Arb = AkkD[bf], ArkD[bf], ArbD[bf]
        Pf = [PmD[bf][a][1] for a in range(2)]
        H0, H1 = Hs[u][0], Hs[u][1]
        pC = psb[7]
        Hc = [H0, H1, H0]
        for c in range(2):
            rc = slice(c * 64, (c + 1) * 64)
            Hin, Hout = Hc[c], Hc[c + 1]
            for a in range(2):
                ra = slice(a * 64, (a + 1) * 64)
                S.mm(pC[rc, a * 64:(a + 1) * 64], kbar[ra, rc], Hin[ra, :], start=True, stop=False)
                S.mm(pC[rc, a * 64:(a + 1) * 64], Akk[a][rc, rc], v_tok[rc, ra], start=False, stop=True)
            yield
            S.copy(Ws[rc, :], pC[rc, 0:128])
            yield
            for a in range(2):
                ra = slice(a * 64, (a + 1) * 64)
                S.mm(pC[rc, 128 + a * 64:128 + (a + 1) * 64], Pf[a][rc, rc], Ws[rc, ra])
            yield
            S.copy(Us[rc, :], pC[rc, 128:256])
            yield
            for a in range(2):
                ra = slice(a * 64, (a + 1) * 64)
                S.mm(pC[ra, 384:448], Khat[rc, ra], v_tok[rc, ra], start=True, stop=False)
                S.mm(pC[ra, 384:448], Bhat[rc, ra], Us[rc, ra], start=False, stop=True)
            for a in range(2):
                ra = slice(a * 64, (a + 1) * 64)
                oy = slice(256 + a * 64, 256 + (a + 1) * 64)
                S.mm(pC[rc, oy], rbar[ra, rc], Hin[ra, :], start=True, stop=False)
                S.mm(pC[rc, oy], Ark[a][rc, rc], v_tok[rc, ra], start=False, stop=False)
                S.mm(pC[rc, oy], Arb[a][rc, rc], Us[rc, ra], start=False, stop=True)
            yield
            S.stt(Hout[:], Hin[:], gC[:, c:c + 1], pC[:, 384:448], ALU.mult, ALU.add)
            S.copy(ytok[rc, :], pC[rc, 256:384], eng="act")
            yield
        for a in range(2):
            S.bn_stats(st6[:, a, :], ytok[:, a * 64:(a + 1) * 64])
            S.bn_aggr(mv2[:, a, :], st6[:, a, :])
        rsqrt_eps(S, rs2[:], mv2[:, :, 1], 64e-5)
        yield
        for a in range(2):
            ra = slice(a * 64, (a + 1) * 64)
            gc = slice(u * 128 + a * 64, u * 128 + (a + 1) * 64)
            S.ts(yn[:, ra], ytok[:, ra], mv2[:, a, 0:1], rs2[:, a:a + 1], ALU.subtract, ALU.mult)
            S.tt(yn[:, ra], yn[:, ra], lngB[:, gc], ALU.mult)
            S.tt(yn[:, ra], yn[:, ra], lnbB[:, gc], ALU.add)
            S.stt(yn[:, ra], v_tok[:, ra], sb2[:, a:a + 1], yn[:, ra], ALU.mult, ALU.add)
        S.tt(brt_[:], yn[:], g_tok[:], ALU.mult)
        S.tr(pC[:, 0:128], brt_[:], ident[:])
        yield
        bb_ = brb[nbr[0] % 2]
        nbr[0] += 1
        S.copy(bb_[:], pC[:, 0:128], eng="act")
        S.dma(brT_d[1, u, :, t0:t0 + 128], bb_[:], eng="sp")

    def run_interleaved(gens):
        active = [x for x in gens if x is not None]
        while active:
            for gi in list(active):
                try:
                    next(gi)
                except StopIteration:
                    active.remove(gi)

    def run_weighted(ga, gb, wa):
        a_live, b_live = ga is not None, gb is not None
        while a_live or b_live:
            if a_live:
                for _ in range(wa):
                    try:
                        next(ga)
                    except StopIteration:
                        a_live = False
                        break
            if b_live:
                try:
                    next(gb)
                except StopIteration:
                    b_live = False

    n_it = len(iters)
    pipelined = DBG.get("rw_pipe", True)
    if pipelined:
        for k in range(n_it + 1):
            if DBG.get("rw_chain_first", False):
                run_weighted(chain(k - 1) if k >= 1 else None, setup(k) if k < n_it else None, DBG.get("rw_ratio", 2))
            else:
                run_weighted(setup(k) if k < n_it else None, chain(k - 1) if k >= 1 else None, DBG.get("rw_ratio", 1))
    else:
        for k in range(n_it):
            run_interleaved([setup(k)])
            run_interleaved([chain(k)])
    P.close()


_CACHE = {}


def kernel(**inputs):
    if "kb" not in _CACHE:
        _CACHE["kb"] = build()
    kb = _CACHE["kb"]
    consts = make_consts()
    shared = {}
    for n, shp in PARAMS:
        if n == "c":
            continue
        shared[n] = np.ascontiguousarray(np.asarray(inputs[n], dtype=np.float32))
    for n, v in consts.items():
        shared["k_" + n] = v
    x = np.asarray(inputs["x"], dtype=np.float32)
    c = np.asarray(inputs["c"], dtype=np.float32)
    in_maps = []
    for b in range(8):
        m = dict(shared)
        m["x"] = np.ascontiguousarray(x[b])
        m["c"] = np.ascontiguousarray(c[b:b + 1])
        in_maps.append(m)
    res = run_bass_kernel_spmd(kb.nc, in_maps, core_ids=list(range(8)))
    return np.stack([np.asarray(r["out"], dtype=np.float32) for r in res.results], axis=0)
```
